# Optimizing a Trainium2 kernel written in Bass

```python
import jax, jax.numpy as jnp
from jax import lax
import numpy as np

D_MODEL = 1024
BATCH = 8
SEQ = 4096
DEPTH = 2

GRID_W = 64
CTX_LEN = 256
D_MIX = D_MODEL
HEAD_DIM = 64
ATT_WIDTH = D_MIX // 2
RWKV_WIDTH = D_MIX - ATT_WIDTH
ATT_HEADS = ATT_WIDTH // HEAD_DIM
ATT_KV_HEADS = 2
ATT_GROUP = ATT_HEADS // ATT_KV_HEADS
ATT_KV_WIDTH = ATT_KV_HEADS * HEAD_DIM
WINDOW = 128
QBLK = 128
ROPE_BASE = 10000.0
ROPE_FREQS = HEAD_DIM // 4
RWKV_HEADS = RWKV_WIDTH // HEAD_DIM
LORA_DECAY = 64
LORA_ICLR = 64
LORA_VRES = 32
LORA_GATE = 128
N_DIRS = 2
ATT_COLS = ATT_WIDTH + 2 * ATT_KV_WIDTH
RWKV_COLS = 3 * RWKV_WIDTH + N_DIRS * (LORA_DECAY + LORA_ICLR) + LORA_GATE
IN_COLS = ATT_COLS + RWKV_COLS
ATT_SPLITS = (ATT_WIDTH, ATT_WIDTH + ATT_KV_WIDTH, ATT_COLS)
RWKV_SPLITS = (RWKV_WIDTH, 2 * RWKV_WIDTH, 3 * RWKV_WIDTH,
               3 * RWKV_WIDTH + N_DIRS * LORA_DECAY,
               3 * RWKV_WIDTH + N_DIRS * (LORA_DECAY + LORA_ICLR))
N_EXPERTS = 16
N_GROUPS = 4
EXPERTS_PER_GROUP = N_EXPERTS // N_GROUPS
TOP_K = 2
GROUP_SCORE_K = 2
D_EXPERT = D_MODEL
MOE_BLK = 256
NORM_EPS = 1e-6
GN_EPS = 64e-5
NEG_INF = -1e30
ATT_SCALE = HEAD_DIM ** -0.5

kernel_name = 'hybrid_swa_rwkv7_moe_prefix_dit'


def rmsnorm(x, g):
    xf = x.astype(jnp.float32)
    y = xf * lax.rsqrt(jnp.mean(xf * xf, axis=-1, keepdims=True) + NORM_EPS)
    return (y * g).astype(x.dtype)


def modulate(h, shift, scale):
    return h * (1.0 + scale) + shift


def axial_rope_tables(rows_count):
    row = jnp.repeat(jnp.arange(rows_count, dtype=jnp.float32), GRID_W)
    col = jnp.tile(jnp.arange(GRID_W, dtype=jnp.float32), rows_count)
    inv_freq = ROPE_BASE ** (-jnp.arange(ROPE_FREQS, dtype=jnp.float32) / ROPE_FREQS)
    ang_r = row[:, None] * inv_freq[None, :]
    ang_c = col[:, None] * inv_freq[None, :]
    ang = jnp.concatenate([ang_r, ang_r, ang_c, ang_c], axis=-1)
    return jnp.cos(ang), jnp.sin(ang)


def rotate_half_axial(t):
    t = t.reshape(t.shape[:-1] + (2, 2, ROPE_FREQS))
    t = jnp.stack([-t[..., 1, :], t[..., 0, :]], axis=-2)
    return t.reshape(t.shape[:-3] + (HEAD_DIM,))


def apply_rope(t, cos, sin):
    return t * cos[:, None, :] + rotate_half_axial(t) * sin[:, None, :]


def windowed_attention(q, k, v, kc, vc, sink):
    B, L = q.shape[0], q.shape[1]
    C = kc.shape[1]
    nb = L // QBLK

    def key_windows(t):
        tp = jnp.pad(t, ((0, 0), (QBLK, QBLK), (0, 0), (0, 0)))
        tp = tp.reshape(B, nb + 2, QBLK, ATT_KV_HEADS, HEAD_DIM)
        tw = jnp.concatenate([tp[:, :-2], tp[:, 1:-1], tp[:, 2:]], axis=2)
        return jnp.moveaxis(tw, 1, 0)

    kw, vw = key_windows(k), key_windows(v)
    qb = jnp.moveaxis(q.reshape(B, nb, QBLK, ATT_KV_HEADS, ATT_GROUP, HEAD_DIM), 1, 0)
    qi = jnp.arange(QBLK)[:, None]
    kj = jnp.arange(3 * QBLK)[None, :] - QBLK
    in_window = jnp.abs(kj - qi) <= WINDOW
    s_sink = jnp.broadcast_to(sink.astype(jnp.float32)[None, :, :, None, None],
                              (B, ATT_KV_HEADS, ATT_GROUP, QBLK, 1))
    W = 3 * QBLK

    def one_block(args):
        n, qn, kn, vn = args
        kpos = n * QBLK + kj
        valid = in_window & (kpos >= 0) & (kpos < L)
        s_loc = jnp.einsum('bqhgd,bkhd->bhgqk', qn, kn).astype(jnp.float32) * ATT_SCALE
        s_loc = jnp.where(valid, s_loc, NEG_INF)
        s_ctx = jnp.einsum('bqhgd,bchd->bhgqc', qn, kc).astype(jnp.float32) * ATT_SCALE
        p = jax.nn.softmax(jnp.concatenate([s_loc, s_ctx, s_sink], axis=-1), axis=-1)
        o = jnp.einsum('bhgqk,bkhd->bqhgd', p[..., :W].astype(vn.dtype), vn)
        o = o + jnp.einsum('bhgqc,bchd->bqhgd', p[..., W:W + C].astype(vc.dtype), vc)
        return o

    out = lax.map(one_block, (jnp.arange(nb), qb, kw, vw))
    return jnp.moveaxis(out, 0, 1).reshape(B, L, ATT_WIDTH)


def context_attention(qc, kc, vc, sink):
    B, C = qc.shape[0], qc.shape[1]
    s = jnp.einsum('bqhgd,bkhd->bhgqk', qc, kc).astype(jnp.float32) * ATT_SCALE
    s_sink = jnp.broadcast_to(sink.astype(jnp.float32)[None, :, :, None, None], s.shape[:-1] + (1,))
    p = jax.nn.softmax(jnp.concatenate([s, s_sink], axis=-1), axis=-1)[..., :C]
    o = jnp.einsum('bhgqk,bkhd->bqhgd', p.astype(vc.dtype), vc)
    return o.reshape(B, C, ATT_WIDTH)


def centred_shift(u, mu_prev, mu_next):
    prev = jnp.pad(u, ((0, 0), (1, 0), (0, 0)))[:, :-1]
    nxt = jnp.pad(u, ((0, 0), (0, 1), (0, 0)))[:, 1:]
    return u + mu_prev * (prev - u) + mu_next * (nxt - u)


def rwkv_features(u, v_first, P):
    u = u.astype(jnp.float32)
    B, T = u.shape[0], u.shape[1]
    r, k, v, wd, ad, gd = jnp.split(u, RWKV_SPLITS, axis=-1)
    if v_first is None:
        v_first = v
    else:
        v = v + (v_first - v) * jax.nn.sigmoid(P['v0'] + (v @ P['v1']) @ P['v2'])
    wd = wd.reshape(B, T, N_DIRS, LORA_DECAY)
    ad = ad.reshape(B, T, N_DIRS, LORA_ICLR)
    decay_in = P['w0'] + jnp.einsum('btdr,drc->btdc', jnp.tanh(wd), P['w2'])
    decay = jnp.exp(-jnp.exp(-jax.nn.softplus(-decay_in) - 0.5))
    a = jax.nn.sigmoid(P['a0'] + jnp.einsum('btdr,drc->btdc', ad, P['a2']))
    heads = lambda t: t.reshape(t.shape[:-1] + (RWKV_HEADS, HEAD_DIM))
    kk = heads(k * P['k_k'])
    kk = kk / jnp.maximum(jnp.sqrt(jnp.sum(kk * kk, axis=-1, keepdims=True)), 1e-12)
    k_dir = k[:, :, None, :] * (1.0 + (a - 1.0) * P['k_a'])
    g = jax.nn.sigmoid(gd) @ P['g2']
    f = {'r': heads(r), 'k': heads(k_dir), 'v': heads(v), 'decay': heads(decay),
         'a': heads(a), 'kk': kk, 'g': heads(g)}
    return f, v_first


def rwkv_scan(S0, r, decay, k, v, kk, a, reverse):
    tm = lambda t: jnp.moveaxis(t, 1, 0)
    emit = r is not None
    xs = (tm(decay), tm(k), tm(v), tm(kk), tm(kk * a))
    if emit:
        xs = xs + (tm(r),)

    def step(S, inp):
        w_t, k_t, v_t, kk_t, b_t = inp[:5]
        s_kk = jnp.einsum('bhvk,bhk->bhv', S, kk_t)
        S = (S * w_t[:, :, None, :] - s_kk[..., None] * b_t[:, :, None, :]
             + v_t[..., None] * k_t[:, :, None, :])
        y = jnp.einsum('bhvk,bhk->bhv', S, inp[5]) if emit else None
        return S, y

    S, ys = lax.scan(step, S0, xs, reverse=reverse)
    return S, (jnp.moveaxis(ys, 0, 1) if emit else None)


def rwkv_readout(y, f, P):
    B, T = y.shape[0], y.shape[1]
    mu = jnp.mean(y, axis=-1, keepdims=True)
    var = jnp.mean(jnp.square(y - mu), axis=-1, keepdims=True)
    gn = (y - mu) * lax.rsqrt(var + GN_EPS) * P['ln_w'] + P['ln_b']
    bonus = jnp.sum(f['r'][:, :, None] * f['k'] * P['r_k'], axis=(2, 4))[..., None] * f['v']
    return ((gn + bonus) * f['g']).reshape(B, T, RWKV_WIDTH)


def rwkv_mixer(fl, fc, P, emit_ctx):
    B = fl['v'].shape[0]
    S_zero = jnp.zeros((B, RWKV_HEADS, HEAD_DIM, HEAD_DIM), jnp.float32)
    ys_l, ys_c = [], []
    for d in range(N_DIRS):
        rev = d == 1
        S_ctx, yc = rwkv_scan(S_zero, fc['r'] if emit_ctx else None, fc['decay'][:, :, d],
                              fc['k'][:, :, d], fc['v'], fc['kk'], fc['a'][:, :, d], rev)
        _, yl = rwkv_scan(S_ctx, fl['r'], fl['decay'][:, :, d], fl['k'][:, :, d],
                          fl['v'], fl['kk'], fl['a'][:, :, d], rev)
        ys_l.append(yl)
        ys_c.append(yc)
    out_l = rwkv_readout(ys_l[0] + ys_l[1], fl, P)
    out_c = rwkv_readout(ys_c[0] + ys_c[1], fc, P) if emit_ctx else None
    return out_l, out_c


def route(h, router_w, router_b):
    T = h.shape[0]
    scores = jax.nn.sigmoid((h @ router_w).astype(jnp.float32))
    grouped = (scores + router_b).reshape(T, N_GROUPS, EXPERTS_PER_GROUP)
    group_score = jnp.sum(lax.top_k(grouped, GROUP_SCORE_K)[0], axis=-1)
    g_idx = jnp.argmax(group_score, axis=-1)
    in_group = jnp.take_along_axis(grouped, g_idx[:, None, None], axis=1)[:, 0]
    _, local = lax.top_k(in_group, TOP_K)
    e_idx = g_idx[:, None] * EXPERTS_PER_GROUP + local
    gates = jnp.take_along_axis(scores, e_idx, axis=1)
    gates = gates / jnp.sum(gates, axis=-1, keepdims=True)
    return e_idx, gates


def moe_ffn(h, router_w, router_b, w_gate, w_up, w_down):
    T, D = h.shape
    e_idx, gates = route(h, router_w, router_b)
    flat_e = e_idx.reshape(-1)
    n_rows = TOP_K * T
    nb = -(-n_rows // MOE_BLK) + N_EXPERTS
    counts = jnp.bincount(flat_e, length=N_EXPERTS)
    padded = (counts + MOE_BLK - 1) // MOE_BLK * MOE_BLK
    pad_ends = jnp.cumsum(padded)
    pad_starts = pad_ends - padded
    starts = jnp.cumsum(counts) - counts
    order = jnp.argsort(flat_e)
    sorted_e = flat_e[order]
    dest = pad_starts[sorted_e] + jnp.arange(n_rows) - starts[sorted_e]
    row_token = jnp.full((nb * MOE_BLK,), T, jnp.int32).at[dest].set((order // TOP_K).astype(jnp.int32))
    row_gate = jnp.zeros((nb * MOE_BLK,), jnp.float32).at[dest].set(gates.reshape(-1)[order])
    blk_expert = jnp.minimum(jnp.searchsorted(pad_ends, jnp.arange(nb) * MOE_BLK, side='right'),
                             N_EXPERTS - 1)
    h_pad = jnp.concatenate([h, jnp.zeros((1, D), h.dtype)], axis=0)
    xb = h_pad[row_token].reshape(nb, MOE_BLK, D)

    def expert_block(args):
        xe, e = args
        return (jax.nn.silu(xe @ w_gate[e]) * (xe @ w_up[e])) @ w_down[e]

    yb = lax.map(expert_block, (xb, blk_expert)).reshape(nb * MOE_BLK, D)
    out = jnp.zeros((T + 1, D), yb.dtype).at[row_token].add(yb * row_gate[:, None].astype(yb.dtype))
    return out[:T]


def hybrid_layer(xl, xc, c, c_ctx, vfl, vfc, P, router_w, router_b, cos, sin, is_last):
    B, L = xl.shape[0], xl.shape[1]
    C = xc.shape[1]
    mod_l = (jax.nn.silu(c) @ P['w_mod'] + P['b_mod'])[:, None, :]
    mod_c = (jax.nn.silu(c_ctx) @ P['w_mod'] + P['b_mod'])[None, None, :]
    sh_a, sc_a, ga, sh_f, sc_f, gf = jnp.split(mod_l, 6, axis=-1)
    csh_a, csc_a, cga, csh_f, csc_f, cgf = jnp.split(mod_c, 6, axis=-1)

    pl = modulate(rmsnorm(xl, P['norm_mix']), sh_a, sc_a) @ P['w_in']
    pc = modulate(rmsnorm(xc, P['norm_mix']), csh_a, csc_a) @ P['w_in']
    ql, kl, vl, rwl = jnp.split(pl, ATT_SPLITS, axis=-1)
    qc, kc, vc, rwc = jnp.split(pc, ATT_SPLITS, axis=-1)

    ql = apply_rope(ql.reshape(B, L, ATT_HEADS, HEAD_DIM), cos, sin)
    ql = ql.reshape(B, L, ATT_KV_HEADS, ATT_GROUP, HEAD_DIM)
    kl = apply_rope(kl.reshape(B, L, ATT_KV_HEADS, HEAD_DIM), cos, sin)
    vl = vl.reshape(B, L, ATT_KV_HEADS, HEAD_DIM)
    kc = kc.reshape(B, C, ATT_KV_HEADS, HEAD_DIM)
    vc = vc.reshape(B, C, ATT_KV_HEADS, HEAD_DIM)
    att_l = windowed_attention(ql, kl, vl, kc, vc, P['sink'])

    fc, vfc = rwkv_features(centred_shift(rwc, P['mu_prev'], P['mu_next']), vfc, P)
    fl, vfl = rwkv_features(centred_shift(rwl, P['mu_prev'], P['mu_next']), vfl, P)
    rwkv_l, rwkv_c = rwkv_mixer(fl, fc, P, not is_last)

    mix_l = jnp.concatenate([att_l, rwkv_l.astype(att_l.dtype)], axis=-1) @ P['w_out']
    xl = xl + ga * mix_l
    if not is_last:
        att_c = context_attention(qc.reshape(B, C, ATT_KV_HEADS, ATT_GROUP, HEAD_DIM), kc, vc, P['sink'])
        mix_c = jnp.concatenate([att_c, rwkv_c.astype(att_c.dtype)], axis=-1) @ P['w_out']
        xc = xc + cga * mix_c

    hfl = modulate(rmsnorm(xl, P['norm_ffn']), sh_f, sc_f).reshape(B * L, D_MODEL)
    if is_last:
        y = moe_ffn(hfl, router_w, router_b, P['e_gate'], P['e_up'], P['e_down'])
        xl = xl + gf * y.reshape(B, L, D_MODEL)
    else:
        hfc = modulate(rmsnorm(xc, P['norm_ffn']), csh_f, csc_f).reshape(B * C, D_MODEL)
        y = moe_ffn(jnp.concatenate([hfl, hfc], axis=0), router_w, router_b,
                    P['e_gate'], P['e_up'], P['e_down'])
        xl = xl + gf * y[:B * L].reshape(B, L, D_MODEL)
        xc = xc + cgf * y[B * L:].reshape(B, C, D_MODEL)
    return xl, xc, vfl, vfc


def setup_inputs(seed: int = 0) -> dict:
    key = jax.random.key(seed)
    ks = jax.random.split(key, 32)
    D = D_MODEL
    RW = RWKV_WIDTH
    E = N_EXPERTS
    nrm = lambda k, shape, s: jax.random.normal(k, shape, jnp.float32) * s
    uni = lambda k, shape, lo, hi: jax.random.uniform(k, shape, jnp.float32, lo, hi)
    return {
        'x': nrm(ks[0], (BATCH, SEQ, D), 1.0),
        'c': nrm(ks[1], (BATCH, D), 1.0),
        'ctx': nrm(ks[2], (BATCH, CTX_LEN, D), 1.0),
        'c_ctx': nrm(ks[3], (D,), 1.0),
        'w_mod': nrm(ks[4], (DEPTH, D, 6 * D), 0.5 * D ** -0.5),
        'b_mod': nrm(ks[5], (DEPTH, 6 * D), 0.02),
        'norm_mix_g': 1.0 + nrm(ks[6], (DEPTH, D), 0.02),
        'norm_ffn_g': 1.0 + nrm(ks[7], (DEPTH, D), 0.02),
        'w_in': nrm(ks[8], (DEPTH, D, IN_COLS), D ** -0.5),
        'w_out': nrm(ks[9], (DEPTH, D_MIX, D), D_MIX ** -0.5),
        'att_sink': nrm(ks[10], (DEPTH, ATT_HEADS), 0.5),
        'shift_mu_prev': uni(ks[11], (DEPTH, RWKV_COLS), 0.0, 0.5),
        'shift_mu_next': uni(ks[12], (DEPTH, RWKV_COLS), 0.0, 0.5),
        'decay_w0': uni(ks[13], (DEPTH, N_DIRS, RW), -2.0, 1.0),
        'decay_w2': nrm(ks[14], (DEPTH, N_DIRS, LORA_DECAY, RW), 0.5 * LORA_DECAY ** -0.5),
        'iclr_a0': nrm(ks[15], (DEPTH, N_DIRS, RW), 0.1),
        'iclr_a2': nrm(ks[16], (DEPTH, N_DIRS, LORA_ICLR, RW), 0.5 * LORA_ICLR ** -0.5),
        'vres_v0': nrm(ks[17], (DEPTH - 1, RW), 0.1),
        'vres_v1': nrm(ks[18], (DEPTH - 1, RW, LORA_VRES), RW ** -0.5),
        'vres_v2': nrm(ks[19], (DEPTH - 1, LORA_VRES, RW), 0.5 * LORA_VRES ** -0.5),
        'gate_g2': nrm(ks[20], (DEPTH, LORA_GATE, RW), LORA_GATE ** -0.5),
        'k_k': 0.85 + nrm(ks[21], (DEPTH, RW), 0.02),
        'k_a': 1.0 + nrm(ks[22], (DEPTH, RW), 0.02),
        'r_k': nrm(ks[23], (DEPTH, RWKV_HEADS, HEAD_DIM), 0.1),
        'ln_x_w': 1.0 + nrm(ks[24], (DEPTH, RW), 0.02),
        'ln_x_b': nrm(ks[25], (DEPTH, RW), 0.02),
        'router_w': nrm(ks[26], (D, E), D ** -0.5),
        'router_b': nrm(ks[27], (E,), 0.01),
        'expert_w_gate': nrm(ks[28], (DEPTH, E, D, D_EXPERT), D ** -0.5),
        'expert_w_up': nrm(ks[29], (DEPTH, E, D, D_EXPERT), D ** -0.5),
        'expert_w_down': nrm(ks[30], (DEPTH, E, D_EXPERT, D), D_EXPERT ** -0.5),
        'final_norm_g': 1.0 + nrm(ks[31], (D,), 0.02),
    }


def reference(x, c, ctx, c_ctx, w_mod, b_mod, norm_mix_g, norm_ffn_g, w_in, w_out, att_sink,
              shift_mu_prev, shift_mu_next, decay_w0, decay_w2, iclr_a0, iclr_a2,
              vres_v0, vres_v1, vres_v2, gate_g2, k_k, k_a, r_k, ln_x_w, ln_x_b,
              router_w, router_b, expert_w_gate, expert_w_up, expert_w_down, final_norm_g):
    L = x.shape[1]
    ROWS = L // GRID_W
    cos, sin = axial_rope_tables(ROWS)
    xl, xc = x, ctx
    vfl, vfc = None, None
    for l in range(DEPTH):
        P = {'w_mod': w_mod[l], 'b_mod': b_mod[l], 'norm_mix': norm_mix_g[l], 'norm_ffn': norm_ffn_g[l],
             'w_in': w_in[l], 'w_out': w_out[l],
             'sink': att_sink[l].reshape(ATT_KV_HEADS, ATT_GROUP),
             'mu_prev': shift_mu_prev[l], 'mu_next': shift_mu_next[l],
             'w0': decay_w0[l], 'w2': decay_w2[l], 'a0': iclr_a0[l], 'a2': iclr_a2[l],
             'g2': gate_g2[l], 'k_k': k_k[l], 'k_a': k_a[l], 'r_k': r_k[l],
             'ln_w': ln_x_w[l].reshape(RWKV_HEADS, HEAD_DIM),
             'ln_b': ln_x_b[l].reshape(RWKV_HEADS, HEAD_DIM),
             'e_gate': expert_w_gate[l], 'e_up': expert_w_up[l], 'e_down': expert_w_down[l]}
        if l > 0:
            P['v0'] = vres_v0[l - 1]
            P['v1'] = vres_v1[l - 1]
            P['v2'] = vres_v2[l - 1]
        xl, xc, vfl, vfc = hybrid_layer(xl, xc, c, c_ctx, vfl, vfc, P, router_w, router_b,
                                        cos, sin, l == DEPTH - 1)
    return rmsnorm(xl, final_norm_g)
```

```python
from contextlib import ExitStack
import os
import numpy as np
import ml_dtypes
import concourse.bass as bass
import concourse.mybir as mybir
from concourse.bass_utils import run_bass_kernel_spmd

F32 = mybir.dt.float32
BF16 = mybir.dt.bfloat16
AF = mybir.ActivationFunctionType
ALU = mybir.AluOpType
AX = mybir.AxisListType

D = 1024
L = 4096
C = 256
T = L + C
NT = T // 128
NCT = C // 128
DEPTH = 2
IN_COLS = 2688
RW = 512
RWC = 1920
NE = 16
ATT_SCALE = 0.125
NEG = -30000.0


class Eng:
    def __init__(self, fw, e, name, ring=0):
        self.fw = fw
        self.e = e
        self.name = name
        self.count = 0
        self.seen = {}
        self.sem = fw.nc.alloc_semaphore("s_" + name)
        self.ring = [fw.nc.alloc_semaphore("r_%s%d" % (name, i)) for i in range(ring)]
        self.ndma = 0

    def wait_tok(self, tok):
        sem, val, key = tok
        if self.seen.get(key, 0) >= val:
            return
        self.e.wait_ge(sem, val)
        self.seen[key] = val


class TT:
    def __init__(self, ap=None):
        self.ap = ap
        self.w = None
        self.r = {}


class FW:
    def __init__(self, nc):
        self.nc = nc
        self.pe = Eng(self, nc.tensor, "pe")
        self.act = Eng(self, nc.scalar, "act")
        self.dve = Eng(self, nc.vector, "dve")
        self.pool = Eng(self, nc.gpsimd, "pool")
        self.sp = Eng(self, nc.sync, "sp", ring=24)
        self.engs = [self.pe, self.act, self.dve, self.pool, self.sp]
        self.ninst = 0

    def _deps(self, eng, outs, ins):
        for t in ins:
            if t.w is not None and not (eng is self.pe and t.w[2] == "pe"):
                eng.wait_tok(t.w)
        for t in outs:
            if t.w is not None and not (eng is self.pe and t.w[2] == "pe"):
                eng.wait_tok(t.w)
            for tok in t.r.values():
                if not (eng is self.pe and tok[2] == "pe"):
                    eng.wait_tok(tok)

    def _mark(self, tok, outs, ins):
        for t in ins:
            t.r[tok[2]] = tok
        for t in outs:
            t.w = tok
            t.r = {}

    def op(self, eng, fn, outs, ins):
        self._deps(eng, outs, ins)
        inst = fn(eng.e)
        eng.count += 1
        inst.then_inc(eng.sem, 1)
        self._mark((eng.sem, eng.count, eng.name), outs, ins)
        self.ninst += 1

    def dma(self, out_ap, in_ap, outs, ins, q=None):
        q = q or self.sp
        self._deps(q, outs, ins)
        K = len(q.ring)
        slot = q.ndma % K
        rnd = q.ndma // K
        key = "%s_r%d" % (q.name, slot)
        if rnd > 0:
            q.wait_tok((q.ring[slot], 16 * rnd, key))
        q.e.dma_start(out=out_ap, in_=in_ap).then_inc(q.ring[slot], 16)
        q.ndma += 1
        self._mark((q.ring[slot], 16 * (rnd + 1), key), outs, ins)
        self.ninst += 1

    def all_toks(self):
        toks = []
        for e in self.engs:
            if e.count:
                toks.append((e.sem, e.count, e.name))
            K = len(e.ring)
            for slot in range(min(K, e.ndma)):
                n = (e.ndma - 1 - slot) // K + 1
                toks.append((e.ring[slot], 16 * n, "%s_r%d" % (e.name, slot)))
        return toks

    def barrier(self):
        toks = self.all_toks()
        for e in self.engs:
            for tok in toks:
                if e is self.pe and tok[2] == "pe":
                    continue
                e.wait_tok(tok)


_uid = [0]


def _nm(name):
    _uid[0] += 1
    return "sb_%s_%d" % (name, _uid[0])


class Pool:
    def __init__(self, es, nc, name, shape, dtype, bufs):
        self.t = [TT(es.enter_context(nc.sbuf_tensor(_nm(name), shape, dtype)).ap()) for i in range(bufs)]
        self.i = 0

    def next(self):
        t = self.t[self.i % len(self.t)]
        self.i += 1
        return t


def sb(es, nc, name, shape, dtype):
    return TT(es.enter_context(nc.sbuf_tensor(_nm(name), shape, dtype)).ap())


def build(debug_outs=(), stop_after=None):
    nc = bass.Bass("TRN2", target_bir_lowering=False)
    fw = FW(nc)
    pe, act, dve, pool, sp = fw.pe, fw.act, fw.dve, fw.pool, fw.sp

    def din(name, shape, dt=F32):
        return nc.dram_tensor(name, list(shape), dt, kind="ExternalInput").ap()

    def dscr(name, shape, dt=F32):
        return nc.dram_tensor(name, list(shape), dt, kind="Internal").ap()

    x_in = din("x", [L, D])
    ctx_in = din("ctx", [C, D])
    ccol_in = din("ccol", [128, 16])
    w_mod = din("w_mod", [DEPTH, D, 6 * D])
    b_mod = din("b_mod", [DEPTH, 6 * D])
    norm_mix_g = din("norm_mix_g", [DEPTH, D])
    norm_ffn_g = din("norm_ffn_g", [DEPTH, D])
    w_in = din("w_in", [DEPTH, D, IN_COLS])
    w_out = din("w_out", [DEPTH, D, D])
    att_sink = din("att_sink", [DEPTH, 8])
    mu_prev = din("shift_mu_prev", [DEPTH, RWC])
    mu_next = din("shift_mu_next", [DEPTH, RWC])
    decay_w0 = din("decay_w0", [DEPTH, 2, RW])
    decay_w2 = din("decay_w2", [DEPTH, 2, 64, RW])
    iclr_a0 = din("iclr_a0", [DEPTH, 2, RW])
    iclr_a2 = din("iclr_a2", [DEPTH, 2, 64, RW])
    vres_v0 = din("vres_v0", [1, RW])
    vres_v1 = din("vres_v1", [1, RW, 32])
    vres_v2 = din("vres_v2", [1, 32, RW])
    gate_g2 = din("gate_g2", [DEPTH, 128, RW])
    k_k = din("k_k", [DEPTH, RW])
    k_a = din("k_a", [DEPTH, RW])
    r_k = din("r_k", [DEPTH, RW])
    ln_x_w = din("ln_x_w", [DEPTH, RW])
    ln_x_b = din("ln_x_b", [DEPTH, RW])
    router_w = din("router_w", [D, NE])
    router_b = din("router_b", [NE])
    e_gate = din("expert_w_gate", [DEPTH, NE, D, D])
    e_up = din("expert_w_up", [DEPTH, NE, D, D])
    e_down = din("expert_w_down", [DEPTH, NE, D, D])
    final_g = din("final_norm_g", [D])
    ident_in = din("ident", [128, 128])
    masks_in = din("masks", [128, 4, 128])
    rope_cos = din("rope_cos", [L, 640])
    rope_sin = din("rope_sin", [L, 640])
    out = nc.dram_tensor("out", [L, D], F32, kind="ExternalOutput").ap()

    xs = dscr("xs", [T, D])
    xs_t = [TT() for _ in range(NT)]
    modbuf = dscr("modbuf", [DEPTH, 12, 128, D])
    mod_t = [[TT() for _ in range(12)] for _ in range(DEPTH)]
    u_scr = dscr("u_scr", [T + 4, RWC])
    u_t = [TT() for _ in range(NT)]
    u_guard = TT()
    mix = dscr("mix", [T, D])
    mix_a = [TT() for _ in range(NT)]
    mix_r = [TT() for _ in range(NT)]
    vfirst = dscr("vfirst", [T, RW])
    vf_t = [TT() for _ in range(NT)]
    feat = dscr("feat", [T, 5632])
    feat_t = [TT() for _ in range(NT)]
    y_scr = dscr("y_scr", [2, T, RW])
    y_t = [[TT() for _ in range(NT)] for _ in range(2)]
    dbg = {}
    for name, shape in debug_outs:
        dbg[name] = nc.dram_tensor("dbg_" + name, list(shape), F32, kind="ExternalOutput").ap()

    def urow(i):
        return 1 + i * 128 if i < NCT else 3 + i * 128

    PS = [TT(nc.alloc_psum_tensor("psb%d" % i, [128, 512], F32).ap()) for i in range(8)]
    psi = [0]

    def psum():
        t = PS[psi[0] % 6]
        psi[0] += 1
        return t

    with ExitStack() as g_es:
        ident = sb(g_es, nc, "ident", [128, 128], F32)
        identb = sb(g_es, nc, "identb", [128, 128], BF16)
        masks = sb(g_es, nc, "masks", [128, 4, 128], F32)
        fw.dma(ident.ap, ident_in, [ident], [])
        fw.dma(masks.ap, masks_in, [masks], [])
        fw.op(dve, lambda e: e.tensor_copy(out=identb.ap, in_=ident.ap), [identb], [ident])
        zrow = sb(g_es, nc, "zrow", [1, RWC], F32)
        fw.op(dve, lambda e: e.memset(zrow.ap, 0.0), [zrow], [])
        for r in (0, C + 1, C + 2, T + 3):
            fw.dma(u_scr[r:r + 1, :], zrow.ap, [u_guard], [zrow])

        for i in range(NT):
            src = ctx_in[i * 128:(i + 1) * 128, :] if i < NCT else x_in[(i - NCT) * 128:(i - NCT + 1) * 128, :]
            fw.dma(xs[i * 128:(i + 1) * 128, :], src, [xs_t[i]], [])

        for l in range(DEPTH):
            last = l == DEPTH - 1
            with ExitStack() as es:
                ccol = sb(es, nc, "ccol", [128, 16], F32)
                csil = sb(es, nc, "csil", [128, 16], F32)
                crep = sb(es, nc, "crep", [128, 16, 128], F32)
                fw.dma(ccol.ap, ccol_in, [ccol], [])
                fw.op(act, lambda e: e.activation(out=csil.ap, in_=ccol.ap, func=AF.Silu), [csil], [ccol])
                fw.op(dve, lambda e: e.tensor_copy(out=crep.ap, in_=csil.ap.unsqueeze(2).to_broadcast([128, 16, 128])), [crep], [csil])
                wm_pool = Pool(es, nc, "wm", [128, 8, 512], F32, 2)
                bm_pool = Pool(es, nc, "bm", [128, 512], F32, 2)
                modraw = [[sb(es, nc, "mr%d_%d" % (s, j), [128, D], F32) for j in range(6)] for s in range(2)]
                gmix = sb(es, nc, "gmix", [128, D], F32)
                gffn = sb(es, nc, "gffn", [128, D], F32)
                fw.dma(gmix.ap, norm_mix_g[l].partition_broadcast(128), [gmix], [])
                fw.dma(gffn.ap, norm_ffn_g[l].partition_broadcast(128), [gffn], [])
                for j in range(12):
                    wm = wm_pool.next()
                    bm = bm_pool.next()
                    fw.dma(wm.ap, w_mod[l][:, j * 512:(j + 1) * 512].rearrange("(kc p) n -> p kc n", p=128), [wm], [])
                    fw.dma(bm.ap, b_mod[l][j * 512:(j + 1) * 512].partition_broadcast(128), [bm], [])
                    for s in range(2):
                        ps = psum()
                        for kc in range(8):
                            fw.op(pe, lambda e, kc=kc, s=s, ps=ps, wm=wm: e.matmul(ps.ap, lhsT=crep.ap[:, s * 8 + kc, :], rhs=wm.ap[:, kc, :], start=(kc == 0), stop=(kc == 7)), [ps], [crep, wm])
                        dst = modraw[s][j // 2]
                        fw.op(dve, lambda e, ps=ps, dst=dst, bm=bm, j=j: e.tensor_tensor(out=dst.ap[:, (j % 2) * 512:(j % 2 + 1) * 512], in0=ps.ap, in1=bm.ap, op=ALU.add), [dst], [ps, bm])
                for s in range(2):
                    sh_a, sc_a, ga, sh_f, sc_f, gf = modraw[s]
                    fw.op(dve, lambda e, sc_a=sc_a: e.scalar_tensor_tensor(out=sc_a.ap, in0=sc_a.ap, scalar=1.0, in1=gmix.ap, op0=ALU.add, op1=ALU.mult), [sc_a], [sc_a, gmix])
                    fw.op(dve, lambda e, sc_f=sc_f: e.scalar_tensor_tensor(out=sc_f.ap, in0=sc_f.ap, scalar=1.0, in1=gffn.ap, op0=ALU.add, op1=ALU.mult), [sc_f], [sc_f, gffn])
                    for idx, t in enumerate((sc_a, sh_a, ga, sc_f, sh_f, gf)):
                        fw.dma(modbuf[l, s * 6 + idx], t.ap, [mod_t[l][s * 6 + idx]], [t])
                fw.barrier()
            if stop_after == "S1":
                break

            with ExitStack() as es_att:
                QT = sb(es_att, nc, "QT", [128, 4, T], BF16)
                KT = sb(es_att, nc, "KT", [128, T], BF16)
                VV = sb(es_att, nc, "VV", [128, NT, 128], BF16)
                qt_t = [TT() for _ in range(NT)]
                with ExitStack() as es:
                    winb = sb(es, nc, "winb", [128, 8, IN_COLS], BF16)
                    wst_pool = Pool(es, nc, "wst", [128, 8, 384], F32, 2)
                    for j in range(7):
                        wst = wst_pool.next()
                        fw.dma(wst.ap, w_in[l][:, j * 384:(j + 1) * 384].rearrange("(kc p) n -> p kc n", p=128), [wst], [])
                        fw.op(pool, lambda e, wst=wst, j=j: e.tensor_copy(out=winb.ap[:, :, j * 384:(j + 1) * 384], in_=wst.ap), [winb], [wst])
                    modA = [sb(es, nc, "modA%d" % s, [128, D], F32) for s in range(2)]
                    modB = [sb(es, nc, "modB%d" % s, [128, D], F32) for s in range(2)]
                    for s in range(2):
                        fw.dma(modA[s].ap, modbuf[l, s * 6 + 0], [modA[s]], [mod_t[l][s * 6 + 0]])
                        fw.dma(modB[s].ap, modbuf[l, s * 6 + 1], [modB[s]], [mod_t[l][s * 6 + 1]])
                    x_pool = Pool(es, nc, "xt", [128, D], F32, 2)
                    junk = sb(es, nc, "junk", [128, D], F32)
                    st_pool = Pool(es, nc, "st", [128, 4], F32, 2)
                    h_pool = Pool(es, nc, "ht", [128, D], BF16, 2)
                    hT_pool = Pool(es, nc, "hT", [128, 8, 128], BF16, 2)
                    pr_pool = Pool(es, nc, "pr", [128, IN_COLS], F32, 2)
                    cos_pool = Pool(es, nc, "cos", [128, 640], F32, 2)
                    sin_pool = Pool(es, nc, "sin", [128, 640], F32, 2)
                    rp_pool = Pool(es, nc, "rp", [128, 640], F32, 2)
                    rq_pool = Pool(es, nc, "rq", [128, 640], BF16, 2)

                    def s2_load(i):
                        xt = x_pool.next()
                        fw.dma(xt.ap, xs[i * 128:(i + 1) * 128, :], [xt], [xs_t[i]])
                        cs = sn = None
                        if i >= NCT:
                            cs = cos_pool.next()
                            sn = sin_pool.next()
                            r0 = (i - NCT) * 128
                            fw.dma(cs.ap, rope_cos[r0:r0 + 128, :], [cs], [])
                            fw.dma(sn.ap, rope_sin[r0:r0 + 128, :], [sn], [])
                        return xt, cs, sn

                    nxt = s2_load(0)
                    for i in range(NT):
                        xt, cs, sn = nxt
                        if i + 1 < NT:
                            nxt = s2_load(i + 1)
                        s = 1 if i < NCT else 0
                        st = st_pool.next()
                        fw.op(act, lambda e, xt=xt, st=st: e.activation(out=junk.ap, in_=xt.ap, func=AF.Square, accum_out=st.ap[:, 0:1]), [junk, st], [xt])
                        fw.op(dve, lambda e, st=st: e.tensor_scalar(out=st.ap[:, 1:2], in0=st.ap[:, 0:1], scalar1=1.0 / D, scalar2=1e-6, op0=ALU.mult, op1=ALU.add), [st], [st])
                        fw.op(act, lambda e, st=st: e.activation(out=st.ap[:, 3:4], in_=st.ap[:, 1:2], func=AF.Sqrt), [st], [st])
                        fw.op(dve, lambda e, st=st: e.reciprocal(out=st.ap[:, 2:3], in_=st.ap[:, 3:4]), [st], [st])
                        fw.op(dve, lambda e, xt=xt, st=st, s=s: e.scalar_tensor_tensor(out=xt.ap, in0=xt.ap, scalar=st.ap[:, 2:3], in1=modA[s].ap, op0=ALU.mult, op1=ALU.mult), [xt], [xt, st, modA[s]])
                        ht = h_pool.next()
                        fw.op(pool, lambda e, xt=xt, ht=ht, s=s: e.tensor_tensor(out=ht.ap, in0=xt.ap, in1=modB[s].ap, op=ALU.add), [ht], [xt, modB[s]])
                        hT = hT_pool.next()
                        for half in range(2):
                            ps = psum()
                            psb = ps.ap.bitcast(BF16)
                            for kk in range(4):
                                kc = half * 4 + kk
                                fw.op(pe, lambda e, kc=kc, kk=kk, psb=psb, ht=ht: e.transpose(out=psb[:, kk * 128:(kk + 1) * 128], in_=ht.ap[:, kc * 128:(kc + 1) * 128], identity=identb.ap), [ps], [ht, identb])
                            fw.op(act if half else dve, lambda e, psb=psb, hT=hT, half=half: (e.copy if half else e.tensor_copy)(out=hT.ap[:, half * 4:(half + 1) * 4, :], in_=psb[:, 0:512].rearrange("p (a b) -> p a b", a=4)), [hT], [ps])
                        pr = pr_pool.next()
                        col = 0
                        ci = 0
                        while col < IN_COLS:
                            n = min(512, IN_COLS - col)
                            ps = psum()
                            for kc in range(8):
                                fw.op(pe, lambda e, kc=kc, ps=ps, hT=hT, col=col, n=n: e.matmul(ps.ap[:, 0:n], lhsT=hT.ap[:, kc, :], rhs=winb.ap[:, kc, col:col + n], start=(kc == 0), stop=(kc == 7)), [ps], [hT, winb])
                            if ci % 2 == 0:
                                fw.op(act, lambda e, ps=ps, pr=pr, col=col, n=n: e.copy(out=pr.ap[:, col:col + n], in_=ps.ap[:, 0:n]), [pr], [ps])
                            else:
                                fw.op(dve, lambda e, ps=ps, pr=pr, col=col, n=n: e.tensor_copy(out=pr.ap[:, col:col + n], in_=ps.ap[:, 0:n]), [pr], [ps])
                            col += n
                            ci += 1
                        if "proj" in dbg and l == 0:
                            fw.dma(dbg["proj"][i * 128:(i + 1) * 128, :], pr.ap, [], [pr])
                        fw.dma(u_scr[urow(i):urow(i) + 128, :], pr.ap[:, 768:IN_COLS], [u_t[i]], [pr])
                        rq = rq_pool.next()
                        if i >= NCT:
                            rp = rp_pool.next()
                            v5 = lambda ap: ap.rearrange("p (m h f) -> p m h f", h=2, f=16)
                            fw.op(dve, lambda e, rp=rp, pr=pr, sn=sn: e.tensor_tensor(out=v5(rp.ap)[:, :, 0, :], in0=v5(pr.ap[:, 0:640])[:, :, 1, :], in1=v5(sn.ap)[:, :, 0, :], op=ALU.mult), [rp], [pr, sn])
                            fw.op(dve, lambda e, rp=rp, pr=pr, sn=sn: e.tensor_tensor(out=v5(rp.ap)[:, :, 1, :], in0=v5(pr.ap[:, 0:640])[:, :, 0, :], in1=v5(sn.ap)[:, :, 1, :], op=ALU.mult), [rp], [pr, sn])
                            fw.op(pool, lambda e, cs=cs, pr=pr: e.tensor_tensor(out=cs.ap, in0=pr.ap[:, 0:640], in1=cs.ap, op=ALU.mult), [cs], [pr, cs])
                            gq = lambda ap: ap.rearrange("p (g j d) -> p g j d", g=2, j=4)
                            jq = lambda ap: ap.rearrange("p (j g d) -> p g j d", g=2, j=4)
                            fw.op(dve, lambda e, rq=rq, rp=rp, cs=cs: e.tensor_tensor(out=jq(rq.ap[:, 0:512]), in0=gq(rp.ap[:, 0:512]), in1=gq(cs.ap[:, 0:512]), op=ALU.add), [rq], [rp, cs])
                            fw.op(dve, lambda e, rq=rq, rp=rp, cs=cs: e.tensor_tensor(out=rq.ap[:, 512:640], in0=rp.ap[:, 512:640], in1=cs.ap[:, 512:640], op=ALU.add), [rq], [rp, cs])
                        else:
                            gq = lambda ap: ap.rearrange("p (g j d) -> p g j d", g=2, j=4)
                            jq = lambda ap: ap.rearrange("p (j g d) -> p g j d", g=2, j=4)
                            fw.op(dve, lambda e, rq=rq, pr=pr: e.tensor_copy(out=jq(rq.ap[:, 0:512]), in_=gq(pr.ap[:, 0:512])), [rq], [pr])
                            fw.op(dve, lambda e, rq=rq, pr=pr: e.tensor_copy(out=rq.ap[:, 512:640], in_=pr.ap[:, 512:640]), [rq], [pr])
                        fw.op(act, lambda e, pr=pr, i=i: e.copy(out=VV.ap[:, i, :], in_=pr.ap[:, 640:768]), [qt_t[i]], [pr])
                        ps = psum()
                        psb = ps.ap.bitcast(BF16)
                        for j in range(4):
                            fw.op(pe, lambda e, j=j, psb=psb, rq=rq: e.transpose(out=psb[:, j * 128:(j + 1) * 128], in_=rq.ap[:, j * 128:(j + 1) * 128], identity=identb.ap), [ps], [rq, identb])
                        fw.op(pe, lambda e, psb=psb, rq=rq: e.transpose(out=psb[:, 512:640], in_=rq.ap[:, 512:640], identity=identb.ap), [ps], [rq, identb])
                        fw.op(dve, lambda e, psb=psb, i=i: e.tensor_copy(out=QT.ap[:, :, i * 128:(i + 1) * 128], in_=psb[:, 0:512].rearrange("p (j t) -> p j t", j=4)), [qt_t[i]], [ps])
                        fw.op(act, lambda e, psb=psb, i=i: e.copy(out=KT.ap[:, i * 128:(i + 1) * 128], in_=psb[:, 512:640]), [qt_t[i]], [ps])
                    fw.barrier()
                if stop_after == "S2":
                    break
                with ExitStack() as es:
                    sinkb = sb(es, nc, "sinkb", [128, 8], F32)
                    fw.dma(sinkb.ap, att_sink[l].partition_broadcast(128), [sinkb], [])
                    mb = sb(es, nc, "mb", [128, 384], F32)
                    fw.op(dve, lambda e: e.memset(mb.ap, 0.0), [mb], [])
                    fw.op(dve, lambda e: e.tensor_scalar(out=mb.ap[:, 0:128], in0=masks.ap[:, 1, :], scalar1=-1.0, scalar2=-NEG, op0=ALU.add, op1=ALU.mult), [mb], [masks])
                    fw.op(dve, lambda e: e.tensor_scalar(out=mb.ap[:, 256:384], in0=masks.ap[:, 3, :], scalar1=-1.0, scalar2=-NEG, op0=ALU.add, op1=ALU.mult), [mb], [masks])
                    s_pool = Pool(es, nc, "ssb", [128, 640], F32, 2)
                    p_pool = Pool(es, nc, "psb", [128, 640], BF16, 2)
                    pT_pool = Pool(es, nc, "pTs", [128, 640], BF16, 2)
                    ast_pool = Pool(es, nc, "ast", [128, 8], F32, 3)
                    mixa_pool = Pool(es, nc, "mixa", [128, 512], F32, 2)
                    for i in (range(NCT, NT) if last else range(NT)):
                        if i < NCT:
                            nloc = 0
                            kblocks = list(range(NCT))
                        else:
                            lo, hi = max(NCT, i - 1), min(NT - 1, i + 1)
                            nloc = (hi - lo + 1) * 128
                            moff = 0 if lo == i - 1 else 128
                            kblocks = list(range(lo, hi + 1)) + list(range(NCT))
                        ncols = nloc + C
                        mixa = mixa_pool.next()
                        ops = PS[6 + (i % 2)]
                        for h in range(8):
                            g, j = h // 4, h % 4
                            lhsT = QT.ap[g * 64:(g + 1) * 64, j, i * 128:(i + 1) * 128]
                            ssb = s_pool.next()
                            st = ast_pool.next()
                            if nloc:
                                ps1 = psum()
                                fw.op(pe, lambda e, ps1=ps1, lhsT=lhsT, g=g, lo=lo, hi=hi, nloc=nloc: e.matmul(ps1.ap[:, 0:nloc], lhsT=lhsT, rhs=KT.ap[g * 64:(g + 1) * 64, lo * 128:(hi + 1) * 128], start=True, stop=True), [ps1], [qt_t[i]])
                                fw.op(dve, lambda e, ps1=ps1, ssb=ssb, nloc=nloc, moff=moff: e.scalar_tensor_tensor(out=ssb.ap[:, 0:nloc], in0=ps1.ap[:, 0:nloc], scalar=ATT_SCALE, in1=mb.ap[:, moff:moff + nloc], op0=ALU.mult, op1=ALU.add), [ssb], [ps1, mb])
                            ps2 = psum()
                            fw.op(pe, lambda e, ps2=ps2, lhsT=lhsT, g=g: e.matmul(ps2.ap[:, 0:C], lhsT=lhsT, rhs=KT.ap[g * 64:(g + 1) * 64, 0:C], start=True, stop=True), [ps2], [qt_t[i]])
                            fw.op(act, lambda e, ps2=ps2, ssb=ssb, nloc=nloc, ncols=ncols: e.activation(out=ssb.ap[:, nloc:ncols], in_=ps2.ap[:, 0:C], func=AF.Copy, scale=ATT_SCALE), [ssb], [ps2])
                            fw.op(dve, lambda e, ssb=ssb, st=st, ncols=ncols: e.reduce_max(out=st.ap[:, 0:1], in_=ssb.ap[:, 0:ncols], axis=AX.X), [st], [ssb])
                            fw.op(dve, lambda e, st=st, h=h: e.tensor_scalar(out=st.ap[:, 1:2], in0=st.ap[:, 0:1], scalar1=sinkb.ap[:, h:h + 1], scalar2=-1.0, op0=ALU.max, op1=ALU.mult), [st], [st, sinkb])
                            pb = p_pool.next()
                            fw.op(act, lambda e, pb=pb, ssb=ssb, st=st, ncols=ncols: e.activation(out=pb.ap[:, 0:ncols], in_=ssb.ap[:, 0:ncols], func=AF.Exp, bias=st.ap[:, 1:2], accum_out=st.ap[:, 2:3]), [pb, st], [ssb, st])
                            fw.op(act, lambda e, st=st, h=h: e.activation(out=st.ap[:, 3:4], in_=sinkb.ap[:, h:h + 1], func=AF.Exp, bias=st.ap[:, 1:2]), [st], [st, sinkb])
                            fw.op(dve, lambda e, st=st: e.tensor_tensor(out=st.ap[:, 4:5], in0=st.ap[:, 2:3], in1=st.ap[:, 3:4], op=ALU.add), [st], [st])
                            fw.op(dve, lambda e, st=st: e.reciprocal(out=st.ap[:, 5:6], in_=st.ap[:, 4:5]), [st], [st])
                            ps3 = psum()
                            ps3b = ps3.ap.bitcast(BF16)
                            nblk = ncols // 128
                            for b in range(nblk):
                                fw.op(pe, lambda e, b=b, ps3b=ps3b, pb=pb: e.transpose(out=ps3b[:, b * 128:(b + 1) * 128], in_=pb.ap[:, b * 128:(b + 1) * 128], identity=identb.ap), [ps3], [pb, identb])
                            pT = pT_pool.next()
                            if h % 2 == 0:
                                fw.op(dve, lambda e, ps3b=ps3b, pT=pT, ncols=ncols: e.tensor_copy(out=pT.ap[:, 0:ncols], in_=ps3b[:, 0:ncols]), [pT], [ps3])
                            else:
                                fw.op(act, lambda e, ps3b=ps3b, pT=pT, ncols=ncols: e.copy(out=pT.ap[:, 0:ncols], in_=ps3b[:, 0:ncols]), [pT], [ps3])
                            for b, kb in enumerate(kblocks):
                                fw.op(pe, lambda e, b=b, kb=kb, pT=pT, g=g, h=h, ops=ops, nb=len(kblocks): e.matmul(ops.ap[:, h * 64:(h + 1) * 64], lhsT=pT.ap[:, b * 128:(b + 1) * 128], rhs=VV.ap[:, kb, g * 64:(g + 1) * 64], start=(b == 0), stop=(b == nb - 1)), [ops], [pT, qt_t[kb]])
                            fw.op(act, lambda e, ops=ops, mixa=mixa, st=st, h=h: e.activation(out=mixa.ap[:, h * 64:(h + 1) * 64], in_=ops.ap[:, h * 64:(h + 1) * 64], func=AF.Copy, scale=st.ap[:, 5:6]), [mixa], [ops, st])
                        fw.dma(mix[i * 128:(i + 1) * 128, 0:512], mixa.ap, [mix_a[i]], [mixa])
                    fw.barrier()
            if stop_after == "S3":
                if "mix" in dbg:
                    fw.barrier()
                    for i in range(NT):
                        fw.dma(dbg["mix"][i * 128:(i + 1) * 128, :], mix[i * 128:(i + 1) * 128, :], [], [mix_a[i], mix_r[i]])
                break
            FV, FR, FG, FKK, FBON = 0, 512, 1024, 1536, 2048
            FLW = lambda d_: 2560 + d_ * 1536
            FB = lambda d_: 2560 + d_ * 1536 + 512
            FKD = lambda d_: 2560 + d_ * 1536 + 1024
            NF = 5632
            with ExitStack() as es:
                def bc(name, src, n):
                    t_ = sb(es, nc, name, [128, n], F32)
                    fw.dma(t_.ap, src.partition_broadcast(128), [t_], [])
                    return t_
                mup = bc("mup", mu_prev[l], RWC)
                mun = bc("mun", mu_next[l], RWC)
                w0b = bc("w0b", decay_w0[l].rearrange("d c -> (d c)"), 1024)
                a0b = bc("a0b", iclr_a0[l].rearrange("d c -> (d c)"), 1024)
                kkb = bc("kkb", k_k[l], RW)
                kab = bc("kab", k_a[l], RW)
                rkb = bc("rkb", r_k[l], RW)
                w2t = sb(es, nc, "w2t", [128, RW], F32)
                a2t = sb(es, nc, "a2t", [128, RW], F32)
                g2t = sb(es, nc, "g2t", [128, RW], F32)
                fw.dma(w2t.ap, decay_w2[l].rearrange("d r c -> (d r) c"), [w2t], [])
                fw.dma(a2t.ap, iclr_a2[l].rearrange("d r c -> (d r) c"), [a2t], [])
                fw.dma(g2t.ap, gate_g2[l], [g2t], [])
                if l > 0:
                    v0b = bc("v0b", vres_v0[0], RW)
                    v1t = sb(es, nc, "v1t", [128, 4, 32], F32)
                    fw.dma(v1t.ap, vres_v1[0].rearrange("(c p) n -> p c n", p=128), [v1t], [])
                    v2t = sb(es, nc, "v2t", [32, RW], F32)
                    fw.dma(v2t.ap, vres_v2[0], [v2t], [])
                uc_pool = Pool(es, nc, "uc", [128, RWC], F32, 2)
                up_pool = Pool(es, nc, "up", [128, RWC], F32, 2)
                un_pool = Pool(es, nc, "un", [128, RWC], F32, 2)
                vf_pool = Pool(es, nc, "vf", [128, RW], F32, 2)
                li_pool = Pool(es, nc, "li", [128, 384], F32, 2)
                liT_pool = Pool(es, nc, "liT", [128, 3, 128], F32, 2)
                fo_pool = Pool(es, nc, "fo", [128, NF], F32, 2)
                tmp_pool = Pool(es, nc, "ftmp", [128, RW], F32, 6)
                a_pool = Pool(es, nc, "fa", [128, RW], F32, 4)
                sm_pool = Pool(es, nc, "fsm", [128, 64], F32, 2)
                vT_pool = Pool(es, nc, "fvT", [128, 4, 128], F32, 2)
                h3 = lambda ap: ap.rearrange("p (h d) -> p h d", h=8)

                def s4a_load(i):
                    uc, up, un = uc_pool.next(), up_pool.next(), un_pool.next()
                    deps = [u_t[i], u_guard] + ([u_t[i - 1]] if i > 0 else []) + ([u_t[i + 1]] if i + 1 < NT else [])
                    r0 = urow(i)
                    fw.dma(uc.ap, u_scr[r0:r0 + 128, :], [uc], deps)
                    fw.dma(up.ap, u_scr[r0 - 1:r0 + 127, :], [up], deps)
                    fw.dma(un.ap, u_scr[r0 + 1:r0 + 129, :], [un], deps)
                    vf = None
                    if l > 0:
                        vf = vf_pool.next()
                        fw.dma(vf.ap, vfirst[i * 128:(i + 1) * 128, :], [vf], [vf_t[i]])
                    return uc, up, un, vf

                nxt = s4a_load(0)
                for i in range(NT):
                    uc, up, un, vf = nxt
                    if i + 1 < NT:
                        nxt = s4a_load(i + 1)
                    fw.op(dve, lambda e: e.tensor_tensor(out=up.ap, in0=up.ap, in1=uc.ap, op=ALU.subtract), [up], [up, uc])
                    fw.op(pool, lambda e: e.tensor_tensor(out=un.ap, in0=un.ap, in1=uc.ap, op=ALU.subtract), [un], [un, uc])
                    fw.op(dve, lambda e: e.tensor_tensor(out=up.ap, in0=up.ap, in1=mup.ap, op=ALU.mult), [up], [up, mup])
                    fw.op(pool, lambda e: e.tensor_tensor(out=un.ap, in0=un.ap, in1=mun.ap, op=ALU.mult), [un], [un, mun])
                    fw.op(dve, lambda e: e.tensor_tensor(out=uc.ap, in0=uc.ap, in1=up.ap, op=ALU.add), [uc], [uc, up])
                    fw.op(dve, lambda e: e.tensor_tensor(out=uc.ap, in0=uc.ap, in1=un.ap, op=ALU.add), [uc], [uc, un])
                    U = uc.ap
                    r_, k_, v_ = U[:, 0:512], U[:, 512:1024], U[:, 1024:1536]
                    li = li_pool.next()
                    fw.op(act, lambda e: e.activation(out=li.ap[:, 0:128], in_=U[:, 1536:1664], func=AF.Tanh), [li], [uc])
                    fw.op(act, lambda e: e.copy(out=li.ap[:, 128:256], in_=U[:, 1664:1792]), [li], [uc])
                    fw.op(act, lambda e: e.activation(out=li.ap[:, 256:384], in_=U[:, 1792:1920], func=AF.Sigmoid), [li], [uc])
                    ps = psum()
                    for q in range(3):
                        fw.op(pe, lambda e, q=q, ps=ps: e.transpose(out=ps.ap[:, q * 128:(q + 1) * 128], in_=li.ap[:, q * 128:(q + 1) * 128], identity=ident.ap), [ps], [li, ident])
                    liT = liT_pool.next()
                    fw.op(dve, lambda e, ps=ps: e.tensor_copy(out=liT.ap, in_=ps.ap[:, 0:384].rearrange("p (a b) -> p a b", a=3)), [liT], [ps])
                    fo = fo_pool.next()
                    FO = fo.ap
                    a_d = []
                    for d_ in range(2):
                        hp = slice(d_ * 64, (d_ + 1) * 64)
                        ps = psum()
                        fw.op(pe, lambda e, ps=ps, hp=hp: e.matmul(ps.ap, lhsT=liT.ap[hp, 0, :], rhs=w2t.ap[hp, :], start=True, stop=True), [ps], [liT, w2t])
                        t1 = tmp_pool.next()
                        fw.op(dve, lambda e, ps=ps, t1=t1, d_=d_: e.tensor_tensor(out=t1.ap, in0=ps.ap, in1=w0b.ap[:, d_ * 512:(d_ + 1) * 512], op=ALU.add), [t1], [ps, w0b])
                        fw.op(act, lambda e, t1=t1: e.activation(out=t1.ap, in_=t1.ap, func=AF.Sigmoid), [t1], [t1])
                        fw.op(pool, lambda e, t1=t1, d_=d_: e.tensor_scalar(out=FO[:, FLW(d_):FLW(d_) + 512], in0=t1.ap, scalar1=-0.6065306597126334, scalar2=None, op0=ALU.mult), [fo], [t1])
                        ps = psum()
                        fw.op(pe, lambda e, ps=ps, hp=hp: e.matmul(ps.ap, lhsT=liT.ap[hp, 1, :], rhs=a2t.ap[hp, :], start=True, stop=True), [ps], [liT, a2t])
                        ad = a_pool.next()
                        fw.op(dve, lambda e, ps=ps, ad=ad, d_=d_: e.tensor_tensor(out=ad.ap, in0=ps.ap, in1=a0b.ap[:, d_ * 512:(d_ + 1) * 512], op=ALU.add), [ad], [ps, a0b])
                        fw.op(act, lambda e, ad=ad: e.activation(out=ad.ap, in_=ad.ap, func=AF.Sigmoid), [ad], [ad])
                        a_d.append(ad)
                    ps = psum()
                    fw.op(pe, lambda e, ps=ps: e.matmul(ps.ap, lhsT=liT.ap[:, 2, :], rhs=g2t.ap, start=True, stop=True), [ps], [liT, g2t])
                    fw.op(act, lambda e, ps=ps: e.copy(out=FO[:, FG:FG + 512], in_=ps.ap), [fo], [ps])
                    if l == 0:
                        fw.op(pool, lambda e: e.tensor_copy(out=FO[:, FV:FV + 512], in_=v_), [fo], [uc])
                        fw.dma(vfirst[i * 128:(i + 1) * 128, :], v_, [vf_t[i]], [uc])
                    else:
                        ps = psum()
                        for c_ in range(4):
                            fw.op(pe, lambda e, c_=c_, ps=ps: e.transpose(out=ps.ap[:, c_ * 128:(c_ + 1) * 128], in_=U[:, 1024 + c_ * 128:1024 + (c_ + 1) * 128], identity=ident.ap), [ps], [uc, ident])
                        vT = vT_pool.next()
                        fw.op(dve, lambda e, ps=ps, vT=vT: e.tensor_copy(out=vT.ap, in_=ps.ap.rearrange("p (a b) -> p a b", a=4)), [vT], [ps])
                        ps = psum()
                        for c_ in range(4):
                            fw.op(pe, lambda e, c_=c_, ps=ps, vT=vT: e.matmul(ps.ap[:, 0:32], lhsT=vT.ap[:, c_, :], rhs=v1t.ap[:, c_, :], start=(c_ == 0), stop=(c_ == 3)), [ps], [vT, v1t])
                        sm = sm_pool.next()
                        fw.op(dve, lambda e, ps=ps, sm=sm: e.tensor_copy(out=sm.ap[:, 0:32], in_=ps.ap[:, 0:32]), [sm], [ps])
                        ps = psum()
                        fw.op(pe, lambda e, ps=ps, sm=sm: e.transpose(out=ps.ap[0:32, 0:128], in_=sm.ap[:, 0:32], identity=ident.ap), [ps], [sm, ident])
                        smT = tmp_pool.next()
                        fw.op(dve, lambda e, ps=ps, smT=smT: e.tensor_copy(out=smT.ap[0:32, 0:128], in_=ps.ap[0:32, 0:128]), [smT], [ps])
                        ps = psum()
                        fw.op(pe, lambda e, ps=ps, smT=smT: e.matmul(ps.ap, lhsT=smT.ap[0:32, 0:128], rhs=v2t.ap, start=True, stop=True), [ps], [smT, v2t])
                        t1 = tmp_pool.next()
                        fw.op(dve, lambda e, ps=ps, t1=t1: e.tensor_tensor(out=t1.ap, in0=ps.ap, in1=v0b.ap, op=ALU.add), [t1], [ps, v0b])
                        fw.op(act, lambda e, t1=t1: e.activation(out=t1.ap, in_=t1.ap, func=AF.Sigmoid), [t1], [t1])
                        fw.op(dve, lambda e, vf=vf: e.tensor_tensor(out=vf.ap, in0=vf.ap, in1=v_, op=ALU.subtract), [vf], [vf, uc])
                        fw.op(dve, lambda e, vf=vf, t1=t1: e.tensor_tensor(out=vf.ap, in0=vf.ap, in1=t1.ap, op=ALU.mult), [vf], [vf, t1])
                        fw.op(dve, lambda e, vf=vf: e.tensor_tensor(out=FO[:, FV:FV + 512], in0=vf.ap, in1=v_, op=ALU.add), [fo], [vf, uc])
                    fw.op(pool, lambda e: e.tensor_copy(out=FO[:, FR:FR + 512], in_=r_), [fo], [uc])
                    kk0 = tmp_pool.next()
                    sq = tmp_pool.next()
                    sm = sm_pool.next()
                    fw.op(dve, lambda e, kk0=kk0: e.tensor_tensor(out=kk0.ap, in0=k_, in1=kkb.ap, op=ALU.mult), [kk0], [uc, kkb])
                    fw.op(pool, lambda e, kk0=kk0, sq=sq: e.tensor_tensor(out=sq.ap, in0=kk0.ap, in1=kk0.ap, op=ALU.mult), [sq], [kk0])
                    fw.op(dve, lambda e, sq=sq, sm=sm: e.reduce_sum(out=sm.ap[:, 0:8], in_=h3(sq.ap), axis=AX.X), [sm], [sq])
                    fw.op(act, lambda e, sm=sm: e.activation(out=sm.ap[:, 8:16], in_=sm.ap[:, 0:8], func=AF.Sqrt), [sm], [sm])
                    fw.op(dve, lambda e, sm=sm: e.tensor_scalar(out=sm.ap[:, 8:16], in0=sm.ap[:, 8:16], scalar1=1e-12, scalar2=None, op0=ALU.max), [sm], [sm])
                    fw.op(dve, lambda e, sm=sm: e.reciprocal(out=sm.ap[:, 16:24], in_=sm.ap[:, 8:16]), [sm], [sm])
                    fw.op(dve, lambda e, sm=sm, kk0=kk0: e.tensor_tensor(out=h3(FO[:, FKK:FKK + 512]), in0=h3(kk0.ap), in1=sm.ap[:, 16:24].unsqueeze(2).to_broadcast([128, 8, 64]), op=ALU.mult), [fo], [kk0, sm])
                    for d_ in range(2):
                        t1 = tmp_pool.next()
                        fw.op(dve, lambda e, t1=t1, d_=d_: e.scalar_tensor_tensor(out=t1.ap, in0=a_d[d_].ap, scalar=-1.0, in1=kab.ap, op0=ALU.add, op1=ALU.mult), [t1], [a_d[d_], kab])
                        fw.op(dve, lambda e, t1=t1, d_=d_: e.scalar_tensor_tensor(out=FO[:, FKD(d_):FKD(d_) + 512], in0=t1.ap, scalar=1.0, in1=k_, op0=ALU.add, op1=ALU.mult), [fo], [t1, uc])
                        fw.op(pool, lambda e, d_=d_: e.tensor_tensor(out=FO[:, FB(d_):FB(d_) + 512], in0=FO[:, FKK:FKK + 512], in1=a_d[d_].ap, op=ALU.mult), [fo], [fo, a_d[d_]])
                    t1 = tmp_pool.next()
                    sm2 = sm_pool.next()
                    fw.op(pool, lambda e, t1=t1: e.tensor_tensor(out=t1.ap, in0=FO[:, FKD(0):FKD(0) + 512], in1=FO[:, FKD(1):FKD(1) + 512], op=ALU.add), [t1], [fo])
                    fw.op(dve, lambda e, t1=t1: e.tensor_tensor(out=t1.ap, in0=t1.ap, in1=r_, op=ALU.mult), [t1], [t1, uc])
                    fw.op(pool, lambda e, t1=t1: e.tensor_tensor(out=t1.ap, in0=t1.ap, in1=rkb.ap, op=ALU.mult), [t1], [t1, rkb])
                    fw.op(dve, lambda e, t1=t1, sm2=sm2: e.reduce_sum(out=sm2.ap[:, 0:8], in_=h3(t1.ap), axis=AX.X), [sm2], [t1])
                    fw.op(dve, lambda e, sm2=sm2: e.tensor_tensor(out=h3(FO[:, FBON:FBON + 512]), in0=h3(FO[:, FV:FV + 512]), in1=sm2.ap[:, 0:8].unsqueeze(2).to_broadcast([128, 8, 64]), op=ALU.mult), [fo], [fo, sm2])
                    fw.dma(feat[i * 128:(i + 1) * 128, :], FO, [feat_t[i]], [fo])
                    if "feat" in dbg and l == 0:
                        fw.dma(dbg["feat"][i * 128:(i + 1) * 128, :], FO[:, 0:4096], [], [fo])
                fw.barrier()
            if stop_after == "S4a":
                break
            with ExitStack() as es:
                ones = sb(es, nc, "ones", [128, 1], F32)
                fw.op(dve, lambda e: e.memset(ones.ap, 1.0), [ones], [])
                Mq1, MnT = [], []
                for d_ in range(2):
                    m1_ = sb(es, nc, "Mq1", [128, 256], F32)
                    fw.op(dve, lambda e, m1_=m1_, d_=d_: e.tensor_scalar(out=m1_.ap[:, 0:128], in0=masks.ap[:, 2 * d_, :], scalar1=-1.0, scalar2=None, op0=ALU.mult), [m1_], [masks])
                    fw.op(dve, lambda e, m1_=m1_, d_=d_: e.tensor_copy(out=m1_.ap[:, 128:256], in_=masks.ap[:, 2 * d_ + 1, :]), [m1_], [masks])
                    Mq1.append(m1_)
                    mt_ = sb(es, nc, "MnT", [128, 128], F32)
                    fw.op(dve, lambda e, mt_=mt_, d_=d_: e.tensor_scalar(out=mt_.ap, in0=masks.ap[:, 2 * (1 - d_), :], scalar1=-1.0, scalar2=None, op0=ALU.mult), [mt_], [masks])
                    MnT.append(mt_)
                Mq2 = [masks.ap[:, 2 * d_:2 * d_ + 2, :].rearrange("p a t -> p (a t)") for d_ in range(2)]
                PSQB = int(os.environ.get('S4B_PSQB', '4'))
                PSQ = [TT(PS[PSQB + k // 4].ap[:, (k % 4) * 128:(k % 4 + 1) * 128]) for k in range(16)]
                pq = [0]

                def psq():
                    t_ = PSQ[pq[0] % 16]
                    pq[0] += 1
                    return t_
                p4 = [0]

                def psumR():
                    t_ = PS[p4[0] % 8]
                    p4[0] += 1
                    return t_
                fa_pool = Pool(es, nc, "sfa", [128, 3, 512], F32, 3)
                fb_pool = Pool(es, nc, "sfb", [128, 3, 512], F32, 3)
                tm_pool = Pool(es, nc, "stm", [128, 4, 512], F32, 2)
                ex_pool = Pool(es, nc, "sex", [128, 3, 512], F32, 2)
                fm_pool = Pool(es, nc, "sfm", [128, 4, 4, 128], F32, 2)
                el_pool = Pool(es, nc, "sel", [128, 4], F32, 2)
                a1_pool = Pool(es, nc, "sa1", [128, 256], F32, 16)
                a2_pool = Pool(es, nc, "sa2", [128, 256], F32, 16)
                inv_pool = Pool(es, nc, "sinv", [128, 128], F32, 64)
                xs_pool = Pool(es, nc, "sxs", [128, 64], F32, 16)
                nu_pool = Pool(es, nc, "snu", [128, 128], F32, 8)
                ys_pool = Pool(es, nc, "sys", [128, 512], F32, 2)
                ST = [[[sb(es, nc, "ST", [128, 64], F32) for c_ in range(4)] for pp in range(2)] for d_ in range(2)]
                for d_ in range(2):
                    for c_ in range(4):
                        fw.op(dve, lambda e, d_=d_, c_=c_: e.memset(ST[d_][0][c_].ap, 0.0), [ST[d_][0][c_]], [])
                orders = [list(range(NT)), [1, 0] + list(range(NT - 1, NCT - 1, -1))]
                evi = [0]

                def evac(fn_dve, fn_act, outs, ins):
                    if evi[0] % 2:
                        fw.op(dve, fn_dve, outs, ins)
                    else:
                        fw.op(act, fn_act, outs, ins)

                def s4b_load(d_, i):
                    fa, fb = fa_pool.next(), fb_pool.next()
                    r0 = i * 128
                    fw.dma(fa.ap, feat[r0:r0 + 128, FLW(d_):FLW(d_) + 1536].rearrange("p (a c) -> p a c", a=3), [fa], [feat_t[i]])
                    fw.dma(fb.ap[:, 0:2, :], feat[r0:r0 + 128, 0:1024].rearrange("p (a c) -> p a c", a=2), [fb], [feat_t[i]])
                    fw.dma(fb.ap[:, 2, :], feat[r0:r0 + 128, FKK:FKK + 512], [fb], [feat_t[i]])
                    return fa, fb

                steps = [(d_, j) for j in range(NT) for d_ in range(2)]
                PH = int(os.environ.get('S4B_PHASE', '9'))
                if os.environ.get('S4B_STEPS'):
                    steps = steps[:int(os.environ['S4B_STEPS'])]
                nxt = s4b_load(0, orders[0][0])
                for si, (d_, j) in enumerate(steps):
                    i = orders[d_][j]
                    fa, fb = nxt
                    if si + 1 < len(steps):
                        nd, nj = steps[si + 1]
                        nxt = s4b_load(nd, orders[nd][nj])
                    pp = j % 2
                    STo, STn = ST[d_][pp], ST[d_][1 - pp]
                    lw, b_, kd = fa.ap[:, 0, :], fa.ap[:, 1, :], fa.ap[:, 2, :]
                    v_, r_, kk = fb.ap[:, 0, :], fb.ap[:, 1, :], fb.ap[:, 2, :]
                    psL = psumR()
                    fw.op(pe, lambda e: e.matmul(psL.ap, lhsT=masks.ap[:, 1 + 2 * d_, :], rhs=lw, start=True, stop=True), [psL], [masks, fa])
                    ex = ex_pool.next()
                    fw.op(act, lambda e: e.activation(out=ex.ap[:, 0, :], in_=psL.ap, func=AF.Exp), [ex], [psL])
                    fw.op(act, lambda e: e.activation(out=ex.ap[:, 1, :], in_=psL.ap, func=AF.Exp, scale=-1.0), [ex], [psL])
                    fw.op(dve, lambda e: e.tensor_tensor(out=ex.ap[:, 2, :], in0=psL.ap, in1=lw, op=ALU.subtract), [ex], [psL, fa])
                    fw.op(act, lambda e: e.activation(out=ex.ap[:, 2, :], in_=ex.ap[:, 2, :], func=AF.Exp), [ex], [ex])
                    tm = tm_pool.next()
                    fw.op(dve, lambda e: e.tensor_tensor(out=tm.ap[:, 0, :], in0=kk, in1=ex.ap[:, 2, :], op=ALU.mult), [tm], [fb, ex])
                    fw.op(pool, lambda e: e.tensor_tensor(out=tm.ap[:, 1, :], in0=r_, in1=ex.ap[:, 0, :], op=ALU.mult), [tm], [fb, ex])
                    fw.op(dve, lambda e: e.tensor_tensor(out=tm.ap[:, 2, :], in0=b_, in1=ex.ap[:, 1, :], op=ALU.mult), [tm], [fa, ex])
                    fw.op(pool, lambda e: e.tensor_tensor(out=tm.ap[:, 3, :], in0=kd, in1=ex.ap[:, 1, :], op=ALU.mult), [tm], [fa, ex])
                    if PH < 1:
                        continue
                    psE = psumR()
                    for c_ in range(4):
                        fw.op(pe, lambda e, c_=c_: e.matmul(psE.ap[:, c_:c_ + 1], lhsT=lw[:, c_ * 128:(c_ + 1) * 128], rhs=ones.ap, start=True, stop=True), [psE], [fa, ones])
                    el = el_pool.next()
                    fw.op(act, lambda e: e.activation(out=el.ap, in_=psE.ap[:, 0:4], func=AF.Exp), [el], [psE])
                    fm = fm_pool.next()
                    for c_ in range(4):
                        psT = psumR()
                        for q in range(4):
                            fw.op(pe, lambda e, c_=c_, q=q, psT=psT: e.transpose(out=psT.ap[:, q * 128:(q + 1) * 128], in_=tm.ap[:, q, c_ * 128:(c_ + 1) * 128], identity=ident.ap), [psT], [tm, ident])
                        evi[0] += 1
                        evac(lambda e, c_=c_, psT=psT: e.tensor_copy(out=fm.ap[:, c_, :, :], in_=psT.ap.rearrange("p (a b) -> p a b", a=4)),
                             lambda e, c_=c_, psT=psT: e.copy(out=fm.ap[:, c_, :, :], in_=psT.ap.rearrange("p (a b) -> p a b", a=4)), [fm], [psT])
                    if PH < 2:
                        continue
                    H = []
                    for h in range(8):
                        c_ = h // 2
                        hp = slice((h % 2) * 64, (h % 2) * 64 + 64)
                        H.append(dict(c=c_, hp=hp, al=fm.ap[hp, c_, 0, :], rh=fm.ap[hp, c_, 1, :], be=fm.ap[hp, c_, 2, :], ka=fm.ap[hp, c_, 3, :],
                                      ar=fm.ap[hp, c_, 0:2, :].rearrange("p a t -> p (a t)"), bk=fm.ap[hp, c_, 2:4, :].rearrange("p a t -> p (a t)"), hh=h % 2, vh=v_[:, h * 64:(h + 1) * 64]))
                    if PH < 3:
                        continue
                    def phase4(mm, ev):
                        for par in (0, 1):
                            bank = psumR()
                            hs = [par, par + 2, par + 4, par + 6]
                            for k_, h in enumerate(hs):
                                mm(H[h], bank.ap[:, k_ * 128:(k_ + 1) * 128], bank)
                            evi[0] += 1
                            for k_, h in enumerate(hs):
                                ev(H[h], bank.ap[:, k_ * 128:(k_ + 1) * 128], bank)

                    for h in range(8):
                        u = H[h]
                        psG = psumR()
                        fw.op(pe, lambda e, u=u, psG=psG: e.matmul(psG.ap[:, 0:256], lhsT=u["be"], rhs=u["ar"], start=True, stop=True), [psG], [fm])
                        fw.op(pe, lambda e, u=u, psG=psG: e.matmul(psG.ap[:, 256:512], lhsT=u["ka"], rhs=u["ar"], start=True, stop=True), [psG], [fm])
                        a1, a2 = a1_pool.next(), a2_pool.next()
                        fw.op(dve, lambda e, a1=a1, psG=psG: e.tensor_tensor(out=a1.ap, in0=psG.ap[:, 0:256], in1=Mq1[d_].ap, op=ALU.mult), [a1], [psG, Mq1[d_]])
                        fw.op(dve, lambda e, a2=a2, psG=psG: e.tensor_tensor(out=a2.ap, in0=psG.ap[:, 256:512], in1=Mq2[d_], op=ALU.mult), [a2], [psG, masks])
                        M = inv_pool.next()
                        if not os.environ.get('NOM'):
                            fw.op(dve, lambda e, M=M, a1=a1: e.tensor_tensor(out=M.ap, in0=a1.ap[:, 0:128], in1=ident.ap, op=ALU.add), [M], [a1, ident])
                        u.update(a1=a1, a2=a2, A=a1, Aap=a1.ap[:, 0:128], M=M)

                    def mmQ(u, o, bank):
                        fw.op(pe, lambda e: e.transpose(out=o, in_=u["a1"].ap[:, 0:128], identity=ident.ap), [bank], [u["a1"], ident])

                    def evQ(u, o, bank):
                        AT = inv_pool.next()
                        fw.op(dve, lambda e: e.tensor_copy(out=AT.ap, in_=o), [AT], [bank])
                        u["AT"] = AT
                    if not os.environ.get('NOQ'):
                        phase4(mmQ, evQ)
                    if PH < 4:
                        continue
                    for it in range(6):
                        def mmB(u, o, bank):
                            fw.op(pe, lambda e: e.matmul(o, lhsT=u["Aap"], rhs=u["AT"].ap, start=True, stop=True), [bank], [u["AT"], u["A"]])

                        def evB(u, o, bank):
                            ATn = inv_pool.next()
                            evac(lambda e: e.tensor_copy(out=ATn.ap, in_=o), lambda e: e.copy(out=ATn.ap, in_=o), [ATn], [bank])
                            u["ATn"] = ATn

                        def mmA(u, o, bank):
                            fw.op(pe, lambda e: e.matmul(o, lhsT=u["AT"].ap, rhs=u["Aap"], start=True, stop=True), [bank], [u["AT"], u["A"]])

                        def evA(u, o, bank):
                            An = inv_pool.next()
                            evac(lambda e: e.tensor_copy(out=An.ap, in_=o), lambda e: e.copy(out=An.ap, in_=o), [An], [bank])
                            u["An"] = An
                        phase4(mmB, evB)
                        if it < 5:
                            phase4(mmA, evA)
                        for h in range(8):
                            u = H[h]
                            u["AT"] = u["ATn"]
                            if it < 5:
                                u["A"], u["Aap"] = u["An"], u["An"].ap

                        def mmM(u, o, bank):
                            fw.op(pe, lambda e: e.matmul(o, lhsT=u["AT"].ap, rhs=u["M"].ap, start=True, stop=True), [bank], [u["AT"], u["M"]])

                        def evM(u, o, bank):
                            Mn = inv_pool.next()
                            fw.op(dve, lambda e: e.tensor_tensor(out=Mn.ap, in0=o, in1=u["M"].ap, op=ALU.add), [Mn], [bank, u["M"]])
                            u["M"] = Mn
                        phase4(mmM, evM)
                    if PH < 5:
                        continue
                    nus = [nu_pool.next() for c_ in range(4)]
                    def mmX(u, o, bank):
                        fw.op(pe, lambda e: e.matmul(o[:, 0:64], lhsT=u["al"], rhs=STo[u["c"]].ap[u["hp"], :], start=True, stop=False), [bank], [fm, STo[u["c"]]])
                        fw.op(pe, lambda e: e.matmul(o[:, 0:64], lhsT=u["a2"].ap[:, 0:128], rhs=u["vh"], start=False, stop=True), [bank], [u["a2"], fb])

                    def evX(u, o, bank):
                        xsb = xs_pool.next()
                        evac(lambda e: e.tensor_copy(out=xsb.ap, in_=o[:, 0:64]), lambda e: e.copy(out=xsb.ap, in_=o[:, 0:64]), [xsb], [bank])
                        u["xs"] = xsb
                    phase4(mmX, evX)

                    def mmU(u, o, bank):
                        fw.op(pe, lambda e: e.matmul(o[:, 0:64], lhsT=u["M"].ap, rhs=u["xs"].ap, start=True, stop=True), [bank], [u["M"], u["xs"]])

                    def evU(u, o, bank):
                        nu = nus[u["c"]]
                        fw.op(act, lambda e: e.activation(out=nu.ap[:, u["hp"]], in_=o[:, 0:64], func=AF.Copy, scale=-1.0), [nu], [bank])
                    phase4(mmU, evU)
                    ysb = ys_pool.next()
                    for par in (0, 1):
                        psY = psumR()
                        for k_, h in enumerate([par, par + 2, par + 4, par + 6]):
                            u = H[h]
                            nu = nus[u["c"]]
                            ysl = psY.ap[:, k_ * 64:(k_ + 1) * 64]
                            fw.op(pe, lambda e, u=u, ysl=ysl: e.matmul(ysl, lhsT=u["rh"], rhs=STo[u["c"]].ap[u["hp"], :], start=True, stop=False), [psY], [fm, STo[u["c"]]])
                            fw.op(pe, lambda e, u=u, ysl=ysl, nu=nu: e.matmul(ysl, lhsT=u["a1"].ap[:, 128:256], rhs=nu.ap[:, u["hp"]], start=False, stop=False), [psY], [u["a1"], nu])
                            fw.op(pe, lambda e, u=u, ysl=ysl: e.matmul(ysl, lhsT=u["a2"].ap[:, 128:256], rhs=u["vh"], start=False, stop=True), [psY], [u["a2"], fb])
                        yv = ysb.ap.rearrange("p (c two d) -> p c two d", two=2, d=64)[:, :, par, :]
                        fw.op(act, lambda e, psY=psY, yv=yv: e.copy(out=yv, in_=psY.ap[:, 0:256].rearrange("p (c d) -> p c d", d=64)), [ysb], [psY])
                    fw.dma(y_scr[d_, i * 128:(i + 1) * 128, :], ysb.ap, [y_t[d_][i]], [ysb])
                    if PH < 6:
                        continue
                    psS = psumR()
                    for c_ in range(4):
                        so = psS.ap[:, c_ * 128:(c_ + 1) * 128]
                        fw.op(pe, lambda e, c_=c_, so=so: e.matmul(so, lhsT=tm.ap[:, 2, c_ * 128:(c_ + 1) * 128], rhs=nus[c_].ap, start=True, stop=False), [psS], [tm, nus[c_]])
                        fw.op(pe, lambda e, c_=c_, so=so: e.matmul(so, lhsT=tm.ap[:, 3, c_ * 128:(c_ + 1) * 128], rhs=v_[:, c_ * 128:(c_ + 1) * 128], start=False, stop=True), [psS], [tm, fb])
                        fw.op(pool, lambda e, c_=c_: e.tensor_scalar(out=STn[c_].ap, in0=STo[c_].ap, scalar1=el.ap[:, c_:c_ + 1], scalar2=None, op0=ALU.mult), [STn[c_]], [STo[c_], el])
                    for c_ in range(4):
                        so = psS.ap[:, c_ * 128:(c_ + 1) * 128]
                        for hh in range(2):
                            hp = slice(hh * 64, hh * 64 + 64)
                            fw.op(dve, lambda e, c_=c_, hp=hp, so=so: e.scalar_tensor_tensor(out=STn[c_].ap[hp, :], in0=so[hp, hp], scalar=el.ap[hp, c_:c_ + 1], in1=STn[c_].ap[hp, :], op0=ALU.mult, op1=ALU.add), [STn[c_]], [psS, el, STn[c_]])
                fw.barrier()
            if stop_after == "S4b":
                break
            with ExitStack() as es:
                lnw = sb(es, nc, "lnw", [128, RW], F32)
                lnb = sb(es, nc, "lnb", [128, RW], F32)
                fw.dma(lnw.ap, ln_x_w[l].partition_broadcast(128), [lnw], [])
                fw.dma(lnb.ap, ln_x_b[l].partition_broadcast(128), [lnb], [])
                y0_pool = Pool(es, nc, "cy0", [128, RW], F32, 2)
                y1_pool = Pool(es, nc, "cy1", [128, RW], F32, 2)
                gb_pool = Pool(es, nc, "cgb", [128, 2, RW], F32, 2)
                sq_pool = Pool(es, nc, "csq", [128, RW], F32, 2)
                cs_pool = Pool(es, nc, "ccs", [128, 32], F32, 2)
                h3 = lambda ap: ap.rearrange("p (h d) -> p h d", h=8)
                bc8 = lambda ap: ap.unsqueeze(2).to_broadcast([128, 8, 64])
                for i in (range(NCT, NT) if last else range(NT)):
                    y0, y1, gb = y0_pool.next(), y1_pool.next(), gb_pool.next()
                    r0 = i * 128
                    fw.dma(y0.ap, y_scr[0, r0:r0 + 128, :], [y0], [y_t[0][i]])
                    fw.dma(y1.ap, y_scr[1, r0:r0 + 128, :], [y1], [y_t[1][i]])
                    fw.dma(gb.ap[:, 0, :], feat[r0:r0 + 128, FG:FG + 512], [gb], [feat_t[i]])
                    fw.dma(gb.ap[:, 1, :], feat[r0:r0 + 128, FBON:FBON + 512], [gb], [feat_t[i]])
                    cs_ = cs_pool.next()
                    sq = sq_pool.next()
                    fw.op(dve, lambda e: e.tensor_tensor(out=y0.ap, in0=y0.ap, in1=y1.ap, op=ALU.add), [y0], [y0, y1])
                    fw.op(dve, lambda e: e.reduce_sum(out=cs_.ap[:, 0:8], in_=h3(y0.ap), axis=AX.X), [cs_], [y0])
                    fw.op(dve, lambda e: e.tensor_scalar(out=cs_.ap[:, 0:8], in0=cs_.ap[:, 0:8], scalar1=1.0 / 64, scalar2=None, op0=ALU.mult), [cs_], [cs_])
                    fw.op(dve, lambda e: e.tensor_tensor(out=h3(y0.ap), in0=h3(y0.ap), in1=bc8(cs_.ap[:, 0:8]), op=ALU.subtract), [y0], [y0, cs_])
                    fw.op(pool, lambda e: e.tensor_tensor(out=sq.ap, in0=y0.ap, in1=y0.ap, op=ALU.mult), [sq], [y0])
                    fw.op(dve, lambda e: e.reduce_sum(out=cs_.ap[:, 8:16], in_=h3(sq.ap), axis=AX.X), [cs_], [sq])
                    fw.op(dve, lambda e: e.tensor_scalar(out=cs_.ap[:, 8:16], in0=cs_.ap[:, 8:16], scalar1=1.0 / 64, scalar2=64e-5, op0=ALU.mult, op1=ALU.add), [cs_], [cs_])
                    fw.op(act, lambda e: e.activation(out=cs_.ap[:, 16:24], in_=cs_.ap[:, 8:16], func=AF.Sqrt), [cs_], [cs_])
                    fw.op(dve, lambda e: e.reciprocal(out=cs_.ap[:, 24:32], in_=cs_.ap[:, 16:24]), [cs_], [cs_])
                    fw.op(dve, lambda e: e.tensor_tensor(out=h3(y0.ap), in0=h3(y0.ap), in1=bc8(cs_.ap[:, 24:32]), op=ALU.mult), [y0], [y0, cs_])
                    fw.op(pool, lambda e: e.tensor_tensor(out=y0.ap, in0=y0.ap, in1=lnw.ap, op=ALU.mult), [y0], [y0, lnw])
                    fw.op(dve, lambda e: e.tensor_tensor(out=y0.ap, in0=y0.ap, in1=lnb.ap, op=ALU.add), [y0], [y0, lnb])
                    fw.op(pool, lambda e: e.tensor_tensor(out=y0.ap, in0=y0.ap, in1=gb.ap[:, 1, :], op=ALU.add), [y0], [y0, gb])
                    fw.op(dve, lambda e: e.tensor_tensor(out=y0.ap, in0=y0.ap, in1=gb.ap[:, 0, :], op=ALU.mult), [y0], [y0, gb])
                    fw.dma(mix[r0:r0 + 128, 512:1024], y0.ap, [mix_r[i]], [y0])
                fw.barrier()
            if stop_after == "S4":
                if "mix" in dbg:
                    for i in range(NT):
                        fw.dma(dbg["mix"][i * 128:(i + 1) * 128, :], mix[i * 128:(i + 1) * 128, :], [], [mix_a[i], mix_r[i]])
                    fw.barrier()
                break
            tiles5 = list(range(NCT, NT)) if last else list(range(NT))
            with ExitStack() as es:
                woutb = sb(es, nc, "woutb", [128, 8, D], BF16)
                wst_pool = Pool(es, nc, "wst5", [128, 8, 256], F32, 2)
                for j in range(4):
                    wst = wst_pool.next()
                    fw.dma(wst.ap, w_out[l][:, j * 256:(j + 1) * 256].rearrange("(kc p) n -> p kc n", p=128), [wst], [])
                    fw.op(pool, lambda e, wst=wst, j=j: e.tensor_copy(out=woutb.ap[:, :, j * 256:(j + 1) * 256], in_=wst.ap), [woutb], [wst])
                modG = [sb(es, nc, "modG%d" % s_, [128, D], F32) for s_ in range(2)]
                for s_ in range(2):
                    fw.dma(modG[s_].ap, modbuf[l, s_ * 6 + 2], [modG[s_]], [mod_t[l][s_ * 6 + 2]])
                mx_pool = Pool(es, nc, "mx", [128, D], F32, 2)
                mxb_pool = Pool(es, nc, "mxb", [128, D], BF16, 2)
                mT_pool = Pool(es, nc, "mT", [128, 8, 128], BF16, 2)
                x5_pool = Pool(es, nc, "x5", [128, D], F32, 2)

                def s5_load(i):
                    mx = mx_pool.next()
                    fw.dma(mx.ap, mix[i * 128:(i + 1) * 128, :], [mx], [mix_a[i], mix_r[i]])
                    xt = x5_pool.next()
                    fw.dma(xt.ap, xs[i * 128:(i + 1) * 128, :], [xt], [xs_t[i]])
                    return mx, xt

                nxt = s5_load(tiles5[0])
                for ti, i in enumerate(tiles5):
                    mx, xt = nxt
                    if ti + 1 < len(tiles5):
                        nxt = s5_load(tiles5[ti + 1])
                    s_ = 1 if i < NCT else 0
                    mxb = mxb_pool.next()
                    fw.op(pool, lambda e, mx=mx, mxb=mxb: e.tensor_copy(out=mxb.ap, in_=mx.ap), [mxb], [mx])
                    mT = mT_pool.next()
                    for half in range(2):
                        ps = psum()
                        psb = ps.ap.bitcast(BF16)
                        for kk in range(4):
                            kc = half * 4 + kk
                            fw.op(pe, lambda e, kc=kc, kk=kk, psb=psb, mxb=mxb: e.transpose(out=psb[:, kk * 128:(kk + 1) * 128], in_=mxb.ap[:, kc * 128:(kc + 1) * 128], identity=identb.ap), [ps], [mxb, identb])
                        fw.op(act if half else dve, lambda e, psb=psb, mT=mT, half=half: (e.copy if half else e.tensor_copy)(out=mT.ap[:, half * 4:(half + 1) * 4, :], in_=psb[:, 0:512].rearrange("p (a b) -> p a b", a=4)), [mT], [ps])
                    for half in range(2):
                        ps = psum()
                        hs = slice(half * 512, (half + 1) * 512)
                        for kc in range(8):
                            fw.op(pe, lambda e, kc=kc, ps=ps, mT=mT, hs=hs: e.matmul(ps.ap, lhsT=mT.ap[:, kc, :], rhs=woutb.ap[:, kc, hs], start=(kc == 0), stop=(kc == 7)), [ps], [mT, woutb])
                        fw.op(dve, lambda e, ps=ps, mx=mx, hs=hs, s_=s_: e.tensor_tensor(out=mx.ap[:, hs], in0=ps.ap, in1=modG[s_].ap[:, hs], op=ALU.mult), [mx], [ps, modG[s_]])
                    fw.op(pool, lambda e, xt=xt, mx=mx: e.tensor_tensor(out=xt.ap, in0=xt.ap, in1=mx.ap, op=ALU.add), [xt], [xt, mx])
                    fw.dma(xs[i * 128:(i + 1) * 128, :], xt.ap, [xs_t[i]], [xt])
                    if "xmix" in dbg and l == 0:
                        fw.dma(dbg["xmix"][i * 128:(i + 1) * 128, :], xt.ap, [], [xt])
                fw.barrier()
            if stop_after == "S5":
                break

            tiles6 = list(range(NCT, NT)) if last else list(range(NT))
            GS = 8
            groups = [tiles6[k:k + GS] for k in range(0, len(tiles6), GS)]
            with ExitStack() as es:
                mA = [sb(es, nc, "mA%d" % s_, [128, D], F32) for s_ in range(2)]
                mB = [sb(es, nc, "mB%d" % s_, [128, D], F32) for s_ in range(2)]
                mG = [sb(es, nc, "mG%d" % s_, [128, D], F32) for s_ in range(2)]
                for s_ in range(2):
                    fw.dma(mA[s_].ap, modbuf[l, s_ * 6 + 3], [mA[s_]], [mod_t[l][s_ * 6 + 3]])
                    fw.dma(mB[s_].ap, modbuf[l, s_ * 6 + 4], [mB[s_]], [mod_t[l][s_ * 6 + 4]])
                    fw.dma(mG[s_].ap, modbuf[l, s_ * 6 + 5], [mG[s_]], [mod_t[l][s_ * 6 + 5]])
                rwt = sb(es, nc, "rwt", [128, 8, NE], F32)
                fw.dma(rwt.ap, router_w.rearrange("(kc p) n -> p kc n", p=128), [rwt], [])
                rbt = sb(es, nc, "rbt", [128, NE], F32)
                fw.dma(rbt.ap, router_b.partition_broadcast(128), [rbt], [])
                fgt = None
                if last:
                    fgt = sb(es, nc, "fgt", [128, D], F32)
                    fw.dma(fgt.ap, final_g.partition_broadcast(128), [fgt], [])
                HT = sb(es, nc, "HT", [128, 8, GS * 128], BF16)
                accs = [sb(es, nc, "acc%d" % k, [128, D], F32) for k in range(GS)]
                gates = sb(es, nc, "gates", [128, GS, NE], F32)
                Wg = sb(es, nc, "Wg", [128, 8, D], BF16)
                Wu = sb(es, nc, "Wu", [128, 8, D], BF16)
                Wd = sb(es, nc, "Wd", [128, 8, D], BF16)
                w6_pool = Pool(es, nc, "w6", [128, 8, 256], F32, 3)
                actb = sb(es, nc, "actb", [128, 8, GS * 128], BF16)
                x6_pool = Pool(es, nc, "x6", [128, D], F32, 2)
                junk6 = sb(es, nc, "junk6", [128, D], F32)
                hT32_pool = Pool(es, nc, "hT32", [128, 8, 128], F32, 2)
                rt_pool = Pool(es, nc, "rt", [128, 96], F32, 2)
                sg_pool = Pool(es, nc, "sg", [128, 512], F32, 2)
                st6_pool = Pool(es, nc, "st6", [128, 4], F32, 2)

                def rms(xt, st):
                    fw.op(act, lambda e: e.activation(out=junk6.ap, in_=xt.ap, func=AF.Square, accum_out=st.ap[:, 0:1]), [junk6, st], [xt])
                    fw.op(dve, lambda e: e.tensor_scalar(out=st.ap[:, 1:2], in0=st.ap[:, 0:1], scalar1=1.0 / D, scalar2=1e-6, op0=ALU.mult, op1=ALU.add), [st], [st])
                    fw.op(act, lambda e: e.activation(out=st.ap[:, 3:4], in_=st.ap[:, 1:2], func=AF.Sqrt), [st], [st])
                    fw.op(dve, lambda e: e.reciprocal(out=st.ap[:, 2:3], in_=st.ap[:, 3:4]), [st], [st])

                def load_w(dst, src):
                    for j in range(4):
                        wst = w6_pool.next()
                        fw.dma(wst.ap, src[:, j * 256:(j + 1) * 256].rearrange("(kc p) n -> p kc n", p=128), [wst], [])
                        fw.op(pool, lambda e, wst=wst, j=j: e.tensor_copy(out=dst.ap[:, :, j * 256:(j + 1) * 256], in_=wst.ap), [dst], [wst])

                for grp in groups:
                    ntok = len(grp) * 128
                    for t, i in enumerate(grp):
                        s_ = 1 if i < NCT else 0
                        xt = x6_pool.next()
                        fw.dma(xt.ap, xs[i * 128:(i + 1) * 128, :], [xt], [xs_t[i]])
                        st = st6_pool.next()
                        rms(xt, st)
                        fw.op(dve, lambda e, xt=xt, st=st, s_=s_: e.scalar_tensor_tensor(out=xt.ap, in0=xt.ap, scalar=st.ap[:, 2:3], in1=mA[s_].ap, op0=ALU.mult, op1=ALU.mult), [xt], [xt, st, mA[s_]])
                        fw.op(pool, lambda e, xt=xt, s_=s_: e.tensor_tensor(out=xt.ap, in0=xt.ap, in1=mB[s_].ap, op=ALU.add), [xt], [xt, mB[s_]])
                        hT32 = hT32_pool.next()
                        for half in range(2):
                            ps = psum()
                            for kk in range(4):
                                kc = half * 4 + kk
                                fw.op(pe, lambda e, kc=kc, kk=kk, ps=ps, xt=xt: e.transpose(out=ps.ap[:, kk * 128:(kk + 1) * 128], in_=xt.ap[:, kc * 128:(kc + 1) * 128], identity=ident.ap), [ps], [xt, ident])
                            fw.op(dve, lambda e, ps=ps, hT32=hT32, half=half: e.tensor_copy(out=hT32.ap[:, half * 4:(half + 1) * 4, :], in_=ps.ap.rearrange("p (a b) -> p a b", a=4)), [hT32], [ps])
                            fw.op(act, lambda e, hT32=hT32, half=half, t=t: e.copy(out=HT.ap[:, half * 4:(half + 1) * 4, t * 128:(t + 1) * 128], in_=hT32.ap[:, half * 4:(half + 1) * 4, :]), [HT], [hT32])
                        ps = psum()
                        for kc in range(8):
                            fw.op(pe, lambda e, kc=kc, ps=ps, hT32=hT32: e.matmul(ps.ap[:, 0:NE], lhsT=hT32.ap[:, kc, :], rhs=rwt.ap[:, kc, :], start=(kc == 0), stop=(kc == 7)), [ps], [hT32, rwt])
                        rt = rt_pool.next()
                        R = rt.ap
                        sc_, bi_ = R[:, 0:16], R[:, 16:32]
                        g4 = lambda ap: ap.rearrange("p (g m) -> p g m", g=4)
                        fw.op(act, lambda e, ps=ps, sc_=sc_: e.activation(out=sc_, in_=ps.ap[:, 0:NE], func=AF.Sigmoid), [rt], [ps])
                        fw.op(dve, lambda e, sc_=sc_, bi_=bi_: e.tensor_tensor(out=bi_, in0=sc_, in1=rbt.ap, op=ALU.add), [rt], [rt, rbt])
                        a = [g4(bi_)[:, :, m] for m in range(4)]
                        hi01, lo01, hi23, lo23 = R[:, 32:36], R[:, 36:40], R[:, 40:44], R[:, 44:48]
                        top1, m1, m2, top2 = R[:, 48:52], R[:, 52:56], R[:, 56:60], R[:, 60:64]
                        gs_, gmx, gm_, rd = R[:, 64:68], R[:, 68:69], R[:, 72:76], R[:, 69:70]
                        sel = R[:, 80:96]
                        tt = lambda o, x0, x1, op: fw.op(dve, lambda e: e.tensor_tensor(out=o, in0=x0, in1=x1, op=op), [rt], [rt])
                        tt(hi01, a[0], a[1], ALU.max)
                        tt(lo01, a[0], a[1], ALU.min)
                        tt(hi23, a[2], a[3], ALU.max)
                        tt(lo23, a[2], a[3], ALU.min)
                        tt(top1, hi01, hi23, ALU.max)
                        tt(m1, hi01, hi23, ALU.min)
                        tt(m2, lo01, lo23, ALU.max)
                        tt(top2, m1, m2, ALU.max)
                        tt(gs_, top1, top2, ALU.add)
                        fw.op(dve, lambda e, gs_=gs_, gmx=gmx: e.reduce_max(out=gmx, in_=gs_, axis=AX.X), [rt], [rt])
                        fw.op(dve, lambda e, gs_=gs_, gmx=gmx, gm_=gm_: e.tensor_scalar(out=gm_, in0=gs_, scalar1=gmx, scalar2=None, op0=ALU.is_ge), [rt], [rt])
                        tt(g4(sel), g4(bi_), top2.unsqueeze(2).to_broadcast([128, 4, 4]), ALU.is_ge)
                        tt(g4(sel), g4(sel), gm_.unsqueeze(2).to_broadcast([128, 4, 4]), ALU.mult)
                        tt(sel, sel, sc_, ALU.mult)
                        fw.op(dve, lambda e, sel=sel, rd=rd: e.reduce_sum(out=rd, in_=sel, axis=AX.X), [rt], [rt])
                        fw.op(dve, lambda e, rd=rd: e.reciprocal(out=rd, in_=rd), [rt], [rt])
                        fw.op(dve, lambda e, sel=sel, rd=rd, t=t: e.tensor_scalar(out=gates.ap[:, t, :], in0=sel, scalar1=rd, scalar2=None, op0=ALU.mult), [gates], [rt])
                        if "gates" in dbg and l == 0:
                            fw.dma(dbg["gates"][i * 128:(i + 1) * 128, :], gates.ap[:, t, :], [], [gates])
                    for ex in range(NE):
                        load_w(Wg, e_gate[l, ex])
                        load_w(Wu, e_up[l, ex])
                        for t0 in range(0, ntok, 512):
                            ntk = min(512, ntok - t0)
                            for fc in range(8):
                                psg = psum()
                                psu = psum()
                                for kc in range(8):
                                    fw.op(pe, lambda e, kc=kc, fc=fc, psg=psg, t0=t0, ntk=ntk: e.matmul(psg.ap[:, 0:ntk], lhsT=Wg.ap[:, kc, fc * 128:(fc + 1) * 128], rhs=HT.ap[:, kc, t0:t0 + ntk], start=(kc == 0), stop=(kc == 7)), [psg], [Wg, HT])
                                for kc in range(8):
                                    fw.op(pe, lambda e, kc=kc, fc=fc, psu=psu, t0=t0, ntk=ntk: e.matmul(psu.ap[:, 0:ntk], lhsT=Wu.ap[:, kc, fc * 128:(fc + 1) * 128], rhs=HT.ap[:, kc, t0:t0 + ntk], start=(kc == 0), stop=(kc == 7)), [psu], [Wu, HT])
                                sg = sg_pool.next()
                                fw.op(act, lambda e, psg=psg, sg=sg, ntk=ntk: e.activation(out=sg.ap[:, 0:ntk], in_=psg.ap[:, 0:ntk], func=AF.Silu), [sg], [psg])
                                fw.op(dve, lambda e, psu=psu, sg=sg, fc=fc, t0=t0, ntk=ntk: e.tensor_tensor(out=actb.ap[:, fc, t0:t0 + ntk], in0=sg.ap[:, 0:ntk], in1=psu.ap[:, 0:ntk], op=ALU.mult), [actb], [sg, psu])
                        load_w(Wd, e_down[l, ex])
                        for t in range(len(grp)):
                            for half in range(2):
                                hs = slice(half * 512, (half + 1) * 512)
                                ps = psum()
                                for fc in range(8):
                                    fw.op(pe, lambda e, fc=fc, ps=ps, t=t, hs=hs: e.matmul(ps.ap, lhsT=actb.ap[:, fc, t * 128:(t + 1) * 128], rhs=Wd.ap[:, fc, hs], start=(fc == 0), stop=(fc == 7)), [ps], [actb, Wd])
                                if ex == 0:
                                    fw.op(dve, lambda e, ps=ps, t=t, hs=hs, ex=ex: e.tensor_scalar(out=accs[t].ap[:, hs], in0=ps.ap, scalar1=gates.ap[:, t, ex:ex + 1], scalar2=None, op0=ALU.mult), [accs[t]], [ps, gates])
                                else:
                                    fw.op(dve, lambda e, ps=ps, t=t, hs=hs, ex=ex: e.scalar_tensor_tensor(out=accs[t].ap[:, hs], in0=ps.ap, scalar=gates.ap[:, t, ex:ex + 1], in1=accs[t].ap[:, hs], op0=ALU.mult, op1=ALU.add), [accs[t]], [ps, gates, accs[t]])
                    for t, i in enumerate(grp):
                        s_ = 1 if i < NCT else 0
                        xt = x6_pool.next()
                        fw.dma(xt.ap, xs[i * 128:(i + 1) * 128, :], [xt], [xs_t[i]])
                        fw.op(pool, lambda e, t=t, s_=s_: e.tensor_tensor(out=accs[t].ap, in0=accs[t].ap, in1=mG[s_].ap, op=ALU.mult), [accs[t]], [accs[t], mG[s_]])
                        fw.op(dve, lambda e, xt=xt, t=t: e.tensor_tensor(out=xt.ap, in0=xt.ap, in1=accs[t].ap, op=ALU.add), [xt], [xt, accs[t]])
                        if "xout" in dbg and l == 0:
                            fw.dma(dbg["xout"][i * 128:(i + 1) * 128, :], xt.ap, [], [xt])
                        if not last:
                            fw.dma(xs[i * 128:(i + 1) * 128, :], xt.ap, [xs_t[i]], [xt])
                        else:
                            st = st6_pool.next()
                            rms(xt, st)
                            fw.op(dve, lambda e, xt=xt, st=st: e.scalar_tensor_tensor(out=xt.ap, in0=xt.ap, scalar=st.ap[:, 2:3], in1=fgt.ap, op0=ALU.mult, op1=ALU.mult), [xt], [xt, st, fgt])
                            fw.dma(out[(i - NCT) * 128:(i - NCT + 1) * 128, :], xt.ap, [], [xt])
                fw.barrier()
            if stop_after == "S6":
                break
        fw.barrier()
    return nc


def prep_inputs(inputs):
    f = lambda a: np.ascontiguousarray(np.asarray(a, dtype=np.float32))
    shared = {}
    for k in ("w_mod", "b_mod", "norm_mix_g", "norm_ffn_g", "w_in", "w_out", "att_sink", "shift_mu_prev", "shift_mu_next",
              "decay_w0", "decay_w2", "iclr_a0", "iclr_a2", "vres_v0", "vres_v1", "vres_v2", "gate_g2", "k_k", "k_a",
              "ln_x_w", "ln_x_b", "router_w", "router_b", "expert_w_gate", "expert_w_up", "expert_w_down", "final_norm_g"):
        shared[k] = f(inputs[k])
    shared["r_k"] = f(inputs["r_k"]).reshape(DEPTH, RW)
    shared["ident"] = np.eye(128, dtype=np.float32)
    s = np.arange(128)[:, None]
    t = np.arange(128)[None, :]
    shared["masks"] = np.ascontiguousarray(np.stack([s < t, s <= t, s > t, s >= t], axis=1).astype(np.float32))
    pos = np.arange(L)
    row = (pos // 64).astype(np.float32)
    colp = (pos % 64).astype(np.float32)
    inv = (10000.0 ** (-np.arange(16, dtype=np.float32) / 16)).astype(np.float32)
    ang = np.concatenate([row[:, None] * inv, row[:, None] * inv, colp[:, None] * inv, colp[:, None] * inv], axis=1)
    cos = np.cos(ang).astype(np.float32)
    sin = np.sin(ang).astype(np.float32)
    sgn = np.concatenate([-np.ones(16), np.ones(16), -np.ones(16), np.ones(16)]).astype(np.float32)
    shared["rope_cos"] = np.ascontiguousarray(np.tile(cos, (1, 10)))
    shared["rope_sin"] = np.ascontiguousarray(np.tile(sin * sgn, (1, 10)))
    x = f(inputs["x"])
    ctx = f(inputs["ctx"])
    c = f(inputs["c"])
    cc = f(inputs["c_ctx"])
    maps = []
    for b in range(8):
        m = dict(shared)
        m["x"] = x[b]
        m["ctx"] = ctx[b]
        m["ccol"] = np.ascontiguousarray(np.concatenate([c[b].reshape(8, 128).T, cc.reshape(8, 128).T], axis=1))
        maps.append(m)
    return maps


def kernel(**inputs):
    maps = prep_inputs(inputs)
    nc = build()
    res = run_bass_kernel_spmd(nc, maps, core_ids=list(range(8)))
    return np.stack([r["out"] for r in res.results], axis=0).astype(np.float32)
```

```python
from contextlib import ExitStack
import os
import numpy as np
import ml_dtypes
import concourse.bass as bass
import concourse.mybir as mybir
from concourse.bass_utils import run_bass_kernel_spmd

F32 = mybir.dt.float32
BF16 = mybir.dt.bfloat16
AF = mybir.ActivationFunctionType
ALU = mybir.AluOpType
AX = mybir.AxisListType

D = 1024
L = 4096
C = 256
T = L + C
NT = T // 128
NCT = C // 128
DEPTH = 2
IN_COLS = 2688
RW = 512
RWC = 1920
NE = 16
ATT_SCALE = 0.125
NEG = -30000.0


class Eng:
    def __init__(self, fw, e, name, ring=0):
        self.fw = fw
        self.e = e
        self.name = name
        self.count = 0
        self.seen = {}
        self.sem = fw.nc.alloc_semaphore("s_" + name)
        self.ring = [fw.nc.alloc_semaphore("r_%s%d" % (name, i)) for i in range(ring)]
        self.ndma = 0

    def wait_tok(self, tok):
        sem, val, key = tok
        if self.seen.get(key, 0) >= val:
            return
        self.e.wait_ge(sem, val)
        self.seen[key] = val


class TT:
    def __init__(self, ap=None):
        self.ap = ap
        self.w = None
        self.r = {}


class FW:
    def __init__(self, nc):
        self.nc = nc
        self.pe = Eng(self, nc.tensor, "pe")
        self.act = Eng(self, nc.scalar, "act")
        self.dve = Eng(self, nc.vector, "dve")
        self.pool = Eng(self, nc.gpsimd, "pool", ring=12)
        self.sp = Eng(self, nc.sync, "sp", ring=24)
        self.engs = [self.pe, self.act, self.dve, self.pool, self.sp]
        self.ninst = 0

    def _deps(self, eng, outs, ins):
        for t in ins:
            if t.w is not None and not (eng is self.pe and t.w[2] == "pe"):
                eng.wait_tok(t.w)
        for t in outs:
            if t.w is not None and not (eng is self.pe and t.w[2] == "pe"):
                eng.wait_tok(t.w)
            for tok in t.r.values():
                if not (eng is self.pe and tok[2] == "pe"):
                    eng.wait_tok(tok)

    def _mark(self, tok, outs, ins):
        for t in ins:
            t.r[tok[2]] = tok
        for t in outs:
            t.w = tok
            t.r = {}

    def op(self, eng, fn, outs, ins):
        self._deps(eng, outs, ins)
        inst = fn(eng.e)
        eng.count += 1
        inst.then_inc(eng.sem, 1)
        self._mark((eng.sem, eng.count, eng.name), outs, ins)
        self.ninst += 1

    def dma(self, out_ap, in_ap, outs, ins, q=None):
        q = q or self.sp
        self._deps(q, outs, ins)
        K = len(q.ring)
        slot = q.ndma % K
        rnd = q.ndma // K
        key = "%s_r%d" % (q.name, slot)
        if rnd > 0:
            q.wait_tok((q.ring[slot], 16 * rnd, key))
        q.e.dma_start(out=out_ap, in_=in_ap).then_inc(q.ring[slot], 16)
        q.ndma += 1
        self._mark((q.ring[slot], 16 * (rnd + 1), key), outs, ins)
        self.ninst += 1

    def all_toks(self):
        toks = []
        for e in self.engs:
            if e.count:
                toks.append((e.sem, e.count, e.name))
            K = len(e.ring)
            for slot in range(min(K, e.ndma)):
                n = (e.ndma - 1 - slot) // K + 1
                toks.append((e.ring[slot], 16 * n, "%s_r%d" % (e.name, slot)))
        return toks

    def barrier(self):
        toks = self.all_toks()
        for e in self.engs:
            for tok in toks:
                if e is self.pe and tok[2] == "pe":
                    continue
                e.wait_tok(tok)


_uid = [0]


def _nm(name):
    _uid[0] += 1
    return "sb_%s_%d" % (name, _uid[0])


class Pool:
    def __init__(self, es, nc, name, shape, dtype, bufs):
        self.t = [TT(es.enter_context(nc.sbuf_tensor(_nm(name), shape, dtype)).ap()) for i in range(bufs)]
        self.i = 0

    def next(self):
        t = self.t[self.i % len(self.t)]
        self.i += 1
        return t


def sb(es, nc, name, shape, dtype):
    return TT(es.enter_context(nc.sbuf_tensor(_nm(name), shape, dtype)).ap())


def build(debug_outs=(), stop_after=None):
    nc = bass.Bass("TRN2", target_bir_lowering=False)
    fw = FW(nc)
    pe, act, dve, pool, sp = fw.pe, fw.act, fw.dve, fw.pool, fw.sp

    def din(name, shape, dt=F32):
        return nc.dram_tensor(name, list(shape), dt, kind="ExternalInput").ap()

    def dscr(name, shape, dt=F32):
        return nc.dram_tensor(name, list(shape), dt, kind="Internal").ap()

    x_in = din("x", [L, D])
    ctx_in = din("ctx", [C, D])
    ccol_in = din("ccol", [128, 16])
    w_mod = din("w_mod", [DEPTH, D, 6 * D])
    b_mod = din("b_mod", [DEPTH, 6 * D])
    norm_mix_g = din("norm_mix_g", [DEPTH, D])
    norm_ffn_g = din("norm_ffn_g", [DEPTH, D])
    w_in = din("w_in", [DEPTH, D, IN_COLS])
    w_out = din("w_out", [DEPTH, D, D])
    att_sink = din("att_sink", [DEPTH, 8])
    mu_prev = din("shift_mu_prev", [DEPTH, RWC])
    mu_next = din("shift_mu_next", [DEPTH, RWC])
    decay_w0 = din("decay_w0", [DEPTH, 2, RW])
    decay_w2 = din("decay_w2", [DEPTH, 2, 64, RW])
    iclr_a0 = din("iclr_a0", [DEPTH, 2, RW])
    iclr_a2 = din("iclr_a2", [DEPTH, 2, 64, RW])
    vres_v0 = din("vres_v0", [1, RW])
    vres_v1 = din("vres_v1", [1, RW, 32])
    vres_v2 = din("vres_v2", [1, 32, RW])
    gate_g2 = din("gate_g2", [DEPTH, 128, RW])
    k_k = din("k_k", [DEPTH, RW])
    k_a = din("k_a", [DEPTH, RW])
    r_k = din("r_k", [DEPTH, RW])
    ln_x_w = din("ln_x_w", [DEPTH, RW])
    ln_x_b = din("ln_x_b", [DEPTH, RW])
    router_w = din("router_w", [D, NE])
    router_b = din("router_b", [NE])
    e_gate = din("expert_w_gate", [DEPTH, NE, D, D])
    e_up = din("expert_w_up", [DEPTH, NE, D, D])
    e_down = din("expert_w_down", [DEPTH, NE, D, D])
    final_g = din("final_norm_g", [D])
    ident_in = din("ident", [128, 128])
    masks_in = din("masks", [128, 4, 128])
    rope_cos = din("rope_cos", [L, 640])
    rope_sin = din("rope_sin", [L, 640])
    out = nc.dram_tensor("out", [L, D], F32, kind="ExternalOutput").ap()

    xs = dscr("xs", [T, D])
    xs_t = [TT() for _ in range(NT)]
    modbuf = dscr("modbuf", [DEPTH, 12, 128, D])
    mod_t = [[TT() for _ in range(12)] for _ in range(DEPTH)]
    u_scr = dscr("u_scr", [T + 4, RWC])
    u_t = [TT() for _ in range(NT)]
    u_guard = TT()
    mix = dscr("mix", [T, D])
    mix_a = [TT() for _ in range(NT)]
    mix_r = [TT() for _ in range(NT)]
    vfirst = dscr("vfirst", [T, RW])
    vf_t = [TT() for _ in range(NT)]
    feat = dscr("feat", [T, 5632])
    feat_t = [TT() for _ in range(NT)]
    y_scr = dscr("y_scr", [2, T, RW])
    y_t = [[TT() for _ in range(NT)] for _ in range(2)]
    dbg = {}
    for name, shape in debug_outs:
        dbg[name] = nc.dram_tensor("dbg_" + name, list(shape), F32, kind="ExternalOutput").ap()

    def urow(i):
        return 1 + i * 128 if i < NCT else 3 + i * 128

    PS = [TT(nc.alloc_psum_tensor("psb%d" % i, [128, 512], F32).ap()) for i in range(8)]
    psi = [0]

    def psum():
        t = PS[psi[0] % 6]
        psi[0] += 1
        return t

    with ExitStack() as g_es:
        ident = sb(g_es, nc, "ident", [128, 128], F32)
        identb = sb(g_es, nc, "identb", [128, 128], BF16)
        masks = sb(g_es, nc, "masks", [128, 4, 128], F32)
        fw.dma(ident.ap, ident_in, [ident], [])
        fw.dma(masks.ap, masks_in, [masks], [])
        fw.op(dve, lambda e: e.tensor_copy(out=identb.ap, in_=ident.ap), [identb], [ident])
        zrow = sb(g_es, nc, "zrow", [1, RWC], F32)
        fw.op(dve, lambda e: e.memset(zrow.ap, 0.0), [zrow], [])
        for r in (0, C + 1, C + 2, T + 3):
            fw.dma(u_scr[r:r + 1, :], zrow.ap, [u_guard], [zrow])

        for i in range(NT):
            src = ctx_in[i * 128:(i + 1) * 128, :] if i < NCT else x_in[(i - NCT) * 128:(i - NCT + 1) * 128, :]
            fw.dma(xs[i * 128:(i + 1) * 128, :], src, [xs_t[i]], [])

        for l in range(DEPTH):
            last = l == DEPTH - 1
            with ExitStack() as es:
                ccol = sb(es, nc, "ccol", [128, 16], F32)
                csil = sb(es, nc, "csil", [128, 16], F32)
                crep = sb(es, nc, "crep", [128, 16, 128], F32)
                fw.dma(ccol.ap, ccol_in, [ccol], [])
                fw.op(act, lambda e: e.activation(out=csil.ap, in_=ccol.ap, func=AF.Silu), [csil], [ccol])
                fw.op(dve, lambda e: e.tensor_copy(out=crep.ap, in_=csil.ap.unsqueeze(2).to_broadcast([128, 16, 128])), [crep], [csil])
                wm_pool = Pool(es, nc, "wm", [128, 8, 512], F32, 2)
                bm_pool = Pool(es, nc, "bm", [128, 512], F32, 2)
                modraw = [[sb(es, nc, "mr%d_%d" % (s, j), [128, D], F32) for j in range(6)] for s in range(2)]
                gmix = sb(es, nc, "gmix", [128, D], F32)
                gffn = sb(es, nc, "gffn", [128, D], F32)
                fw.dma(gmix.ap, norm_mix_g[l].partition_broadcast(128), [gmix], [])
                fw.dma(gffn.ap, norm_ffn_g[l].partition_broadcast(128), [gffn], [])
                for j in range(12):
                    wm = wm_pool.next()
                    bm = bm_pool.next()
                    fw.dma(wm.ap, w_mod[l][:, j * 512:(j + 1) * 512].rearrange("(kc p) n -> p kc n", p=128), [wm], [])
                    fw.dma(bm.ap, b_mod[l][j * 512:(j + 1) * 512].partition_broadcast(128), [bm], [])
                    for s in range(2):
                        ps = psum()
                        for kc in range(8):
                            fw.op(pe, lambda e, kc=kc, s=s, ps=ps, wm=wm: e.matmul(ps.ap, lhsT=crep.ap[:, s * 8 + kc, :], rhs=wm.ap[:, kc, :], start=(kc == 0), stop=(kc == 7)), [ps], [crep, wm])
                        dst = modraw[s][j // 2]
                        fw.op(dve, lambda e, ps=ps, dst=dst, bm=bm, j=j: e.tensor_tensor(out=dst.ap[:, (j % 2) * 512:(j % 2 + 1) * 512], in0=ps.ap, in1=bm.ap, op=ALU.add), [dst], [ps, bm])
                for s in range(2):
                    sh_a, sc_a, ga, sh_f, sc_f, gf = modraw[s]
                    fw.op(dve, lambda e, sc_a=sc_a: e.scalar_tensor_tensor(out=sc_a.ap, in0=sc_a.ap, scalar=1.0, in1=gmix.ap, op0=ALU.add, op1=ALU.mult), [sc_a], [sc_a, gmix])
                    fw.op(dve, lambda e, sc_f=sc_f: e.scalar_tensor_tensor(out=sc_f.ap, in0=sc_f.ap, scalar=1.0, in1=gffn.ap, op0=ALU.add, op1=ALU.mult), [sc_f], [sc_f, gffn])
                    for idx, t in enumerate((sc_a, sh_a, ga, sc_f, sh_f, gf)):
                        fw.dma(modbuf[l, s * 6 + idx], t.ap, [mod_t[l][s * 6 + idx]], [t])
                fw.barrier()
            if stop_after == "S1":
                break

            with ExitStack() as es_att:
                QT = sb(es_att, nc, "QT", [128, 4, T], BF16)
                KT = sb(es_att, nc, "KT", [128, T], BF16)
                VV = sb(es_att, nc, "VV", [128, NT, 128], BF16)
                qt_t = [TT() for _ in range(NT)]
                with ExitStack() as es:
                    winb = sb(es, nc, "winb", [128, 8, IN_COLS], BF16)
                    wst_pool = Pool(es, nc, "wst", [128, 8, 384], F32, 2)
                    for j in range(7):
                        wst = wst_pool.next()
                        fw.dma(wst.ap, w_in[l][:, j * 384:(j + 1) * 384].rearrange("(kc p) n -> p kc n", p=128), [wst], [])
                        fw.op(pool, lambda e, wst=wst, j=j: e.tensor_copy(out=winb.ap[:, :, j * 384:(j + 1) * 384], in_=wst.ap), [winb], [wst])
                    modA = [sb(es, nc, "modA%d" % s, [128, D], F32) for s in range(2)]
                    modB = [sb(es, nc, "modB%d" % s, [128, D], F32) for s in range(2)]
                    for s in range(2):
                        fw.dma(modA[s].ap, modbuf[l, s * 6 + 0], [modA[s]], [mod_t[l][s * 6 + 0]])
                        fw.dma(modB[s].ap, modbuf[l, s * 6 + 1], [modB[s]], [mod_t[l][s * 6 + 1]])
                    x_pool = Pool(es, nc, "xt", [128, D], F32, 2)
                    junk = sb(es, nc, "junk", [128, D], F32)
                    st_pool = Pool(es, nc, "st", [128, 4], F32, 2)
                    h_pool = Pool(es, nc, "ht", [128, D], BF16, 2)
                    hT_pool = Pool(es, nc, "hT", [128, 8, 128], BF16, 2)
                    pr_pool = Pool(es, nc, "pr", [128, IN_COLS], F32, 2)
                    cos_pool = Pool(es, nc, "cos", [128, 640], F32, 2)
                    sin_pool = Pool(es, nc, "sin", [128, 640], F32, 2)
                    rp_pool = Pool(es, nc, "rp", [128, 640], F32, 2)
                    rq_pool = Pool(es, nc, "rq", [128, 640], BF16, 2)

                    def s2_load(i):
                        xt = x_pool.next()
                        fw.dma(xt.ap, xs[i * 128:(i + 1) * 128, :], [xt], [xs_t[i]])
                        cs = sn = None
                        if i >= NCT:
                            cs = cos_pool.next()
                            sn = sin_pool.next()
                            r0 = (i - NCT) * 128
                            fw.dma(cs.ap, rope_cos[r0:r0 + 128, :], [cs], [])
                            fw.dma(sn.ap, rope_sin[r0:r0 + 128, :], [sn], [])
                        return xt, cs, sn

                    nxt = s2_load(0)
                    for i in range(NT):
                        xt, cs, sn = nxt
                        if i + 1 < NT:
                            nxt = s2_load(i + 1)
                        s = 1 if i < NCT else 0
                        st = st_pool.next()
                        fw.op(act, lambda e, xt=xt, st=st: e.activation(out=junk.ap, in_=xt.ap, func=AF.Square, accum_out=st.ap[:, 0:1]), [junk, st], [xt])
                        fw.op(dve, lambda e, st=st: e.tensor_scalar(out=st.ap[:, 1:2], in0=st.ap[:, 0:1], scalar1=1.0 / D, scalar2=1e-6, op0=ALU.mult, op1=ALU.add), [st], [st])
                        fw.op(act, lambda e, st=st: e.activation(out=st.ap[:, 3:4], in_=st.ap[:, 1:2], func=AF.Sqrt), [st], [st])
                        fw.op(dve, lambda e, st=st: e.reciprocal(out=st.ap[:, 2:3], in_=st.ap[:, 3:4]), [st], [st])
                        fw.op(dve, lambda e, xt=xt, st=st, s=s: e.scalar_tensor_tensor(out=xt.ap, in0=xt.ap, scalar=st.ap[:, 2:3], in1=modA[s].ap, op0=ALU.mult, op1=ALU.mult), [xt], [xt, st, modA[s]])
                        ht = h_pool.next()
                        fw.op(pool, lambda e, xt=xt, ht=ht, s=s: e.tensor_tensor(out=ht.ap, in0=xt.ap, in1=modB[s].ap, op=ALU.add), [ht], [xt, modB[s]])
                        hT = hT_pool.next()
                        for half in range(2):
                            ps = psum()
                            psb = ps.ap.bitcast(BF16)
                            for kk in range(4):
                                kc = half * 4 + kk
                                fw.op(pe, lambda e, kc=kc, kk=kk, psb=psb, ht=ht: e.transpose(out=psb[:, kk * 128:(kk + 1) * 128], in_=ht.ap[:, kc * 128:(kc + 1) * 128], identity=identb.ap), [ps], [ht, identb])
                            fw.op(act if half else dve, lambda e, psb=psb, hT=hT, half=half: (e.copy if half else e.tensor_copy)(out=hT.ap[:, half * 4:(half + 1) * 4, :], in_=psb[:, 0:512].rearrange("p (a b) -> p a b", a=4)), [hT], [ps])
                        pr = pr_pool.next()
                        col = 0
                        ci = 0
                        while col < IN_COLS:
                            n = min(512, IN_COLS - col)
                            ps = psum()
                            for kc in range(8):
                                fw.op(pe, lambda e, kc=kc, ps=ps, hT=hT, col=col, n=n: e.matmul(ps.ap[:, 0:n], lhsT=hT.ap[:, kc, :], rhs=winb.ap[:, kc, col:col + n], start=(kc == 0), stop=(kc == 7)), [ps], [hT, winb])
                            if ci % 2 == 0:
                                fw.op(act, lambda e, ps=ps, pr=pr, col=col, n=n: e.copy(out=pr.ap[:, col:col + n], in_=ps.ap[:, 0:n]), [pr], [ps])
                            else:
                                fw.op(dve, lambda e, ps=ps, pr=pr, col=col, n=n: e.tensor_copy(out=pr.ap[:, col:col + n], in_=ps.ap[:, 0:n]), [pr], [ps])
                            col += n
                            ci += 1
                        if "proj" in dbg and l == 0:
                            fw.dma(dbg["proj"][i * 128:(i + 1) * 128, :], pr.ap, [], [pr])
                        fw.dma(u_scr[urow(i):urow(i) + 128, :], pr.ap[:, 768:IN_COLS], [u_t[i]], [pr])
                        rq = rq_pool.next()
                        if i >= NCT:
                            rp = rp_pool.next()
                            v5 = lambda ap: ap.rearrange("p (m h f) -> p m h f", h=2, f=16)
                            fw.op(dve, lambda e, rp=rp, pr=pr, sn=sn: e.tensor_tensor(out=v5(rp.ap)[:, :, 0, :], in0=v5(pr.ap[:, 0:640])[:, :, 1, :], in1=v5(sn.ap)[:, :, 0, :], op=ALU.mult), [rp], [pr, sn])
                            fw.op(dve, lambda e, rp=rp, pr=pr, sn=sn: e.tensor_tensor(out=v5(rp.ap)[:, :, 1, :], in0=v5(pr.ap[:, 0:640])[:, :, 0, :], in1=v5(sn.ap)[:, :, 1, :], op=ALU.mult), [rp], [pr, sn])
                            fw.op(pool, lambda e, cs=cs, pr=pr: e.tensor_tensor(out=cs.ap, in0=pr.ap[:, 0:640], in1=cs.ap, op=ALU.mult), [cs], [pr, cs])
                            gq = lambda ap: ap.rearrange("p (g j d) -> p g j d", g=2, j=4)
                            jq = lambda ap: ap.rearrange("p (j g d) -> p g j d", g=2, j=4)
                            fw.op(dve, lambda e, rq=rq, rp=rp, cs=cs: e.tensor_tensor(out=jq(rq.ap[:, 0:512]), in0=gq(rp.ap[:, 0:512]), in1=gq(cs.ap[:, 0:512]), op=ALU.add), [rq], [rp, cs])
                            fw.op(dve, lambda e, rq=rq, rp=rp, cs=cs: e.tensor_tensor(out=rq.ap[:, 512:640], in0=rp.ap[:, 512:640], in1=cs.ap[:, 512:640], op=ALU.add), [rq], [rp, cs])
                        else:
                            gq = lambda ap: ap.rearrange("p (g j d) -> p g j d", g=2, j=4)
                            jq = lambda ap: ap.rearrange("p (j g d) -> p g j d", g=2, j=4)
                            fw.op(dve, lambda e, rq=rq, pr=pr: e.tensor_copy(out=jq(rq.ap[:, 0:512]), in_=gq(pr.ap[:, 0:512])), [rq], [pr])
                            fw.op(dve, lambda e, rq=rq, pr=pr: e.tensor_copy(out=rq.ap[:, 512:640], in_=pr.ap[:, 512:640]), [rq], [pr])
                        fw.op(act, lambda e, pr=pr, i=i: e.copy(out=VV.ap[:, i, :], in_=pr.ap[:, 640:768]), [qt_t[i]], [pr])
                        ps = psum()
                        psb = ps.ap.bitcast(BF16)
                        for j in range(4):
                            fw.op(pe, lambda e, j=j, psb=psb, rq=rq: e.transpose(out=psb[:, j * 128:(j + 1) * 128], in_=rq.ap[:, j * 128:(j + 1) * 128], identity=identb.ap), [ps], [rq, identb])
                        fw.op(pe, lambda e, psb=psb, rq=rq: e.transpose(out=psb[:, 512:640], in_=rq.ap[:, 512:640], identity=identb.ap), [ps], [rq, identb])
                        fw.op(dve, lambda e, psb=psb, i=i: e.tensor_copy(out=QT.ap[:, :, i * 128:(i + 1) * 128], in_=psb[:, 0:512].rearrange("p (j t) -> p j t", j=4)), [qt_t[i]], [ps])
                        fw.op(act, lambda e, psb=psb, i=i: e.copy(out=KT.ap[:, i * 128:(i + 1) * 128], in_=psb[:, 512:640]), [qt_t[i]], [ps])
                    fw.barrier()
                if stop_after == "S2":
                    break
                with ExitStack() as es:
                    sinkb = sb(es, nc, "sinkb", [128, 8], F32)
                    fw.dma(sinkb.ap, att_sink[l].partition_broadcast(128), [sinkb], [])
                    mb = sb(es, nc, "mb", [128, 384], F32)
                    fw.op(dve, lambda e: e.memset(mb.ap, 0.0), [mb], [])
                    fw.op(dve, lambda e: e.tensor_scalar(out=mb.ap[:, 0:128], in0=masks.ap[:, 1, :], scalar1=-1.0, scalar2=-NEG, op0=ALU.add, op1=ALU.mult), [mb], [masks])
                    fw.op(dve, lambda e: e.tensor_scalar(out=mb.ap[:, 256:384], in0=masks.ap[:, 3, :], scalar1=-1.0, scalar2=-NEG, op0=ALU.add, op1=ALU.mult), [mb], [masks])
                    s_pool = Pool(es, nc, "ssb", [128, 640], F32, 2)
                    p_pool = Pool(es, nc, "psb", [128, 640], BF16, 2)
                    pT_pool = Pool(es, nc, "pTs", [128, 640], BF16, 2)
                    ast_pool = Pool(es, nc, "ast", [128, 8], F32, 3)
                    mixa_pool = Pool(es, nc, "mixa", [128, 512], F32, 2)
                    for i in (range(NCT, NT) if last else range(NT)):
                        if i < NCT:
                            nloc = 0
                            kblocks = list(range(NCT))
                        else:
                            lo, hi = max(NCT, i - 1), min(NT - 1, i + 1)
                            nloc = (hi - lo + 1) * 128
                            moff = 0 if lo == i - 1 else 128
                            kblocks = list(range(lo, hi + 1)) + list(range(NCT))
                        ncols = nloc + C
                        mixa = mixa_pool.next()
                        ops = PS[6 + (i % 2)]
                        for h in range(8):
                            g, j = h // 4, h % 4
                            lhsT = QT.ap[g * 64:(g + 1) * 64, j, i * 128:(i + 1) * 128]
                            ssb = s_pool.next()
                            st = ast_pool.next()
                            if nloc:
                                ps1 = psum()
                                fw.op(pe, lambda e, ps1=ps1, lhsT=lhsT, g=g, lo=lo, hi=hi, nloc=nloc: e.matmul(ps1.ap[:, 0:nloc], lhsT=lhsT, rhs=KT.ap[g * 64:(g + 1) * 64, lo * 128:(hi + 1) * 128], start=True, stop=True), [ps1], [qt_t[i]])
                                fw.op(dve, lambda e, ps1=ps1, ssb=ssb, nloc=nloc, moff=moff: e.scalar_tensor_tensor(out=ssb.ap[:, 0:nloc], in0=ps1.ap[:, 0:nloc], scalar=ATT_SCALE, in1=mb.ap[:, moff:moff + nloc], op0=ALU.mult, op1=ALU.add), [ssb], [ps1, mb])
                            ps2 = psum()
                            fw.op(pe, lambda e, ps2=ps2, lhsT=lhsT, g=g: e.matmul(ps2.ap[:, 0:C], lhsT=lhsT, rhs=KT.ap[g * 64:(g + 1) * 64, 0:C], start=True, stop=True), [ps2], [qt_t[i]])
                            fw.op(act, lambda e, ps2=ps2, ssb=ssb, nloc=nloc, ncols=ncols: e.activation(out=ssb.ap[:, nloc:ncols], in_=ps2.ap[:, 0:C], func=AF.Copy, scale=ATT_SCALE), [ssb], [ps2])
                            fw.op(dve, lambda e, ssb=ssb, st=st, ncols=ncols: e.reduce_max(out=st.ap[:, 0:1], in_=ssb.ap[:, 0:ncols], axis=AX.X), [st], [ssb])
                            fw.op(dve, lambda e, st=st, h=h: e.tensor_scalar(out=st.ap[:, 1:2], in0=st.ap[:, 0:1], scalar1=sinkb.ap[:, h:h + 1], scalar2=-1.0, op0=ALU.max, op1=ALU.mult), [st], [st, sinkb])
                            pb = p_pool.next()
                            fw.op(act, lambda e, pb=pb, ssb=ssb, st=st, ncols=ncols: e.activation(out=pb.ap[:, 0:ncols], in_=ssb.ap[:, 0:ncols], func=AF.Exp, bias=st.ap[:, 1:2], accum_out=st.ap[:, 2:3]), [pb, st], [ssb, st])
                            fw.op(act, lambda e, st=st, h=h: e.activation(out=st.ap[:, 3:4], in_=sinkb.ap[:, h:h + 1], func=AF.Exp, bias=st.ap[:, 1:2]), [st], [st, sinkb])
                            fw.op(dve, lambda e, st=st: e.tensor_tensor(out=st.ap[:, 4:5], in0=st.ap[:, 2:3], in1=st.ap[:, 3:4], op=ALU.add), [st], [st])
                            fw.op(dve, lambda e, st=st: e.reciprocal(out=st.ap[:, 5:6], in_=st.ap[:, 4:5]), [st], [st])
                            ps3 = psum()
                            ps3b = ps3.ap.bitcast(BF16)
                            nblk = ncols // 128
                            for b in range(nblk):
                                fw.op(pe, lambda e, b=b, ps3b=ps3b, pb=pb: e.transpose(out=ps3b[:, b * 128:(b + 1) * 128], in_=pb.ap[:, b * 128:(b + 1) * 128], identity=identb.ap), [ps3], [pb, identb])
                            pT = pT_pool.next()
                            if h % 2 == 0:
                                fw.op(dve, lambda e, ps3b=ps3b, pT=pT, ncols=ncols: e.tensor_copy(out=pT.ap[:, 0:ncols], in_=ps3b[:, 0:ncols]), [pT], [ps3])
                            else:
                                fw.op(act, lambda e, ps3b=ps3b, pT=pT, ncols=ncols: e.copy(out=pT.ap[:, 0:ncols], in_=ps3b[:, 0:ncols]), [pT], [ps3])
                            for b, kb in enumerate(kblocks):
                                fw.op(pe, lambda e, b=b, kb=kb, pT=pT, g=g, h=h, ops=ops, nb=len(kblocks): e.matmul(ops.ap[:, h * 64:(h + 1) * 64], lhsT=pT.ap[:, b * 128:(b + 1) * 128], rhs=VV.ap[:, kb, g * 64:(g + 1) * 64], start=(b == 0), stop=(b == nb - 1)), [ops], [pT, qt_t[kb]])
                            fw.op(act, lambda e, ops=ops, mixa=mixa, st=st, h=h: e.activation(out=mixa.ap[:, h * 64:(h + 1) * 64], in_=ops.ap[:, h * 64:(h + 1) * 64], func=AF.Copy, scale=st.ap[:, 5:6]), [mixa], [ops, st])
                        fw.dma(mix[i * 128:(i + 1) * 128, 0:512], mixa.ap, [mix_a[i]], [mixa])
                    fw.barrier()
            if stop_after == "S3":
                if "mix" in dbg:
                    fw.barrier()
                    for i in range(NT):
                        fw.dma(dbg["mix"][i * 128:(i + 1) * 128, :], mix[i * 128:(i + 1) * 128, :], [], [mix_a[i], mix_r[i]])
                break
            FV, FR, FG, FKK, FBON = 0, 512, 1024, 1536, 2048
            FLW = lambda d_: 2560 + d_ * 1536
            FB = lambda d_: 2560 + d_ * 1536 + 512
            FKD = lambda d_: 2560 + d_ * 1536 + 1024
            NF = 5632
            with ExitStack() as es:
                def bc(name, src, n):
                    t_ = sb(es, nc, name, [128, n], F32)
                    fw.dma(t_.ap, src.partition_broadcast(128), [t_], [])
                    return t_
                mup = bc("mup", mu_prev[l], RWC)
                mun = bc("mun", mu_next[l], RWC)
                w0b = bc("w0b", decay_w0[l].rearrange("d c -> (d c)"), 1024)
                a0b = bc("a0b", iclr_a0[l].rearrange("d c -> (d c)"), 1024)
                kkb = bc("kkb", k_k[l], RW)
                kab = bc("kab", k_a[l], RW)
                rkb = bc("rkb", r_k[l], RW)
                w2t = sb(es, nc, "w2t", [128, RW], F32)
                a2t = sb(es, nc, "a2t", [128, RW], F32)
                g2t = sb(es, nc, "g2t", [128, RW], F32)
                fw.dma(w2t.ap, decay_w2[l].rearrange("d r c -> (d r) c"), [w2t], [])
                fw.dma(a2t.ap, iclr_a2[l].rearrange("d r c -> (d r) c"), [a2t], [])
                fw.dma(g2t.ap, gate_g2[l], [g2t], [])
                if l > 0:
                    v0b = bc("v0b", vres_v0[0], RW)
                    v1t = sb(es, nc, "v1t", [128, 4, 32], F32)
                    fw.dma(v1t.ap, vres_v1[0].rearrange("(c p) n -> p c n", p=128), [v1t], [])
                    v2t = sb(es, nc, "v2t", [32, RW], F32)
                    fw.dma(v2t.ap, vres_v2[0], [v2t], [])
                uc_pool = Pool(es, nc, "uc", [128, RWC], F32, 2)
                up_pool = Pool(es, nc, "up", [128, RWC], F32, 2)
                un_pool = Pool(es, nc, "un", [128, RWC], F32, 2)
                vf_pool = Pool(es, nc, "vf", [128, RW], F32, 2)
                li_pool = Pool(es, nc, "li", [128, 384], F32, 2)
                liT_pool = Pool(es, nc, "liT", [128, 3, 128], F32, 2)
                fo_pool = Pool(es, nc, "fo", [128, NF], F32, 2)
                tmp_pool = Pool(es, nc, "ftmp", [128, RW], F32, 6)
                a_pool = Pool(es, nc, "fa", [128, RW], F32, 4)
                sm_pool = Pool(es, nc, "fsm", [128, 64], F32, 2)
                vT_pool = Pool(es, nc, "fvT", [128, 4, 128], F32, 2)
                h3 = lambda ap: ap.rearrange("p (h d) -> p h d", h=8)

                def s4a_load(i):
                    uc, up, un = uc_pool.next(), up_pool.next(), un_pool.next()
                    deps = [u_t[i], u_guard] + ([u_t[i - 1]] if i > 0 else []) + ([u_t[i + 1]] if i + 1 < NT else [])
                    r0 = urow(i)
                    fw.dma(uc.ap, u_scr[r0:r0 + 128, :], [uc], deps)
                    fw.dma(up.ap, u_scr[r0 - 1:r0 + 127, :], [up], deps)
                    fw.dma(un.ap, u_scr[r0 + 1:r0 + 129, :], [un], deps)
                    vf = None
                    if l > 0:
                        vf = vf_pool.next()
                        fw.dma(vf.ap, vfirst[i * 128:(i + 1) * 128, :], [vf], [vf_t[i]])
                    return uc, up, un, vf

                nxt = s4a_load(0)
                for i in range(NT):
                    uc, up, un, vf = nxt
                    if i + 1 < NT:
                        nxt = s4a_load(i + 1)
                    fw.op(dve, lambda e: e.tensor_tensor(out=up.ap, in0=up.ap, in1=uc.ap, op=ALU.subtract), [up], [up, uc])
                    fw.op(pool, lambda e: e.tensor_tensor(out=un.ap, in0=un.ap, in1=uc.ap, op=ALU.subtract), [un], [un, uc])
                    fw.op(dve, lambda e: e.tensor_tensor(out=up.ap, in0=up.ap, in1=mup.ap, op=ALU.mult), [up], [up, mup])
                    fw.op(pool, lambda e: e.tensor_tensor(out=un.ap, in0=un.ap, in1=mun.ap, op=ALU.mult), [un], [un, mun])
                    fw.op(dve, lambda e: e.tensor_tensor(out=uc.ap, in0=uc.ap, in1=up.ap, op=ALU.add), [uc], [uc, up])
                    fw.op(dve, lambda e: e.tensor_tensor(out=uc.ap, in0=uc.ap, in1=un.ap, op=ALU.add), [uc], [uc, un])
                    U = uc.ap
                    r_, k_, v_ = U[:, 0:512], U[:, 512:1024], U[:, 1024:1536]
                    li = li_pool.next()
                    fw.op(act, lambda e: e.activation(out=li.ap[:, 0:128], in_=U[:, 1536:1664], func=AF.Tanh), [li], [uc])
                    fw.op(act, lambda e: e.copy(out=li.ap[:, 128:256], in_=U[:, 1664:1792]), [li], [uc])
                    fw.op(act, lambda e: e.activation(out=li.ap[:, 256:384], in_=U[:, 1792:1920], func=AF.Sigmoid), [li], [uc])
                    ps = psum()
                    for q in range(3):
                        fw.op(pe, lambda e, q=q, ps=ps: e.transpose(out=ps.ap[:, q * 128:(q + 1) * 128], in_=li.ap[:, q * 128:(q + 1) * 128], identity=ident.ap), [ps], [li, ident])
                    liT = liT_pool.next()
                    fw.op(dve, lambda e, ps=ps: e.tensor_copy(out=liT.ap, in_=ps.ap[:, 0:384].rearrange("p (a b) -> p a b", a=3)), [liT], [ps])
                    fo = fo_pool.next()
                    FO = fo.ap
                    a_d = []
                    for d_ in range(2):
                        hp = slice(d_ * 64, (d_ + 1) * 64)
                        ps = psum()
                        fw.op(pe, lambda e, ps=ps, hp=hp: e.matmul(ps.ap, lhsT=liT.ap[hp, 0, :], rhs=w2t.ap[hp, :], start=True, stop=True), [ps], [liT, w2t])
                        t1 = tmp_pool.next()
                        fw.op(dve, lambda e, ps=ps, t1=t1, d_=d_: e.tensor_tensor(out=t1.ap, in0=ps.ap, in1=w0b.ap[:, d_ * 512:(d_ + 1) * 512], op=ALU.add), [t1], [ps, w0b])
                        fw.op(act, lambda e, t1=t1: e.activation(out=t1.ap, in_=t1.ap, func=AF.Sigmoid), [t1], [t1])
                        fw.op(pool, lambda e, t1=t1, d_=d_: e.tensor_scalar(out=FO[:, FLW(d_):FLW(d_) + 512], in0=t1.ap, scalar1=-0.6065306597126334, scalar2=None, op0=ALU.mult), [fo], [t1])
                        ps = psum()
                        fw.op(pe, lambda e, ps=ps, hp=hp: e.matmul(ps.ap, lhsT=liT.ap[hp, 1, :], rhs=a2t.ap[hp, :], start=True, stop=True), [ps], [liT, a2t])
                        ad = a_pool.next()
                        fw.op(dve, lambda e, ps=ps, ad=ad, d_=d_: e.tensor_tensor(out=ad.ap, in0=ps.ap, in1=a0b.ap[:, d_ * 512:(d_ + 1) * 512], op=ALU.add), [ad], [ps, a0b])
                        fw.op(act, lambda e, ad=ad: e.activation(out=ad.ap, in_=ad.ap, func=AF.Sigmoid), [ad], [ad])
                        a_d.append(ad)
                    ps = psum()
                    fw.op(pe, lambda e, ps=ps: e.matmul(ps.ap, lhsT=liT.ap[:, 2, :], rhs=g2t.ap, start=True, stop=True), [ps], [liT, g2t])
                    fw.op(act, lambda e, ps=ps: e.copy(out=FO[:, FG:FG + 512], in_=ps.ap), [fo], [ps])
                    if l == 0:
                        fw.op(pool, lambda e: e.tensor_copy(out=FO[:, FV:FV + 512], in_=v_), [fo], [uc])
                        fw.dma(vfirst[i * 128:(i + 1) * 128, :], v_, [vf_t[i]], [uc])
                    else:
                        ps = psum()
                        for c_ in range(4):
                            fw.op(pe, lambda e, c_=c_, ps=ps: e.transpose(out=ps.ap[:, c_ * 128:(c_ + 1) * 128], in_=U[:, 1024 + c_ * 128:1024 + (c_ + 1) * 128], identity=ident.ap), [ps], [uc, ident])
                        vT = vT_pool.next()
                        fw.op(dve, lambda e, ps=ps, vT=vT: e.tensor_copy(out=vT.ap, in_=ps.ap.rearrange("p (a b) -> p a b", a=4)), [vT], [ps])
                        ps = psum()
                        for c_ in range(4):
                            fw.op(pe, lambda e, c_=c_, ps=ps, vT=vT: e.matmul(ps.ap[:, 0:32], lhsT=vT.ap[:, c_, :], rhs=v1t.ap[:, c_, :], start=(c_ == 0), stop=(c_ == 3)), [ps], [vT, v1t])
                        sm = sm_pool.next()
                        fw.op(dve, lambda e, ps=ps, sm=sm: e.tensor_copy(out=sm.ap[:, 0:32], in_=ps.ap[:, 0:32]), [sm], [ps])
                        ps = psum()
                        fw.op(pe, lambda e, ps=ps, sm=sm: e.transpose(out=ps.ap[0:32, 0:128], in_=sm.ap[:, 0:32], identity=ident.ap), [ps], [sm, ident])
                        smT = tmp_pool.next()
                        fw.op(dve, lambda e, ps=ps, smT=smT: e.tensor_copy(out=smT.ap[0:32, 0:128], in_=ps.ap[0:32, 0:128]), [smT], [ps])
                        ps = psum()
                        fw.op(pe, lambda e, ps=ps, smT=smT: e.matmul(ps.ap, lhsT=smT.ap[0:32, 0:128], rhs=v2t.ap, start=True, stop=True), [ps], [smT, v2t])
                        t1 = tmp_pool.next()
                        fw.op(dve, lambda e, ps=ps, t1=t1: e.tensor_tensor(out=t1.ap, in0=ps.ap, in1=v0b.ap, op=ALU.add), [t1], [ps, v0b])
                        fw.op(act, lambda e, t1=t1: e.activation(out=t1.ap, in_=t1.ap, func=AF.Sigmoid), [t1], [t1])
                        fw.op(dve, lambda e, vf=vf: e.tensor_tensor(out=vf.ap, in0=vf.ap, in1=v_, op=ALU.subtract), [vf], [vf, uc])
                        fw.op(dve, lambda e, vf=vf, t1=t1: e.tensor_tensor(out=vf.ap, in0=vf.ap, in1=t1.ap, op=ALU.mult), [vf], [vf, t1])
                        fw.op(dve, lambda e, vf=vf: e.tensor_tensor(out=FO[:, FV:FV + 512], in0=vf.ap, in1=v_, op=ALU.add), [fo], [vf, uc])
                    fw.op(pool, lambda e: e.tensor_copy(out=FO[:, FR:FR + 512], in_=r_), [fo], [uc])
                    kk0 = tmp_pool.next()
                    sq = tmp_pool.next()
                    sm = sm_pool.next()
                    fw.op(dve, lambda e, kk0=kk0: e.tensor_tensor(out=kk0.ap, in0=k_, in1=kkb.ap, op=ALU.mult), [kk0], [uc, kkb])
                    fw.op(pool, lambda e, kk0=kk0, sq=sq: e.tensor_tensor(out=sq.ap, in0=kk0.ap, in1=kk0.ap, op=ALU.mult), [sq], [kk0])
                    fw.op(dve, lambda e, sq=sq, sm=sm: e.reduce_sum(out=sm.ap[:, 0:8], in_=h3(sq.ap), axis=AX.X), [sm], [sq])
                    fw.op(act, lambda e, sm=sm: e.activation(out=sm.ap[:, 8:16], in_=sm.ap[:, 0:8], func=AF.Sqrt), [sm], [sm])
                    fw.op(dve, lambda e, sm=sm: e.tensor_scalar(out=sm.ap[:, 8:16], in0=sm.ap[:, 8:16], scalar1=1e-12, scalar2=None, op0=ALU.max), [sm], [sm])
                    fw.op(dve, lambda e, sm=sm: e.reciprocal(out=sm.ap[:, 16:24], in_=sm.ap[:, 8:16]), [sm], [sm])
                    fw.op(dve, lambda e, sm=sm, kk0=kk0: e.tensor_tensor(out=h3(FO[:, FKK:FKK + 512]), in0=h3(kk0.ap), in1=sm.ap[:, 16:24].unsqueeze(2).to_broadcast([128, 8, 64]), op=ALU.mult), [fo], [kk0, sm])
                    for d_ in range(2):
                        t1 = tmp_pool.next()
                        fw.op(dve, lambda e, t1=t1, d_=d_: e.scalar_tensor_tensor(out=t1.ap, in0=a_d[d_].ap, scalar=-1.0, in1=kab.ap, op0=ALU.add, op1=ALU.mult), [t1], [a_d[d_], kab])
                        fw.op(dve, lambda e, t1=t1, d_=d_: e.scalar_tensor_tensor(out=FO[:, FKD(d_):FKD(d_) + 512], in0=t1.ap, scalar=1.0, in1=k_, op0=ALU.add, op1=ALU.mult), [fo], [t1, uc])
                        fw.op(pool, lambda e, d_=d_: e.tensor_tensor(out=FO[:, FB(d_):FB(d_) + 512], in0=FO[:, FKK:FKK + 512], in1=a_d[d_].ap, op=ALU.mult), [fo], [fo, a_d[d_]])
                    t1 = tmp_pool.next()
                    sm2 = sm_pool.next()
                    fw.op(pool, lambda e, t1=t1: e.tensor_tensor(out=t1.ap, in0=FO[:, FKD(0):FKD(0) + 512], in1=FO[:, FKD(1):FKD(1) + 512], op=ALU.add), [t1], [fo])
                    fw.op(dve, lambda e, t1=t1: e.tensor_tensor(out=t1.ap, in0=t1.ap, in1=r_, op=ALU.mult), [t1], [t1, uc])
                    fw.op(pool, lambda e, t1=t1: e.tensor_tensor(out=t1.ap, in0=t1.ap, in1=rkb.ap, op=ALU.mult), [t1], [t1, rkb])
                    fw.op(dve, lambda e, t1=t1, sm2=sm2: e.reduce_sum(out=sm2.ap[:, 0:8], in_=h3(t1.ap), axis=AX.X), [sm2], [t1])
                    fw.op(dve, lambda e, sm2=sm2: e.tensor_tensor(out=h3(FO[:, FBON:FBON + 512]), in0=h3(FO[:, FV:FV + 512]), in1=sm2.ap[:, 0:8].unsqueeze(2).to_broadcast([128, 8, 64]), op=ALU.mult), [fo], [fo, sm2])
                    fw.dma(feat[i * 128:(i + 1) * 128, :], FO, [feat_t[i]], [fo])
                    if "feat" in dbg and l == 0:
                        fw.dma(dbg["feat"][i * 128:(i + 1) * 128, :], FO[:, 0:4096], [], [fo])
                fw.barrier()
            if stop_after == "S4a":
                break
            with ExitStack() as es:
                ones = sb(es, nc, "ones", [128, 1], F32)
                fw.op(dve, lambda e: e.memset(ones.ap, 1.0), [ones], [])
                Mq1, MnT = [], []
                for d_ in range(2):
                    m1_ = sb(es, nc, "Mq1", [128, 256], F32)
                    fw.op(dve, lambda e, m1_=m1_, d_=d_: e.tensor_scalar(out=m1_.ap[:, 0:128], in0=masks.ap[:, 2 * d_, :], scalar1=-1.0, scalar2=None, op0=ALU.mult), [m1_], [masks])
                    fw.op(dve, lambda e, m1_=m1_, d_=d_: e.tensor_copy(out=m1_.ap[:, 128:256], in_=masks.ap[:, 2 * d_ + 1, :]), [m1_], [masks])
                    Mq1.append(m1_)
                    mt_ = sb(es, nc, "MnT", [128, 128], F32)
                    fw.op(dve, lambda e, mt_=mt_, d_=d_: e.tensor_scalar(out=mt_.ap, in0=masks.ap[:, 2 * (1 - d_), :], scalar1=-1.0, scalar2=None, op0=ALU.mult), [mt_], [masks])
                    MnT.append(mt_)
                Mq2 = [masks.ap[:, 2 * d_:2 * d_ + 2, :].rearrange("p a t -> p (a t)") for d_ in range(2)]
                PSQB = int(os.environ.get('S4B_PSQB', '4'))
                PSQ = [TT(PS[PSQB + k // 4].ap[:, (k % 4) * 128:(k % 4 + 1) * 128]) for k in range(16)]
                pq = [0]

                def psq():
                    t_ = PSQ[pq[0] % 16]
                    pq[0] += 1
                    return t_
                p4 = [0]

                def psumR():
                    t_ = PS[p4[0] % 8]
                    p4[0] += 1
                    return t_
                fa_pool = Pool(es, nc, "sfa", [128, 3, 512], F32, 3)
                fb_pool = Pool(es, nc, "sfb", [128, 3, 512], F32, 3)
                tm_pool = Pool(es, nc, "stm", [128, 4, 512], F32, 2)
                ex_pool = Pool(es, nc, "sex", [128, 3, 512], F32, 2)
                fm_pool = Pool(es, nc, "sfm", [128, 4, 4, 128], F32, 2)
                el_pool = Pool(es, nc, "sel", [128, 4], F32, 2)
                g1_pool = Pool(es, nc, "sg1", [128, 2, 4, 128], F32, 4)
                a2_pool = Pool(es, nc, "sa2", [128, 256], F32, 16)
                inv_pool = Pool(es, nc, "sinv", [128, 4, 128], F32, 20)
                xs_pool = Pool(es, nc, "sxs", [128, 4, 64], F32, 4)
                nu_pool = Pool(es, nc, "snu", [128, 512], F32, 2)
                ys_pool = Pool(es, nc, "sys", [128, 512], F32, 2)
                ST = [[[sb(es, nc, "ST", [128, 64], F32) for c_ in range(4)] for pp in range(2)] for d_ in range(2)]
                for d_ in range(2):
                    for c_ in range(4):
                        fw.op(dve, lambda e, d_=d_, c_=c_: e.memset(ST[d_][0][c_].ap, 0.0), [ST[d_][0][c_]], [])
                orders = [list(range(NT)), [1, 0] + list(range(NT - 1, NCT - 1, -1))]
                evi = [0]

                def evac(fn_dve, fn_act, outs, ins):
                    if evi[0] % 2:
                        fw.op(dve, fn_dve, outs, ins)
                    else:
                        fw.op(act, fn_act, outs, ins)

                def s4b_load(d_, i):
                    fa, fb = fa_pool.next(), fb_pool.next()
                    r0 = i * 128
                    fw.dma(fa.ap, feat[r0:r0 + 128, FLW(d_):FLW(d_) + 1536].rearrange("p (a c) -> p a c", a=3), [fa], [feat_t[i]])
                    fw.dma(fb.ap[:, 0:2, :], feat[r0:r0 + 128, 0:1024].rearrange("p (a c) -> p a c", a=2), [fb], [feat_t[i]])
                    fw.dma(fb.ap[:, 2, :], feat[r0:r0 + 128, FKK:FKK + 512], [fb], [feat_t[i]])
                    return fa, fb

                steps = [(d_, j) for j in range(NT) for d_ in range(2)]
                PH = int(os.environ.get('S4B_PHASE', '9'))
                if os.environ.get('S4B_STEPS'):
                    steps = steps[:int(os.environ['S4B_STEPS'])]
                nxt = s4b_load(0, orders[0][0])
                for si, (d_, j) in enumerate(steps):
                    i = orders[d_][j]
                    fa, fb = nxt
                    if si + 1 < len(steps):
                        nd, nj = steps[si + 1]
                        nxt = s4b_load(nd, orders[nd][nj])
                    pp = j % 2
                    STo, STn = ST[d_][pp], ST[d_][1 - pp]
                    lw, b_, kd = fa.ap[:, 0, :], fa.ap[:, 1, :], fa.ap[:, 2, :]
                    v_, r_, kk = fb.ap[:, 0, :], fb.ap[:, 1, :], fb.ap[:, 2, :]
                    psL = psumR()
                    fw.op(pe, lambda e: e.matmul(psL.ap, lhsT=masks.ap[:, 1 + 2 * d_, :], rhs=lw, start=True, stop=True), [psL], [masks, fa])
                    ex = ex_pool.next()
                    fw.op(act, lambda e: e.activation(out=ex.ap[:, 0, :], in_=psL.ap, func=AF.Exp), [ex], [psL])
                    fw.op(act, lambda e: e.activation(out=ex.ap[:, 1, :], in_=psL.ap, func=AF.Exp, scale=-1.0), [ex], [psL])
                    fw.op(dve, lambda e: e.tensor_tensor(out=ex.ap[:, 2, :], in0=psL.ap, in1=lw, op=ALU.subtract), [ex], [psL, fa])
                    fw.op(act, lambda e: e.activation(out=ex.ap[:, 2, :], in_=ex.ap[:, 2, :], func=AF.Exp), [ex], [ex])
                    tm = tm_pool.next()
                    fw.op(dve, lambda e: e.tensor_tensor(out=tm.ap[:, 0, :], in0=kk, in1=ex.ap[:, 2, :], op=ALU.mult), [tm], [fb, ex])
                    fw.op(pool, lambda e: e.tensor_tensor(out=tm.ap[:, 1, :], in0=r_, in1=ex.ap[:, 0, :], op=ALU.mult), [tm], [fb, ex])
                    fw.op(dve, lambda e: e.tensor_tensor(out=tm.ap[:, 2, :], in0=b_, in1=ex.ap[:, 1, :], op=ALU.mult), [tm], [fa, ex])
                    fw.op(pool, lambda e: e.tensor_tensor(out=tm.ap[:, 3, :], in0=kd, in1=ex.ap[:, 1, :], op=ALU.mult), [tm], [fa, ex])
                    if PH < 1:
                        continue
                    psE = psumR()
                    for c_ in range(4):
                        fw.op(pe, lambda e, c_=c_: e.matmul(psE.ap[:, c_:c_ + 1], lhsT=lw[:, c_ * 128:(c_ + 1) * 128], rhs=ones.ap, start=True, stop=True), [psE], [fa, ones])
                    el = el_pool.next()
                    fw.op(act, lambda e: e.activation(out=el.ap, in_=psE.ap[:, 0:4], func=AF.Exp), [el], [psE])
                    fm = fm_pool.next()
                    for c_ in range(4):
                        psT = psumR()
                        for q in range(4):
                            fw.op(pe, lambda e, c_=c_, q=q, psT=psT: e.transpose(out=psT.ap[:, q * 128:(q + 1) * 128], in_=tm.ap[:, q, c_ * 128:(c_ + 1) * 128], identity=ident.ap), [psT], [tm, ident])
                        evi[0] += 1
                        evac(lambda e, c_=c_, psT=psT: e.tensor_copy(out=fm.ap[:, c_, :, :], in_=psT.ap.rearrange("p (a b) -> p a b", a=4)),
                             lambda e, c_=c_, psT=psT: e.copy(out=fm.ap[:, c_, :, :], in_=psT.ap.rearrange("p (a b) -> p a b", a=4)), [fm], [psT])
                    if PH < 2:
                        continue
                    H = []
                    for h in range(8):
                        c_ = h // 2
                        hp = slice((h % 2) * 64, (h % 2) * 64 + 64)
                        H.append(dict(c=c_, hp=hp, par=h % 2, k=h // 2, al=fm.ap[hp, c_, 0, :], rh=fm.ap[hp, c_, 1, :], be=fm.ap[hp, c_, 2, :], ka=fm.ap[hp, c_, 3, :],
                                      ar=fm.ap[hp, c_, 0:2, :].rearrange("p a t -> p (a t)"), vh=v_[:, h * 64:(h + 1) * 64]))
                    G1 = [g1_pool.next() for _ in range(2)]
                    for h in range(8):
                        u = H[h]
                        psG = psumR()
                        fw.op(pe, lambda e, u=u, psG=psG: e.matmul(psG.ap[:, 0:256], lhsT=u["be"], rhs=u["ar"], start=True, stop=True), [psG], [fm])
                        fw.op(pe, lambda e, u=u, psG=psG: e.matmul(psG.ap[:, 256:512], lhsT=u["ka"], rhs=u["ar"], start=True, stop=True), [psG], [fm])
                        a2 = a2_pool.next()
                        g1 = G1[u["par"]]
                        fw.op(dve, lambda e, u=u, psG=psG, g1=g1: e.tensor_tensor(out=g1.ap[:, :, u["k"], :], in0=psG.ap[:, 0:256].rearrange("p (a t) -> p a t", a=2), in1=Mq1[d_].ap.rearrange("p (a t) -> p a t", a=2), op=ALU.mult), [g1], [psG, Mq1[d_]])
                        fw.op(dve, lambda e, a2=a2, psG=psG: e.tensor_tensor(out=a2.ap, in0=psG.ap[:, 256:512], in1=Mq2[d_], op=ALU.mult), [a2], [psG, masks])
                        u.update(a2=a2, abr=g1.ap[:, 1, u["k"], :], g1=g1)
                    GP = []
                    f4 = lambda ap: ap.rearrange("p k t -> p (k t)")
                    for par in range(2):
                        g1 = G1[par]
                        M = inv_pool.next()
                        fw.op(dve, lambda e, M=M, g1=g1: e.tensor_tensor(out=M.ap, in0=g1.ap[:, 0, :, :], in1=ident.ap.unsqueeze(1).to_broadcast([128, 4, 128]), op=ALU.add), [M], [g1, ident])
                        bank = psumR()
                        for k_ in range(4):
                            fw.op(pe, lambda e, k_=k_, bank=bank, g1=g1: e.transpose(out=bank.ap[:, k_ * 128:(k_ + 1) * 128], in_=g1.ap[:, 0, k_, :], identity=ident.ap), [bank], [g1, ident])
                        AT = inv_pool.next()
                        evi[0] += 1
                        evac(lambda e, AT=AT, bank=bank: e.tensor_copy(out=f4(AT.ap), in_=bank.ap), lambda e, AT=AT, bank=bank: e.copy(out=f4(AT.ap), in_=bank.ap), [AT], [bank])
                        GP.append(dict(A=g1, Aap=g1.ap[:, 0, :, :], AT=AT, M=M))
                    if PH < 4:
                        continue
                    for it in range(6):
                        for g in GP:
                            bank = psumR()
                            for k_ in range(4):
                                fw.op(pe, lambda e, k_=k_, bank=bank, g=g: e.matmul(bank.ap[:, k_ * 128:(k_ + 1) * 128], lhsT=g["Aap"][:, k_, :], rhs=g["AT"].ap[:, k_, :], start=True, stop=True), [bank], [g["A"], g["AT"]])
                            ATn = inv_pool.next()
                            evi[0] += 1
                            evac(lambda e, ATn=ATn, bank=bank: e.tensor_copy(out=f4(ATn.ap), in_=bank.ap), lambda e, ATn=ATn, bank=bank: e.copy(out=f4(ATn.ap), in_=bank.ap), [ATn], [bank])
                            g["ATn"] = ATn
                            if it < 5:
                                bank = psumR()
                                for k_ in range(4):
                                    fw.op(pe, lambda e, k_=k_, bank=bank, g=g: e.matmul(bank.ap[:, k_ * 128:(k_ + 1) * 128], lhsT=g["AT"].ap[:, k_, :], rhs=g["Aap"][:, k_, :], start=True, stop=True), [bank], [g["A"], g["AT"]])
                                An = inv_pool.next()
                                evi[0] += 1
                                evac(lambda e, An=An, bank=bank: e.tensor_copy(out=f4(An.ap), in_=bank.ap), lambda e, An=An, bank=bank: e.copy(out=f4(An.ap), in_=bank.ap), [An], [bank])
                                g["An"] = An
                        for g in GP:
                            g["AT"] = g["ATn"]
                            if it < 5:
                                g["A"], g["Aap"] = g["An"], g["An"].ap
                        for g in GP:
                            bank = psumR()
                            for k_ in range(4):
                                fw.op(pe, lambda e, k_=k_, bank=bank, g=g: e.matmul(bank.ap[:, k_ * 128:(k_ + 1) * 128], lhsT=g["AT"].ap[:, k_, :], rhs=g["M"].ap[:, k_, :], start=True, stop=True), [bank], [g["AT"], g["M"]])
                            Mn = inv_pool.next()
                            fw.op(dve, lambda e, Mn=Mn, bank=bank, g=g: e.tensor_tensor(out=f4(Mn.ap), in0=bank.ap, in1=f4(g["M"].ap), op=ALU.add), [Mn], [bank, g["M"]])
                            g["M"] = Mn
                    if PH < 5:
                        continue
                    NU = nu_pool.next()
                    q4 = lambda ap: ap.rearrange("p (k t) -> p k t", k=4)[:, :, 0:64]
                    for par, g in enumerate(GP):
                        bank = psumR()
                        for k_ in range(4):
                            u = H[par + 2 * k_]
                            o = bank.ap[:, k_ * 128:k_ * 128 + 64]
                            fw.op(pe, lambda e, u=u, o=o: e.matmul(o, lhsT=u["al"], rhs=STo[u["c"]].ap[u["hp"], :], start=True, stop=False), [bank], [fm, STo[u["c"]]])
                            fw.op(pe, lambda e, u=u, o=o: e.matmul(o, lhsT=u["a2"].ap[:, 0:128], rhs=u["vh"], start=False, stop=True), [bank], [u["a2"], fb])
                        xsg = xs_pool.next()
                        evi[0] += 1
                        evac(lambda e, xsg=xsg, bank=bank: e.tensor_copy(out=xsg.ap, in_=q4(bank.ap)), lambda e, xsg=xsg, bank=bank: e.copy(out=xsg.ap, in_=q4(bank.ap)), [xsg], [bank])
                        g["xs"] = xsg
                    for par, g in enumerate(GP):
                        bank = psumR()
                        for k_ in range(4):
                            fw.op(pe, lambda e, k_=k_, bank=bank, g=g: e.matmul(bank.ap[:, k_ * 128:k_ * 128 + 64], lhsT=g["M"].ap[:, k_, :], rhs=g["xs"].ap[:, k_, :], start=True, stop=True), [bank], [g["M"], g["xs"]])
                        nuv = NU.ap.rearrange("p (c two d) -> p c two d", two=2, d=64)[:, :, par, :]
                        fw.op(act, lambda e, bank=bank, nuv=nuv: e.activation(out=nuv, in_=q4(bank.ap), func=AF.Copy, scale=-1.0), [NU], [bank])
                    ysb = ys_pool.next()
                    for par in (0, 1):
                        psY = psumR()
                        for k_ in range(4):
                            h = par + 2 * k_
                            u = H[h]
                            ysl = psY.ap[:, k_ * 64:(k_ + 1) * 64]
                            fw.op(pe, lambda e, u=u, ysl=ysl: e.matmul(ysl, lhsT=u["rh"], rhs=STo[u["c"]].ap[u["hp"], :], start=True, stop=False), [psY], [fm, STo[u["c"]]])
                            fw.op(pe, lambda e, u=u, ysl=ysl, h=h: e.matmul(ysl, lhsT=u["abr"], rhs=NU.ap[:, h * 64:(h + 1) * 64], start=False, stop=False), [psY], [u["g1"], NU])
                            fw.op(pe, lambda e, u=u, ysl=ysl: e.matmul(ysl, lhsT=u["a2"].ap[:, 128:256], rhs=u["vh"], start=False, stop=True), [psY], [u["a2"], fb])
                        yv = ysb.ap.rearrange("p (c two d) -> p c two d", two=2, d=64)[:, :, par, :]
                        fw.op(act, lambda e, psY=psY, yv=yv: e.copy(out=yv, in_=psY.ap[:, 0:256].rearrange("p (c d) -> p c d", d=64)), [ysb], [psY])
                    fw.dma(y_scr[d_, i * 128:(i + 1) * 128, :], ysb.ap, [y_t[d_][i]], [ysb])
                    if PH < 6:
                        continue
                    psS = psumR()
                    for c_ in range(4):
                        so = psS.ap[:, c_ * 128:(c_ + 1) * 128]
                        fw.op(pe, lambda e, c_=c_, so=so: e.matmul(so, lhsT=tm.ap[:, 2, c_ * 128:(c_ + 1) * 128], rhs=NU.ap[:, c_ * 128:(c_ + 1) * 128], start=True, stop=False), [psS], [tm, NU])
                        fw.op(pe, lambda e, c_=c_, so=so: e.matmul(so, lhsT=tm.ap[:, 3, c_ * 128:(c_ + 1) * 128], rhs=v_[:, c_ * 128:(c_ + 1) * 128], start=False, stop=True), [psS], [tm, fb])
                        fw.op(pool, lambda e, c_=c_: e.tensor_scalar(out=STn[c_].ap, in0=STo[c_].ap, scalar1=el.ap[:, c_:c_ + 1], scalar2=None, op0=ALU.mult), [STn[c_]], [STo[c_], el])
                    for c_ in range(4):
                        so = psS.ap[:, c_ * 128:(c_ + 1) * 128]
                        for hh in range(2):
                            hp = slice(hh * 64, hh * 64 + 64)
                            fw.op(dve, lambda e, c_=c_, hp=hp, so=so: e.scalar_tensor_tensor(out=STn[c_].ap[hp, :], in0=so[hp, hp], scalar=el.ap[hp, c_:c_ + 1], in1=STn[c_].ap[hp, :], op0=ALU.mult, op1=ALU.add), [STn[c_]], [psS, el, STn[c_]])
                fw.barrier()
            if stop_after == "S4b":
                break
            with ExitStack() as es:
                lnw = sb(es, nc, "lnw", [128, RW], F32)
                lnb = sb(es, nc, "lnb", [128, RW], F32)
                fw.dma(lnw.ap, ln_x_w[l].partition_broadcast(128), [lnw], [])
                fw.dma(lnb.ap, ln_x_b[l].partition_broadcast(128), [lnb], [])
                y0_pool = Pool(es, nc, "cy0", [128, RW], F32, 2)
                y1_pool = Pool(es, nc, "cy1", [128, RW], F32, 2)
                gb_pool = Pool(es, nc, "cgb", [128, 2, RW], F32, 2)
                sq_pool = Pool(es, nc, "csq", [128, RW], F32, 2)
                cs_pool = Pool(es, nc, "ccs", [128, 32], F32, 2)
                h3 = lambda ap: ap.rearrange("p (h d) -> p h d", h=8)
                bc8 = lambda ap: ap.unsqueeze(2).to_broadcast([128, 8, 64])
                for i in (range(NCT, NT) if last else range(NT)):
                    y0, y1, gb = y0_pool.next(), y1_pool.next(), gb_pool.next()
                    r0 = i * 128
                    fw.dma(y0.ap, y_scr[0, r0:r0 + 128, :], [y0], [y_t[0][i]])
                    fw.dma(y1.ap, y_scr[1, r0:r0 + 128, :], [y1], [y_t[1][i]])
                    fw.dma(gb.ap[:, 0, :], feat[r0:r0 + 128, FG:FG + 512], [gb], [feat_t[i]])
                    fw.dma(gb.ap[:, 1, :], feat[r0:r0 + 128, FBON:FBON + 512], [gb], [feat_t[i]])
                    cs_ = cs_pool.next()
                    sq = sq_pool.next()
                    fw.op(dve, lambda e: e.tensor_tensor(out=y0.ap, in0=y0.ap, in1=y1.ap, op=ALU.add), [y0], [y0, y1])
                    fw.op(dve, lambda e: e.reduce_sum(out=cs_.ap[:, 0:8], in_=h3(y0.ap), axis=AX.X), [cs_], [y0])
                    fw.op(dve, lambda e: e.tensor_scalar(out=cs_.ap[:, 0:8], in0=cs_.ap[:, 0:8], scalar1=1.0 / 64, scalar2=None, op0=ALU.mult), [cs_], [cs_])
                    fw.op(dve, lambda e: e.tensor_tensor(out=h3(y0.ap), in0=h3(y0.ap), in1=bc8(cs_.ap[:, 0:8]), op=ALU.subtract), [y0], [y0, cs_])
                    fw.op(pool, lambda e: e.tensor_tensor(out=sq.ap, in0=y0.ap, in1=y0.ap, op=ALU.mult), [sq], [y0])
                    fw.op(dve, lambda e: e.reduce_sum(out=cs_.ap[:, 8:16], in_=h3(sq.ap), axis=AX.X), [cs_], [sq])
                    fw.op(dve, lambda e: e.tensor_scalar(out=cs_.ap[:, 8:16], in0=cs_.ap[:, 8:16], scalar1=1.0 / 64, scalar2=64e-5, op0=ALU.mult, op1=ALU.add), [cs_], [cs_])
                    fw.op(act, lambda e: e.activation(out=cs_.ap[:, 16:24], in_=cs_.ap[:, 8:16], func=AF.Sqrt), [cs_], [cs_])
                    fw.op(dve, lambda e: e.reciprocal(out=cs_.ap[:, 24:32], in_=cs_.ap[:, 16:24]), [cs_], [cs_])
                    fw.op(dve, lambda e: e.tensor_tensor(out=h3(y0.ap), in0=h3(y0.ap), in1=bc8(cs_.ap[:, 24:32]), op=ALU.mult), [y0], [y0, cs_])
                    fw.op(pool, lambda e: e.tensor_tensor(out=y0.ap, in0=y0.ap, in1=lnw.ap, op=ALU.mult), [y0], [y0, lnw])
                    fw.op(dve, lambda e: e.tensor_tensor(out=y0.ap, in0=y0.ap, in1=lnb.ap, op=ALU.add), [y0], [y0, lnb])
                    fw.op(pool, lambda e: e.tensor_tensor(out=y0.ap, in0=y0.ap, in1=gb.ap[:, 1, :], op=ALU.add), [y0], [y0, gb])
                    fw.op(dve, lambda e: e.tensor_tensor(out=y0.ap, in0=y0.ap, in1=gb.ap[:, 0, :], op=ALU.mult), [y0], [y0, gb])
                    fw.dma(mix[r0:r0 + 128, 512:1024], y0.ap, [mix_r[i]], [y0])
                fw.barrier()
            if stop_after == "S4":
                if "mix" in dbg:
                    for i in range(NT):
                        fw.dma(dbg["mix"][i * 128:(i + 1) * 128, :], mix[i * 128:(i + 1) * 128, :], [], [mix_a[i], mix_r[i]])
                    fw.barrier()
                break
            tiles5 = list(range(NCT, NT)) if last else list(range(NT))
            with ExitStack() as es:
                woutb = sb(es, nc, "woutb", [128, 8, D], BF16)
                wst_pool = Pool(es, nc, "wst5", [128, 8, 256], F32, 2)
                for j in range(4):
                    wst = wst_pool.next()
                    fw.dma(wst.ap, w_out[l][:, j * 256:(j + 1) * 256].rearrange("(kc p) n -> p kc n", p=128), [wst], [])
                    fw.op(pool, lambda e, wst=wst, j=j: e.tensor_copy(out=woutb.ap[:, :, j * 256:(j + 1) * 256], in_=wst.ap), [woutb], [wst])
                modG = [sb(es, nc, "modG%d" % s_, [128, D], F32) for s_ in range(2)]
                for s_ in range(2):
                    fw.dma(modG[s_].ap, modbuf[l, s_ * 6 + 2], [modG[s_]], [mod_t[l][s_ * 6 + 2]])
                mx_pool = Pool(es, nc, "mx", [128, D], F32, 2)
                mxb_pool = Pool(es, nc, "mxb", [128, D], BF16, 2)
                mT_pool = Pool(es, nc, "mT", [128, 8, 128], BF16, 2)
                x5_pool = Pool(es, nc, "x5", [128, D], F32, 2)

                def s5_load(i):
                    mx = mx_pool.next()
                    fw.dma(mx.ap, mix[i * 128:(i + 1) * 128, :], [mx], [mix_a[i], mix_r[i]])
                    xt = x5_pool.next()
                    fw.dma(xt.ap, xs[i * 128:(i + 1) * 128, :], [xt], [xs_t[i]])
                    return mx, xt

                nxt = s5_load(tiles5[0])
                for ti, i in enumerate(tiles5):
                    mx, xt = nxt
                    if ti + 1 < len(tiles5):
                        nxt = s5_load(tiles5[ti + 1])
                    s_ = 1 if i < NCT else 0
                    mxb = mxb_pool.next()
                    fw.op(pool, lambda e, mx=mx, mxb=mxb: e.tensor_copy(out=mxb.ap, in_=mx.ap), [mxb], [mx])
                    mT = mT_pool.next()
                    for half in range(2):
                        ps = psum()
                        psb = ps.ap.bitcast(BF16)
                        for kk in range(4):
                            kc = half * 4 + kk
                            fw.op(pe, lambda e, kc=kc, kk=kk, psb=psb, mxb=mxb: e.transpose(out=psb[:, kk * 128:(kk + 1) * 128], in_=mxb.ap[:, kc * 128:(kc + 1) * 128], identity=identb.ap), [ps], [mxb, identb])
                        fw.op(act if half else dve, lambda e, psb=psb, mT=mT, half=half: (e.copy if half else e.tensor_copy)(out=mT.ap[:, half * 4:(half + 1) * 4, :], in_=psb[:, 0:512].rearrange("p (a b) -> p a b", a=4)), [mT], [ps])
                    for half in range(2):
                        ps = psum()
                        hs = slice(half * 512, (half + 1) * 512)
                        for kc in range(8):
                            fw.op(pe, lambda e, kc=kc, ps=ps, mT=mT, hs=hs: e.matmul(ps.ap, lhsT=mT.ap[:, kc, :], rhs=woutb.ap[:, kc, hs], start=(kc == 0), stop=(kc == 7)), [ps], [mT, woutb])
                        fw.op(dve, lambda e, ps=ps, mx=mx, hs=hs, s_=s_: e.tensor_tensor(out=mx.ap[:, hs], in0=ps.ap, in1=modG[s_].ap[:, hs], op=ALU.mult), [mx], [ps, modG[s_]])
                    fw.op(pool, lambda e, xt=xt, mx=mx: e.tensor_tensor(out=xt.ap, in0=xt.ap, in1=mx.ap, op=ALU.add), [xt], [xt, mx])
                    fw.dma(xs[i * 128:(i + 1) * 128, :], xt.ap, [xs_t[i]], [xt])
                    if "xmix" in dbg and l == 0:
                        fw.dma(dbg["xmix"][i * 128:(i + 1) * 128, :], xt.ap, [], [xt])
                fw.barrier()
            if stop_after == "S5":
                break

            tiles6 = list(range(NCT, NT)) if last else list(range(NT))
            GS = 8
            groups = [tiles6[k:k + GS] for k in range(0, len(tiles6), GS)]
            with ExitStack() as es:
                mA = [sb(es, nc, "mA%d" % s_, [128, D], F32) for s_ in range(2)]
                mB = [sb(es, nc, "mB%d" % s_, [128, D], F32) for s_ in range(2)]
                mG = [sb(es, nc, "mG%d" % s_, [128, D], F32) for s_ in range(2)]
                for s_ in range(2):
                    fw.dma(mA[s_].ap, modbuf[l, s_ * 6 + 3], [mA[s_]], [mod_t[l][s_ * 6 + 3]])
                    fw.dma(mB[s_].ap, modbuf[l, s_ * 6 + 4], [mB[s_]], [mod_t[l][s_ * 6 + 4]])
                    fw.dma(mG[s_].ap, modbuf[l, s_ * 6 + 5], [mG[s_]], [mod_t[l][s_ * 6 + 5]])
                rwt = sb(es, nc, "rwt", [128, 8, NE], F32)
                fw.dma(rwt.ap, router_w.rearrange("(kc p) n -> p kc n", p=128), [rwt], [])
                rbt = sb(es, nc, "rbt", [128, NE], F32)
                fw.dma(rbt.ap, router_b.partition_broadcast(128), [rbt], [])
                fgt = None
                if last:
                    fgt = sb(es, nc, "fgt", [128, D], F32)
                    fw.dma(fgt.ap, final_g.partition_broadcast(128), [fgt], [])
                HT = sb(es, nc, "HT", [128, 8, GS * 128], BF16)
                accs = [sb(es, nc, "acc%d" % k, [128, D], F32) for k in range(GS)]
                gates = sb(es, nc, "gates", [128, GS, NE], F32)
                Wg2 = [sb(es, nc, "Wg", [128, 8, D], BF16) for _ in range(2)]
                Wu2 = [sb(es, nc, "Wu", [128, 8, D], BF16) for _ in range(2)]
                Wd = sb(es, nc, "Wd", [128, 8, D], BF16)
                actb = sb(es, nc, "actb", [128, 8, GS * 128], BF16)
                x6_pool = Pool(es, nc, "x6", [128, D], F32, 2)
                hT32_pool = Pool(es, nc, "hT32", [128, 8, 128], F32, 1)
                rt_pool = Pool(es, nc, "rt", [128, 96], F32, 2)
                sg_pool = Pool(es, nc, "sg", [128, 512], F32, 2)
                st6_pool = Pool(es, nc, "st6", [128, 4], F32, 2)

                def rms(xt, st, junk6):
                    fw.op(act, lambda e: e.activation(out=junk6.ap, in_=xt.ap, func=AF.Square, accum_out=st.ap[:, 0:1]), [junk6, st], [xt])
                    fw.op(dve, lambda e: e.tensor_scalar(out=st.ap[:, 1:2], in0=st.ap[:, 0:1], scalar1=1.0 / D, scalar2=1e-6, op0=ALU.mult, op1=ALU.add), [st], [st])
                    fw.op(act, lambda e: e.activation(out=st.ap[:, 3:4], in_=st.ap[:, 1:2], func=AF.Sqrt), [st], [st])
                    fw.op(dve, lambda e: e.reciprocal(out=st.ap[:, 2:3], in_=st.ap[:, 3:4]), [st], [st])

                def load_w(dst, src):
                    for j in range(2):
                        fw.dma(dst.ap[:, :, j * 512:(j + 1) * 512], src[:, j * 512:(j + 1) * 512].rearrange("(kc p) n -> p kc n", p=128), [dst], [], q=pool)

                seq = [(gi, ex) for gi in range(len(groups)) for ex in range(NE)]
                load_w(Wg2[0], e_gate[l, 0])
                load_w(Wu2[0], e_up[l, 0])

                for gi, grp in enumerate(groups):
                    ntok = len(grp) * 128
                    for t, i in enumerate(grp):
                        s_ = 1 if i < NCT else 0
                        xt = x6_pool.next()
                        fw.dma(xt.ap, xs[i * 128:(i + 1) * 128, :], [xt], [xs_t[i]])
                        st = st6_pool.next()
                        rms(xt, st, accs[t])
                        fw.op(dve, lambda e, xt=xt, st=st, s_=s_: e.scalar_tensor_tensor(out=xt.ap, in0=xt.ap, scalar=st.ap[:, 2:3], in1=mA[s_].ap, op0=ALU.mult, op1=ALU.mult), [xt], [xt, st, mA[s_]])
                        fw.op(pool, lambda e, xt=xt, s_=s_: e.tensor_tensor(out=xt.ap, in0=xt.ap, in1=mB[s_].ap, op=ALU.add), [xt], [xt, mB[s_]])
                        hT32 = hT32_pool.next()
                        for half in range(2):
                            ps = psum()
                            for kk in range(4):
                                kc = half * 4 + kk
                                fw.op(pe, lambda e, kc=kc, kk=kk, ps=ps, xt=xt: e.transpose(out=ps.ap[:, kk * 128:(kk + 1) * 128], in_=xt.ap[:, kc * 128:(kc + 1) * 128], identity=ident.ap), [ps], [xt, ident])
                            fw.op(dve, lambda e, ps=ps, hT32=hT32, half=half: e.tensor_copy(out=hT32.ap[:, half * 4:(half + 1) * 4, :], in_=ps.ap.rearrange("p (a b) -> p a b", a=4)), [hT32], [ps])
                            fw.op(act, lambda e, hT32=hT32, half=half, t=t: e.copy(out=HT.ap[:, half * 4:(half + 1) * 4, t * 128:(t + 1) * 128], in_=hT32.ap[:, half * 4:(half + 1) * 4, :]), [HT], [hT32])
                        ps = psum()
                        for kc in range(8):
                            fw.op(pe, lambda e, kc=kc, ps=ps, hT32=hT32: e.matmul(ps.ap[:, 0:NE], lhsT=hT32.ap[:, kc, :], rhs=rwt.ap[:, kc, :], start=(kc == 0), stop=(kc == 7)), [ps], [hT32, rwt])
                        rt = rt_pool.next()
                        R = rt.ap
                        sc_, bi_ = R[:, 0:16], R[:, 16:32]
                        g4 = lambda ap: ap.rearrange("p (g m) -> p g m", g=4)
                        fw.op(act, lambda e, ps=ps, sc_=sc_: e.activation(out=sc_, in_=ps.ap[:, 0:NE], func=AF.Sigmoid), [rt], [ps])
                        fw.op(dve, lambda e, sc_=sc_, bi_=bi_: e.tensor_tensor(out=bi_, in0=sc_, in1=rbt.ap, op=ALU.add), [rt], [rt, rbt])
                        a = [g4(bi_)[:, :, m] for m in range(4)]
                        hi01, lo01, hi23, lo23 = R[:, 32:36], R[:, 36:40], R[:, 40:44], R[:, 44:48]
                        top1, m1, m2, top2 = R[:, 48:52], R[:, 52:56], R[:, 56:60], R[:, 60:64]
                        gs_, gmx, gm_, rd = R[:, 64:68], R[:, 68:69], R[:, 72:76], R[:, 69:70]
                        sel = R[:, 80:96]
                        tt = lambda o, x0, x1, op: fw.op(dve, lambda e: e.tensor_tensor(out=o, in0=x0, in1=x1, op=op), [rt], [rt])
                        tt(hi01, a[0], a[1], ALU.max)
                        tt(lo01, a[0], a[1], ALU.min)
                        tt(hi23, a[2], a[3], ALU.max)
                        tt(lo23, a[2], a[3], ALU.min)
                        tt(top1, hi01, hi23, ALU.max)
                        tt(m1, hi01, hi23, ALU.min)
                        tt(m2, lo01, lo23, ALU.max)
                        tt(top2, m1, m2, ALU.max)
                        tt(gs_, top1, top2, ALU.add)
                        fw.op(dve, lambda e, gs_=gs_, gmx=gmx: e.reduce_max(out=gmx, in_=gs_, axis=AX.X), [rt], [rt])
                        fw.op(dve, lambda e, gs_=gs_, gmx=gmx, gm_=gm_: e.tensor_scalar(out=gm_, in0=gs_, scalar1=gmx, scalar2=None, op0=ALU.is_ge), [rt], [rt])
                        tt(g4(sel), g4(bi_), top2.unsqueeze(2).to_broadcast([128, 4, 4]), ALU.is_ge)
                        tt(g4(sel), g4(sel), gm_.unsqueeze(2).to_broadcast([128, 4, 4]), ALU.mult)
                        tt(sel, sel, sc_, ALU.mult)
                        fw.op(dve, lambda e, sel=sel, rd=rd: e.reduce_sum(out=rd, in_=sel, axis=AX.X), [rt], [rt])
                        fw.op(dve, lambda e, rd=rd: e.reciprocal(out=rd, in_=rd), [rt], [rt])
                        fw.op(dve, lambda e, sel=sel, rd=rd, t=t: e.tensor_scalar(out=gates.ap[:, t, :], in0=sel, scalar1=rd, scalar2=None, op0=ALU.mult), [gates], [rt])
                        if "gates" in dbg and l == 0:
                            fw.dma(dbg["gates"][i * 128:(i + 1) * 128, :], gates.ap[:, t, :], [], [gates])
                    for ex in range(NE):
                        qi = gi * NE + ex
                        Wg, Wu = Wg2[qi % 2], Wu2[qi % 2]
                        load_w(Wd, e_down[l, ex])
                        if qi + 1 < len(seq):
                            nex = seq[qi + 1][1]
                            load_w(Wg2[(qi + 1) % 2], e_gate[l, nex])
                            load_w(Wu2[(qi + 1) % 2], e_up[l, nex])
                        for t0 in range(0, ntok, 512):
                            ntk = min(512, ntok - t0)
                            for fc in range(8):
                                psg = psum()
                                psu = psum()
                                for kc in range(8):
                                    fw.op(pe, lambda e, kc=kc, fc=fc, psg=psg, t0=t0, ntk=ntk: e.matmul(psg.ap[:, 0:ntk], lhsT=Wg.ap[:, kc, fc * 128:(fc + 1) * 128], rhs=HT.ap[:, kc, t0:t0 + ntk], start=(kc == 0), stop=(kc == 7)), [psg], [Wg, HT])
                                for kc in range(8):
                                    fw.op(pe, lambda e, kc=kc, fc=fc, psu=psu, t0=t0, ntk=ntk: e.matmul(psu.ap[:, 0:ntk], lhsT=Wu.ap[:, kc, fc * 128:(fc + 1) * 128], rhs=HT.ap[:, kc, t0:t0 + ntk], start=(kc == 0), stop=(kc == 7)), [psu], [Wu, HT])
                                sg = sg_pool.next()
                                fw.op(act, lambda e, psg=psg, sg=sg, ntk=ntk: e.activation(out=sg.ap[:, 0:ntk], in_=psg.ap[:, 0:ntk], func=AF.Silu), [sg], [psg])
                                fw.op(dve, lambda e, psu=psu, sg=sg, fc=fc, t0=t0, ntk=ntk: e.tensor_tensor(out=actb.ap[:, fc, t0:t0 + ntk], in0=sg.ap[:, 0:ntk], in1=psu.ap[:, 0:ntk], op=ALU.mult), [actb], [sg, psu])
                        for t in range(len(grp)):
                            for half in range(2):
                                hs = slice(half * 512, (half + 1) * 512)
                                ps = psum()
                                for fc in range(8):
                                    fw.op(pe, lambda e, fc=fc, ps=ps, t=t, hs=hs: e.matmul(ps.ap, lhsT=actb.ap[:, fc, t * 128:(t + 1) * 128], rhs=Wd.ap[:, fc, hs], start=(fc == 0), stop=(fc == 7)), [ps], [actb, Wd])
                                if ex == 0:
                                    fw.op(dve, lambda e, ps=ps, t=t, hs=hs, ex=ex: e.tensor_scalar(out=accs[t].ap[:, hs], in0=ps.ap, scalar1=gates.ap[:, t, ex:ex + 1], scalar2=None, op0=ALU.mult), [accs[t]], [ps, gates])
                                else:
                                    fw.op(dve, lambda e, ps=ps, t=t, hs=hs, ex=ex: e.scalar_tensor_tensor(out=accs[t].ap[:, hs], in0=ps.ap, scalar=gates.ap[:, t, ex:ex + 1], in1=accs[t].ap[:, hs], op0=ALU.mult, op1=ALU.add), [accs[t]], [ps, gates, accs[t]])
                    for t, i in enumerate(grp):
                        s_ = 1 if i < NCT else 0
                        xt = x6_pool.next()
                        fw.dma(xt.ap, xs[i * 128:(i + 1) * 128, :], [xt], [xs_t[i]])
                        fw.op(pool, lambda e, t=t, s_=s_: e.tensor_tensor(out=accs[t].ap, in0=accs[t].ap, in1=mG[s_].ap, op=ALU.mult), [accs[t]], [accs[t], mG[s_]])
                        fw.op(dve, lambda e, xt=xt, t=t: e.tensor_tensor(out=xt.ap, in0=xt.ap, in1=accs[t].ap, op=ALU.add), [xt], [xt, accs[t]])
                        if "xout" in dbg and l == 0:
                            fw.dma(dbg["xout"][i * 128:(i + 1) * 128, :], xt.ap, [], [xt])
                        if not last:
                            fw.dma(xs[i * 128:(i + 1) * 128, :], xt.ap, [xs_t[i]], [xt])
                        else:
                            st = st6_pool.next()
                            rms(xt, st, accs[t])
                            fw.op(dve, lambda e, xt=xt, st=st: e.scalar_tensor_tensor(out=xt.ap, in0=xt.ap, scalar=st.ap[:, 2:3], in1=fgt.ap, op0=ALU.mult, op1=ALU.mult), [xt], [xt, st, fgt])
                            fw.dma(out[(i - NCT) * 128:(i - NCT + 1) * 128, :], xt.ap, [], [xt])
                fw.barrier()
            if stop_after == "S6":
                break
        fw.barrier()
    return nc


def prep_inputs(inputs):
    f = lambda a: np.ascontiguousarray(np.asarray(a, dtype=np.float32))
    shared = {}
    for k in ("w_mod", "b_mod", "norm_mix_g", "norm_ffn_g", "w_in", "w_out", "att_sink", "shift_mu_prev", "shift_mu_next",
              "decay_w0", "decay_w2", "iclr_a0", "iclr_a2", "vres_v0", "vres_v1", "vres_v2", "gate_g2", "k_k", "k_a",
              "ln_x_w", "ln_x_b", "router_w", "router_b", "expert_w_gate", "expert_w_up", "expert_w_down", "final_norm_g"):
        shared[k] = f(inputs[k])
    shared["r_k"] = f(inputs["r_k"]).reshape(DEPTH, RW)
    shared["ident"] = np.eye(128, dtype=np.float32)
    s = np.arange(128)[:, None]
    t = np.arange(128)[None, :]
    shared["masks"] = np.ascontiguousarray(np.stack([s < t, s <= t, s > t, s >= t], axis=1).astype(np.float32))
    pos = np.arange(L)
    row = (pos // 64).astype(np.float32)
    colp = (pos % 64).astype(np.float32)
    inv = (10000.0 ** (-np.arange(16, dtype=np.float32) / 16)).astype(np.float32)
    ang = np.concatenate([row[:, None] * inv, row[:, None] * inv, colp[:, None] * inv, colp[:, None] * inv], axis=1)
    cos = np.cos(ang).astype(np.float32)
    sin = np.sin(ang).astype(np.float32)
    sgn = np.concatenate([-np.ones(16), np.ones(16), -np.ones(16), np.ones(16)]).astype(np.float32)
    shared["rope_cos"] = np.ascontiguousarray(np.tile(cos, (1, 10)))
    shared["rope_sin"] = np.ascontiguousarray(np.tile(sin * sgn, (1, 10)))
    x = f(inputs["x"])
    ctx = f(inputs["ctx"])
    c = f(inputs["c"])
    cc = f(inputs["c_ctx"])
    maps = []
    for b in range(8):
        m = dict(shared)
        m["x"] = x[b]
        m["ctx"] = ctx[b]
        m["ccol"] = np.ascontiguousarray(np.concatenate([c[b].reshape(8, 128).T, cc.reshape(8, 128).T], axis=1))
        maps.append(m)
    return maps


def kernel(**inputs):
    maps = prep_inputs(inputs)
    nc = build()
    res = run_bass_kernel_spmd(nc, maps, core_ids=list(range(8)))
    return np.stack([r["out"] for r in res.results], axis=0).astype(np.float32)
```

```python
from contextlib import ExitStack
import os
import numpy as np
import ml_dtypes
import concourse.bass as bass
import concourse.mybir as mybir
from concourse.bass_utils import run_bass_kernel_spmd

F32 = mybir.dt.float32
BF16 = mybir.dt.bfloat16
AF = mybir.ActivationFunctionType
ALU = mybir.AluOpType
AX = mybir.AxisListType

D = 1024
L = 4096
C = 256
T = L + C
NT = T // 128
NCT = C // 128
DEPTH = 2
IN_COLS = 2688
RW = 512
RWC = 1920
NE = 16
ATT_SCALE = 0.125
NEG = -30000.0


class Eng:
    def __init__(self, fw, e, name, ring=0):
        self.fw = fw
        self.e = e
        self.name = name
        self.count = 0
        self.seen = {}
        self.sem = fw.nc.alloc_semaphore("s_" + name)
        self.ring = [fw.nc.alloc_semaphore("r_%s%d" % (name, i)) for i in range(ring)]
        self.ndma = 0

    def wait_tok(self, tok):
        sem, val, key = tok
        if self.seen.get(key, 0) >= val:
            return
        self.e.wait_ge(sem, val)
        self.seen[key] = val


class TT:
    def __init__(self, ap=None):
        self.ap = ap
        self.w = None
        self.r = {}


class FW:
    def __init__(self, nc):
        self.nc = nc
        self.pe = Eng(self, nc.tensor, "pe")
        self.act = Eng(self, nc.scalar, "act")
        self.dve = Eng(self, nc.vector, "dve")
        self.pool = Eng(self, nc.gpsimd, "pool", ring=12)
        self.sp = Eng(self, nc.sync, "sp", ring=24)
        self.engs = [self.pe, self.act, self.dve, self.pool, self.sp]
        self.ninst = 0

    def _deps(self, eng, outs, ins):
        for t in ins:
            if t.w is not None and not (eng is self.pe and t.w[2] == "pe"):
                eng.wait_tok(t.w)
        for t in outs:
            if t.w is not None and not (eng is self.pe and t.w[2] == "pe"):
                eng.wait_tok(t.w)
            for tok in t.r.values():
                if not (eng is self.pe and tok[2] == "pe"):
                    eng.wait_tok(tok)

    def _mark(self, tok, outs, ins):
        for t in ins:
            t.r[tok[2]] = tok
        for t in outs:
            t.w = tok
            t.r = {}

    def op(self, eng, fn, outs, ins):
        self._deps(eng, outs, ins)
        inst = fn(eng.e)
        eng.count += 1
        inst.then_inc(eng.sem, 1)
        self._mark((eng.sem, eng.count, eng.name), outs, ins)
        self.ninst += 1

    def dma(self, out_ap, in_ap, outs, ins, q=None):
        q = q or self.sp
        self._deps(q, outs, ins)
        K = len(q.ring)
        slot = q.ndma % K
        rnd = q.ndma // K
        key = "%s_r%d" % (q.name, slot)
        if rnd > 0:
            q.wait_tok((q.ring[slot], 16 * rnd, key))
        q.e.dma_start(out=out_ap, in_=in_ap).then_inc(q.ring[slot], 16)
        q.ndma += 1
        self._mark((q.ring[slot], 16 * (rnd + 1), key), outs, ins)
        self.ninst += 1

    def all_toks(self):
        toks = []
        for e in self.engs:
            if e.count:
                toks.append((e.sem, e.count, e.name))
            K = len(e.ring)
            for slot in range(min(K, e.ndma)):
                n = (e.ndma - 1 - slot) // K + 1
                toks.append((e.ring[slot], 16 * n, "%s_r%d" % (e.name, slot)))
        return toks

    def barrier(self):
        toks = self.all_toks()
        for e in self.engs:
            for tok in toks:
                if e is self.pe and tok[2] == "pe":
                    continue
                e.wait_tok(tok)


_uid = [0]


def _nm(name):
    _uid[0] += 1
    return "sb_%s_%d" % (name, _uid[0])


class Pool:
    def __init__(self, es, nc, name, shape, dtype, bufs):
        self.t = [TT(es.enter_context(nc.sbuf_tensor(_nm(name), shape, dtype)).ap()) for i in range(bufs)]
        self.i = 0

    def next(self):
        t = self.t[self.i % len(self.t)]
        self.i += 1
        return t


def sb(es, nc, name, shape, dtype):
    return TT(es.enter_context(nc.sbuf_tensor(_nm(name), shape, dtype)).ap())


def build(debug_outs=(), stop_after=None):
    nc = bass.Bass("TRN2", target_bir_lowering=False)
    fw = FW(nc)
    pe, act, dve, pool, sp = fw.pe, fw.act, fw.dve, fw.pool, fw.sp

    def din(name, shape, dt=F32):
        return nc.dram_tensor(name, list(shape), dt, kind="ExternalInput").ap()

    def dscr(name, shape, dt=F32):
        return nc.dram_tensor(name, list(shape), dt, kind="Internal").ap()

    x_in = din("x", [L, D])
    ctx_in = din("ctx", [C, D])
    ccol_in = din("ccol", [128, 16])
    w_mod = din("w_mod", [DEPTH, D, 6 * D])
    b_mod = din("b_mod", [DEPTH, 6 * D])
    norm_mix_g = din("norm_mix_g", [DEPTH, D])
    norm_ffn_g = din("norm_ffn_g", [DEPTH, D])
    w_in = din("w_in", [DEPTH, D, IN_COLS])
    w_out = din("w_out", [DEPTH, D, D])
    att_sink = din("att_sink", [DEPTH, 8])
    mu_prev = din("shift_mu_prev", [DEPTH, RWC])
    mu_next = din("shift_mu_next", [DEPTH, RWC])
    decay_w0 = din("decay_w0", [DEPTH, 2, RW])
    decay_w2 = din("decay_w2", [DEPTH, 2, 64, RW])
    iclr_a0 = din("iclr_a0", [DEPTH, 2, RW])
    iclr_a2 = din("iclr_a2", [DEPTH, 2, 64, RW])
    vres_v0 = din("vres_v0", [1, RW])
    vres_v1 = din("vres_v1", [1, RW, 32])
    vres_v2 = din("vres_v2", [1, 32, RW])
    gate_g2 = din("gate_g2", [DEPTH, 128, RW])
    k_k = din("k_k", [DEPTH, RW])
    k_a = din("k_a", [DEPTH, RW])
    r_k = din("r_k", [DEPTH, RW])
    ln_x_w = din("ln_x_w", [DEPTH, RW])
    ln_x_b = din("ln_x_b", [DEPTH, RW])
    router_w = din("router_w", [D, NE])
    router_b = din("router_b", [NE])
    e_gate = din("expert_w_gate", [DEPTH, NE, D, D])
    e_up = din("expert_w_up", [DEPTH, NE, D, D])
    e_down = din("expert_w_down", [DEPTH, NE, D, D])
    final_g = din("final_norm_g", [D])
    ident_in = din("ident", [128, 128])
    masks_in = din("masks", [128, 4, 128])
    rope_cos = din("rope_cos", [L, 640])
    rope_sin = din("rope_sin", [L, 640])
    out = nc.dram_tensor("out", [L, D], F32, kind="ExternalOutput").ap()

    xs = dscr("xs", [T, D])
    xs_t = [TT() for _ in range(NT)]
    modbuf = dscr("modbuf", [DEPTH, 12, 128, D])
    mod_t = [[TT() for _ in range(12)] for _ in range(DEPTH)]
    u_scr = dscr("u_scr", [T + 4, RWC])
    u_t = [TT() for _ in range(NT)]
    u_guard = TT()
    mix = dscr("mix", [T, D])
    mix_a = [TT() for _ in range(NT)]
    mix_r = [TT() for _ in range(NT)]
    vfirst = dscr("vfirst", [T, RW])
    vf_t = [TT() for _ in range(NT)]
    feat = dscr("feat", [T, 5632])
    feat_t = [TT() for _ in range(NT)]
    y_scr = dscr("y_scr", [2, T, RW])
    y_t = [[TT() for _ in range(NT)] for _ in range(2)]
    dbg = {}
    for name, shape in debug_outs:
        dbg[name] = nc.dram_tensor("dbg_" + name, list(shape), F32, kind="ExternalOutput").ap()

    def urow(i):
        return 1 + i * 128 if i < NCT else 3 + i * 128

    PS = [TT(nc.alloc_psum_tensor("psb%d" % i, [128, 512], F32).ap()) for i in range(8)]
    psi = [0]

    def psum():
        t = PS[psi[0] % 6]
        psi[0] += 1
        return t

    with ExitStack() as g_es:
        ident = sb(g_es, nc, "ident", [128, 128], F32)
        identb = sb(g_es, nc, "identb", [128, 128], BF16)
        masks = sb(g_es, nc, "masks", [128, 4, 128], F32)
        fw.dma(ident.ap, ident_in, [ident], [])
        fw.dma(masks.ap, masks_in, [masks], [])
        fw.op(dve, lambda e: e.tensor_copy(out=identb.ap, in_=ident.ap), [identb], [ident])
        zrow = sb(g_es, nc, "zrow", [1, RWC], F32)
        fw.op(dve, lambda e: e.memset(zrow.ap, 0.0), [zrow], [])
        for r in (0, C + 1, C + 2, T + 3):
            fw.dma(u_scr[r:r + 1, :], zrow.ap, [u_guard], [zrow])

        for i in range(NT):
            src = ctx_in[i * 128:(i + 1) * 128, :] if i < NCT else x_in[(i - NCT) * 128:(i - NCT + 1) * 128, :]
            fw.dma(xs[i * 128:(i + 1) * 128, :], src, [xs_t[i]], [])

        for l in range(DEPTH):
            last = l == DEPTH - 1
            with ExitStack() as es:
                ccol = sb(es, nc, "ccol", [128, 16], F32)
                csil = sb(es, nc, "csil", [128, 16], F32)
                crep = sb(es, nc, "crep", [128, 16, 128], F32)
                fw.dma(ccol.ap, ccol_in, [ccol], [])
                fw.op(act, lambda e: e.activation(out=csil.ap, in_=ccol.ap, func=AF.Silu), [csil], [ccol])
                fw.op(dve, lambda e: e.tensor_copy(out=crep.ap, in_=csil.ap.unsqueeze(2).to_broadcast([128, 16, 128])), [crep], [csil])
                wm_pool = Pool(es, nc, "wm", [128, 8, 512], F32, 2)
                bm_pool = Pool(es, nc, "bm", [128, 512], F32, 2)
                modraw = [[sb(es, nc, "mr%d_%d" % (s, j), [128, D], F32) for j in range(6)] for s in range(2)]
                gmix = sb(es, nc, "gmix", [128, D], F32)
                gffn = sb(es, nc, "gffn", [128, D], F32)
                fw.dma(gmix.ap, norm_mix_g[l].partition_broadcast(128), [gmix], [])
                fw.dma(gffn.ap, norm_ffn_g[l].partition_broadcast(128), [gffn], [])
                for j in range(12):
                    wm = wm_pool.next()
                    bm = bm_pool.next()
                    fw.dma(wm.ap, w_mod[l][:, j * 512:(j + 1) * 512].rearrange("(kc p) n -> p kc n", p=128), [wm], [])
                    fw.dma(bm.ap, b_mod[l][j * 512:(j + 1) * 512].partition_broadcast(128), [bm], [])
                    for s in range(2):
                        ps = psum()
                        for kc in range(8):
                            fw.op(pe, lambda e, kc=kc, s=s, ps=ps, wm=wm: e.matmul(ps.ap, lhsT=crep.ap[:, s * 8 + kc, :], rhs=wm.ap[:, kc, :], start=(kc == 0), stop=(kc == 7)), [ps], [crep, wm])
                        dst = modraw[s][j // 2]
                        fw.op(dve, lambda e, ps=ps, dst=dst, bm=bm, j=j: e.tensor_tensor(out=dst.ap[:, (j % 2) * 512:(j % 2 + 1) * 512], in0=ps.ap, in1=bm.ap, op=ALU.add), [dst], [ps, bm])
                for s in range(2):
                    sh_a, sc_a, ga, sh_f, sc_f, gf = modraw[s]
                    fw.op(dve, lambda e, sc_a=sc_a: e.scalar_tensor_tensor(out=sc_a.ap, in0=sc_a.ap, scalar=1.0, in1=gmix.ap, op0=ALU.add, op1=ALU.mult), [sc_a], [sc_a, gmix])
                    fw.op(dve, lambda e, sc_f=sc_f: e.scalar_tensor_tensor(out=sc_f.ap, in0=sc_f.ap, scalar=1.0, in1=gffn.ap, op0=ALU.add, op1=ALU.mult), [sc_f], [sc_f, gffn])
                    for idx, t in enumerate((sc_a, sh_a, ga, sc_f, sh_f, gf)):
                        fw.dma(modbuf[l, s * 6 + idx], t.ap, [mod_t[l][s * 6 + idx]], [t])
                fw.barrier()
            if stop_after == "S1":
                break

            with ExitStack() as es_att:
                QT = sb(es_att, nc, "QT", [128, 4, T], BF16)
                KT = sb(es_att, nc, "KT", [128, T], BF16)
                VV = sb(es_att, nc, "VV", [128, NT, 128], BF16)
                qt_t = [TT() for _ in range(NT)]
                with ExitStack() as es:
                    winb = sb(es, nc, "winb", [128, 8, IN_COLS], BF16)
                    wst_pool = Pool(es, nc, "wst", [128, 8, 384], F32, 2)
                    for j in range(7):
                        wst = wst_pool.next()
                        fw.dma(wst.ap, w_in[l][:, j * 384:(j + 1) * 384].rearrange("(kc p) n -> p kc n", p=128), [wst], [])
                        fw.op(pool, lambda e, wst=wst, j=j: e.tensor_copy(out=winb.ap[:, :, j * 384:(j + 1) * 384], in_=wst.ap), [winb], [wst])
                    modA = [sb(es, nc, "modA%d" % s, [128, D], F32) for s in range(2)]
                    modB = [sb(es, nc, "modB%d" % s, [128, D], F32) for s in range(2)]
                    for s in range(2):
                        fw.dma(modA[s].ap, modbuf[l, s * 6 + 0], [modA[s]], [mod_t[l][s * 6 + 0]])
                        fw.dma(modB[s].ap, modbuf[l, s * 6 + 1], [modB[s]], [mod_t[l][s * 6 + 1]])
                    x_pool = Pool(es, nc, "xt", [128, D], F32, 2)
                    junk = sb(es, nc, "junk", [128, D], F32)
                    st_pool = Pool(es, nc, "st", [128, 4], F32, 2)
                    h_pool = Pool(es, nc, "ht", [128, D], BF16, 2)
                    hT_pool = Pool(es, nc, "hT", [128, 8, 128], BF16, 2)
                    pr_pool = Pool(es, nc, "pr", [128, IN_COLS], F32, 2)
                    cos_pool = Pool(es, nc, "cos", [128, 640], F32, 2)
                    sin_pool = Pool(es, nc, "sin", [128, 640], F32, 2)
                    rp_pool = Pool(es, nc, "rp", [128, 640], F32, 2)
                    rq_pool = Pool(es, nc, "rq", [128, 640], BF16, 2)

                    def s2_load(i):
                        xt = x_pool.next()
                        fw.dma(xt.ap, xs[i * 128:(i + 1) * 128, :], [xt], [xs_t[i]])
                        cs = sn = None
                        if i >= NCT:
                            cs = cos_pool.next()
                            sn = sin_pool.next()
                            r0 = (i - NCT) * 128
                            fw.dma(cs.ap, rope_cos[r0:r0 + 128, :], [cs], [])
                            fw.dma(sn.ap, rope_sin[r0:r0 + 128, :], [sn], [])
                        return xt, cs, sn

                    nxt = s2_load(0)
                    for i in range(NT):
                        xt, cs, sn = nxt
                        if i + 1 < NT:
                            nxt = s2_load(i + 1)
                        s = 1 if i < NCT else 0
                        st = st_pool.next()
                        fw.op(act, lambda e, xt=xt, st=st: e.activation(out=junk.ap, in_=xt.ap, func=AF.Square, accum_out=st.ap[:, 0:1]), [junk, st], [xt])
                        fw.op(dve, lambda e, st=st: e.tensor_scalar(out=st.ap[:, 1:2], in0=st.ap[:, 0:1], scalar1=1.0 / D, scalar2=1e-6, op0=ALU.mult, op1=ALU.add), [st], [st])
                        fw.op(act, lambda e, st=st: e.activation(out=st.ap[:, 3:4], in_=st.ap[:, 1:2], func=AF.Sqrt), [st], [st])
                        fw.op(dve, lambda e, st=st: e.reciprocal(out=st.ap[:, 2:3], in_=st.ap[:, 3:4]), [st], [st])
                        fw.op(dve, lambda e, xt=xt, st=st, s=s: e.scalar_tensor_tensor(out=xt.ap, in0=xt.ap, scalar=st.ap[:, 2:3], in1=modA[s].ap, op0=ALU.mult, op1=ALU.mult), [xt], [xt, st, modA[s]])
                        ht = h_pool.next()
                        fw.op(pool, lambda e, xt=xt, ht=ht, s=s: e.tensor_tensor(out=ht.ap, in0=xt.ap, in1=modB[s].ap, op=ALU.add), [ht], [xt, modB[s]])
                        hT = hT_pool.next()
                        for half in range(2):
                            ps = psum()
                            psb = ps.ap.bitcast(BF16)
                            for kk in range(4):
                                kc = half * 4 + kk
                                fw.op(pe, lambda e, kc=kc, kk=kk, psb=psb, ht=ht: e.transpose(out=psb[:, kk * 128:(kk + 1) * 128], in_=ht.ap[:, kc * 128:(kc + 1) * 128], identity=identb.ap), [ps], [ht, identb])
                            fw.op(act if half else dve, lambda e, psb=psb, hT=hT, half=half: (e.copy if half else e.tensor_copy)(out=hT.ap[:, half * 4:(half + 1) * 4, :], in_=psb[:, 0:512].rearrange("p (a b) -> p a b", a=4)), [hT], [ps])
                        pr = pr_pool.next()
                        col = 0
                        ci = 0
                        while col < IN_COLS:
                            n = min(512, IN_COLS - col)
                            ps = psum()
                            for kc in range(8):
                                fw.op(pe, lambda e, kc=kc, ps=ps, hT=hT, col=col, n=n: e.matmul(ps.ap[:, 0:n], lhsT=hT.ap[:, kc, :], rhs=winb.ap[:, kc, col:col + n], start=(kc == 0), stop=(kc == 7)), [ps], [hT, winb])
                            if ci % 2 == 0:
                                fw.op(act, lambda e, ps=ps, pr=pr, col=col, n=n: e.copy(out=pr.ap[:, col:col + n], in_=ps.ap[:, 0:n]), [pr], [ps])
                            else:
                                fw.op(dve, lambda e, ps=ps, pr=pr, col=col, n=n: e.tensor_copy(out=pr.ap[:, col:col + n], in_=ps.ap[:, 0:n]), [pr], [ps])
                            col += n
                            ci += 1
                        if "proj" in dbg and l == 0:
                            fw.dma(dbg["proj"][i * 128:(i + 1) * 128, :], pr.ap, [], [pr])
                        fw.dma(u_scr[urow(i):urow(i) + 128, :], pr.ap[:, 768:IN_COLS], [u_t[i]], [pr])
                        rq = rq_pool.next()
                        if i >= NCT:
                            rp = rp_pool.next()
                            v5 = lambda ap: ap.rearrange("p (m h f) -> p m h f", h=2, f=16)
                            fw.op(dve, lambda e, rp=rp, pr=pr, sn=sn: e.tensor_tensor(out=v5(rp.ap)[:, :, 0, :], in0=v5(pr.ap[:, 0:640])[:, :, 1, :], in1=v5(sn.ap)[:, :, 0, :], op=ALU.mult), [rp], [pr, sn])
                            fw.op(dve, lambda e, rp=rp, pr=pr, sn=sn: e.tensor_tensor(out=v5(rp.ap)[:, :, 1, :], in0=v5(pr.ap[:, 0:640])[:, :, 0, :], in1=v5(sn.ap)[:, :, 1, :], op=ALU.mult), [rp], [pr, sn])
                            fw.op(pool, lambda e, cs=cs, pr=pr: e.tensor_tensor(out=cs.ap, in0=pr.ap[:, 0:640], in1=cs.ap, op=ALU.mult), [cs], [pr, cs])
                            gq = lambda ap: ap.rearrange("p (g j d) -> p g j d", g=2, j=4)
                            jq = lambda ap: ap.rearrange("p (j g d) -> p g j d", g=2, j=4)
                            fw.op(dve, lambda e, rq=rq, rp=rp, cs=cs: e.tensor_tensor(out=jq(rq.ap[:, 0:512]), in0=gq(rp.ap[:, 0:512]), in1=gq(cs.ap[:, 0:512]), op=ALU.add), [rq], [rp, cs])
                            fw.op(dve, lambda e, rq=rq, rp=rp, cs=cs: e.tensor_tensor(out=rq.ap[:, 512:640], in0=rp.ap[:, 512:640], in1=cs.ap[:, 512:640], op=ALU.add), [rq], [rp, cs])
                        else:
                            gq = lambda ap: ap.rearrange("p (g j d) -> p g j d", g=2, j=4)
                            jq = lambda ap: ap.rearrange("p (j g d) -> p g j d", g=2, j=4)
                            fw.op(dve, lambda e, rq=rq, pr=pr: e.tensor_copy(out=jq(rq.ap[:, 0:512]), in_=gq(pr.ap[:, 0:512])), [rq], [pr])
                            fw.op(dve, lambda e, rq=rq, pr=pr: e.tensor_copy(out=rq.ap[:, 512:640], in_=pr.ap[:, 512:640]), [rq], [pr])
                        fw.op(act, lambda e, pr=pr, i=i: e.copy(out=VV.ap[:, i, :], in_=pr.ap[:, 640:768]), [qt_t[i]], [pr])
                        ps = psum()
                        psb = ps.ap.bitcast(BF16)
                        for j in range(4):
                            fw.op(pe, lambda e, j=j, psb=psb, rq=rq: e.transpose(out=psb[:, j * 128:(j + 1) * 128], in_=rq.ap[:, j * 128:(j + 1) * 128], identity=identb.ap), [ps], [rq, identb])
                        fw.op(pe, lambda e, psb=psb, rq=rq: e.transpose(out=psb[:, 512:640], in_=rq.ap[:, 512:640], identity=identb.ap), [ps], [rq, identb])
                        fw.op(dve, lambda e, psb=psb, i=i: e.tensor_copy(out=QT.ap[:, :, i * 128:(i + 1) * 128], in_=psb[:, 0:512].rearrange("p (j t) -> p j t", j=4)), [qt_t[i]], [ps])
                        fw.op(act, lambda e, psb=psb, i=i: e.copy(out=KT.ap[:, i * 128:(i + 1) * 128], in_=psb[:, 512:640]), [qt_t[i]], [ps])
                    fw.barrier()
                if stop_after == "S2":
                    break
                with ExitStack() as es:
                    sinkb = sb(es, nc, "sinkb", [128, 8], F32)
                    fw.dma(sinkb.ap, att_sink[l].partition_broadcast(128), [sinkb], [])
                    mb = sb(es, nc, "mb", [128, 384], F32)
                    fw.op(dve, lambda e: e.memset(mb.ap, 0.0), [mb], [])
                    fw.op(dve, lambda e: e.tensor_scalar(out=mb.ap[:, 0:128], in0=masks.ap[:, 1, :], scalar1=-1.0, scalar2=-NEG, op0=ALU.add, op1=ALU.mult), [mb], [masks])
                    fw.op(dve, lambda e: e.tensor_scalar(out=mb.ap[:, 256:384], in0=masks.ap[:, 3, :], scalar1=-1.0, scalar2=-NEG, op0=ALU.add, op1=ALU.mult), [mb], [masks])
                    s_pool = Pool(es, nc, "ssb", [128, 640], F32, 2)
                    p_pool = Pool(es, nc, "psb", [128, 640], BF16, 2)
                    pT_pool = Pool(es, nc, "pTs", [128, 640], BF16, 2)
                    ast_pool = Pool(es, nc, "ast", [128, 8], F32, 4)
                    mixa_pool = Pool(es, nc, "mixa", [128, 512], F32, 3)
                    tiles3 = list(range(NCT, NT) if last else range(NT))
                    tinfo = {}
                    for i in tiles3:
                        if i < NCT:
                            nloc, lo, hi, moff = 0, 0, 0, 0
                            kblocks = list(range(NCT))
                        else:
                            lo, hi = max(NCT, i - 1), min(NT - 1, i + 1)
                            nloc = (hi - lo + 1) * 128
                            moff = 0 if lo == i - 1 else 128
                            kblocks = list(range(lo, hi + 1)) + list(range(NCT))
                        tinfo[i] = dict(nloc=nloc, lo=lo, hi=hi, moff=moff, kblocks=kblocks, ncols=nloc + C, mixa=None, ops=PS[6 + (i % 2)])

                    def att_front(i, h):
                        ti = tinfo[i]
                        nloc, lo, hi, moff, ncols = ti["nloc"], ti["lo"], ti["hi"], ti["moff"], ti["ncols"]
                        if ti["mixa"] is None:
                            ti["mixa"] = mixa_pool.next()
                        g, j = h // 4, h % 4
                        lhsT = QT.ap[g * 64:(g + 1) * 64, j, i * 128:(i + 1) * 128]
                        ssb = s_pool.next()
                        st = ast_pool.next()
                        if nloc:
                            ps1 = psum()
                            fw.op(pe, lambda e: e.matmul(ps1.ap[:, 0:nloc], lhsT=lhsT, rhs=KT.ap[g * 64:(g + 1) * 64, lo * 128:(hi + 1) * 128], start=True, stop=True), [ps1], [qt_t[i]])
                            fw.op(dve, lambda e: e.scalar_tensor_tensor(out=ssb.ap[:, 0:nloc], in0=ps1.ap[:, 0:nloc], scalar=ATT_SCALE, in1=mb.ap[:, moff:moff + nloc], op0=ALU.mult, op1=ALU.add), [ssb], [ps1, mb])
                        ps2 = psum()
                        fw.op(pe, lambda e: e.matmul(ps2.ap[:, 0:C], lhsT=lhsT, rhs=KT.ap[g * 64:(g + 1) * 64, 0:C], start=True, stop=True), [ps2], [qt_t[i]])
                        fw.op(act, lambda e: e.activation(out=ssb.ap[:, nloc:ncols], in_=ps2.ap[:, 0:C], func=AF.Copy, scale=ATT_SCALE), [ssb], [ps2])
                        fw.op(dve, lambda e: e.reduce_max(out=st.ap[:, 0:1], in_=ssb.ap[:, 0:ncols], axis=AX.X), [st], [ssb])
                        fw.op(dve, lambda e: e.tensor_scalar(out=st.ap[:, 1:2], in0=st.ap[:, 0:1], scalar1=sinkb.ap[:, h:h + 1], scalar2=-1.0, op0=ALU.max, op1=ALU.mult), [st], [st, sinkb])
                        pb = p_pool.next()
                        fw.op(act, lambda e: e.activation(out=pb.ap[:, 0:ncols], in_=ssb.ap[:, 0:ncols], func=AF.Exp, bias=st.ap[:, 1:2], accum_out=st.ap[:, 2:3]), [pb, st], [ssb, st])
                        fw.op(act, lambda e: e.activation(out=st.ap[:, 3:4], in_=sinkb.ap[:, h:h + 1], func=AF.Exp, bias=st.ap[:, 1:2]), [st], [st, sinkb])
                        fw.op(dve, lambda e: e.tensor_tensor(out=st.ap[:, 4:5], in0=st.ap[:, 2:3], in1=st.ap[:, 3:4], op=ALU.add), [st], [st])
                        fw.op(dve, lambda e: e.reciprocal(out=st.ap[:, 5:6], in_=st.ap[:, 4:5]), [st], [st])
                        return dict(i=i, h=h, g=g, pb=pb, st=st)

                    def att_back(cx):
                        i, h, g, pb, st = cx["i"], cx["h"], cx["g"], cx["pb"], cx["st"]
                        ti = tinfo[i]
                        ncols, kblocks, mixa, ops = ti["ncols"], ti["kblocks"], ti["mixa"], ti["ops"]
                        ps3 = psum()
                        ps3b = ps3.ap.bitcast(BF16)
                        nblk = ncols // 128
                        for b in range(nblk):
                            fw.op(pe, lambda e, b=b: e.transpose(out=ps3b[:, b * 128:(b + 1) * 128], in_=pb.ap[:, b * 128:(b + 1) * 128], identity=identb.ap), [ps3], [pb, identb])
                        pT = pT_pool.next()
                        if h % 2 == 0:
                            fw.op(dve, lambda e: e.tensor_copy(out=pT.ap[:, 0:ncols], in_=ps3b[:, 0:ncols]), [pT], [ps3])
                        else:
                            fw.op(act, lambda e: e.copy(out=pT.ap[:, 0:ncols], in_=ps3b[:, 0:ncols]), [pT], [ps3])
                        nb = len(kblocks)
                        for b, kb in enumerate(kblocks):
                            fw.op(pe, lambda e, b=b, kb=kb: e.matmul(ops.ap[:, h * 64:(h + 1) * 64], lhsT=pT.ap[:, b * 128:(b + 1) * 128], rhs=VV.ap[:, kb, g * 64:(g + 1) * 64], start=(b == 0), stop=(b == nb - 1)), [ops], [pT, qt_t[kb]])
                        fw.op(act, lambda e: e.activation(out=mixa.ap[:, h * 64:(h + 1) * 64], in_=ops.ap[:, h * 64:(h + 1) * 64], func=AF.Copy, scale=st.ap[:, 5:6]), [mixa], [ops, st])
                        if h == 7:
                            fw.dma(mix[i * 128:(i + 1) * 128, 0:512], mixa.ap, [mix_a[i]], [mixa])

                    units = [(i, h) for i in tiles3 for h in range(8)]
                    cxn = att_front(*units[0])
                    for n in range(len(units)):
                        cxc = cxn
                        if n + 1 < len(units):
                            cxn = att_front(*units[n + 1])
                        att_back(cxc)
                    fw.barrier()
            if stop_after == "S3":
                if "mix" in dbg:
                    fw.barrier()
                    for i in range(NT):
                        fw.dma(dbg["mix"][i * 128:(i + 1) * 128, :], mix[i * 128:(i + 1) * 128, :], [], [mix_a[i], mix_r[i]])
                break
            FV, FR, FG, FKK, FBON = 0, 512, 1024, 1536, 2048
            FLW = lambda d_: 2560 + d_ * 1536
            FB = lambda d_: 2560 + d_ * 1536 + 512
            FKD = lambda d_: 2560 + d_ * 1536 + 1024
            NF = 5632
            with ExitStack() as es:
                def bc(name, src, n):
                    t_ = sb(es, nc, name, [128, n], F32)
                    fw.dma(t_.ap, src.partition_broadcast(128), [t_], [])
                    return t_
                mup = bc("mup", mu_prev[l], RWC)
                mun = bc("mun", mu_next[l], RWC)
                w0b = bc("w0b", decay_w0[l].rearrange("d c -> (d c)"), 1024)
                a0b = bc("a0b", iclr_a0[l].rearrange("d c -> (d c)"), 1024)
                kkb = bc("kkb", k_k[l], RW)
                kab = bc("kab", k_a[l], RW)
                rkb = bc("rkb", r_k[l], RW)
                w2t = sb(es, nc, "w2t", [128, RW], F32)
                a2t = sb(es, nc, "a2t", [128, RW], F32)
                g2t = sb(es, nc, "g2t", [128, RW], F32)
                fw.dma(w2t.ap, decay_w2[l].rearrange("d r c -> (d r) c"), [w2t], [])
                fw.dma(a2t.ap, iclr_a2[l].rearrange("d r c -> (d r) c"), [a2t], [])
                fw.dma(g2t.ap, gate_g2[l], [g2t], [])
                if l > 0:
                    v0b = bc("v0b", vres_v0[0], RW)
                    v1t = sb(es, nc, "v1t", [128, 4, 32], F32)
                    fw.dma(v1t.ap, vres_v1[0].rearrange("(c p) n -> p c n", p=128), [v1t], [])
                    v2t = sb(es, nc, "v2t", [32, RW], F32)
                    fw.dma(v2t.ap, vres_v2[0], [v2t], [])
                uc_pool = Pool(es, nc, "uc", [128, RWC], F32, 2)
                up_pool = Pool(es, nc, "up", [128, RWC], F32, 2)
                un_pool = Pool(es, nc, "un", [128, RWC], F32, 2)
                vf_pool = Pool(es, nc, "vf", [128, RW], F32, 2)
                li_pool = Pool(es, nc, "li", [128, 384], F32, 2)
                liT_pool = Pool(es, nc, "liT", [128, 3, 128], F32, 2)
                fo_pool = Pool(es, nc, "fo", [128, NF], F32, 2)
                tmp_pool = Pool(es, nc, "ftmp", [128, RW], F32, 6)
                a_pool = Pool(es, nc, "fa", [128, RW], F32, 4)
                sm_pool = Pool(es, nc, "fsm", [128, 64], F32, 2)
                vT_pool = Pool(es, nc, "fvT", [128, 4, 128], F32, 2)
                h3 = lambda ap: ap.rearrange("p (h d) -> p h d", h=8)

                def s4a_load(i):
                    uc, up, un = uc_pool.next(), up_pool.next(), un_pool.next()
                    deps = [u_t[i], u_guard] + ([u_t[i - 1]] if i > 0 else []) + ([u_t[i + 1]] if i + 1 < NT else [])
                    r0 = urow(i)
                    fw.dma(uc.ap, u_scr[r0:r0 + 128, :], [uc], deps)
                    fw.dma(up.ap, u_scr[r0 - 1:r0 + 127, :], [up], deps)
                    fw.dma(un.ap, u_scr[r0 + 1:r0 + 129, :], [un], deps)
                    vf = None
                    if l > 0:
                        vf = vf_pool.next()
                        fw.dma(vf.ap, vfirst[i * 128:(i + 1) * 128, :], [vf], [vf_t[i]])
                    return uc, up, un, vf

                nxt = s4a_load(0)
                for i in range(NT):
                    uc, up, un, vf = nxt
                    if i + 1 < NT:
                        nxt = s4a_load(i + 1)
                    fw.op(dve, lambda e: e.tensor_tensor(out=up.ap, in0=up.ap, in1=uc.ap, op=ALU.subtract), [up], [up, uc])
                    fw.op(pool, lambda e: e.tensor_tensor(out=un.ap, in0=un.ap, in1=uc.ap, op=ALU.subtract), [un], [un, uc])
                    fw.op(dve, lambda e: e.tensor_tensor(out=up.ap, in0=up.ap, in1=mup.ap, op=ALU.mult), [up], [up, mup])
                    fw.op(pool, lambda e: e.tensor_tensor(out=un.ap, in0=un.ap, in1=mun.ap, op=ALU.mult), [un], [un, mun])
                    fw.op(dve, lambda e: e.tensor_tensor(out=uc.ap, in0=uc.ap, in1=up.ap, op=ALU.add), [uc], [uc, up])
                    fw.op(dve, lambda e: e.tensor_tensor(out=uc.ap, in0=uc.ap, in1=un.ap, op=ALU.add), [uc], [uc, un])
                    U = uc.ap
                    r_, k_, v_ = U[:, 0:512], U[:, 512:1024], U[:, 1024:1536]
                    li = li_pool.next()
                    fw.op(act, lambda e: e.activation(out=li.ap[:, 0:128], in_=U[:, 1536:1664], func=AF.Tanh), [li], [uc])
                    fw.op(act, lambda e: e.copy(out=li.ap[:, 128:256], in_=U[:, 1664:1792]), [li], [uc])
                    fw.op(act, lambda e: e.activation(out=li.ap[:, 256:384], in_=U[:, 1792:1920], func=AF.Sigmoid), [li], [uc])
                    ps = psum()
                    for q in range(3):
                        fw.op(pe, lambda e, q=q, ps=ps: e.transpose(out=ps.ap[:, q * 128:(q + 1) * 128], in_=li.ap[:, q * 128:(q + 1) * 128], identity=ident.ap), [ps], [li, ident])
                    liT = liT_pool.next()
                    fw.op(dve, lambda e, ps=ps: e.tensor_copy(out=liT.ap, in_=ps.ap[:, 0:384].rearrange("p (a b) -> p a b", a=3)), [liT], [ps])
                    fo = fo_pool.next()
                    FO = fo.ap
                    a_d = []
                    for d_ in range(2):
                        hp = slice(d_ * 64, (d_ + 1) * 64)
                        ps = psum()
                        fw.op(pe, lambda e, ps=ps, hp=hp: e.matmul(ps.ap, lhsT=liT.ap[hp, 0, :], rhs=w2t.ap[hp, :], start=True, stop=True), [ps], [liT, w2t])
                        t1 = tmp_pool.next()
                        fw.op(dve, lambda e, ps=ps, t1=t1, d_=d_: e.tensor_tensor(out=t1.ap, in0=ps.ap, in1=w0b.ap[:, d_ * 512:(d_ + 1) * 512], op=ALU.add), [t1], [ps, w0b])
                        fw.op(act, lambda e, t1=t1: e.activation(out=t1.ap, in_=t1.ap, func=AF.Sigmoid), [t1], [t1])
                        fw.op(pool, lambda e, t1=t1, d_=d_: e.tensor_scalar(out=FO[:, FLW(d_):FLW(d_) + 512], in0=t1.ap, scalar1=-0.6065306597126334, scalar2=None, op0=ALU.mult), [fo], [t1])
                        ps = psum()
                        fw.op(pe, lambda e, ps=ps, hp=hp: e.matmul(ps.ap, lhsT=liT.ap[hp, 1, :], rhs=a2t.ap[hp, :], start=True, stop=True), [ps], [liT, a2t])
                        ad = a_pool.next()
                        fw.op(dve, lambda e, ps=ps, ad=ad, d_=d_: e.tensor_tensor(out=ad.ap, in0=ps.ap, in1=a0b.ap[:, d_ * 512:(d_ + 1) * 512], op=ALU.add), [ad], [ps, a0b])
                        fw.op(act, lambda e, ad=ad: e.activation(out=ad.ap, in_=ad.ap, func=AF.Sigmoid), [ad], [ad])
                        a_d.append(ad)
                    ps = psum()
                    fw.op(pe, lambda e, ps=ps: e.matmul(ps.ap, lhsT=liT.ap[:, 2, :], rhs=g2t.ap, start=True, stop=True), [ps], [liT, g2t])
                    fw.op(act, lambda e, ps=ps: e.copy(out=FO[:, FG:FG + 512], in_=ps.ap), [fo], [ps])
                    if l == 0:
                        fw.op(pool, lambda e: e.tensor_copy(out=FO[:, FV:FV + 512], in_=v_), [fo], [uc])
                        fw.dma(vfirst[i * 128:(i + 1) * 128, :], v_, [vf_t[i]], [uc])
                    else:
                        ps = psum()
                        for c_ in range(4):
                            fw.op(pe, lambda e, c_=c_, ps=ps: e.transpose(out=ps.ap[:, c_ * 128:(c_ + 1) * 128], in_=U[:, 1024 + c_ * 128:1024 + (c_ + 1) * 128], identity=ident.ap), [ps], [uc, ident])
                        vT = vT_pool.next()
                        fw.op(dve, lambda e, ps=ps, vT=vT: e.tensor_copy(out=vT.ap, in_=ps.ap.rearrange("p (a b) -> p a b", a=4)), [vT], [ps])
                        ps = psum()
                        for c_ in range(4):
                            fw.op(pe, lambda e, c_=c_, ps=ps, vT=vT: e.matmul(ps.ap[:, 0:32], lhsT=vT.ap[:, c_, :], rhs=v1t.ap[:, c_, :], start=(c_ == 0), stop=(c_ == 3)), [ps], [vT, v1t])
                        sm = sm_pool.next()
                        fw.op(dve, lambda e, ps=ps, sm=sm: e.tensor_copy(out=sm.ap[:, 0:32], in_=ps.ap[:, 0:32]), [sm], [ps])
                        ps = psum()
                        fw.op(pe, lambda e, ps=ps, sm=sm: e.transpose(out=ps.ap[0:32, 0:128], in_=sm.ap[:, 0:32], identity=ident.ap), [ps], [sm, ident])
                        smT = tmp_pool.next()
                        fw.op(dve, lambda e, ps=ps, smT=smT: e.tensor_copy(out=smT.ap[0:32, 0:128], in_=ps.ap[0:32, 0:128]), [smT], [ps])
                        ps = psum()
                        fw.op(pe, lambda e, ps=ps, smT=smT: e.matmul(ps.ap, lhsT=smT.ap[0:32, 0:128], rhs=v2t.ap, start=True, stop=True), [ps], [smT, v2t])
                        t1 = tmp_pool.next()
                        fw.op(dve, lambda e, ps=ps, t1=t1: e.tensor_tensor(out=t1.ap, in0=ps.ap, in1=v0b.ap, op=ALU.add), [t1], [ps, v0b])
                        fw.op(act, lambda e, t1=t1: e.activation(out=t1.ap, in_=t1.ap, func=AF.Sigmoid), [t1], [t1])
                        fw.op(dve, lambda e, vf=vf: e.tensor_tensor(out=vf.ap, in0=vf.ap, in1=v_, op=ALU.subtract), [vf], [vf, uc])
                        fw.op(dve, lambda e, vf=vf, t1=t1: e.tensor_tensor(out=vf.ap, in0=vf.ap, in1=t1.ap, op=ALU.mult), [vf], [vf, t1])
                        fw.op(dve, lambda e, vf=vf: e.tensor_tensor(out=FO[:, FV:FV + 512], in0=vf.ap, in1=v_, op=ALU.add), [fo], [vf, uc])
                    fw.op(pool, lambda e: e.tensor_copy(out=FO[:, FR:FR + 512], in_=r_), [fo], [uc])
                    kk0 = tmp_pool.next()
                    sq = tmp_pool.next()
                    sm = sm_pool.next()
                    fw.op(dve, lambda e, kk0=kk0: e.tensor_tensor(out=kk0.ap, in0=k_, in1=kkb.ap, op=ALU.mult), [kk0], [uc, kkb])
                    fw.op(pool, lambda e, kk0=kk0, sq=sq: e.tensor_tensor(out=sq.ap, in0=kk0.ap, in1=kk0.ap, op=ALU.mult), [sq], [kk0])
                    fw.op(dve, lambda e, sq=sq, sm=sm: e.reduce_sum(out=sm.ap[:, 0:8], in_=h3(sq.ap), axis=AX.X), [sm], [sq])
                    fw.op(act, lambda e, sm=sm: e.activation(out=sm.ap[:, 8:16], in_=sm.ap[:, 0:8], func=AF.Sqrt), [sm], [sm])
                    fw.op(dve, lambda e, sm=sm: e.tensor_scalar(out=sm.ap[:, 8:16], in0=sm.ap[:, 8:16], scalar1=1e-12, scalar2=None, op0=ALU.max), [sm], [sm])
                    fw.op(dve, lambda e, sm=sm: e.reciprocal(out=sm.ap[:, 16:24], in_=sm.ap[:, 8:16]), [sm], [sm])
                    fw.op(dve, lambda e, sm=sm, kk0=kk0: e.tensor_tensor(out=h3(FO[:, FKK:FKK + 512]), in0=h3(kk0.ap), in1=sm.ap[:, 16:24].unsqueeze(2).to_broadcast([128, 8, 64]), op=ALU.mult), [fo], [kk0, sm])
                    for d_ in range(2):
                        t1 = tmp_pool.next()
                        fw.op(dve, lambda e, t1=t1, d_=d_: e.scalar_tensor_tensor(out=t1.ap, in0=a_d[d_].ap, scalar=-1.0, in1=kab.ap, op0=ALU.add, op1=ALU.mult), [t1], [a_d[d_], kab])
                        fw.op(dve, lambda e, t1=t1, d_=d_: e.scalar_tensor_tensor(out=FO[:, FKD(d_):FKD(d_) + 512], in0=t1.ap, scalar=1.0, in1=k_, op0=ALU.add, op1=ALU.mult), [fo], [t1, uc])
                        fw.op(pool, lambda e, d_=d_: e.tensor_tensor(out=FO[:, FB(d_):FB(d_) + 512], in0=FO[:, FKK:FKK + 512], in1=a_d[d_].ap, op=ALU.mult), [fo], [fo, a_d[d_]])
                    t1 = tmp_pool.next()
                    sm2 = sm_pool.next()
                    fw.op(pool, lambda e, t1=t1: e.tensor_tensor(out=t1.ap, in0=FO[:, FKD(0):FKD(0) + 512], in1=FO[:, FKD(1):FKD(1) + 512], op=ALU.add), [t1], [fo])
                    fw.op(dve, lambda e, t1=t1: e.tensor_tensor(out=t1.ap, in0=t1.ap, in1=r_, op=ALU.mult), [t1], [t1, uc])
                    fw.op(pool, lambda e, t1=t1: e.tensor_tensor(out=t1.ap, in0=t1.ap, in1=rkb.ap, op=ALU.mult), [t1], [t1, rkb])
                    fw.op(dve, lambda e, t1=t1, sm2=sm2: e.reduce_sum(out=sm2.ap[:, 0:8], in_=h3(t1.ap), axis=AX.X), [sm2], [t1])
                    fw.op(dve, lambda e, sm2=sm2: e.tensor_tensor(out=h3(FO[:, FBON:FBON + 512]), in0=h3(FO[:, FV:FV + 512]), in1=sm2.ap[:, 0:8].unsqueeze(2).to_broadcast([128, 8, 64]), op=ALU.mult), [fo], [fo, sm2])
                    fw.dma(feat[i * 128:(i + 1) * 128, :], FO, [feat_t[i]], [fo])
                    if "feat" in dbg and l == 0:
                        fw.dma(dbg["feat"][i * 128:(i + 1) * 128, :], FO[:, 0:4096], [], [fo])
                fw.barrier()
            if stop_after == "S4a":
                break
            with ExitStack() as es:
                ones = sb(es, nc, "ones", [128, 1], F32)
                fw.op(dve, lambda e: e.memset(ones.ap, 1.0), [ones], [])
                Mq1, MnT = [], []
                for d_ in range(2):
                    m1_ = sb(es, nc, "Mq1", [128, 256], F32)
                    fw.op(dve, lambda e, m1_=m1_, d_=d_: e.tensor_scalar(out=m1_.ap[:, 0:128], in0=masks.ap[:, 2 * d_, :], scalar1=-1.0, scalar2=None, op0=ALU.mult), [m1_], [masks])
                    fw.op(dve, lambda e, m1_=m1_, d_=d_: e.tensor_copy(out=m1_.ap[:, 128:256], in_=masks.ap[:, 2 * d_ + 1, :]), [m1_], [masks])
                    Mq1.append(m1_)
                    mt_ = sb(es, nc, "MnT", [128, 128], F32)
                    fw.op(dve, lambda e, mt_=mt_, d_=d_: e.tensor_scalar(out=mt_.ap, in0=masks.ap[:, 2 * (1 - d_), :], scalar1=-1.0, scalar2=None, op0=ALU.mult), [mt_], [masks])
                    MnT.append(mt_)
                Mq2 = [masks.ap[:, 2 * d_:2 * d_ + 2, :].rearrange("p a t -> p (a t)") for d_ in range(2)]
                PSQB = int(os.environ.get('S4B_PSQB', '4'))
                PSQ = [TT(PS[PSQB + k // 4].ap[:, (k % 4) * 128:(k % 4 + 1) * 128]) for k in range(16)]
                pq = [0]

                def psq():
                    t_ = PSQ[pq[0] % 16]
                    pq[0] += 1
                    return t_
                p4 = [0]

                def psumR():
                    t_ = PS[p4[0] % 8]
                    p4[0] += 1
                    return t_
                fa_pool = Pool(es, nc, "sfa", [128, 3, 512], F32, 4)
                fb_pool = Pool(es, nc, "sfb", [128, 3, 512], F32, 4)
                tm_pool = Pool(es, nc, "stm", [128, 4, 512], BF16, 2)
                vb_pool = Pool(es, nc, "svb", [128, 512], BF16, 2)
                ex_pool = Pool(es, nc, "sex", [128, 3, 512], F32, 2)
                fm_pool = Pool(es, nc, "sfm", [128, 4, 4, 128], BF16, 2)
                el_pool = Pool(es, nc, "sel", [128, 4], F32, 2)
                g1_pool = Pool(es, nc, "sg1", [128, 2, 4, 128], BF16, 4)
                a2_pool = Pool(es, nc, "sa2", [128, 256], BF16, 16)
                inv_pool = Pool(es, nc, "sinv", [128, 4, 128], BF16, 32)
                invf_pool = Pool(es, nc, "sinvf", [128, 4, 128], F32, 10)
                xs_pool = Pool(es, nc, "sxs", [128, 4, 64], BF16, 4)
                nu_pool = Pool(es, nc, "snu", [128, 512], BF16, 2)
                ys_pool = Pool(es, nc, "sys", [128, 512], F32, 2)
                ST = [[[sb(es, nc, "ST", [128, 64], F32) for c_ in range(4)] for pp in range(2)] for d_ in range(2)]
                STb = [[[sb(es, nc, "STb", [128, 64], BF16) for c_ in range(4)] for pp in range(2)] for d_ in range(2)]
                for d_ in range(2):
                    for c_ in range(4):
                        fw.op(dve, lambda e, d_=d_, c_=c_: e.memset(ST[d_][0][c_].ap, 0.0), [ST[d_][0][c_]], [])
                        fw.op(dve, lambda e, d_=d_, c_=c_: e.memset(STb[d_][0][c_].ap, 0.0), [STb[d_][0][c_]], [])
                orders = [list(range(NT)), [1, 0] + list(range(NT - 1, NCT - 1, -1))]
                evi = [0]

                def evac(fn_dve, fn_act, outs, ins):
                    if evi[0] % 2:
                        fw.op(dve, fn_dve, outs, ins)
                    else:
                        fw.op(act, fn_act, outs, ins)

                def s4b_load(d_, i):
                    fa, fb = fa_pool.next(), fb_pool.next()
                    r0 = i * 128
                    fw.dma(fa.ap, feat[r0:r0 + 128, FLW(d_):FLW(d_) + 1536].rearrange("p (a c) -> p a c", a=3), [fa], [feat_t[i]])
                    fw.dma(fb.ap[:, 0:2, :], feat[r0:r0 + 128, 0:1024].rearrange("p (a c) -> p a c", a=2), [fb], [feat_t[i]])
                    fw.dma(fb.ap[:, 2, :], feat[r0:r0 + 128, FKK:FKK + 512], [fb], [feat_t[i]])
                    return fa, fb

                def step_prep(d_, j, fa, fb):
                    i = orders[d_][j]
                    pp = j % 2
                    STo, STn = ST[d_][pp], ST[d_][1 - pp]
                    SBo, SBn = STb[d_][pp], STb[d_][1 - pp]
                    vb = vb_pool.next()
                    fw.op(act, lambda e: e.copy(out=vb.ap, in_=fb.ap[:, 0, :]), [vb], [fb])
                    lw, b_, kd = fa.ap[:, 0, :], fa.ap[:, 1, :], fa.ap[:, 2, :]
                    v_, r_, kk = fb.ap[:, 0, :], fb.ap[:, 1, :], fb.ap[:, 2, :]
                    psL = psumR()
                    fw.op(pe, lambda e: e.matmul(psL.ap, lhsT=masks.ap[:, 1 + 2 * d_, :], rhs=lw, start=True, stop=True), [psL], [masks, fa])
                    ex = ex_pool.next()
                    fw.op(act, lambda e: e.activation(out=ex.ap[:, 0, :], in_=psL.ap, func=AF.Exp), [ex], [psL])
                    fw.op(act, lambda e: e.activation(out=ex.ap[:, 1, :], in_=psL.ap, func=AF.Exp, scale=-1.0), [ex], [psL])
                    fw.op(dve, lambda e: e.tensor_tensor(out=ex.ap[:, 2, :], in0=psL.ap, in1=lw, op=ALU.subtract), [ex], [psL, fa])
                    fw.op(act, lambda e: e.activation(out=ex.ap[:, 2, :], in_=ex.ap[:, 2, :], func=AF.Exp), [ex], [ex])
                    tm = tm_pool.next()
                    fw.op(dve, lambda e: e.tensor_tensor(out=tm.ap[:, 0, :], in0=kk, in1=ex.ap[:, 2, :], op=ALU.mult), [tm], [fb, ex])
                    fw.op(pool, lambda e: e.tensor_tensor(out=tm.ap[:, 1, :], in0=r_, in1=ex.ap[:, 0, :], op=ALU.mult), [tm], [fb, ex])
                    fw.op(dve, lambda e: e.tensor_tensor(out=tm.ap[:, 2, :], in0=b_, in1=ex.ap[:, 1, :], op=ALU.mult), [tm], [fa, ex])
                    fw.op(pool, lambda e: e.tensor_tensor(out=tm.ap[:, 3, :], in0=kd, in1=ex.ap[:, 1, :], op=ALU.mult), [tm], [fa, ex])
                    psE = psumR()
                    for c_ in range(4):
                        fw.op(pe, lambda e, c_=c_: e.matmul(psE.ap[:, c_:c_ + 1], lhsT=lw[:, c_ * 128:(c_ + 1) * 128], rhs=ones.ap, start=True, stop=True), [psE], [fa, ones])
                    el = el_pool.next()
                    fw.op(act, lambda e: e.activation(out=el.ap, in_=psE.ap[:, 0:4], func=AF.Exp), [el], [psE])
                    fm = fm_pool.next()
                    for c_ in range(4):
                        psT = psumR()
                        for q in range(4):
                            fw.op(pe, lambda e, c_=c_, q=q, psT=psT: e.transpose(out=psT.ap.bitcast(BF16)[:, q * 128:(q + 1) * 128], in_=tm.ap[:, q, c_ * 128:(c_ + 1) * 128], identity=identb.ap), [psT], [tm, identb])
                        evi[0] += 1
                        evac(lambda e, c_=c_, psT=psT: e.tensor_copy(out=fm.ap[:, c_, :, :], in_=psT.ap.bitcast(BF16)[:, 0:512].rearrange("p (a b) -> p a b", a=4)),
                             lambda e, c_=c_, psT=psT: e.copy(out=fm.ap[:, c_, :, :], in_=psT.ap.bitcast(BF16)[:, 0:512].rearrange("p (a b) -> p a b", a=4)), [fm], [psT])
                    H = []
                    for h in range(8):
                        c_ = h // 2
                        hp = slice((h % 2) * 64, (h % 2) * 64 + 64)
                        H.append(dict(c=c_, hp=hp, par=h % 2, k=h // 2, al=fm.ap[hp, c_, 0, :], rh=fm.ap[hp, c_, 1, :], be=fm.ap[hp, c_, 2, :], ka=fm.ap[hp, c_, 3, :],
                                      ar=fm.ap[hp, c_, 0:2, :].rearrange("p a t -> p (a t)"), vh=vb.ap[:, h * 64:(h + 1) * 64]))
                    G1 = [g1_pool.next() for _ in range(2)]
                    for h in range(8):
                        u = H[h]
                        psG = psumR()
                        fw.op(pe, lambda e, u=u, psG=psG: e.matmul(psG.ap[:, 0:256], lhsT=u["be"], rhs=u["ar"], start=True, stop=True), [psG], [fm])
                        fw.op(pe, lambda e, u=u, psG=psG: e.matmul(psG.ap[:, 256:512], lhsT=u["ka"], rhs=u["ar"], start=True, stop=True), [psG], [fm])
                        a2 = a2_pool.next()
                        g1 = G1[u["par"]]
                        fw.op(dve, lambda e, u=u, psG=psG, g1=g1: e.tensor_tensor(out=g1.ap[:, :, u["k"], :], in0=psG.ap[:, 0:256].rearrange("p (a t) -> p a t", a=2), in1=Mq1[d_].ap.rearrange("p (a t) -> p a t", a=2), op=ALU.mult), [g1], [psG, Mq1[d_]])
                        fw.op(dve, lambda e, a2=a2, psG=psG: e.tensor_tensor(out=a2.ap, in0=psG.ap[:, 256:512], in1=Mq2[d_], op=ALU.mult), [a2], [psG, masks])
                        u.update(a2=a2, abr=g1.ap[:, 1, u["k"], :], g1=g1)
                    GP = []
                    for par in range(2):
                        g1 = G1[par]
                        Mf = invf_pool.next()
                        fw.op(dve, lambda e, Mf=Mf, g1=g1: e.tensor_tensor(out=Mf.ap, in0=g1.ap[:, 0, :, :], in1=ident.ap.unsqueeze(1).to_broadcast([128, 4, 128]), op=ALU.add), [Mf], [g1, ident])
                        Mb = inv_pool.next()
                        fw.op(act, lambda e, Mf=Mf, Mb=Mb: e.copy(out=Mb.ap, in_=Mf.ap), [Mb], [Mf])
                        bank = psumR()
                        bkb = bank.ap.bitcast(BF16)
                        for k_ in range(4):
                            fw.op(pe, lambda e, k_=k_, bkb=bkb, g1=g1: e.transpose(out=bkb[:, k_ * 128:(k_ + 1) * 128], in_=g1.ap[:, 0, k_, :], identity=identb.ap), [bank], [g1, identb])
                        AT = inv_pool.next()
                        evi[0] += 1
                        evac(lambda e, AT=AT, bkb=bkb: e.tensor_copy(out=f4(AT.ap), in_=bkb[:, 0:512]), lambda e, AT=AT, bkb=bkb: e.copy(out=f4(AT.ap), in_=bkb[:, 0:512]), [AT], [bank])
                        GP.append(dict(A=g1, Aap=g1.ap[:, 0, :, :], AT=AT, M=Mf, Mb=Mb))
                    return dict(d_=d_, i=i, STo=STo, STn=STn, tm=tm, el=el, fm=fm, fb=fb, v_=v_, H=H, GP=GP, SBo=SBo, SBn=SBn, vb=vb)

                f4 = lambda ap: ap.rearrange("p k t -> p (k t)")

                def inv_all(GP):
                    for it in range(6):
                        for g in GP:
                            bank = psumR()
                            for k_ in range(4):
                                fw.op(pe, lambda e, k_=k_, bank=bank, g=g: e.matmul(bank.ap[:, k_ * 128:(k_ + 1) * 128], lhsT=g["Aap"][:, k_, :], rhs=g["AT"].ap[:, k_, :], start=True, stop=True), [bank], [g["A"], g["AT"]])
                            ATn = inv_pool.next()
                            evi[0] += 1
                            evac(lambda e, ATn=ATn, bank=bank: e.tensor_copy(out=f4(ATn.ap), in_=bank.ap), lambda e, ATn=ATn, bank=bank: e.copy(out=f4(ATn.ap), in_=bank.ap), [ATn], [bank])
                            g["ATn"] = ATn
                            if it < 5:
                                bank = psumR()
                                for k_ in range(4):
                                    fw.op(pe, lambda e, k_=k_, bank=bank, g=g: e.matmul(bank.ap[:, k_ * 128:(k_ + 1) * 128], lhsT=g["AT"].ap[:, k_, :], rhs=g["Aap"][:, k_, :], start=True, stop=True), [bank], [g["A"], g["AT"]])
                                An = inv_pool.next()
                                evi[0] += 1
                                evac(lambda e, An=An, bank=bank: e.tensor_copy(out=f4(An.ap), in_=bank.ap), lambda e, An=An, bank=bank: e.copy(out=f4(An.ap), in_=bank.ap), [An], [bank])
                                g["An"] = An
                        for g in GP:
                            g["AT"] = g["ATn"]
                            if it < 5:
                                g["A"], g["Aap"] = g["An"], g["An"].ap
                        for g in GP:
                            bank = psumR()
                            for k_ in range(4):
                                fw.op(pe, lambda e, k_=k_, bank=bank, g=g: e.matmul(bank.ap[:, k_ * 128:(k_ + 1) * 128], lhsT=g["AT"].ap[:, k_, :], rhs=g["Mb"].ap[:, k_, :], start=True, stop=True), [bank], [g["AT"], g["Mb"]])
                            Mn = invf_pool.next()
                            fw.op(dve, lambda e, Mn=Mn, bank=bank, g=g: e.tensor_tensor(out=f4(Mn.ap), in0=bank.ap, in1=f4(g["M"].ap), op=ALU.add), [Mn], [bank, g["M"]])
                            g["M"] = Mn
                            Mbn = inv_pool.next()
                            fw.op(act, lambda e, Mn=Mn, Mbn=Mbn: e.copy(out=Mbn.ap, in_=Mn.ap), [Mbn], [Mn])
                            g["Mb"] = Mbn

                def step_state(cx):
                    d_, i, STo, STn, tm, el, fm, fb, v_, H, GP, SBo, SBn, vb = (cx[k] for k in ('d_', 'i', 'STo', 'STn', 'tm', 'el', 'fm', 'fb', 'v_', 'H', 'GP', 'SBo', 'SBn', 'vb'))
                    NU = nu_pool.next()
                    q4 = lambda ap: ap.rearrange("p (k t) -> p k t", k=4)[:, :, 0:64]
                    for par, g in enumerate(GP):
                        bank = psumR()
                        for k_ in range(4):
                            u = H[par + 2 * k_]
                            o = bank.ap[:, k_ * 128:k_ * 128 + 64]
                            fw.op(pe, lambda e, u=u, o=o: e.matmul(o, lhsT=u["al"], rhs=SBo[u["c"]].ap[u["hp"], :], start=True, stop=False), [bank], [fm, SBo[u["c"]]])
                            fw.op(pe, lambda e, u=u, o=o: e.matmul(o, lhsT=u["a2"].ap[:, 0:128], rhs=u["vh"], start=False, stop=True), [bank], [u["a2"], vb])
                        xsg = xs_pool.next()
                        evi[0] += 1
                        evac(lambda e, xsg=xsg, bank=bank: e.tensor_copy(out=xsg.ap, in_=q4(bank.ap)), lambda e, xsg=xsg, bank=bank: e.copy(out=xsg.ap, in_=q4(bank.ap)), [xsg], [bank])
                        g["xs"] = xsg
                    for par, g in enumerate(GP):
                        bank = psumR()
                        for k_ in range(4):
                            fw.op(pe, lambda e, k_=k_, bank=bank, g=g: e.matmul(bank.ap[:, k_ * 128:k_ * 128 + 64], lhsT=g["Mb"].ap[:, k_, :], rhs=g["xs"].ap[:, k_, :], start=True, stop=True), [bank], [g["Mb"], g["xs"]])
                        nuv = NU.ap.rearrange("p (c two d) -> p c two d", two=2, d=64)[:, :, par, :]
                        fw.op(act, lambda e, bank=bank, nuv=nuv: e.activation(out=nuv, in_=q4(bank.ap), func=AF.Copy, scale=-1.0), [NU], [bank])
                    ysb = ys_pool.next()
                    for par in (0, 1):
                        psY = psumR()
                        for k_ in range(4):
                            h = par + 2 * k_
                            u = H[h]
                            ysl = psY.ap[:, k_ * 64:(k_ + 1) * 64]
                            fw.op(pe, lambda e, u=u, ysl=ysl: e.matmul(ysl, lhsT=u["rh"], rhs=SBo[u["c"]].ap[u["hp"], :], start=True, stop=False), [psY], [fm, SBo[u["c"]]])
                            fw.op(pe, lambda e, u=u, ysl=ysl, h=h: e.matmul(ysl, lhsT=u["abr"], rhs=NU.ap[:, h * 64:(h + 1) * 64], start=False, stop=False), [psY], [u["g1"], NU])
                            fw.op(pe, lambda e, u=u, ysl=ysl: e.matmul(ysl, lhsT=u["a2"].ap[:, 128:256], rhs=u["vh"], start=False, stop=True), [psY], [u["a2"], vb])
                        yv = ysb.ap.rearrange("p (c two d) -> p c two d", two=2, d=64)[:, :, par, :]
                        fw.op(act, lambda e, psY=psY, yv=yv: e.copy(out=yv, in_=psY.ap[:, 0:256].rearrange("p (c d) -> p c d", d=64)), [ysb], [psY])
                    fw.dma(y_scr[d_, i * 128:(i + 1) * 128, :], ysb.ap, [y_t[d_][i]], [ysb])
                    psS = psumR()
                    for c_ in range(4):
                        so = psS.ap[:, c_ * 128:(c_ + 1) * 128]
                        fw.op(pe, lambda e, c_=c_, so=so: e.matmul(so, lhsT=tm.ap[:, 2, c_ * 128:(c_ + 1) * 128], rhs=NU.ap[:, c_ * 128:(c_ + 1) * 128], start=True, stop=False), [psS], [tm, NU])
                        fw.op(pe, lambda e, c_=c_, so=so: e.matmul(so, lhsT=tm.ap[:, 3, c_ * 128:(c_ + 1) * 128], rhs=vb.ap[:, c_ * 128:(c_ + 1) * 128], start=False, stop=True), [psS], [tm, vb])
                        fw.op(pool, lambda e, c_=c_: e.tensor_scalar(out=STn[c_].ap, in0=STo[c_].ap, scalar1=el.ap[:, c_:c_ + 1], scalar2=None, op0=ALU.mult), [STn[c_]], [STo[c_], el])
                    for c_ in range(4):
                        so = psS.ap[:, c_ * 128:(c_ + 1) * 128]
                        for hh in range(2):
                            hp = slice(hh * 64, hh * 64 + 64)
                            fw.op(dve, lambda e, c_=c_, hp=hp, so=so: e.scalar_tensor_tensor(out=STn[c_].ap[hp, :], in0=so[hp, hp], scalar=el.ap[hp, c_:c_ + 1], in1=STn[c_].ap[hp, :], op0=ALU.mult, op1=ALU.add), [STn[c_]], [psS, el, STn[c_]])
                    for c_ in range(4):
                        fw.op(act, lambda e, c_=c_: e.copy(out=SBn[c_].ap, in_=STn[c_].ap), [SBn[c_]], [STn[c_]])

                nsteps = NT if not os.environ.get('S4B_STEPS') else int(os.environ['S4B_STEPS'])
                nxt = [s4b_load(d_, orders[d_][0]) for d_ in range(2)]
                for j in range(nsteps):
                    cur = list(nxt)
                    cxs = []
                    for d_ in range(2):
                        cxs.append(step_prep(d_, j, *cur[d_]))
                        if j + 1 < nsteps:
                            nxt[d_] = s4b_load(d_, orders[d_][j + 1])
                    inv_all([g for cx in cxs for g in cx["GP"]])
                    for cx in cxs:
                        step_state(cx)
                fw.barrier()
            if stop_after == "S4b":
                break
            with ExitStack() as es:
                lnw = sb(es, nc, "lnw", [128, RW], F32)
                lnb = sb(es, nc, "lnb", [128, RW], F32)
                fw.dma(lnw.ap, ln_x_w[l].partition_broadcast(128), [lnw], [])
                fw.dma(lnb.ap, ln_x_b[l].partition_broadcast(128), [lnb], [])
                y0_pool = Pool(es, nc, "cy0", [128, RW], F32, 2)
                y1_pool = Pool(es, nc, "cy1", [128, RW], F32, 2)
                gb_pool = Pool(es, nc, "cgb", [128, 2, RW], F32, 2)
                sq_pool = Pool(es, nc, "csq", [128, RW], F32, 2)
                cs_pool = Pool(es, nc, "ccs", [128, 32], F32, 2)
                h3 = lambda ap: ap.rearrange("p (h d) -> p h d", h=8)
                bc8 = lambda ap: ap.unsqueeze(2).to_broadcast([128, 8, 64])
                for i in (range(NCT, NT) if last else range(NT)):
                    y0, y1, gb = y0_pool.next(), y1_pool.next(), gb_pool.next()
                    r0 = i * 128
                    fw.dma(y0.ap, y_scr[0, r0:r0 + 128, :], [y0], [y_t[0][i]])
                    fw.dma(y1.ap, y_scr[1, r0:r0 + 128, :], [y1], [y_t[1][i]])
                    fw.dma(gb.ap[:, 0, :], feat[r0:r0 + 128, FG:FG + 512], [gb], [feat_t[i]])
                    fw.dma(gb.ap[:, 1, :], feat[r0:r0 + 128, FBON:FBON + 512], [gb], [feat_t[i]])
                    cs_ = cs_pool.next()
                    sq = sq_pool.next()
                    fw.op(dve, lambda e: e.tensor_tensor(out=y0.ap, in0=y0.ap, in1=y1.ap, op=ALU.add), [y0], [y0, y1])
                    fw.op(dve, lambda e: e.reduce_sum(out=cs_.ap[:, 0:8], in_=h3(y0.ap), axis=AX.X), [cs_], [y0])
                    fw.op(dve, lambda e: e.tensor_scalar(out=cs_.ap[:, 0:8], in0=cs_.ap[:, 0:8], scalar1=1.0 / 64, scalar2=None, op0=ALU.mult), [cs_], [cs_])
                    fw.op(dve, lambda e: e.tensor_tensor(out=h3(y0.ap), in0=h3(y0.ap), in1=bc8(cs_.ap[:, 0:8]), op=ALU.subtract), [y0], [y0, cs_])
                    fw.op(pool, lambda e: e.tensor_tensor(out=sq.ap, in0=y0.ap, in1=y0.ap, op=ALU.mult), [sq], [y0])
                    fw.op(dve, lambda e: e.reduce_sum(out=cs_.ap[:, 8:16], in_=h3(sq.ap), axis=AX.X), [cs_], [sq])
                    fw.op(dve, lambda e: e.tensor_scalar(out=cs_.ap[:, 8:16], in0=cs_.ap[:, 8:16], scalar1=1.0 / 64, scalar2=64e-5, op0=ALU.mult, op1=ALU.add), [cs_], [cs_])
                    fw.op(act, lambda e: e.activation(out=cs_.ap[:, 16:24], in_=cs_.ap[:, 8:16], func=AF.Sqrt), [cs_], [cs_])
                    fw.op(dve, lambda e: e.reciprocal(out=cs_.ap[:, 24:32], in_=cs_.ap[:, 16:24]), [cs_], [cs_])
                    fw.op(dve, lambda e: e.tensor_tensor(out=h3(y0.ap), in0=h3(y0.ap), in1=bc8(cs_.ap[:, 24:32]), op=ALU.mult), [y0], [y0, cs_])
                    fw.op(pool, lambda e: e.tensor_tensor(out=y0.ap, in0=y0.ap, in1=lnw.ap, op=ALU.mult), [y0], [y0, lnw])
                    fw.op(dve, lambda e: e.tensor_tensor(out=y0.ap, in0=y0.ap, in1=lnb.ap, op=ALU.add), [y0], [y0, lnb])
                    fw.op(pool, lambda e: e.tensor_tensor(out=y0.ap, in0=y0.ap, in1=gb.ap[:, 1, :], op=ALU.add), [y0], [y0, gb])
                    fw.op(dve, lambda e: e.tensor_tensor(out=y0.ap, in0=y0.ap, in1=gb.ap[:, 0, :], op=ALU.mult), [y0], [y0, gb])
                    fw.dma(mix[r0:r0 + 128, 512:1024], y0.ap, [mix_r[i]], [y0])
                fw.barrier()
            if stop_after == "S4":
                if "mix" in dbg:
                    for i in range(NT):
                        fw.dma(dbg["mix"][i * 128:(i + 1) * 128, :], mix[i * 128:(i + 1) * 128, :], [], [mix_a[i], mix_r[i]])
                    fw.barrier()
                break
            tiles5 = list(range(NCT, NT)) if last else list(range(NT))
            with ExitStack() as es:
                woutb = sb(es, nc, "woutb", [128, 8, D], BF16)
                wst_pool = Pool(es, nc, "wst5", [128, 8, 256], F32, 2)
                for j in range(4):
                    wst = wst_pool.next()
                    fw.dma(wst.ap, w_out[l][:, j * 256:(j + 1) * 256].rearrange("(kc p) n -> p kc n", p=128), [wst], [])
                    fw.op(pool, lambda e, wst=wst, j=j: e.tensor_copy(out=woutb.ap[:, :, j * 256:(j + 1) * 256], in_=wst.ap), [woutb], [wst])
                modG = [sb(es, nc, "modG%d" % s_, [128, D], F32) for s_ in range(2)]
                for s_ in range(2):
                    fw.dma(modG[s_].ap, modbuf[l, s_ * 6 + 2], [modG[s_]], [mod_t[l][s_ * 6 + 2]])
                mx_pool = Pool(es, nc, "mx", [128, D], F32, 2)
                mxb_pool = Pool(es, nc, "mxb", [128, D], BF16, 2)
                mT_pool = Pool(es, nc, "mT", [128, 8, 128], BF16, 2)
                x5_pool = Pool(es, nc, "x5", [128, D], F32, 2)

                def s5_load(i):
                    mx = mx_pool.next()
                    fw.dma(mx.ap, mix[i * 128:(i + 1) * 128, :], [mx], [mix_a[i], mix_r[i]])
                    xt = x5_pool.next()
                    fw.dma(xt.ap, xs[i * 128:(i + 1) * 128, :], [xt], [xs_t[i]])
                    return mx, xt

                nxt = s5_load(tiles5[0])
                for ti, i in enumerate(tiles5):
                    mx, xt = nxt
                    if ti + 1 < len(tiles5):
                        nxt = s5_load(tiles5[ti + 1])
                    s_ = 1 if i < NCT else 0
                    mxb = mxb_pool.next()
                    fw.op(pool, lambda e, mx=mx, mxb=mxb: e.tensor_copy(out=mxb.ap, in_=mx.ap), [mxb], [mx])
                    mT = mT_pool.next()
                    for half in range(2):
                        ps = psum()
                        psb = ps.ap.bitcast(BF16)
                        for kk in range(4):
                            kc = half * 4 + kk
                            fw.op(pe, lambda e, kc=kc, kk=kk, psb=psb, mxb=mxb: e.transpose(out=psb[:, kk * 128:(kk + 1) * 128], in_=mxb.ap[:, kc * 128:(kc + 1) * 128], identity=identb.ap), [ps], [mxb, identb])
                        fw.op(act if half else dve, lambda e, psb=psb, mT=mT, half=half: (e.copy if half else e.tensor_copy)(out=mT.ap[:, half * 4:(half + 1) * 4, :], in_=psb[:, 0:512].rearrange("p (a b) -> p a b", a=4)), [mT], [ps])
                    for half in range(2):
                        ps = psum()
                        hs = slice(half * 512, (half + 1) * 512)
                        for kc in range(8):
                            fw.op(pe, lambda e, kc=kc, ps=ps, mT=mT, hs=hs: e.matmul(ps.ap, lhsT=mT.ap[:, kc, :], rhs=woutb.ap[:, kc, hs], start=(kc == 0), stop=(kc == 7)), [ps], [mT, woutb])
                        fw.op(dve, lambda e, ps=ps, mx=mx, hs=hs, s_=s_: e.tensor_tensor(out=mx.ap[:, hs], in0=ps.ap, in1=modG[s_].ap[:, hs], op=ALU.mult), [mx], [ps, modG[s_]])
                    fw.op(pool, lambda e, xt=xt, mx=mx: e.tensor_tensor(out=xt.ap, in0=xt.ap, in1=mx.ap, op=ALU.add), [xt], [xt, mx])
                    fw.dma(xs[i * 128:(i + 1) * 128, :], xt.ap, [xs_t[i]], [xt])
                    if "xmix" in dbg and l == 0:
                        fw.dma(dbg["xmix"][i * 128:(i + 1) * 128, :], xt.ap, [], [xt])
                fw.barrier()
            if stop_after == "S5":
                break

            tiles6 = list(range(NCT, NT)) if last else list(range(NT))
            GS = 8
            groups = [tiles6[k:k + GS] for k in range(0, len(tiles6), GS)]
            with ExitStack() as es:
                mA = [sb(es, nc, "mA%d" % s_, [128, D], F32) for s_ in range(2)]
                mB = [sb(es, nc, "mB%d" % s_, [128, D], F32) for s_ in range(2)]
                mG = [sb(es, nc, "mG%d" % s_, [128, D], F32) for s_ in range(2)]
                for s_ in range(2):
                    fw.dma(mA[s_].ap, modbuf[l, s_ * 6 + 3], [mA[s_]], [mod_t[l][s_ * 6 + 3]])
                    fw.dma(mB[s_].ap, modbuf[l, s_ * 6 + 4], [mB[s_]], [mod_t[l][s_ * 6 + 4]])
                    fw.dma(mG[s_].ap, modbuf[l, s_ * 6 + 5], [mG[s_]], [mod_t[l][s_ * 6 + 5]])
                rwt = sb(es, nc, "rwt", [128, 8, NE], F32)
                fw.dma(rwt.ap, router_w.rearrange("(kc p) n -> p kc n", p=128), [rwt], [])
                rbt = sb(es, nc, "rbt", [128, NE], F32)
                fw.dma(rbt.ap, router_b.partition_broadcast(128), [rbt], [])
                fgt = None
                if last:
                    fgt = sb(es, nc, "fgt", [128, D], F32)
                    fw.dma(fgt.ap, final_g.partition_broadcast(128), [fgt], [])
                HT = sb(es, nc, "HT", [128, 8, GS * 128], BF16)
                accs = [sb(es, nc, "acc%d" % k, [128, D], F32) for k in range(GS)]
                gates = sb(es, nc, "gates", [128, GS, NE], F32)
                Wg2 = [sb(es, nc, "Wg", [128, 8, D], BF16) for _ in range(2)]
                Wu2 = [sb(es, nc, "Wu", [128, 8, D], BF16) for _ in range(2)]
                Wd = sb(es, nc, "Wd", [128, 8, D], BF16)
                actb = sb(es, nc, "actb", [128, 8, GS * 128], BF16)
                x6_pool = Pool(es, nc, "x6", [128, D], F32, 2)
                hT32_pool = Pool(es, nc, "hT32", [128, 8, 128], F32, 1)
                rt_pool = Pool(es, nc, "rt", [128, 96], F32, 2)
                sg_pool = Pool(es, nc, "sg", [128, 512], F32, 2)
                st6_pool = Pool(es, nc, "st6", [128, 4], F32, 2)

                def rms(xt, st, junk6):
                    fw.op(act, lambda e: e.activation(out=junk6.ap, in_=xt.ap, func=AF.Square, accum_out=st.ap[:, 0:1]), [junk6, st], [xt])
                    fw.op(dve, lambda e: e.tensor_scalar(out=st.ap[:, 1:2], in0=st.ap[:, 0:1], scalar1=1.0 / D, scalar2=1e-6, op0=ALU.mult, op1=ALU.add), [st], [st])
                    fw.op(act, lambda e: e.activation(out=st.ap[:, 3:4], in_=st.ap[:, 1:2], func=AF.Sqrt), [st], [st])
                    fw.op(dve, lambda e: e.reciprocal(out=st.ap[:, 2:3], in_=st.ap[:, 3:4]), [st], [st])

                def load_w(dst, src):
                    for j in range(2):
                        fw.dma(dst.ap[:, :, j * 512:(j + 1) * 512], src[:, j * 512:(j + 1) * 512].rearrange("(kc p) n -> p kc n", p=128), [dst], [], q=pool)

                seq = [(gi, ex) for gi in range(len(groups)) for ex in range(NE)]
                load_w(Wg2[0], e_gate[l, 0])
                load_w(Wu2[0], e_up[l, 0])

                for gi, grp in enumerate(groups):
                    ntok = len(grp) * 128
                    for t, i in enumerate(grp):
                        s_ = 1 if i < NCT else 0
                        xt = x6_pool.next()
                        fw.dma(xt.ap, xs[i * 128:(i + 1) * 128, :], [xt], [xs_t[i]])
                        st = st6_pool.next()
                        rms(xt, st, accs[t])
                        fw.op(dve, lambda e, xt=xt, st=st, s_=s_: e.scalar_tensor_tensor(out=xt.ap, in0=xt.ap, scalar=st.ap[:, 2:3], in1=mA[s_].ap, op0=ALU.mult, op1=ALU.mult), [xt], [xt, st, mA[s_]])
                        fw.op(pool, lambda e, xt=xt, s_=s_: e.tensor_tensor(out=xt.ap, in0=xt.ap, in1=mB[s_].ap, op=ALU.add), [xt], [xt, mB[s_]])
                        hT32 = hT32_pool.next()
                        for half in range(2):
                            ps = psum()
                            for kk in range(4):
                                kc = half * 4 + kk
                                fw.op(pe, lambda e, kc=kc, kk=kk, ps=ps, xt=xt: e.transpose(out=ps.ap[:, kk * 128:(kk + 1) * 128], in_=xt.ap[:, kc * 128:(kc + 1) * 128], identity=ident.ap), [ps], [xt, ident])
                            fw.op(dve, lambda e, ps=ps, hT32=hT32, half=half: e.tensor_copy(out=hT32.ap[:, half * 4:(half + 1) * 4, :], in_=ps.ap.rearrange("p (a b) -> p a b", a=4)), [hT32], [ps])
                            fw.op(act, lambda e, hT32=hT32, half=half, t=t: e.copy(out=HT.ap[:, half * 4:(half + 1) * 4, t * 128:(t + 1) * 128], in_=hT32.ap[:, half * 4:(half + 1) * 4, :]), [HT], [hT32])
                        ps = psum()
                        for kc in range(8):
                            fw.op(pe, lambda e, kc=kc, ps=ps, hT32=hT32: e.matmul(ps.ap[:, 0:NE], lhsT=hT32.ap[:, kc, :], rhs=rwt.ap[:, kc, :], start=(kc == 0), stop=(kc == 7)), [ps], [hT32, rwt])
                        rt = rt_pool.next()
                        R = rt.ap
                        sc_, bi_ = R[:, 0:16], R[:, 16:32]
                        g4 = lambda ap: ap.rearrange("p (g m) -> p g m", g=4)
                        fw.op(act, lambda e, ps=ps, sc_=sc_: e.activation(out=sc_, in_=ps.ap[:, 0:NE], func=AF.Sigmoid), [rt], [ps])
                        fw.op(dve, lambda e, sc_=sc_, bi_=bi_: e.tensor_tensor(out=bi_, in0=sc_, in1=rbt.ap, op=ALU.add), [rt], [rt, rbt])
                        a = [g4(bi_)[:, :, m] for m in range(4)]
                        hi01, lo01, hi23, lo23 = R[:, 32:36], R[:, 36:40], R[:, 40:44], R[:, 44:48]
                        top1, m1, m2, top2 = R[:, 48:52], R[:, 52:56], R[:, 56:60], R[:, 60:64]
                        gs_, gmx, gm_, rd = R[:, 64:68], R[:, 68:69], R[:, 72:76], R[:, 69:70]
                        sel = R[:, 80:96]
                        tt = lambda o, x0, x1, op: fw.op(dve, lambda e: e.tensor_tensor(out=o, in0=x0, in1=x1, op=op), [rt], [rt])
                        tt(hi01, a[0], a[1], ALU.max)
                        tt(lo01, a[0], a[1], ALU.min)
                        tt(hi23, a[2], a[3], ALU.max)
                        tt(lo23, a[2], a[3], ALU.min)
                        tt(top1, hi01, hi23, ALU.max)
                        tt(m1, hi01, hi23, ALU.min)
                        tt(m2, lo01, lo23, ALU.max)
                        tt(top2, m1, m2, ALU.max)
                        tt(gs_, top1, top2, ALU.add)
                        fw.op(dve, lambda e, gs_=gs_, gmx=gmx: e.reduce_max(out=gmx, in_=gs_, axis=AX.X), [rt], [rt])
                        fw.op(dve, lambda e, gs_=gs_, gmx=gmx, gm_=gm_: e.tensor_scalar(out=gm_, in0=gs_, scalar1=gmx, scalar2=None, op0=ALU.is_ge), [rt], [rt])
                        tt(g4(sel), g4(bi_), top2.unsqueeze(2).to_broadcast([128, 4, 4]), ALU.is_ge)
                        tt(g4(sel), g4(sel), gm_.unsqueeze(2).to_broadcast([128, 4, 4]), ALU.mult)
                        tt(sel, sel, sc_, ALU.mult)
                        fw.op(dve, lambda e, sel=sel, rd=rd: e.reduce_sum(out=rd, in_=sel, axis=AX.X), [rt], [rt])
                        fw.op(dve, lambda e, rd=rd: e.reciprocal(out=rd, in_=rd), [rt], [rt])
                        fw.op(dve, lambda e, sel=sel, rd=rd, t=t: e.tensor_scalar(out=gates.ap[:, t, :], in0=sel, scalar1=rd, scalar2=None, op0=ALU.mult), [gates], [rt])
                        if "gates" in dbg and l == 0:
                            fw.dma(dbg["gates"][i * 128:(i + 1) * 128, :], gates.ap[:, t, :], [], [gates])
                    for ex in range(NE):
                        qi = gi * NE + ex
                        Wg, Wu = Wg2[qi % 2], Wu2[qi % 2]
                        load_w(Wd, e_down[l, ex])
                        if qi + 1 < len(seq):
                            nex = seq[qi + 1][1]
                            load_w(Wg2[(qi + 1) % 2], e_gate[l, nex])
                            load_w(Wu2[(qi + 1) % 2], e_up[l, nex])
                        for t0 in range(0, ntok, 512):
                            ntk = min(512, ntok - t0)
                            for fc in range(8):
                                psg = psum()
                                psu = psum()
                                for kc in range(8):
                                    fw.op(pe, lambda e, kc=kc, fc=fc, psg=psg, t0=t0, ntk=ntk: e.matmul(psg.ap[:, 0:ntk], lhsT=Wg.ap[:, kc, fc * 128:(fc + 1) * 128], rhs=HT.ap[:, kc, t0:t0 + ntk], start=(kc == 0), stop=(kc == 7)), [psg], [Wg, HT])
                                for kc in range(8):
                                    fw.op(pe, lambda e, kc=kc, fc=fc, psu=psu, t0=t0, ntk=ntk: e.matmul(psu.ap[:, 0:ntk], lhsT=Wu.ap[:, kc, fc * 128:(fc + 1) * 128], rhs=HT.ap[:, kc, t0:t0 + ntk], start=(kc == 0), stop=(kc == 7)), [psu], [Wu, HT])
                                sg = sg_pool.next()
                                fw.op(act, lambda e, psg=psg, sg=sg, ntk=ntk: e.activation(out=sg.ap[:, 0:ntk], in_=psg.ap[:, 0:ntk], func=AF.Silu), [sg], [psg])
                                fw.op(dve, lambda e, psu=psu, sg=sg, fc=fc, t0=t0, ntk=ntk: e.tensor_tensor(out=actb.ap[:, fc, t0:t0 + ntk], in0=sg.ap[:, 0:ntk], in1=psu.ap[:, 0:ntk], op=ALU.mult), [actb], [sg, psu])
                        for t in range(len(grp)):
                            for half in range(2):
                                hs = slice(half * 512, (half + 1) * 512)
                                ps = psum()
                                for fc in range(8):
                                    fw.op(pe, lambda e, fc=fc, ps=ps, t=t, hs=hs: e.matmul(ps.ap, lhsT=actb.ap[:, fc, t * 128:(t + 1) * 128], rhs=Wd.ap[:, fc, hs], start=(fc == 0), stop=(fc == 7)), [ps], [actb, Wd])
                                if ex == 0:
                                    fw.op(dve, lambda e, ps=ps, t=t, hs=hs, ex=ex: e.tensor_scalar(out=accs[t].ap[:, hs], in0=ps.ap, scalar1=gates.ap[:, t, ex:ex + 1], scalar2=None, op0=ALU.mult), [accs[t]], [ps, gates])
                                else:
                                    fw.op(dve, lambda e, ps=ps, t=t, hs=hs, ex=ex: e.scalar_tensor_tensor(out=accs[t].ap[:, hs], in0=ps.ap, scalar=gates.ap[:, t, ex:ex + 1], in1=accs[t].ap[:, hs], op0=ALU.mult, op1=ALU.add), [accs[t]], [ps, gates, accs[t]])
                    for t, i in enumerate(grp):
                        s_ = 1 if i < NCT else 0
                        xt = x6_pool.next()
                        fw.dma(xt.ap, xs[i * 128:(i + 1) * 128, :], [xt], [xs_t[i]])
                        fw.op(pool, lambda e, t=t, s_=s_: e.tensor_tensor(out=accs[t].ap, in0=accs[t].ap, in1=mG[s_].ap, op=ALU.mult), [accs[t]], [accs[t], mG[s_]])
                        fw.op(dve, lambda e, xt=xt, t=t: e.tensor_tensor(out=xt.ap, in0=xt.ap, in1=accs[t].ap, op=ALU.add), [xt], [xt, accs[t]])
                        if "xout" in dbg and l == 0:
                            fw.dma(dbg["xout"][i * 128:(i + 1) * 128, :], xt.ap, [], [xt])
                        if not last:
                            fw.dma(xs[i * 128:(i + 1) * 128, :], xt.ap, [xs_t[i]], [xt])
                        else:
                            st = st6_pool.next()
                            rms(xt, st, accs[t])
                            fw.op(dve, lambda e, xt=xt, st=st: e.scalar_tensor_tensor(out=xt.ap, in0=xt.ap, scalar=st.ap[:, 2:3], in1=fgt.ap, op0=ALU.mult, op1=ALU.mult), [xt], [xt, st, fgt])
                            fw.dma(out[(i - NCT) * 128:(i - NCT + 1) * 128, :], xt.ap, [], [xt])
                fw.barrier()
            if stop_after == "S6":
                break
        fw.barrier()
    return nc


def prep_inputs(inputs):
    f = lambda a: np.ascontiguousarray(np.asarray(a, dtype=np.float32))
    shared = {}
    for k in ("w_mod", "b_mod", "norm_mix_g", "norm_ffn_g", "w_in", "w_out", "att_sink", "shift_mu_prev", "shift_mu_next",
              "decay_w0", "decay_w2", "iclr_a0", "iclr_a2", "vres_v0", "vres_v1", "vres_v2", "gate_g2", "k_k", "k_a",
              "ln_x_w", "ln_x_b", "router_w", "router_b", "expert_w_gate", "expert_w_up", "expert_w_down", "final_norm_g"):
        shared[k] = f(inputs[k])
    shared["r_k"] = f(inputs["r_k"]).reshape(DEPTH, RW)
    shared["ident"] = np.eye(128, dtype=np.float32)
    s = np.arange(128)[:, None]
    t = np.arange(128)[None, :]
    shared["masks"] = np.ascontiguousarray(np.stack([s < t, s <= t, s > t, s >= t], axis=1).astype(np.float32))
    pos = np.arange(L)
    row = (pos // 64).astype(np.float32)
    colp = (pos % 64).astype(np.float32)
    inv = (10000.0 ** (-np.arange(16, dtype=np.float32) / 16)).astype(np.float32)
    ang = np.concatenate([row[:, None] * inv, row[:, None] * inv, colp[:, None] * inv, colp[:, None] * inv], axis=1)
    cos = np.cos(ang).astype(np.float32)
    sin = np.sin(ang).astype(np.float32)
    sgn = np.concatenate([-np.ones(16), np.ones(16), -np.ones(16), np.ones(16)]).astype(np.float32)
    shared["rope_cos"] = np.ascontiguousarray(np.tile(cos, (1, 10)))
    shared["rope_sin"] = np.ascontiguousarray(np.tile(sin * sgn, (1, 10)))
    x = f(inputs["x"])
    ctx = f(inputs["ctx"])
    c = f(inputs["c"])
    cc = f(inputs["c_ctx"])
    maps = []
    for b in range(8):
        m = dict(shared)
        m["x"] = x[b]
        m["ctx"] = ctx[b]
        m["ccol"] = np.ascontiguousarray(np.concatenate([c[b].reshape(8, 128).T, cc.reshape(8, 128).T], axis=1))
        maps.append(m)
    return maps


def kernel(**inputs):
    maps = prep_inputs(inputs)
    nc = build()
    res = run_bass_kernel_spmd(nc, maps, core_ids=list(range(8)))
    return np.stack([r["out"] for r in res.results], axis=0).astype(np.float32)
```

```python
from contextlib import ExitStack
import os
import numpy as np
import ml_dtypes
import concourse.bass as bass
import concourse.mybir as mybir
from concourse.bass_utils import run_bass_kernel_spmd

F32 = mybir.dt.float32
BF16 = mybir.dt.bfloat16
AF = mybir.ActivationFunctionType
ALU = mybir.AluOpType
AX = mybir.AxisListType

D = 1024
L = 4096
C = 256
T = L + C
NT = T // 128
NCT = C // 128
DEPTH = 2
IN_COLS = 2688
RW = 512
RWC = 1920
NE = 16
ATT_SCALE = 0.125
NEG = -30000.0


class Eng:
    def __init__(self, fw, e, name, ring=0):
        self.fw = fw
        self.e = e
        self.name = name
        self.count = 0
        self.seen = {}
        self.sem = fw.nc.alloc_semaphore("s_" + name)
        self.ring = [fw.nc.alloc_semaphore("r_%s%d" % (name, i)) for i in range(ring)]
        self.ndma = 0

    def wait_tok(self, tok):
        sem, val, key = tok
        if self.seen.get(key, 0) >= val:
            return
        self.e.wait_ge(sem, val)
        self.seen[key] = val


class TT:
    def __init__(self, ap=None):
        self.ap = ap
        self.w = None
        self.r = {}


class FW:
    def __init__(self, nc):
        self.nc = nc
        self.pe = Eng(self, nc.tensor, "pe")
        self.act = Eng(self, nc.scalar, "act")
        self.dve = Eng(self, nc.vector, "dve")
        self.pool = Eng(self, nc.gpsimd, "pool", ring=12)
        self.sp = Eng(self, nc.sync, "sp", ring=24)
        self.engs = [self.pe, self.act, self.dve, self.pool, self.sp]
        self.ninst = 0

    def _deps(self, eng, outs, ins):
        for t in ins:
            if t.w is not None and not (eng is self.pe and t.w[2] == "pe"):
                eng.wait_tok(t.w)
        for t in outs:
            if t.w is not None and not (eng is self.pe and t.w[2] == "pe"):
                eng.wait_tok(t.w)
            for tok in t.r.values():
                if not (eng is self.pe and tok[2] == "pe"):
                    eng.wait_tok(tok)

    def _mark(self, tok, outs, ins):
        for t in ins:
            t.r[tok[2]] = tok
        for t in outs:
            t.w = tok
            t.r = {}

    def op(self, eng, fn, outs, ins):
        self._deps(eng, outs, ins)
        inst = fn(eng.e)
        eng.count += 1
        inst.then_inc(eng.sem, 1)
        self._mark((eng.sem, eng.count, eng.name), outs, ins)
        self.ninst += 1

    def dma(self, out_ap, in_ap, outs, ins, q=None):
        q = q or self.sp
        self._deps(q, outs, ins)
        K = len(q.ring)
        slot = q.ndma % K
        rnd = q.ndma // K
        key = "%s_r%d" % (q.name, slot)
        if rnd > 0:
            q.wait_tok((q.ring[slot], 16 * rnd, key))
        q.e.dma_start(out=out_ap, in_=in_ap).then_inc(q.ring[slot], 16)
        q.ndma += 1
        self._mark((q.ring[slot], 16 * (rnd + 1), key), outs, ins)
        self.ninst += 1

    def all_toks(self):
        toks = []
        for e in self.engs:
            if e.count:
                toks.append((e.sem, e.count, e.name))
            K = len(e.ring)
            for slot in range(min(K, e.ndma)):
                n = (e.ndma - 1 - slot) // K + 1
                toks.append((e.ring[slot], 16 * n, "%s_r%d" % (e.name, slot)))
        return toks

    def barrier(self):
        toks = self.all_toks()
        for e in self.engs:
            for tok in toks:
                if e is self.pe and tok[2] == "pe":
                    continue
                e.wait_tok(tok)


_uid = [0]


def _nm(name):
    _uid[0] += 1
    return "sb_%s_%d" % (name, _uid[0])


class Pool:
    def __init__(self, es, nc, name, shape, dtype, bufs):
        self.t = [TT(es.enter_context(nc.sbuf_tensor(_nm(name), shape, dtype)).ap()) for i in range(bufs)]
        self.i = 0

    def next(self):
        t = self.t[self.i % len(self.t)]
        self.i += 1
        return t


def sb(es, nc, name, shape, dtype):
    return TT(es.enter_context(nc.sbuf_tensor(_nm(name), shape, dtype)).ap())


def build(debug_outs=(), stop_after=None):
    nc = bass.Bass("TRN2", target_bir_lowering=False)
    fw = FW(nc)
    pe, act, dve, pool, sp = fw.pe, fw.act, fw.dve, fw.pool, fw.sp

    def din(name, shape, dt=F32):
        return nc.dram_tensor(name, list(shape), dt, kind="ExternalInput").ap()

    def dscr(name, shape, dt=F32):
        return nc.dram_tensor(name, list(shape), dt, kind="Internal").ap()

    x_in = din("x", [L, D])
    ctx_in = din("ctx", [C, D])
    ccol_in = din("ccol", [128, 16])
    w_mod = din("w_mod", [DEPTH, D, 6 * D])
    b_mod = din("b_mod", [DEPTH, 6 * D])
    norm_mix_g = din("norm_mix_g", [DEPTH, D])
    norm_ffn_g = din("norm_ffn_g", [DEPTH, D])
    w_in = din("w_in", [DEPTH, D, IN_COLS])
    w_out = din("w_out", [DEPTH, D, D])
    att_sink = din("att_sink", [DEPTH, 8])
    mu_prev = din("shift_mu_prev", [DEPTH, RWC])
    mu_next = din("shift_mu_next", [DEPTH, RWC])
    decay_w0 = din("decay_w0", [DEPTH, 2, RW])
    decay_w2 = din("decay_w2", [DEPTH, 2, 64, RW])
    iclr_a0 = din("iclr_a0", [DEPTH, 2, RW])
    iclr_a2 = din("iclr_a2", [DEPTH, 2, 64, RW])
    vres_v0 = din("vres_v0", [1, RW])
    vres_v1 = din("vres_v1", [1, RW, 32])
    vres_v2 = din("vres_v2", [1, 32, RW])
    gate_g2 = din("gate_g2", [DEPTH, 128, RW])
    k_k = din("k_k", [DEPTH, RW])
    k_a = din("k_a", [DEPTH, RW])
    r_k = din("r_k", [DEPTH, RW])
    ln_x_w = din("ln_x_w", [DEPTH, RW])
    ln_x_b = din("ln_x_b", [DEPTH, RW])
    router_w = din("router_w", [D, NE])
    router_b = din("router_b", [NE])
    e_gate = din("expert_w_gate", [DEPTH, NE, D, D])
    e_up = din("expert_w_up", [DEPTH, NE, D, D])
    e_down = din("expert_w_down", [DEPTH, NE, D, D])
    final_g = din("final_norm_g", [D])
    ident_in = din("ident", [128, 128])
    masks_in = din("masks", [128, 4, 128])
    rope_cos = din("rope_cos", [L, 640])
    rope_sin = din("rope_sin", [L, 640])
    out = nc.dram_tensor("out", [L, D], F32, kind="ExternalOutput").ap()

    xs = dscr("xs", [T, D])
    xs_t = [TT() for _ in range(NT)]
    modbuf = dscr("modbuf", [DEPTH, 12, 128, D])
    mod_t = [[TT() for _ in range(12)] for _ in range(DEPTH)]
    u_scr = dscr("u_scr", [T + 4, RWC])
    u_t = [TT() for _ in range(NT)]
    u_guard = TT()
    mix = dscr("mix", [T, D])
    mix_a = [TT() for _ in range(NT)]
    mix_r = [TT() for _ in range(NT)]
    vfirst = dscr("vfirst", [T, RW])
    vf_t = [TT() for _ in range(NT)]
    feat = dscr("feat", [T, 5632])
    feat_t = [TT() for _ in range(NT)]
    y_scr = dscr("y_scr", [2, T, RW])
    y_t = [[TT() for _ in range(NT)] for _ in range(2)]
    dbg = {}
    for name, shape in debug_outs:
        dbg[name] = nc.dram_tensor("dbg_" + name, list(shape), F32, kind="ExternalOutput").ap()

    def urow(i):
        return 1 + i * 128 if i < NCT else 3 + i * 128

    PS = [TT(nc.alloc_psum_tensor("psb%d" % i, [128, 512], F32).ap()) for i in range(8)]
    psi = [0]

    def psum():
        t = PS[psi[0] % 6]
        psi[0] += 1
        return t

    with ExitStack() as g_es:
        ident = sb(g_es, nc, "ident", [128, 128], F32)
        identb = sb(g_es, nc, "identb", [128, 128], BF16)
        masks = sb(g_es, nc, "masks", [128, 4, 128], F32)
        fw.dma(ident.ap, ident_in, [ident], [])
        fw.dma(masks.ap, masks_in, [masks], [])
        fw.op(dve, lambda e: e.tensor_copy(out=identb.ap, in_=ident.ap), [identb], [ident])
        with ExitStack() as es0:
            zrow = sb(es0, nc, "zrow", [1, RWC], F32)
            fw.op(dve, lambda e: e.memset(zrow.ap, 0.0), [zrow], [])
            for r in (0, C + 1, C + 2, T + 3):
                fw.dma(u_scr[r:r + 1, :], zrow.ap, [u_guard], [zrow])
            fw.barrier()

        for i in range(NT):
            src = ctx_in[i * 128:(i + 1) * 128, :] if i < NCT else x_in[(i - NCT) * 128:(i - NCT + 1) * 128, :]
            fw.dma(xs[i * 128:(i + 1) * 128, :], src, [xs_t[i]], [])

        for l in range(DEPTH):
            last = l == DEPTH - 1
            with ExitStack() as es:
                ccol = sb(es, nc, "ccol", [128, 16], F32)
                csil = sb(es, nc, "csil", [128, 16], F32)
                crep = sb(es, nc, "crep", [128, 16, 128], F32)
                fw.dma(ccol.ap, ccol_in, [ccol], [])
                fw.op(act, lambda e: e.activation(out=csil.ap, in_=ccol.ap, func=AF.Silu), [csil], [ccol])
                fw.op(dve, lambda e: e.tensor_copy(out=crep.ap, in_=csil.ap.unsqueeze(2).to_broadcast([128, 16, 128])), [crep], [csil])
                wm_pool = Pool(es, nc, "wm", [128, 8, 512], F32, 2)
                bm_pool = Pool(es, nc, "bm", [128, 512], F32, 2)
                modraw = [[sb(es, nc, "mr%d_%d" % (s, j), [128, D], F32) for j in range(6)] for s in range(2)]
                gmix = sb(es, nc, "gmix", [128, D], F32)
                gffn = sb(es, nc, "gffn", [128, D], F32)
                fw.dma(gmix.ap, norm_mix_g[l].partition_broadcast(128), [gmix], [])
                fw.dma(gffn.ap, norm_ffn_g[l].partition_broadcast(128), [gffn], [])
                for j in range(12):
                    wm = wm_pool.next()
                    bm = bm_pool.next()
                    fw.dma(wm.ap, w_mod[l][:, j * 512:(j + 1) * 512].rearrange("(kc p) n -> p kc n", p=128), [wm], [])
                    fw.dma(bm.ap, b_mod[l][j * 512:(j + 1) * 512].partition_broadcast(128), [bm], [])
                    for s in range(2):
                        ps = psum()
                        for kc in range(8):
                            fw.op(pe, lambda e, kc=kc, s=s, ps=ps, wm=wm: e.matmul(ps.ap, lhsT=crep.ap[:, s * 8 + kc, :], rhs=wm.ap[:, kc, :], start=(kc == 0), stop=(kc == 7)), [ps], [crep, wm])
                        dst = modraw[s][j // 2]
                        fw.op(dve, lambda e, ps=ps, dst=dst, bm=bm, j=j: e.tensor_tensor(out=dst.ap[:, (j % 2) * 512:(j % 2 + 1) * 512], in0=ps.ap, in1=bm.ap, op=ALU.add), [dst], [ps, bm])
                for s in range(2):
                    sh_a, sc_a, ga, sh_f, sc_f, gf = modraw[s]
                    fw.op(dve, lambda e, sc_a=sc_a: e.scalar_tensor_tensor(out=sc_a.ap, in0=sc_a.ap, scalar=1.0, in1=gmix.ap, op0=ALU.add, op1=ALU.mult), [sc_a], [sc_a, gmix])
                    fw.op(dve, lambda e, sc_f=sc_f: e.scalar_tensor_tensor(out=sc_f.ap, in0=sc_f.ap, scalar=1.0, in1=gffn.ap, op0=ALU.add, op1=ALU.mult), [sc_f], [sc_f, gffn])
                    for idx, t in enumerate((sc_a, sh_a, ga, sc_f, sh_f, gf)):
                        fw.dma(modbuf[l, s * 6 + idx], t.ap, [mod_t[l][s * 6 + idx]], [t])
                fw.barrier()
            if stop_after == "S1":
                break

            with ExitStack() as es_att:
                QT = sb(es_att, nc, "QT", [128, 4, T], BF16)
                KT = sb(es_att, nc, "KT", [128, T], BF16)
                VV = sb(es_att, nc, "VV", [128, NT, 128], BF16)
                qt_t = [TT() for _ in range(NT)]
                with ExitStack() as es:
                    winb = sb(es, nc, "winb", [128, 8, IN_COLS], BF16)
                    wst_pool = Pool(es, nc, "wst", [128, 8, 384], F32, 2)
                    for j in range(7):
                        wst = wst_pool.next()
                        fw.dma(wst.ap, w_in[l][:, j * 384:(j + 1) * 384].rearrange("(kc p) n -> p kc n", p=128), [wst], [])
                        fw.op(pool, lambda e, wst=wst, j=j: e.tensor_copy(out=winb.ap[:, :, j * 384:(j + 1) * 384], in_=wst.ap), [winb], [wst])
                    modA = [sb(es, nc, "modA%d" % s, [128, D], F32) for s in range(2)]
                    modB = [sb(es, nc, "modB%d" % s, [128, D], F32) for s in range(2)]
                    for s in range(2):
                        fw.dma(modA[s].ap, modbuf[l, s * 6 + 0], [modA[s]], [mod_t[l][s * 6 + 0]])
                        fw.dma(modB[s].ap, modbuf[l, s * 6 + 1], [modB[s]], [mod_t[l][s * 6 + 1]])
                    x_pool = Pool(es, nc, "xt", [128, D], F32, 2)
                    junk = sb(es, nc, "junk", [128, D], F32)
                    st_pool = Pool(es, nc, "st", [128, 4], F32, 2)
                    h_pool = Pool(es, nc, "ht", [128, D], BF16, 2)
                    hT_pool = Pool(es, nc, "hT", [128, 8, 128], BF16, 2)
                    pr_pool = Pool(es, nc, "pr", [128, IN_COLS], F32, 2)
                    cos_pool = Pool(es, nc, "cos", [128, 640], F32, 2)
                    sin_pool = Pool(es, nc, "sin", [128, 640], F32, 2)
                    rp_pool = Pool(es, nc, "rp", [128, 640], F32, 2)
                    rq_pool = Pool(es, nc, "rq", [128, 640], BF16, 2)

                    def s2_load(i):
                        xt = x_pool.next()
                        fw.dma(xt.ap, xs[i * 128:(i + 1) * 128, :], [xt], [xs_t[i]])
                        cs = sn = None
                        if i >= NCT:
                            cs = cos_pool.next()
                            sn = sin_pool.next()
                            r0 = (i - NCT) * 128
                            fw.dma(cs.ap, rope_cos[r0:r0 + 128, :], [cs], [])
                            fw.dma(sn.ap, rope_sin[r0:r0 + 128, :], [sn], [])
                        return xt, cs, sn

                    nxt = s2_load(0)
                    for i in range(NT):
                        xt, cs, sn = nxt
                        if i + 1 < NT:
                            nxt = s2_load(i + 1)
                        s = 1 if i < NCT else 0
                        st = st_pool.next()
                        fw.op(act, lambda e, xt=xt, st=st: e.activation(out=junk.ap, in_=xt.ap, func=AF.Square, accum_out=st.ap[:, 0:1]), [junk, st], [xt])
                        fw.op(dve, lambda e, st=st: e.tensor_scalar(out=st.ap[:, 1:2], in0=st.ap[:, 0:1], scalar1=1.0 / D, scalar2=1e-6, op0=ALU.mult, op1=ALU.add), [st], [st])
                        fw.op(act, lambda e, st=st: e.activation(out=st.ap[:, 3:4], in_=st.ap[:, 1:2], func=AF.Sqrt), [st], [st])
                        fw.op(dve, lambda e, st=st: e.reciprocal(out=st.ap[:, 2:3], in_=st.ap[:, 3:4]), [st], [st])
                        fw.op(dve, lambda e, xt=xt, st=st, s=s: e.scalar_tensor_tensor(out=xt.ap, in0=xt.ap, scalar=st.ap[:, 2:3], in1=modA[s].ap, op0=ALU.mult, op1=ALU.mult), [xt], [xt, st, modA[s]])
                        ht = h_pool.next()
                        fw.op(pool, lambda e, xt=xt, ht=ht, s=s: e.tensor_tensor(out=ht.ap, in0=xt.ap, in1=modB[s].ap, op=ALU.add), [ht], [xt, modB[s]])
                        hT = hT_pool.next()
                        for half in range(2):
                            ps = psum()
                            psb = ps.ap.bitcast(BF16)
                            for kk in range(4):
                                kc = half * 4 + kk
                                fw.op(pe, lambda e, kc=kc, kk=kk, psb=psb, ht=ht: e.transpose(out=psb[:, kk * 128:(kk + 1) * 128], in_=ht.ap[:, kc * 128:(kc + 1) * 128], identity=identb.ap), [ps], [ht, identb])
                            fw.op(act if half else dve, lambda e, psb=psb, hT=hT, half=half: (e.copy if half else e.tensor_copy)(out=hT.ap[:, half * 4:(half + 1) * 4, :], in_=psb[:, 0:512].rearrange("p (a b) -> p a b", a=4)), [hT], [ps])
                        pr = pr_pool.next()
                        col = 0
                        ci = 0
                        while col < IN_COLS:
                            n = min(512, IN_COLS - col)
                            ps = psum()
                            for kc in range(8):
                                fw.op(pe, lambda e, kc=kc, ps=ps, hT=hT, col=col, n=n: e.matmul(ps.ap[:, 0:n], lhsT=hT.ap[:, kc, :], rhs=winb.ap[:, kc, col:col + n], start=(kc == 0), stop=(kc == 7)), [ps], [hT, winb])
                            if ci % 2 == 0:
                                fw.op(act, lambda e, ps=ps, pr=pr, col=col, n=n: e.copy(out=pr.ap[:, col:col + n], in_=ps.ap[:, 0:n]), [pr], [ps])
                            else:
                                fw.op(dve, lambda e, ps=ps, pr=pr, col=col, n=n: e.tensor_copy(out=pr.ap[:, col:col + n], in_=ps.ap[:, 0:n]), [pr], [ps])
                            col += n
                            ci += 1
                        if "proj" in dbg and l == 0:
                            fw.dma(dbg["proj"][i * 128:(i + 1) * 128, :], pr.ap, [], [pr])
                        fw.dma(u_scr[urow(i):urow(i) + 128, :], pr.ap[:, 768:IN_COLS], [u_t[i]], [pr])
                        rq = rq_pool.next()
                        if i >= NCT:
                            rp = rp_pool.next()
                            v5 = lambda ap: ap.rearrange("p (m h f) -> p m h f", h=2, f=16)
                            fw.op(dve, lambda e, rp=rp, pr=pr, sn=sn: e.tensor_tensor(out=v5(rp.ap)[:, :, 0, :], in0=v5(pr.ap[:, 0:640])[:, :, 1, :], in1=v5(sn.ap)[:, :, 0, :], op=ALU.mult), [rp], [pr, sn])
                            fw.op(dve, lambda e, rp=rp, pr=pr, sn=sn: e.tensor_tensor(out=v5(rp.ap)[:, :, 1, :], in0=v5(pr.ap[:, 0:640])[:, :, 0, :], in1=v5(sn.ap)[:, :, 1, :], op=ALU.mult), [rp], [pr, sn])
                            fw.op(pool, lambda e, cs=cs, pr=pr: e.tensor_tensor(out=cs.ap, in0=pr.ap[:, 0:640], in1=cs.ap, op=ALU.mult), [cs], [pr, cs])
                            gq = lambda ap: ap.rearrange("p (g j d) -> p g j d", g=2, j=4)
                            jq = lambda ap: ap.rearrange("p (j g d) -> p g j d", g=2, j=4)
                            fw.op(dve, lambda e, rq=rq, rp=rp, cs=cs: e.tensor_tensor(out=jq(rq.ap[:, 0:512]), in0=gq(rp.ap[:, 0:512]), in1=gq(cs.ap[:, 0:512]), op=ALU.add), [rq], [rp, cs])
                            fw.op(dve, lambda e, rq=rq, rp=rp, cs=cs: e.tensor_tensor(out=rq.ap[:, 512:640], in0=rp.ap[:, 512:640], in1=cs.ap[:, 512:640], op=ALU.add), [rq], [rp, cs])
                        else:
                            gq = lambda ap: ap.rearrange("p (g j d) -> p g j d", g=2, j=4)
                            jq = lambda ap: ap.rearrange("p (j g d) -> p g j d", g=2, j=4)
                            fw.op(dve, lambda e, rq=rq, pr=pr: e.tensor_copy(out=jq(rq.ap[:, 0:512]), in_=gq(pr.ap[:, 0:512])), [rq], [pr])
                            fw.op(dve, lambda e, rq=rq, pr=pr: e.tensor_copy(out=rq.ap[:, 512:640], in_=pr.ap[:, 512:640]), [rq], [pr])
                        fw.op(act, lambda e, pr=pr, i=i: e.copy(out=VV.ap[:, i, :], in_=pr.ap[:, 640:768]), [qt_t[i]], [pr])
                        ps = psum()
                        psb = ps.ap.bitcast(BF16)
                        for j in range(4):
                            fw.op(pe, lambda e, j=j, psb=psb, rq=rq: e.transpose(out=psb[:, j * 128:(j + 1) * 128], in_=rq.ap[:, j * 128:(j + 1) * 128], identity=identb.ap), [ps], [rq, identb])
                        fw.op(pe, lambda e, psb=psb, rq=rq: e.transpose(out=psb[:, 512:640], in_=rq.ap[:, 512:640], identity=identb.ap), [ps], [rq, identb])
                        fw.op(dve, lambda e, psb=psb, i=i: e.tensor_copy(out=QT.ap[:, :, i * 128:(i + 1) * 128], in_=psb[:, 0:512].rearrange("p (j t) -> p j t", j=4)), [qt_t[i]], [ps])
                        fw.op(act, lambda e, psb=psb, i=i: e.copy(out=KT.ap[:, i * 128:(i + 1) * 128], in_=psb[:, 512:640]), [qt_t[i]], [ps])
                    fw.barrier()
                if stop_after == "S2":
                    break
                with ExitStack() as es:
                    sinkb = sb(es, nc, "sinkb", [128, 8], F32)
                    fw.dma(sinkb.ap, att_sink[l].partition_broadcast(128), [sinkb], [])
                    mb = sb(es, nc, "mb", [128, 384], F32)
                    fw.op(dve, lambda e: e.memset(mb.ap, 0.0), [mb], [])
                    fw.op(dve, lambda e: e.tensor_scalar(out=mb.ap[:, 0:128], in0=masks.ap[:, 1, :], scalar1=-1.0, scalar2=-NEG, op0=ALU.add, op1=ALU.mult), [mb], [masks])
                    fw.op(dve, lambda e: e.tensor_scalar(out=mb.ap[:, 256:384], in0=masks.ap[:, 3, :], scalar1=-1.0, scalar2=-NEG, op0=ALU.add, op1=ALU.mult), [mb], [masks])
                    s_pool = Pool(es, nc, "ssb", [128, 640], F32, 2)
                    p_pool = Pool(es, nc, "psb", [128, 640], BF16, 2)
                    pT_pool = Pool(es, nc, "pTs", [128, 640], BF16, 2)
                    ast_pool = Pool(es, nc, "ast", [128, 8], F32, 4)
                    mixa_pool = Pool(es, nc, "mixa", [128, 512], F32, 3)
                    tiles3 = list(range(NCT, NT) if last else range(NT))
                    tinfo = {}
                    for i in tiles3:
                        if i < NCT:
                            nloc, lo, hi, moff = 0, 0, 0, 0
                            kblocks = list(range(NCT))
                        else:
                            lo, hi = max(NCT, i - 1), min(NT - 1, i + 1)
                            nloc = (hi - lo + 1) * 128
                            moff = 0 if lo == i - 1 else 128
                            kblocks = list(range(lo, hi + 1)) + list(range(NCT))
                        tinfo[i] = dict(nloc=nloc, lo=lo, hi=hi, moff=moff, kblocks=kblocks, ncols=nloc + C, mixa=None, ops=PS[6 + (i % 2)])

                    def att_front(i, h):
                        ti = tinfo[i]
                        nloc, lo, hi, moff, ncols = ti["nloc"], ti["lo"], ti["hi"], ti["moff"], ti["ncols"]
                        if ti["mixa"] is None:
                            ti["mixa"] = mixa_pool.next()
                        g, j = h // 4, h % 4
                        lhsT = QT.ap[g * 64:(g + 1) * 64, j, i * 128:(i + 1) * 128]
                        ssb = s_pool.next()
                        st = ast_pool.next()
                        if nloc:
                            ps1 = psum()
                            fw.op(pe, lambda e: e.matmul(ps1.ap[:, 0:nloc], lhsT=lhsT, rhs=KT.ap[g * 64:(g + 1) * 64, lo * 128:(hi + 1) * 128], start=True, stop=True), [ps1], [qt_t[i]])
                            fw.op(dve, lambda e: e.scalar_tensor_tensor(out=ssb.ap[:, 0:nloc], in0=ps1.ap[:, 0:nloc], scalar=ATT_SCALE, in1=mb.ap[:, moff:moff + nloc], op0=ALU.mult, op1=ALU.add), [ssb], [ps1, mb])
                        ps2 = psum()
                        fw.op(pe, lambda e: e.matmul(ps2.ap[:, 0:C], lhsT=lhsT, rhs=KT.ap[g * 64:(g + 1) * 64, 0:C], start=True, stop=True), [ps2], [qt_t[i]])
                        fw.op(act, lambda e: e.activation(out=ssb.ap[:, nloc:ncols], in_=ps2.ap[:, 0:C], func=AF.Copy, scale=ATT_SCALE), [ssb], [ps2])
                        fw.op(dve, lambda e: e.reduce_max(out=st.ap[:, 0:1], in_=ssb.ap[:, 0:ncols], axis=AX.X), [st], [ssb])
                        fw.op(dve, lambda e: e.tensor_scalar(out=st.ap[:, 1:2], in0=st.ap[:, 0:1], scalar1=sinkb.ap[:, h:h + 1], scalar2=-1.0, op0=ALU.max, op1=ALU.mult), [st], [st, sinkb])
                        pb = p_pool.next()
                        fw.op(act, lambda e: e.activation(out=pb.ap[:, 0:ncols], in_=ssb.ap[:, 0:ncols], func=AF.Exp, bias=st.ap[:, 1:2], accum_out=st.ap[:, 2:3]), [pb, st], [ssb, st])
                        fw.op(act, lambda e: e.activation(out=st.ap[:, 3:4], in_=sinkb.ap[:, h:h + 1], func=AF.Exp, bias=st.ap[:, 1:2]), [st], [st, sinkb])
                        fw.op(dve, lambda e: e.tensor_tensor(out=st.ap[:, 4:5], in0=st.ap[:, 2:3], in1=st.ap[:, 3:4], op=ALU.add), [st], [st])
                        fw.op(dve, lambda e: e.reciprocal(out=st.ap[:, 5:6], in_=st.ap[:, 4:5]), [st], [st])
                        return dict(i=i, h=h, g=g, pb=pb, st=st)

                    def att_back(cx):
                        i, h, g, pb, st = cx["i"], cx["h"], cx["g"], cx["pb"], cx["st"]
                        ti = tinfo[i]
                        ncols, kblocks, mixa, ops = ti["ncols"], ti["kblocks"], ti["mixa"], ti["ops"]
                        ps3 = psum()
                        ps3b = ps3.ap.bitcast(BF16)
                        nblk = ncols // 128
                        for b in range(nblk):
                            fw.op(pe, lambda e, b=b: e.transpose(out=ps3b[:, b * 128:(b + 1) * 128], in_=pb.ap[:, b * 128:(b + 1) * 128], identity=identb.ap), [ps3], [pb, identb])
                        pT = pT_pool.next()
                        if h % 2 == 0:
                            fw.op(dve, lambda e: e.tensor_copy(out=pT.ap[:, 0:ncols], in_=ps3b[:, 0:ncols]), [pT], [ps3])
                        else:
                            fw.op(act, lambda e: e.copy(out=pT.ap[:, 0:ncols], in_=ps3b[:, 0:ncols]), [pT], [ps3])
                        nb = len(kblocks)
                        for b, kb in enumerate(kblocks):
                            fw.op(pe, lambda e, b=b, kb=kb: e.matmul(ops.ap[:, h * 64:(h + 1) * 64], lhsT=pT.ap[:, b * 128:(b + 1) * 128], rhs=VV.ap[:, kb, g * 64:(g + 1) * 64], start=(b == 0), stop=(b == nb - 1)), [ops], [pT, qt_t[kb]])
                        fw.op(act, lambda e: e.activation(out=mixa.ap[:, h * 64:(h + 1) * 64], in_=ops.ap[:, h * 64:(h + 1) * 64], func=AF.Copy, scale=st.ap[:, 5:6]), [mixa], [ops, st])
                        if h == 7:
                            fw.dma(mix[i * 128:(i + 1) * 128, 0:512], mixa.ap, [mix_a[i]], [mixa])

                    units = [(i, h) for i in tiles3 for h in range(8)]
                    cxn = att_front(*units[0])
                    for n in range(len(units)):
                        cxc = cxn
                        if n + 1 < len(units):
                            cxn = att_front(*units[n + 1])
                        att_back(cxc)
                    fw.barrier()
            if stop_after == "S3":
                if "mix" in dbg:
                    fw.barrier()
                    for i in range(NT):
                        fw.dma(dbg["mix"][i * 128:(i + 1) * 128, :], mix[i * 128:(i + 1) * 128, :], [], [mix_a[i], mix_r[i]])
                break
            FV, FR, FG, FKK, FBON = 0, 512, 1024, 1536, 2048
            FLW = lambda d_: 2560 + d_ * 1536
            FB = lambda d_: 2560 + d_ * 1536 + 512
            FKD = lambda d_: 2560 + d_ * 1536 + 1024
            NF = 5632
            with ExitStack() as es:
                def bc(name, src, n):
                    t_ = sb(es, nc, name, [128, n], F32)
                    fw.dma(t_.ap, src.partition_broadcast(128), [t_], [])
                    return t_
                mup = bc("mup", mu_prev[l], RWC)
                mun = bc("mun", mu_next[l], RWC)
                w0b = bc("w0b", decay_w0[l].rearrange("d c -> (d c)"), 1024)
                a0b = bc("a0b", iclr_a0[l].rearrange("d c -> (d c)"), 1024)
                kkb = bc("kkb", k_k[l], RW)
                kab = bc("kab", k_a[l], RW)
                rkb = bc("rkb", r_k[l], RW)
                w2t = sb(es, nc, "w2t", [128, RW], F32)
                a2t = sb(es, nc, "a2t", [128, RW], F32)
                g2t = sb(es, nc, "g2t", [128, RW], F32)
                fw.dma(w2t.ap, decay_w2[l].rearrange("d r c -> (d r) c"), [w2t], [])
                fw.dma(a2t.ap, iclr_a2[l].rearrange("d r c -> (d r) c"), [a2t], [])
                fw.dma(g2t.ap, gate_g2[l], [g2t], [])
                if l > 0:
                    v0b = bc("v0b", vres_v0[0], RW)
                    v1t = sb(es, nc, "v1t", [128, 4, 32], F32)
                    fw.dma(v1t.ap, vres_v1[0].rearrange("(c p) n -> p c n", p=128), [v1t], [])
                    v2t = sb(es, nc, "v2t", [32, RW], F32)
                    fw.dma(v2t.ap, vres_v2[0], [v2t], [])
                uc_pool = Pool(es, nc, "uc", [128, RWC], F32, 2)
                up_pool = Pool(es, nc, "up", [128, RWC], F32, 2)
                un_pool = Pool(es, nc, "un", [128, RWC], F32, 2)
                vf_pool = Pool(es, nc, "vf", [128, RW], F32, 2)
                li_pool = Pool(es, nc, "li", [128, 384], F32, 2)
                liT_pool = Pool(es, nc, "liT", [128, 3, 128], F32, 2)
                fo_pool = Pool(es, nc, "fo", [128, NF], F32, 2)
                tmp_pool = Pool(es, nc, "ftmp", [128, RW], F32, 6)
                a_pool = Pool(es, nc, "fa", [128, RW], F32, 4)
                sm_pool = Pool(es, nc, "fsm", [128, 64], F32, 2)
                vT_pool = Pool(es, nc, "fvT", [128, 4, 128], F32, 2)
                h3 = lambda ap: ap.rearrange("p (h d) -> p h d", h=8)

                def s4a_load(i):
                    uc, up, un = uc_pool.next(), up_pool.next(), un_pool.next()
                    deps = [u_t[i], u_guard] + ([u_t[i - 1]] if i > 0 else []) + ([u_t[i + 1]] if i + 1 < NT else [])
                    r0 = urow(i)
                    fw.dma(uc.ap, u_scr[r0:r0 + 128, :], [uc], deps)
                    fw.dma(up.ap, u_scr[r0 - 1:r0 + 127, :], [up], deps)
                    fw.dma(un.ap, u_scr[r0 + 1:r0 + 129, :], [un], deps)
                    vf = None
                    if l > 0:
                        vf = vf_pool.next()
                        fw.dma(vf.ap, vfirst[i * 128:(i + 1) * 128, :], [vf], [vf_t[i]])
                    return uc, up, un, vf

                nxt = s4a_load(0)
                for i in range(NT):
                    uc, up, un, vf = nxt
                    if i + 1 < NT:
                        nxt = s4a_load(i + 1)
                    fw.op(dve, lambda e: e.tensor_tensor(out=up.ap, in0=up.ap, in1=uc.ap, op=ALU.subtract), [up], [up, uc])
                    fw.op(pool, lambda e: e.tensor_tensor(out=un.ap, in0=un.ap, in1=uc.ap, op=ALU.subtract), [un], [un, uc])
                    fw.op(dve, lambda e: e.tensor_tensor(out=up.ap, in0=up.ap, in1=mup.ap, op=ALU.mult), [up], [up, mup])
                    fw.op(pool, lambda e: e.tensor_tensor(out=un.ap, in0=un.ap, in1=mun.ap, op=ALU.mult), [un], [un, mun])
                    fw.op(dve, lambda e: e.tensor_tensor(out=uc.ap, in0=uc.ap, in1=up.ap, op=ALU.add), [uc], [uc, up])
                    fw.op(dve, lambda e: e.tensor_tensor(out=uc.ap, in0=uc.ap, in1=un.ap, op=ALU.add), [uc], [uc, un])
                    U = uc.ap
                    r_, k_, v_ = U[:, 0:512], U[:, 512:1024], U[:, 1024:1536]
                    li = li_pool.next()
                    fw.op(act, lambda e: e.activation(out=li.ap[:, 0:128], in_=U[:, 1536:1664], func=AF.Tanh), [li], [uc])
                    fw.op(act, lambda e: e.copy(out=li.ap[:, 128:256], in_=U[:, 1664:1792]), [li], [uc])
                    fw.op(act, lambda e: e.activation(out=li.ap[:, 256:384], in_=U[:, 1792:1920], func=AF.Sigmoid), [li], [uc])
                    ps = psum()
                    for q in range(3):
                        fw.op(pe, lambda e, q=q, ps=ps: e.transpose(out=ps.ap[:, q * 128:(q + 1) * 128], in_=li.ap[:, q * 128:(q + 1) * 128], identity=ident.ap), [ps], [li, ident])
                    liT = liT_pool.next()
                    fw.op(dve, lambda e, ps=ps: e.tensor_copy(out=liT.ap, in_=ps.ap[:, 0:384].rearrange("p (a b) -> p a b", a=3)), [liT], [ps])
                    fo = fo_pool.next()
                    FO = fo.ap
                    a_d = []
                    for d_ in range(2):
                        hp = slice(d_ * 64, (d_ + 1) * 64)
                        ps = psum()
                        fw.op(pe, lambda e, ps=ps, hp=hp: e.matmul(ps.ap, lhsT=liT.ap[hp, 0, :], rhs=w2t.ap[hp, :], start=True, stop=True), [ps], [liT, w2t])
                        t1 = tmp_pool.next()
                        fw.op(dve, lambda e, ps=ps, t1=t1, d_=d_: e.tensor_tensor(out=t1.ap, in0=ps.ap, in1=w0b.ap[:, d_ * 512:(d_ + 1) * 512], op=ALU.add), [t1], [ps, w0b])
                        fw.op(act, lambda e, t1=t1: e.activation(out=t1.ap, in_=t1.ap, func=AF.Sigmoid), [t1], [t1])
                        fw.op(act, lambda e, t1=t1, d_=d_: e.activation(out=FO[:, FLW(d_):FLW(d_) + 512], in_=t1.ap, func=AF.Copy, scale=-0.6065306597126334), [fo], [t1])
                        ps = psum()
                        fw.op(pe, lambda e, ps=ps, hp=hp: e.matmul(ps.ap, lhsT=liT.ap[hp, 1, :], rhs=a2t.ap[hp, :], start=True, stop=True), [ps], [liT, a2t])
                        ad = a_pool.next()
                        fw.op(dve, lambda e, ps=ps, ad=ad, d_=d_: e.tensor_tensor(out=ad.ap, in0=ps.ap, in1=a0b.ap[:, d_ * 512:(d_ + 1) * 512], op=ALU.add), [ad], [ps, a0b])
                        fw.op(act, lambda e, ad=ad: e.activation(out=ad.ap, in_=ad.ap, func=AF.Sigmoid), [ad], [ad])
                        a_d.append(ad)
                    ps = psum()
                    fw.op(pe, lambda e, ps=ps: e.matmul(ps.ap, lhsT=liT.ap[:, 2, :], rhs=g2t.ap, start=True, stop=True), [ps], [liT, g2t])
                    fw.op(act, lambda e, ps=ps: e.copy(out=FO[:, FG:FG + 512], in_=ps.ap), [fo], [ps])
                    if l == 0:
                        fw.op(act, lambda e: e.copy(out=FO[:, FV:FV + 512], in_=v_), [fo], [uc])
                        fw.dma(vfirst[i * 128:(i + 1) * 128, :], v_, [vf_t[i]], [uc])
                    else:
                        ps = psum()
                        for c_ in range(4):
                            fw.op(pe, lambda e, c_=c_, ps=ps: e.transpose(out=ps.ap[:, c_ * 128:(c_ + 1) * 128], in_=U[:, 1024 + c_ * 128:1024 + (c_ + 1) * 128], identity=ident.ap), [ps], [uc, ident])
                        vT = vT_pool.next()
                        fw.op(dve, lambda e, ps=ps, vT=vT: e.tensor_copy(out=vT.ap, in_=ps.ap.rearrange("p (a b) -> p a b", a=4)), [vT], [ps])
                        ps = psum()
                        for c_ in range(4):
                            fw.op(pe, lambda e, c_=c_, ps=ps, vT=vT: e.matmul(ps.ap[:, 0:32], lhsT=vT.ap[:, c_, :], rhs=v1t.ap[:, c_, :], start=(c_ == 0), stop=(c_ == 3)), [ps], [vT, v1t])
                        sm = sm_pool.next()
                        fw.op(dve, lambda e, ps=ps, sm=sm: e.tensor_copy(out=sm.ap[:, 0:32], in_=ps.ap[:, 0:32]), [sm], [ps])
                        ps = psum()
                        fw.op(pe, lambda e, ps=ps, sm=sm: e.transpose(out=ps.ap[0:32, 0:128], in_=sm.ap[:, 0:32], identity=ident.ap), [ps], [sm, ident])
                        smT = tmp_pool.next()
                        fw.op(dve, lambda e, ps=ps, smT=smT: e.tensor_copy(out=smT.ap[0:32, 0:128], in_=ps.ap[0:32, 0:128]), [smT], [ps])
                        ps = psum()
                        fw.op(pe, lambda e, ps=ps, smT=smT: e.matmul(ps.ap, lhsT=smT.ap[0:32, 0:128], rhs=v2t.ap, start=True, stop=True), [ps], [smT, v2t])
                        t1 = tmp_pool.next()
                        fw.op(dve, lambda e, ps=ps, t1=t1: e.tensor_tensor(out=t1.ap, in0=ps.ap, in1=v0b.ap, op=ALU.add), [t1], [ps, v0b])
                        fw.op(act, lambda e, t1=t1: e.activation(out=t1.ap, in_=t1.ap, func=AF.Sigmoid), [t1], [t1])
                        fw.op(dve, lambda e, vf=vf: e.tensor_tensor(out=vf.ap, in0=vf.ap, in1=v_, op=ALU.subtract), [vf], [vf, uc])
                        fw.op(dve, lambda e, vf=vf, t1=t1: e.tensor_tensor(out=vf.ap, in0=vf.ap, in1=t1.ap, op=ALU.mult), [vf], [vf, t1])
                        fw.op(dve, lambda e, vf=vf: e.tensor_tensor(out=FO[:, FV:FV + 512], in0=vf.ap, in1=v_, op=ALU.add), [fo], [vf, uc])
                    fw.op(act, lambda e: e.copy(out=FO[:, FR:FR + 512], in_=r_), [fo], [uc])
                    kk0 = tmp_pool.next()
                    sq = tmp_pool.next()
                    sm = sm_pool.next()
                    fw.op(dve, lambda e, kk0=kk0: e.tensor_tensor(out=kk0.ap, in0=k_, in1=kkb.ap, op=ALU.mult), [kk0], [uc, kkb])
                    fw.op(act, lambda e, kk0=kk0, sq=sq: e.activation(out=sq.ap, in_=kk0.ap, func=AF.Square), [sq], [kk0])
                    fw.op(dve, lambda e, sq=sq, sm=sm: e.reduce_sum(out=sm.ap[:, 0:8], in_=h3(sq.ap), axis=AX.X), [sm], [sq])
                    fw.op(act, lambda e, sm=sm: e.activation(out=sm.ap[:, 8:16], in_=sm.ap[:, 0:8], func=AF.Sqrt), [sm], [sm])
                    fw.op(dve, lambda e, sm=sm: e.tensor_scalar(out=sm.ap[:, 8:16], in0=sm.ap[:, 8:16], scalar1=1e-12, scalar2=None, op0=ALU.max), [sm], [sm])
                    fw.op(dve, lambda e, sm=sm: e.reciprocal(out=sm.ap[:, 16:24], in_=sm.ap[:, 8:16]), [sm], [sm])
                    fw.op(dve, lambda e, sm=sm, kk0=kk0: e.tensor_tensor(out=h3(FO[:, FKK:FKK + 512]), in0=h3(kk0.ap), in1=sm.ap[:, 16:24].unsqueeze(2).to_broadcast([128, 8, 64]), op=ALU.mult), [fo], [kk0, sm])
                    for d_ in range(2):
                        t1 = tmp_pool.next()
                        fw.op(dve, lambda e, t1=t1, d_=d_: e.scalar_tensor_tensor(out=t1.ap, in0=a_d[d_].ap, scalar=-1.0, in1=kab.ap, op0=ALU.add, op1=ALU.mult), [t1], [a_d[d_], kab])
                        fw.op(dve, lambda e, t1=t1, d_=d_: e.scalar_tensor_tensor(out=FO[:, FKD(d_):FKD(d_) + 512], in0=t1.ap, scalar=1.0, in1=k_, op0=ALU.add, op1=ALU.mult), [fo], [t1, uc])
                        fw.op(pool, lambda e, d_=d_: e.tensor_tensor(out=FO[:, FB(d_):FB(d_) + 512], in0=FO[:, FKK:FKK + 512], in1=a_d[d_].ap, op=ALU.mult), [fo], [fo, a_d[d_]])
                    t1 = tmp_pool.next()
                    sm2 = sm_pool.next()
                    fw.op(pool, lambda e, t1=t1: e.tensor_tensor(out=t1.ap, in0=FO[:, FKD(0):FKD(0) + 512], in1=FO[:, FKD(1):FKD(1) + 512], op=ALU.add), [t1], [fo])
                    fw.op(dve, lambda e, t1=t1: e.tensor_tensor(out=t1.ap, in0=t1.ap, in1=r_, op=ALU.mult), [t1], [t1, uc])
                    fw.op(pool, lambda e, t1=t1: e.tensor_tensor(out=t1.ap, in0=t1.ap, in1=rkb.ap, op=ALU.mult), [t1], [t1, rkb])
                    fw.op(dve, lambda e, t1=t1, sm2=sm2: e.reduce_sum(out=sm2.ap[:, 0:8], in_=h3(t1.ap), axis=AX.X), [sm2], [t1])
                    fw.op(dve, lambda e, sm2=sm2: e.tensor_tensor(out=h3(FO[:, FBON:FBON + 512]), in0=h3(FO[:, FV:FV + 512]), in1=sm2.ap[:, 0:8].unsqueeze(2).to_broadcast([128, 8, 64]), op=ALU.mult), [fo], [fo, sm2])
                    fw.dma(feat[i * 128:(i + 1) * 128, :], FO, [feat_t[i]], [fo])
                    if "feat" in dbg and l == 0:
                        fw.dma(dbg["feat"][i * 128:(i + 1) * 128, :], FO[:, 0:4096], [], [fo])
                fw.barrier()
            if stop_after == "S4a":
                break
            with ExitStack() as es:
                ones = sb(es, nc, "ones", [128, 1], F32)
                fw.op(dve, lambda e: e.memset(ones.ap, 1.0), [ones], [])
                Mq1, MnT = [], []
                for d_ in range(2):
                    m1_ = sb(es, nc, "Mq1", [128, 256], F32)
                    fw.op(dve, lambda e, m1_=m1_, d_=d_: e.tensor_scalar(out=m1_.ap[:, 0:128], in0=masks.ap[:, 2 * d_, :], scalar1=-1.0, scalar2=None, op0=ALU.mult), [m1_], [masks])
                    fw.op(dve, lambda e, m1_=m1_, d_=d_: e.tensor_copy(out=m1_.ap[:, 128:256], in_=masks.ap[:, 2 * d_ + 1, :]), [m1_], [masks])
                    Mq1.append(m1_)
                    mt_ = sb(es, nc, "MnT", [128, 128], F32)
                    fw.op(dve, lambda e, mt_=mt_, d_=d_: e.tensor_scalar(out=mt_.ap, in0=masks.ap[:, 2 * (1 - d_), :], scalar1=-1.0, scalar2=None, op0=ALU.mult), [mt_], [masks])
                    MnT.append(mt_)
                Mq2 = [masks.ap[:, 2 * d_:2 * d_ + 2, :].rearrange("p a t -> p (a t)") for d_ in range(2)]
                PSQB = int(os.environ.get('S4B_PSQB', '4'))
                PSQ = [TT(PS[PSQB + k // 4].ap[:, (k % 4) * 128:(k % 4 + 1) * 128]) for k in range(16)]
                pq = [0]

                def psq():
                    t_ = PSQ[pq[0] % 16]
                    pq[0] += 1
                    return t_
                p4 = [0]

                def psumR():
                    t_ = PS[p4[0] % 8]
                    p4[0] += 1
                    return t_
                fa_pool = Pool(es, nc, "sfa", [128, 3, 512], F32, 4)
                fb_pool = Pool(es, nc, "sfb", [128, 3, 512], F32, 4)
                tm_pool = Pool(es, nc, "stm", [128, 4, 512], BF16, 2)
                vb_pool = Pool(es, nc, "svb", [128, 512], BF16, 2)
                ex_pool = Pool(es, nc, "sex", [128, 3, 512], F32, 2)
                fm_pool = Pool(es, nc, "sfm", [128, 4, 4, 128], BF16, 2)
                el_pool = Pool(es, nc, "sel", [128, 4], F32, 2)
                g1_pool = Pool(es, nc, "sg1", [128, 2, 4, 128], BF16, 4)
                a2_pool = Pool(es, nc, "sa2", [128, 256], BF16, 16)
                inv_pool = Pool(es, nc, "sinv", [128, 4, 128], BF16, 32)
                invf_pool = Pool(es, nc, "sinvf", [128, 4, 128], F32, 10)
                xs_pool = Pool(es, nc, "sxs", [128, 4, 64], BF16, 4)
                nu_pool = Pool(es, nc, "snu", [128, 512], BF16, 2)
                ys_pool = Pool(es, nc, "sys", [128, 512], F32, 2)
                ST = [[[sb(es, nc, "ST", [128, 64], F32) for c_ in range(4)] for pp in range(2)] for d_ in range(2)]
                STb = [[[sb(es, nc, "STb", [128, 64], BF16) for c_ in range(4)] for pp in range(2)] for d_ in range(2)]
                for d_ in range(2):
                    for c_ in range(4):
                        fw.op(dve, lambda e, d_=d_, c_=c_: e.memset(ST[d_][0][c_].ap, 0.0), [ST[d_][0][c_]], [])
                        fw.op(dve, lambda e, d_=d_, c_=c_: e.memset(STb[d_][0][c_].ap, 0.0), [STb[d_][0][c_]], [])
                orders = [list(range(NT)), [1, 0] + list(range(NT - 1, NCT - 1, -1))]
                evi = [0]

                def evac(fn_dve, fn_act, outs, ins):
                    if evi[0] % 2:
                        fw.op(dve, fn_dve, outs, ins)
                    else:
                        fw.op(act, fn_act, outs, ins)

                def s4b_load(d_, i):
                    fa, fb = fa_pool.next(), fb_pool.next()
                    r0 = i * 128
                    fw.dma(fa.ap, feat[r0:r0 + 128, FLW(d_):FLW(d_) + 1536].rearrange("p (a c) -> p a c", a=3), [fa], [feat_t[i]])
                    fw.dma(fb.ap[:, 0:2, :], feat[r0:r0 + 128, 0:1024].rearrange("p (a c) -> p a c", a=2), [fb], [feat_t[i]])
                    fw.dma(fb.ap[:, 2, :], feat[r0:r0 + 128, FKK:FKK + 512], [fb], [feat_t[i]])
                    return fa, fb

                def step_prep(d_, j, fa, fb):
                    i = orders[d_][j]
                    pp = j % 2
                    STo, STn = ST[d_][pp], ST[d_][1 - pp]
                    SBo, SBn = STb[d_][pp], STb[d_][1 - pp]
                    vb = vb_pool.next()
                    fw.op(act, lambda e: e.copy(out=vb.ap, in_=fb.ap[:, 0, :]), [vb], [fb])
                    lw, b_, kd = fa.ap[:, 0, :], fa.ap[:, 1, :], fa.ap[:, 2, :]
                    v_, r_, kk = fb.ap[:, 0, :], fb.ap[:, 1, :], fb.ap[:, 2, :]
                    psL = psumR()
                    fw.op(pe, lambda e: e.matmul(psL.ap, lhsT=masks.ap[:, 1 + 2 * d_, :], rhs=lw, start=True, stop=True), [psL], [masks, fa])
                    ex = ex_pool.next()
                    fw.op(act, lambda e: e.activation(out=ex.ap[:, 0, :], in_=psL.ap, func=AF.Exp), [ex], [psL])
                    fw.op(act, lambda e: e.activation(out=ex.ap[:, 1, :], in_=psL.ap, func=AF.Exp, scale=-1.0), [ex], [psL])
                    fw.op(dve, lambda e: e.tensor_tensor(out=ex.ap[:, 2, :], in0=psL.ap, in1=lw, op=ALU.subtract), [ex], [psL, fa])
                    fw.op(act, lambda e: e.activation(out=ex.ap[:, 2, :], in_=ex.ap[:, 2, :], func=AF.Exp), [ex], [ex])
                    tm = tm_pool.next()
                    fw.op(dve, lambda e: e.tensor_tensor(out=tm.ap[:, 0, :], in0=kk, in1=ex.ap[:, 2, :], op=ALU.mult), [tm], [fb, ex])
                    fw.op(pool, lambda e: e.tensor_tensor(out=tm.ap[:, 1, :], in0=r_, in1=ex.ap[:, 0, :], op=ALU.mult), [tm], [fb, ex])
                    fw.op(dve, lambda e: e.tensor_tensor(out=tm.ap[:, 2, :], in0=b_, in1=ex.ap[:, 1, :], op=ALU.mult), [tm], [fa, ex])
                    fw.op(pool, lambda e: e.tensor_tensor(out=tm.ap[:, 3, :], in0=kd, in1=ex.ap[:, 1, :], op=ALU.mult), [tm], [fa, ex])
                    psE = psumR()
                    for c_ in range(4):
                        fw.op(pe, lambda e, c_=c_: e.matmul(psE.ap[:, c_:c_ + 1], lhsT=lw[:, c_ * 128:(c_ + 1) * 128], rhs=ones.ap, start=True, stop=True), [psE], [fa, ones])
                    el = el_pool.next()
                    fw.op(act, lambda e: e.activation(out=el.ap, in_=psE.ap[:, 0:4], func=AF.Exp), [el], [psE])
                    fm = fm_pool.next()
                    for c_ in range(4):
                        psT = psumR()
                        for q in range(4):
                            fw.op(pe, lambda e, c_=c_, q=q, psT=psT: e.transpose(out=psT.ap.bitcast(BF16)[:, q * 128:(q + 1) * 128], in_=tm.ap[:, q, c_ * 128:(c_ + 1) * 128], identity=identb.ap), [psT], [tm, identb])
                        evi[0] += 1
                        evac(lambda e, c_=c_, psT=psT: e.tensor_copy(out=fm.ap[:, c_, :, :], in_=psT.ap.bitcast(BF16)[:, 0:512].rearrange("p (a b) -> p a b", a=4)),
                             lambda e, c_=c_, psT=psT: e.copy(out=fm.ap[:, c_, :, :], in_=psT.ap.bitcast(BF16)[:, 0:512].rearrange("p (a b) -> p a b", a=4)), [fm], [psT])
                    H = []
                    for h in range(8):
                        c_ = h // 2
                        hp = slice((h % 2) * 64, (h % 2) * 64 + 64)
                        H.append(dict(c=c_, hp=hp, par=h % 2, k=h // 2, al=fm.ap[hp, c_, 0, :], rh=fm.ap[hp, c_, 1, :], be=fm.ap[hp, c_, 2, :], ka=fm.ap[hp, c_, 3, :],
                                      ar=fm.ap[hp, c_, 0:2, :].rearrange("p a t -> p (a t)"), vh=vb.ap[:, h * 64:(h + 1) * 64]))
                    G1 = [g1_pool.next() for _ in range(2)]
                    for h in range(8):
                        u = H[h]
                        psG = psumR()
                        fw.op(pe, lambda e, u=u, psG=psG: e.matmul(psG.ap[:, 0:256], lhsT=u["be"], rhs=u["ar"], start=True, stop=True), [psG], [fm])
                        fw.op(pe, lambda e, u=u, psG=psG: e.matmul(psG.ap[:, 256:512], lhsT=u["ka"], rhs=u["ar"], start=True, stop=True), [psG], [fm])
                        a2 = a2_pool.next()
                        g1 = G1[u["par"]]
                        fw.op(dve, lambda e, u=u, psG=psG, g1=g1: e.tensor_tensor(out=g1.ap[:, :, u["k"], :], in0=psG.ap[:, 0:256].rearrange("p (a t) -> p a t", a=2), in1=Mq1[d_].ap.rearrange("p (a t) -> p a t", a=2), op=ALU.mult), [g1], [psG, Mq1[d_]])
                        fw.op(dve, lambda e, a2=a2, psG=psG: e.tensor_tensor(out=a2.ap, in0=psG.ap[:, 256:512], in1=Mq2[d_], op=ALU.mult), [a2], [psG, masks])
                        u.update(a2=a2, abr=g1.ap[:, 1, u["k"], :], g1=g1)
                    GP = []
                    for par in range(2):
                        g1 = G1[par]
                        Mf = invf_pool.next()
                        fw.op(dve, lambda e, Mf=Mf, g1=g1: e.tensor_tensor(out=Mf.ap, in0=g1.ap[:, 0, :, :], in1=ident.ap.unsqueeze(1).to_broadcast([128, 4, 128]), op=ALU.add), [Mf], [g1, ident])
                        Mb = inv_pool.next()
                        fw.op(act, lambda e, Mf=Mf, Mb=Mb: e.copy(out=Mb.ap, in_=Mf.ap), [Mb], [Mf])
                        bank = psumR()
                        bkb = bank.ap.bitcast(BF16)
                        for k_ in range(4):
                            fw.op(pe, lambda e, k_=k_, bkb=bkb, g1=g1: e.transpose(out=bkb[:, k_ * 128:(k_ + 1) * 128], in_=g1.ap[:, 0, k_, :], identity=identb.ap), [bank], [g1, identb])
                        AT = inv_pool.next()
                        evi[0] += 1
                        evac(lambda e, AT=AT, bkb=bkb: e.tensor_copy(out=f4(AT.ap), in_=bkb[:, 0:512]), lambda e, AT=AT, bkb=bkb: e.copy(out=f4(AT.ap), in_=bkb[:, 0:512]), [AT], [bank])
                        GP.append(dict(A=g1, Aap=g1.ap[:, 0, :, :], AT=AT, M=Mf, Mb=Mb))
                    return dict(d_=d_, i=i, STo=STo, STn=STn, tm=tm, el=el, fm=fm, fb=fb, v_=v_, H=H, GP=GP, SBo=SBo, SBn=SBn, vb=vb)

                f4 = lambda ap: ap.rearrange("p k t -> p (k t)")

                def inv_all(GP):
                    for it in range(6):
                        for g in GP:
                            bank = psumR()
                            for k_ in range(4):
                                fw.op(pe, lambda e, k_=k_, bank=bank, g=g: e.matmul(bank.ap[:, k_ * 128:(k_ + 1) * 128], lhsT=g["Aap"][:, k_, :], rhs=g["AT"].ap[:, k_, :], start=True, stop=True), [bank], [g["A"], g["AT"]])
                            ATn = inv_pool.next()
                            evi[0] += 1
                            evac(lambda e, ATn=ATn, bank=bank: e.tensor_copy(out=f4(ATn.ap), in_=bank.ap), lambda e, ATn=ATn, bank=bank: e.copy(out=f4(ATn.ap), in_=bank.ap), [ATn], [bank])
                            g["ATn"] = ATn
                            if it < 5:
                                bank = psumR()
                                for k_ in range(4):
                                    fw.op(pe, lambda e, k_=k_, bank=bank, g=g: e.matmul(bank.ap[:, k_ * 128:(k_ + 1) * 128], lhsT=g["AT"].ap[:, k_, :], rhs=g["Aap"][:, k_, :], start=True, stop=True), [bank], [g["A"], g["AT"]])
                                An = inv_pool.next()
                                evi[0] += 1
                                evac(lambda e, An=An, bank=bank: e.tensor_copy(out=f4(An.ap), in_=bank.ap), lambda e, An=An, bank=bank: e.copy(out=f4(An.ap), in_=bank.ap), [An], [bank])
                                g["An"] = An
                        for g in GP:
                            g["AT"] = g["ATn"]
                            if it < 5:
                                g["A"], g["Aap"] = g["An"], g["An"].ap
                        for g in GP:
                            bank = psumR()
                            for k_ in range(4):
                                fw.op(pe, lambda e, k_=k_, bank=bank, g=g: e.matmul(bank.ap[:, k_ * 128:(k_ + 1) * 128], lhsT=g["AT"].ap[:, k_, :], rhs=g["Mb"].ap[:, k_, :], start=True, stop=True), [bank], [g["AT"], g["Mb"]])
                            Mn = invf_pool.next()
                            fw.op(dve, lambda e, Mn=Mn, bank=bank, g=g: e.tensor_tensor(out=f4(Mn.ap), in0=bank.ap, in1=f4(g["M"].ap), op=ALU.add), [Mn], [bank, g["M"]])
                            g["M"] = Mn
                            Mbn = inv_pool.next()
                            fw.op(act, lambda e, Mn=Mn, Mbn=Mbn: e.copy(out=Mbn.ap, in_=Mn.ap), [Mbn], [Mn])
                            g["Mb"] = Mbn

                def step_state(cx):
                    d_, i, STo, STn, tm, el, fm, fb, v_, H, GP, SBo, SBn, vb = (cx[k] for k in ('d_', 'i', 'STo', 'STn', 'tm', 'el', 'fm', 'fb', 'v_', 'H', 'GP', 'SBo', 'SBn', 'vb'))
                    NU = nu_pool.next()
                    q4 = lambda ap: ap.rearrange("p (k t) -> p k t", k=4)[:, :, 0:64]
                    for par, g in enumerate(GP):
                        bank = psumR()
                        for k_ in range(4):
                            u = H[par + 2 * k_]
                            o = bank.ap[:, k_ * 128:k_ * 128 + 64]
                            fw.op(pe, lambda e, u=u, o=o: e.matmul(o, lhsT=u["al"], rhs=SBo[u["c"]].ap[u["hp"], :], start=True, stop=False), [bank], [fm, SBo[u["c"]]])
                            fw.op(pe, lambda e, u=u, o=o: e.matmul(o, lhsT=u["a2"].ap[:, 0:128], rhs=u["vh"], start=False, stop=True), [bank], [u["a2"], vb])
                        xsg = xs_pool.next()
                        evi[0] += 1
                        evac(lambda e, xsg=xsg, bank=bank: e.tensor_copy(out=xsg.ap, in_=q4(bank.ap)), lambda e, xsg=xsg, bank=bank: e.copy(out=xsg.ap, in_=q4(bank.ap)), [xsg], [bank])
                        g["xs"] = xsg
                    for par, g in enumerate(GP):
                        bank = psumR()
                        for k_ in range(4):
                            fw.op(pe, lambda e, k_=k_, bank=bank, g=g: e.matmul(bank.ap[:, k_ * 128:k_ * 128 + 64], lhsT=g["Mb"].ap[:, k_, :], rhs=g["xs"].ap[:, k_, :], start=True, stop=True), [bank], [g["Mb"], g["xs"]])
                        nuv = NU.ap.rearrange("p (c two d) -> p c two d", two=2, d=64)[:, :, par, :]
                        fw.op(act, lambda e, bank=bank, nuv=nuv: e.activation(out=nuv, in_=q4(bank.ap), func=AF.Copy, scale=-1.0), [NU], [bank])
                    ysb = ys_pool.next()
                    for par in (0, 1):
                        psY = psumR()
                        for k_ in range(4):
                            h = par + 2 * k_
                            u = H[h]
                            ysl = psY.ap[:, k_ * 64:(k_ + 1) * 64]
                            fw.op(pe, lambda e, u=u, ysl=ysl: e.matmul(ysl, lhsT=u["rh"], rhs=SBo[u["c"]].ap[u["hp"], :], start=True, stop=False), [psY], [fm, SBo[u["c"]]])
                            fw.op(pe, lambda e, u=u, ysl=ysl, h=h: e.matmul(ysl, lhsT=u["abr"], rhs=NU.ap[:, h * 64:(h + 1) * 64], start=False, stop=False), [psY], [u["g1"], NU])
                            fw.op(pe, lambda e, u=u, ysl=ysl: e.matmul(ysl, lhsT=u["a2"].ap[:, 128:256], rhs=u["vh"], start=False, stop=True), [psY], [u["a2"], vb])
                        yv = ysb.ap.rearrange("p (c two d) -> p c two d", two=2, d=64)[:, :, par, :]
                        fw.op(act, lambda e, psY=psY, yv=yv: e.copy(out=yv, in_=psY.ap[:, 0:256].rearrange("p (c d) -> p c d", d=64)), [ysb], [psY])
                    fw.dma(y_scr[d_, i * 128:(i + 1) * 128, :], ysb.ap, [y_t[d_][i]], [ysb])
                    psS = psumR()
                    for c_ in range(4):
                        so = psS.ap[:, c_ * 128:(c_ + 1) * 128]
                        fw.op(pe, lambda e, c_=c_, so=so: e.matmul(so, lhsT=tm.ap[:, 2, c_ * 128:(c_ + 1) * 128], rhs=NU.ap[:, c_ * 128:(c_ + 1) * 128], start=True, stop=False), [psS], [tm, NU])
                        fw.op(pe, lambda e, c_=c_, so=so: e.matmul(so, lhsT=tm.ap[:, 3, c_ * 128:(c_ + 1) * 128], rhs=vb.ap[:, c_ * 128:(c_ + 1) * 128], start=False, stop=True), [psS], [tm, vb])
                        fw.op(pool, lambda e, c_=c_: e.tensor_scalar(out=STn[c_].ap, in0=STo[c_].ap, scalar1=el.ap[:, c_:c_ + 1], scalar2=None, op0=ALU.mult), [STn[c_]], [STo[c_], el])
                    for c_ in range(4):
                        so = psS.ap[:, c_ * 128:(c_ + 1) * 128]
                        for hh in range(2):
                            hp = slice(hh * 64, hh * 64 + 64)
                            fw.op(dve, lambda e, c_=c_, hp=hp, so=so: e.scalar_tensor_tensor(out=STn[c_].ap[hp, :], in0=so[hp, hp], scalar=el.ap[hp, c_:c_ + 1], in1=STn[c_].ap[hp, :], op0=ALU.mult, op1=ALU.add), [STn[c_]], [psS, el, STn[c_]])
                    for c_ in range(4):
                        fw.op(act, lambda e, c_=c_: e.copy(out=SBn[c_].ap, in_=STn[c_].ap), [SBn[c_]], [STn[c_]])

                nsteps = NT if not os.environ.get('S4B_STEPS') else int(os.environ['S4B_STEPS'])
                nxt = [s4b_load(d_, orders[d_][0]) for d_ in range(2)]
                for j in range(nsteps):
                    cur = list(nxt)
                    cxs = []
                    for d_ in range(2):
                        cxs.append(step_prep(d_, j, *cur[d_]))
                        if j + 1 < nsteps:
                            nxt[d_] = s4b_load(d_, orders[d_][j + 1])
                    inv_all([g for cx in cxs for g in cx["GP"]])
                    for cx in cxs:
                        step_state(cx)
                fw.barrier()
            if stop_after == "S4b":
                break
            with ExitStack() as es:
                lnw = sb(es, nc, "lnw", [128, RW], F32)
                lnb = sb(es, nc, "lnb", [128, RW], F32)
                fw.dma(lnw.ap, ln_x_w[l].partition_broadcast(128), [lnw], [])
                fw.dma(lnb.ap, ln_x_b[l].partition_broadcast(128), [lnb], [])
                y0_pool = Pool(es, nc, "cy0", [128, RW], F32, 2)
                y1_pool = Pool(es, nc, "cy1", [128, RW], F32, 2)
                gb_pool = Pool(es, nc, "cgb", [128, 2, RW], F32, 2)
                sq_pool = Pool(es, nc, "csq", [128, RW], F32, 2)
                cs_pool = Pool(es, nc, "ccs", [128, 32], F32, 2)
                h3 = lambda ap: ap.rearrange("p (h d) -> p h d", h=8)
                bc8 = lambda ap: ap.unsqueeze(2).to_broadcast([128, 8, 64])
                for i in (range(NCT, NT) if last else range(NT)):
                    y0, y1, gb = y0_pool.next(), y1_pool.next(), gb_pool.next()
                    r0 = i * 128
                    fw.dma(y0.ap, y_scr[0, r0:r0 + 128, :], [y0], [y_t[0][i]])
                    fw.dma(y1.ap, y_scr[1, r0:r0 + 128, :], [y1], [y_t[1][i]])
                    fw.dma(gb.ap[:, 0, :], feat[r0:r0 + 128, FG:FG + 512], [gb], [feat_t[i]])
                    fw.dma(gb.ap[:, 1, :], feat[r0:r0 + 128, FBON:FBON + 512], [gb], [feat_t[i]])
                    cs_ = cs_pool.next()
                    sq = sq_pool.next()
                    fw.op(dve, lambda e: e.tensor_tensor(out=y0.ap, in0=y0.ap, in1=y1.ap, op=ALU.add), [y0], [y0, y1])
                    fw.op(dve, lambda e: e.reduce_sum(out=cs_.ap[:, 0:8], in_=h3(y0.ap), axis=AX.X), [cs_], [y0])
                    fw.op(dve, lambda e: e.tensor_scalar(out=cs_.ap[:, 0:8], in0=cs_.ap[:, 0:8], scalar1=1.0 / 64, scalar2=None, op0=ALU.mult), [cs_], [cs_])
                    fw.op(dve, lambda e: e.tensor_tensor(out=h3(y0.ap), in0=h3(y0.ap), in1=bc8(cs_.ap[:, 0:8]), op=ALU.subtract), [y0], [y0, cs_])
                    fw.op(pool, lambda e: e.tensor_tensor(out=sq.ap, in0=y0.ap, in1=y0.ap, op=ALU.mult), [sq], [y0])
                    fw.op(dve, lambda e: e.reduce_sum(out=cs_.ap[:, 8:16], in_=h3(sq.ap), axis=AX.X), [cs_], [sq])
                    fw.op(dve, lambda e: e.tensor_scalar(out=cs_.ap[:, 8:16], in0=cs_.ap[:, 8:16], scalar1=1.0 / 64, scalar2=64e-5, op0=ALU.mult, op1=ALU.add), [cs_], [cs_])
                    fw.op(act, lambda e: e.activation(out=cs_.ap[:, 16:24], in_=cs_.ap[:, 8:16], func=AF.Sqrt), [cs_], [cs_])
                    fw.op(dve, lambda e: e.reciprocal(out=cs_.ap[:, 24:32], in_=cs_.ap[:, 16:24]), [cs_], [cs_])
                    fw.op(dve, lambda e: e.tensor_tensor(out=h3(y0.ap), in0=h3(y0.ap), in1=bc8(cs_.ap[:, 24:32]), op=ALU.mult), [y0], [y0, cs_])
                    fw.op(pool, lambda e: e.tensor_tensor(out=y0.ap, in0=y0.ap, in1=lnw.ap, op=ALU.mult), [y0], [y0, lnw])
                    fw.op(dve, lambda e: e.tensor_tensor(out=y0.ap, in0=y0.ap, in1=lnb.ap, op=ALU.add), [y0], [y0, lnb])
                    fw.op(pool, lambda e: e.tensor_tensor(out=y0.ap, in0=y0.ap, in1=gb.ap[:, 1, :], op=ALU.add), [y0], [y0, gb])
                    fw.op(dve, lambda e: e.tensor_tensor(out=y0.ap, in0=y0.ap, in1=gb.ap[:, 0, :], op=ALU.mult), [y0], [y0, gb])
                    fw.dma(mix[r0:r0 + 128, 512:1024], y0.ap, [mix_r[i]], [y0])
                fw.barrier()
            if stop_after == "S4":
                if "mix" in dbg:
                    for i in range(NT):
                        fw.dma(dbg["mix"][i * 128:(i + 1) * 128, :], mix[i * 128:(i + 1) * 128, :], [], [mix_a[i], mix_r[i]])
                    fw.barrier()
                break
            tiles5 = list(range(NCT, NT)) if last else list(range(NT))
            with ExitStack() as es:
                woutb = sb(es, nc, "woutb", [128, 8, D], BF16)
                wst_pool = Pool(es, nc, "wst5", [128, 8, 256], F32, 2)
                for j in range(4):
                    wst = wst_pool.next()
                    fw.dma(wst.ap, w_out[l][:, j * 256:(j + 1) * 256].rearrange("(kc p) n -> p kc n", p=128), [wst], [])
                    fw.op(pool, lambda e, wst=wst, j=j: e.tensor_copy(out=woutb.ap[:, :, j * 256:(j + 1) * 256], in_=wst.ap), [woutb], [wst])
                modG = [sb(es, nc, "modG%d" % s_, [128, D], F32) for s_ in range(2)]
                for s_ in range(2):
                    fw.dma(modG[s_].ap, modbuf[l, s_ * 6 + 2], [modG[s_]], [mod_t[l][s_ * 6 + 2]])
                mx_pool = Pool(es, nc, "mx", [128, D], F32, 2)
                mxb_pool = Pool(es, nc, "mxb", [128, D], BF16, 2)
                mT_pool = Pool(es, nc, "mT", [128, 8, 128], BF16, 2)
                x5_pool = Pool(es, nc, "x5", [128, D], F32, 2)

                def s5_load(i):
                    mx = mx_pool.next()
                    fw.dma(mx.ap, mix[i * 128:(i + 1) * 128, :], [mx], [mix_a[i], mix_r[i]])
                    xt = x5_pool.next()
                    fw.dma(xt.ap, xs[i * 128:(i + 1) * 128, :], [xt], [xs_t[i]])
                    return mx, xt

                nxt = s5_load(tiles5[0])
                for ti, i in enumerate(tiles5):
                    mx, xt = nxt
                    if ti + 1 < len(tiles5):
                        nxt = s5_load(tiles5[ti + 1])
                    s_ = 1 if i < NCT else 0
                    mxb = mxb_pool.next()
                    fw.op(pool, lambda e, mx=mx, mxb=mxb: e.tensor_copy(out=mxb.ap, in_=mx.ap), [mxb], [mx])
                    mT = mT_pool.next()
                    for half in range(2):
                        ps = psum()
                        psb = ps.ap.bitcast(BF16)
                        for kk in range(4):
                            kc = half * 4 + kk
                            fw.op(pe, lambda e, kc=kc, kk=kk, psb=psb, mxb=mxb: e.transpose(out=psb[:, kk * 128:(kk + 1) * 128], in_=mxb.ap[:, kc * 128:(kc + 1) * 128], identity=identb.ap), [ps], [mxb, identb])
                        fw.op(act if half else dve, lambda e, psb=psb, mT=mT, half=half: (e.copy if half else e.tensor_copy)(out=mT.ap[:, half * 4:(half + 1) * 4, :], in_=psb[:, 0:512].rearrange("p (a b) -> p a b", a=4)), [mT], [ps])
                    for half in range(2):
                        ps = psum()
                        hs = slice(half * 512, (half + 1) * 512)
                        for kc in range(8):
                            fw.op(pe, lambda e, kc=kc, ps=ps, mT=mT, hs=hs: e.matmul(ps.ap, lhsT=mT.ap[:, kc, :], rhs=woutb.ap[:, kc, hs], start=(kc == 0), stop=(kc == 7)), [ps], [mT, woutb])
                        fw.op(dve, lambda e, ps=ps, mx=mx, hs=hs, s_=s_: e.tensor_tensor(out=mx.ap[:, hs], in0=ps.ap, in1=modG[s_].ap[:, hs], op=ALU.mult), [mx], [ps, modG[s_]])
                    fw.op(pool, lambda e, xt=xt, mx=mx: e.tensor_tensor(out=xt.ap, in0=xt.ap, in1=mx.ap, op=ALU.add), [xt], [xt, mx])
                    fw.dma(xs[i * 128:(i + 1) * 128, :], xt.ap, [xs_t[i]], [xt])
                    if "xmix" in dbg and l == 0:
                        fw.dma(dbg["xmix"][i * 128:(i + 1) * 128, :], xt.ap, [], [xt])
                fw.barrier()
            if stop_after == "S5":
                break

            tiles6 = list(range(NCT, NT)) if last else list(range(NT))
            GS = 8
            ng_ = -(-len(tiles6) // GS)
            gsz = [len(tiles6) // ng_ + (1 if k < len(tiles6) % ng_ else 0) for k in range(ng_)]
            groups, k0_ = [], 0
            for z in gsz:
                groups.append(tiles6[k0_:k0_ + z])
                k0_ += z
            with ExitStack() as es:
                nseg = 1 if last else 2
                mA = [sb(es, nc, "mA%d" % s_, [128, D], F32) for s_ in range(nseg)]
                mB = [sb(es, nc, "mB%d" % s_, [128, D], F32) for s_ in range(nseg)]
                mG = [sb(es, nc, "mG%d" % s_, [128, D], F32) for s_ in range(nseg)]
                for s_ in range(nseg):
                    fw.dma(mA[s_].ap, modbuf[l, s_ * 6 + 3], [mA[s_]], [mod_t[l][s_ * 6 + 3]])
                    fw.dma(mB[s_].ap, modbuf[l, s_ * 6 + 4], [mB[s_]], [mod_t[l][s_ * 6 + 4]])
                    fw.dma(mG[s_].ap, modbuf[l, s_ * 6 + 5], [mG[s_]], [mod_t[l][s_ * 6 + 5]])
                rwt = sb(es, nc, "rwt", [128, 8, NE], F32)
                fw.dma(rwt.ap, router_w.rearrange("(kc p) n -> p kc n", p=128), [rwt], [])
                rbt = sb(es, nc, "rbt", [128, NE], F32)
                fw.dma(rbt.ap, router_b.partition_broadcast(128), [rbt], [])
                fgt = None
                if last:
                    fgt = sb(es, nc, "fgt", [128, D], F32)
                    fw.dma(fgt.ap, final_g.partition_broadcast(128), [fgt], [])
                HT2 = [sb(es, nc, "HT", [128, 8, GS * 128], BF16) for _ in range(2)]
                accs = [sb(es, nc, "acc%d" % k, [128, D], F32) for k in range(GS)]
                gates2 = [sb(es, nc, "gates", [128, GS, NE], F32) for _ in range(2)]
                Wg2 = [sb(es, nc, "Wg", [128, 8, D], BF16) for _ in range(2)]
                Wu2 = [sb(es, nc, "Wu", [128, 8, D], BF16) for _ in range(2)]
                Wd = sb(es, nc, "Wd", [128, 8, D], BF16)
                actb = sb(es, nc, "actb", [128, 8, GS * 128], BF16)
                x6_pool = Pool(es, nc, "x6", [128, D], F32, 2)
                hT32_pool = Pool(es, nc, "hT32", [128, 8, 128], F32, 1)
                hooks = {}
                rt_pool = Pool(es, nc, "rt", [128, 96], F32, 2)
                sg_pool = Pool(es, nc, "sg", [128, 512], F32, 2)
                st6_pool = Pool(es, nc, "st6", [128, 4], F32, 2)

                def rms(xt, st, junk6):
                    jap = junk6.ap if len(junk6.ap.shape) == 2 else junk6.ap.rearrange("p a b -> p (a b)")
                    fw.op(act, lambda e: e.activation(out=jap, in_=xt.ap, func=AF.Square, accum_out=st.ap[:, 0:1]), [junk6, st], [xt])
                    fw.op(dve, lambda e: e.tensor_scalar(out=st.ap[:, 1:2], in0=st.ap[:, 0:1], scalar1=1.0 / D, scalar2=1e-6, op0=ALU.mult, op1=ALU.add), [st], [st])
                    fw.op(act, lambda e: e.activation(out=st.ap[:, 3:4], in_=st.ap[:, 1:2], func=AF.Sqrt), [st], [st])
                    fw.op(dve, lambda e: e.reciprocal(out=st.ap[:, 2:3], in_=st.ap[:, 3:4]), [st], [st])

                def load_w(dst, src):
                    for j in range(2):
                        fw.dma(dst.ap[:, :, j * 512:(j + 1) * 512], src[:, j * 512:(j + 1) * 512].rearrange("(kc p) n -> p kc n", p=128), [dst], [], q=pool)

                seq = [(gi, ex) for gi in range(len(groups)) for ex in range(NE)]
                load_w(Wg2[0], e_gate[l, 0])
                load_w(Wu2[0], e_up[l, 0])

                def moe_A_front(gi, grp, t):
                    i = grp[t]
                    if True:
                        s_ = 1 if i < NCT else 0
                        xt = x6_pool.next()
                        fw.dma(xt.ap, xs[i * 128:(i + 1) * 128, :], [xt], [xs_t[i]])
                        st = st6_pool.next()
                        hT32 = hT32_pool.next()
                        rms(xt, st, hT32)
                        fw.op(dve, lambda e, xt=xt, st=st, s_=s_: e.scalar_tensor_tensor(out=xt.ap, in0=xt.ap, scalar=st.ap[:, 2:3], in1=mA[s_].ap, op0=ALU.mult, op1=ALU.mult), [xt], [xt, st, mA[s_]])
                        fw.op(dve, lambda e, xt=xt, s_=s_: e.tensor_tensor(out=xt.ap, in0=xt.ap, in1=mB[s_].ap, op=ALU.add), [xt], [xt, mB[s_]])
                        return dict(xt=xt, hT32=hT32, t=t, i=i)

                def moe_A_back(gi, grp, cx):
                    HT, gates = HT2[gi % 2], gates2[gi % 2]
                    xt, hT32, t, i = cx["xt"], cx["hT32"], cx["t"], cx["i"]
                    if True:
                        for half in range(2):
                            ps = psum()
                            for kk in range(4):
                                kc = half * 4 + kk
                                fw.op(pe, lambda e, kc=kc, kk=kk, ps=ps, xt=xt: e.transpose(out=ps.ap[:, kk * 128:(kk + 1) * 128], in_=xt.ap[:, kc * 128:(kc + 1) * 128], identity=ident.ap), [ps], [xt, ident])
                            fw.op(dve, lambda e, ps=ps, hT32=hT32, half=half: e.tensor_copy(out=hT32.ap[:, half * 4:(half + 1) * 4, :], in_=ps.ap.rearrange("p (a b) -> p a b", a=4)), [hT32], [ps])
                            fw.op(act, lambda e, hT32=hT32, half=half, t=t: e.copy(out=HT.ap[:, half * 4:(half + 1) * 4, t * 128:(t + 1) * 128], in_=hT32.ap[:, half * 4:(half + 1) * 4, :]), [HT], [hT32])
                        ps = psum()
                        for kc in range(8):
                            fw.op(pe, lambda e, kc=kc, ps=ps, hT32=hT32: e.matmul(ps.ap[:, 0:NE], lhsT=hT32.ap[:, kc, :], rhs=rwt.ap[:, kc, :], start=(kc == 0), stop=(kc == 7)), [ps], [hT32, rwt])
                        rt = rt_pool.next()
                        R = rt.ap
                        sc_, bi_ = R[:, 0:16], R[:, 16:32]
                        g4 = lambda ap: ap.rearrange("p (g m) -> p g m", g=4)
                        fw.op(act, lambda e, ps=ps, sc_=sc_: e.activation(out=sc_, in_=ps.ap[:, 0:NE], func=AF.Sigmoid), [rt], [ps])
                        fw.op(dve, lambda e, sc_=sc_, bi_=bi_: e.tensor_tensor(out=bi_, in0=sc_, in1=rbt.ap, op=ALU.add), [rt], [rt, rbt])
                        a = [g4(bi_)[:, :, m] for m in range(4)]
                        hi01, lo01, hi23, lo23 = R[:, 32:36], R[:, 36:40], R[:, 40:44], R[:, 44:48]
                        top1, m1, m2, top2 = R[:, 48:52], R[:, 52:56], R[:, 56:60], R[:, 60:64]
                        gs_, gmx, gm_, rd = R[:, 64:68], R[:, 68:69], R[:, 72:76], R[:, 69:70]
                        sel = R[:, 80:96]
                        tt = lambda o, x0, x1, op: fw.op(dve, lambda e: e.tensor_tensor(out=o, in0=x0, in1=x1, op=op), [rt], [rt])
                        tt(hi01, a[0], a[1], ALU.max)
                        tt(lo01, a[0], a[1], ALU.min)
                        tt(hi23, a[2], a[3], ALU.max)
                        tt(lo23, a[2], a[3], ALU.min)
                        tt(top1, hi01, hi23, ALU.max)
                        tt(m1, hi01, hi23, ALU.min)
                        tt(m2, lo01, lo23, ALU.max)
                        tt(top2, m1, m2, ALU.max)
                        tt(gs_, top1, top2, ALU.add)
                        fw.op(dve, lambda e, gs_=gs_, gmx=gmx: e.reduce_max(out=gmx, in_=gs_, axis=AX.X), [rt], [rt])
                        fw.op(dve, lambda e, gs_=gs_, gmx=gmx, gm_=gm_: e.tensor_scalar(out=gm_, in0=gs_, scalar1=gmx, scalar2=None, op0=ALU.is_ge), [rt], [rt])
                        tt(g4(sel), g4(bi_), top2.unsqueeze(2).to_broadcast([128, 4, 4]), ALU.is_ge)
                        tt(g4(sel), g4(sel), gm_.unsqueeze(2).to_broadcast([128, 4, 4]), ALU.mult)
                        tt(sel, sel, sc_, ALU.mult)
                        fw.op(dve, lambda e, sel=sel, rd=rd: e.reduce_sum(out=rd, in_=sel, axis=AX.X), [rt], [rt])
                        fw.op(dve, lambda e, rd=rd: e.reciprocal(out=rd, in_=rd), [rt], [rt])
                        fw.op(dve, lambda e, sel=sel, rd=rd, t=t: e.tensor_scalar(out=gates.ap[:, t, :], in0=sel, scalar1=rd, scalar2=None, op0=ALU.mult), [gates], [rt])
                        if "gates" in dbg and l == 0:
                            fw.dma(dbg["gates"][i * 128:(i + 1) * 128, :], gates.ap[:, t, :], [], [gates])

                def moe_B(gi, grp, exs):
                    ntok = len(grp) * 128
                    HT, gates = HT2[gi % 2], gates2[gi % 2]
                    for ex in exs:
                        pre = hooks.get((gi, ex, 0))
                        cxh = pre() if pre else None
                        qi = gi * NE + ex
                        Wg, Wu = Wg2[qi % 2], Wu2[qi % 2]
                        load_w(Wd, e_down[l, ex])
                        if qi + 1 < len(seq):
                            nex = seq[qi + 1][1]
                            load_w(Wg2[(qi + 1) % 2], e_gate[l, nex])
                            load_w(Wu2[(qi + 1) % 2], e_up[l, nex])
                        for t0 in range(0, ntok, 512):
                            ntk = min(512, ntok - t0)
                            for fc in range(8):
                                psg = psum()
                                psu = psum()
                                for kc in range(8):
                                    fw.op(pe, lambda e, kc=kc, fc=fc, psg=psg, t0=t0, ntk=ntk: e.matmul(psg.ap[:, 0:ntk], lhsT=Wg.ap[:, kc, fc * 128:(fc + 1) * 128], rhs=HT.ap[:, kc, t0:t0 + ntk], start=(kc == 0), stop=(kc == 7)), [psg], [Wg, HT])
                                for kc in range(8):
                                    fw.op(pe, lambda e, kc=kc, fc=fc, psu=psu, t0=t0, ntk=ntk: e.matmul(psu.ap[:, 0:ntk], lhsT=Wu.ap[:, kc, fc * 128:(fc + 1) * 128], rhs=HT.ap[:, kc, t0:t0 + ntk], start=(kc == 0), stop=(kc == 7)), [psu], [Wu, HT])
                                sg = sg_pool.next()
                                fw.op(act, lambda e, psg=psg, sg=sg, ntk=ntk: e.activation(out=sg.ap[:, 0:ntk], in_=psg.ap[:, 0:ntk], func=AF.Silu), [sg], [psg])
                                fw.op(dve, lambda e, psu=psu, sg=sg, fc=fc, t0=t0, ntk=ntk: e.tensor_tensor(out=actb.ap[:, fc, t0:t0 + ntk], in0=sg.ap[:, 0:ntk], in1=psu.ap[:, 0:ntk], op=ALU.mult), [actb], [sg, psu])
                        for t in range(len(grp)):
                            for half in range(2):
                                hs = slice(half * 512, (half + 1) * 512)
                                ps = psum()
                                for fc in range(8):
                                    fw.op(pe, lambda e, fc=fc, ps=ps, t=t, hs=hs: e.matmul(ps.ap, lhsT=actb.ap[:, fc, t * 128:(t + 1) * 128], rhs=Wd.ap[:, fc, hs], start=(fc == 0), stop=(fc == 7)), [ps], [actb, Wd])
                                if ex == 0:
                                    fw.op(dve, lambda e, ps=ps, t=t, hs=hs, ex=ex: e.tensor_scalar(out=accs[t].ap[:, hs], in0=ps.ap, scalar1=gates.ap[:, t, ex:ex + 1], scalar2=None, op0=ALU.mult), [accs[t]], [ps, gates])
                                else:
                                    fw.op(dve, lambda e, ps=ps, t=t, hs=hs, ex=ex: e.scalar_tensor_tensor(out=accs[t].ap[:, hs], in0=ps.ap, scalar=gates.ap[:, t, ex:ex + 1], in1=accs[t].ap[:, hs], op0=ALU.mult, op1=ALU.add), [accs[t]], [ps, gates, accs[t]])
                        post = hooks.get((gi, ex, 1))
                        if post:
                            post(cxh)

                def moe_C(gi, grp):
                    ntok = len(grp) * 128
                    HT, gates = HT2[gi % 2], gates2[gi % 2]
                    for t, i in enumerate(grp):
                        s_ = 1 if i < NCT else 0
                        xt = x6_pool.next()
                        fw.dma(xt.ap, xs[i * 128:(i + 1) * 128, :], [xt], [xs_t[i]])
                        fw.op(dve, lambda e, t=t, s_=s_: e.tensor_tensor(out=accs[t].ap, in0=accs[t].ap, in1=mG[s_].ap, op=ALU.mult), [accs[t]], [accs[t], mG[s_]])
                        fw.op(dve, lambda e, xt=xt, t=t: e.tensor_tensor(out=xt.ap, in0=xt.ap, in1=accs[t].ap, op=ALU.add), [xt], [xt, accs[t]])
                        if "xout" in dbg and l == 0:
                            fw.dma(dbg["xout"][i * 128:(i + 1) * 128, :], xt.ap, [], [xt])
                        if not last:
                            fw.dma(xs[i * 128:(i + 1) * 128, :], xt.ap, [xs_t[i]], [xt])
                        else:
                            st = st6_pool.next()
                            rms(xt, st, accs[t])
                            fw.op(dve, lambda e, xt=xt, st=st: e.scalar_tensor_tensor(out=xt.ap, in0=xt.ap, scalar=st.ap[:, 2:3], in1=fgt.ap, op0=ALU.mult, op1=ALU.mult), [xt], [xt, st, fgt])
                            fw.dma(out[(i - NCT) * 128:(i - NCT + 1) * 128, :], xt.ap, [], [xt])

                hooks = {}
                for t in range(len(groups[0])):
                    moe_A_back(0, groups[0], moe_A_front(0, groups[0], t))
                for gi, grp in enumerate(groups):
                    if gi + 1 < len(groups):
                        ng = groups[gi + 1]
                        for t in range(len(ng)):
                            ex = 4 + t
                            hooks[(gi, ex, 0)] = (lambda gi=gi, ng=ng, t=t: moe_A_front(gi + 1, ng, t))
                            hooks[(gi, ex, 1)] = (lambda cx, gi=gi, ng=ng: moe_A_back(gi + 1, ng, cx))
                    moe_B(gi, grp, range(NE))
                    moe_C(gi, grp)
                fw.barrier()
            if stop_after == "S6":
                break
        fw.barrier()
    return nc


def prep_inputs(inputs):
    f = lambda a: np.ascontiguousarray(np.asarray(a, dtype=np.float32))
    shared = {}
    for k in ("w_mod", "b_mod", "norm_mix_g", "norm_ffn_g", "w_in", "w_out", "att_sink", "shift_mu_prev", "shift_mu_next",
              "decay_w0", "decay_w2", "iclr_a0", "iclr_a2", "vres_v0", "vres_v1", "vres_v2", "gate_g2", "k_k", "k_a",
              "ln_x_w", "ln_x_b", "router_w", "router_b", "expert_w_gate", "expert_w_up", "expert_w_down", "final_norm_g"):
        shared[k] = f(inputs[k])
    shared["r_k"] = f(inputs["r_k"]).reshape(DEPTH, RW)
    shared["ident"] = np.eye(128, dtype=np.float32)
    s = np.arange(128)[:, None]
    t = np.arange(128)[None, :]
    shared["masks"] = np.ascontiguousarray(np.stack([s < t, s <= t, s > t, s >= t], axis=1).astype(np.float32))
    pos = np.arange(L)
    row = (pos // 64).astype(np.float32)
    colp = (pos % 64).astype(np.float32)
    inv = (10000.0 ** (-np.arange(16, dtype=np.float32) / 16)).astype(np.float32)
    ang = np.concatenate([row[:, None] * inv, row[:, None] * inv, colp[:, None] * inv, colp[:, None] * inv], axis=1)
    cos = np.cos(ang).astype(np.float32)
    sin = np.sin(ang).astype(np.float32)
    sgn = np.concatenate([-np.ones(16), np.ones(16), -np.ones(16), np.ones(16)]).astype(np.float32)
    shared["rope_cos"] = np.ascontiguousarray(np.tile(cos, (1, 10)))
    shared["rope_sin"] = np.ascontiguousarray(np.tile(sin * sgn, (1, 10)))
    x = f(inputs["x"])
    ctx = f(inputs["ctx"])
    c = f(inputs["c"])
    cc = f(inputs["c_ctx"])
    maps = []
    for b in range(8):
        m = dict(shared)
        m["x"] = x[b]
        m["ctx"] = ctx[b]
        m["ccol"] = np.ascontiguousarray(np.concatenate([c[b].reshape(8, 128).T, cc.reshape(8, 128).T], axis=1))
        maps.append(m)
    return maps


def kernel(**inputs):
    maps = prep_inputs(inputs)
    nc = build()
    res = run_bass_kernel_spmd(nc, maps, core_ids=list(range(8)))
    return np.stack([r["out"] for r in res.results], axis=0).astype(np.float32)
```

```python
from contextlib import ExitStack
import os
import numpy as np
import ml_dtypes
import concourse.bass as bass
import concourse.mybir as mybir
from concourse.bass_utils import run_bass_kernel_spmd

F32 = mybir.dt.float32
BF16 = mybir.dt.bfloat16
AF = mybir.ActivationFunctionType
ALU = mybir.AluOpType
AX = mybir.AxisListType

D = 1024
L = 4096
C = 256
T = L + C
NT = T // 128
NCT = C // 128
DEPTH = 2
IN_COLS = 2688
RW = 512
RWC = 1920
NE = 16
ATT_SCALE = 0.125
NEG = -30000.0


class Eng:
    def __init__(self, fw, e, name, ring=0):
        self.fw = fw
        self.e = e
        self.name = name
        self.count = 0
        self.seen = {}
        self.sem = fw.nc.alloc_semaphore("s_" + name)
        self.ring = [fw.nc.alloc_semaphore("r_%s%d" % (name, i)) for i in range(ring)]
        self.ndma = 0

    def wait_tok(self, tok):
        sem, val, key = tok
        if self.seen.get(key, 0) >= val:
            return
        self.e.wait_ge(sem, val)
        self.seen[key] = val


class TT:
    def __init__(self, ap=None):
        self.ap = ap
        self.w = None
        self.r = {}


class FW:
    def __init__(self, nc):
        self.nc = nc
        self.pe = Eng(self, nc.tensor, "pe")
        self.act = Eng(self, nc.scalar, "act")
        self.dve = Eng(self, nc.vector, "dve")
        self.pool = Eng(self, nc.gpsimd, "pool", ring=12)
        self.sp = Eng(self, nc.sync, "sp", ring=24)
        self.engs = [self.pe, self.act, self.dve, self.pool, self.sp]
        self.ninst = 0

    def _deps(self, eng, outs, ins):
        for t in ins:
            if t.w is not None and not (eng is self.pe and t.w[2] == "pe"):
                eng.wait_tok(t.w)
        for t in outs:
            if t.w is not None and not (eng is self.pe and t.w[2] == "pe"):
                eng.wait_tok(t.w)
            for tok in t.r.values():
                if not (eng is self.pe and tok[2] == "pe"):
                    eng.wait_tok(tok)

    def _mark(self, tok, outs, ins):
        for t in ins:
            t.r[tok[2]] = tok
        for t in outs:
            t.w = tok
            t.r = {}

    def op(self, eng, fn, outs, ins):
        self._deps(eng, outs, ins)
        inst = fn(eng.e)
        eng.count += 1
        inst.then_inc(eng.sem, 1)
        self._mark((eng.sem, eng.count, eng.name), outs, ins)
        self.ninst += 1

    def dma(self, out_ap, in_ap, outs, ins, q=None):
        q = q or self.sp
        self._deps(q, outs, ins)
        K = len(q.ring)
        slot = q.ndma % K
        rnd = q.ndma // K
        key = "%s_r%d" % (q.name, slot)
        if rnd > 0:
            q.wait_tok((q.ring[slot], 16 * rnd, key))
        q.e.dma_start(out=out_ap, in_=in_ap).then_inc(q.ring[slot], 16)
        q.ndma += 1
        self._mark((q.ring[slot], 16 * (rnd + 1), key), outs, ins)
        self.ninst += 1

    def all_toks(self):
        toks = []
        for e in self.engs:
            if e.count:
                toks.append((e.sem, e.count, e.name))
            K = len(e.ring)
            for slot in range(min(K, e.ndma)):
                n = (e.ndma - 1 - slot) // K + 1
                toks.append((e.ring[slot], 16 * n, "%s_r%d" % (e.name, slot)))
        return toks

    def barrier(self):
        toks = self.all_toks()
        for e in self.engs:
            for tok in toks:
                if e is self.pe and tok[2] == "pe":
                    continue
                e.wait_tok(tok)


_uid = [0]


def _nm(name):
    _uid[0] += 1
    return "sb_%s_%d" % (name, _uid[0])


class Pool:
    def __init__(self, es, nc, name, shape, dtype, bufs):
        self.t = [TT(es.enter_context(nc.sbuf_tensor(_nm(name), shape, dtype)).ap()) for i in range(bufs)]
        self.i = 0

    def next(self):
        t = self.t[self.i % len(self.t)]
        self.i += 1
        return t


def sb(es, nc, name, shape, dtype):
    return TT(es.enter_context(nc.sbuf_tensor(_nm(name), shape, dtype)).ap())


def build(debug_outs=(), stop_after=None):
    nc = bass.Bass("TRN2", target_bir_lowering=False)
    fw = FW(nc)
    pe, act, dve, pool, sp = fw.pe, fw.act, fw.dve, fw.pool, fw.sp

    def din(name, shape, dt=F32):
        return nc.dram_tensor(name, list(shape), dt, kind="ExternalInput").ap()

    def dscr(name, shape, dt=F32):
        return nc.dram_tensor(name, list(shape), dt, kind="Internal").ap()

    x_in = din("x", [L, D])
    ctx_in = din("ctx", [C, D])
    ccol_in = din("ccol", [128, 16])
    w_mod = din("w_mod", [DEPTH, D, 6 * D])
    b_mod = din("b_mod", [DEPTH, 6 * D])
    norm_mix_g = din("norm_mix_g", [DEPTH, D])
    norm_ffn_g = din("norm_ffn_g", [DEPTH, D])
    w_in = din("w_in", [DEPTH, D, IN_COLS])
    w_out = din("w_out", [DEPTH, D, D])
    att_sink = din("att_sink", [DEPTH, 8])
    mu_prev = din("shift_mu_prev", [DEPTH, RWC])
    mu_next = din("shift_mu_next", [DEPTH, RWC])
    decay_w0 = din("decay_w0", [DEPTH, 2, RW])
    decay_w2 = din("decay_w2", [DEPTH, 2, 64, RW])
    iclr_a0 = din("iclr_a0", [DEPTH, 2, RW])
    iclr_a2 = din("iclr_a2", [DEPTH, 2, 64, RW])
    vres_v0 = din("vres_v0", [1, RW])
    vres_v1 = din("vres_v1", [1, RW, 32])
    vres_v2 = din("vres_v2", [1, 32, RW])
    gate_g2 = din("gate_g2", [DEPTH, 128, RW])
    k_k = din("k_k", [DEPTH, RW])
    k_a = din("k_a", [DEPTH, RW])
    r_k = din("r_k", [DEPTH, RW])
    ln_x_w = din("ln_x_w", [DEPTH, RW])
    ln_x_b = din("ln_x_b", [DEPTH, RW])
    router_w = din("router_w", [D, NE])
    router_b = din("router_b", [NE])
    e_gate = din("expert_w_gate", [DEPTH, NE, D, D])
    e_up = din("expert_w_up", [DEPTH, NE, D, D])
    e_down = din("expert_w_down", [DEPTH, NE, D, D])
    final_g = din("final_norm_g", [D])
    ident_in = din("ident", [128, 128])
    masks_in = din("masks", [128, 4, 128])
    rope_cos = din("rope_cos", [L, 640])
    rope_sin = din("rope_sin", [L, 640])
    out = nc.dram_tensor("out", [L, D], F32, kind="ExternalOutput").ap()

    xs = dscr("xs", [T, D])
    xs_t = [TT() for _ in range(NT)]
    modbuf = dscr("modbuf", [DEPTH, 12, 128, D])
    mod_t = [[TT() for _ in range(12)] for _ in range(DEPTH)]
    u_scr = dscr("u_scr", [T + 4, RWC])
    u_t = [TT() for _ in range(NT)]
    u_guard = TT()
    mix = dscr("mix", [T, D])
    mix_a = [TT() for _ in range(NT)]
    mix_r = [TT() for _ in range(NT)]
    vfirst = dscr("vfirst", [T, RW])
    vf_t = [TT() for _ in range(NT)]
    feat = dscr("feat", [T, 5632])
    feat_t = [TT() for _ in range(NT)]
    y_scr = dscr("y_scr", [2, T, RW])
    y_t = [[TT() for _ in range(NT)] for _ in range(2)]
    dbg = {}
    for name, shape in debug_outs:
        dbg[name] = nc.dram_tensor("dbg_" + name, list(shape), F32, kind="ExternalOutput").ap()

    def urow(i):
        return 1 + i * 128 if i < NCT else 3 + i * 128

    PS = [TT(nc.alloc_psum_tensor("psb%d" % i, [128, 512], F32).ap()) for i in range(8)]
    psi = [0]

    def psum():
        t = PS[psi[0] % 6]
        psi[0] += 1
        return t

    with ExitStack() as g_es:
        ident = sb(g_es, nc, "ident", [128, 128], F32)
        identb = sb(g_es, nc, "identb", [128, 128], BF16)
        masks = sb(g_es, nc, "masks", [128, 4, 128], F32)
        fw.dma(ident.ap, ident_in, [ident], [])
        fw.dma(masks.ap, masks_in, [masks], [])
        fw.op(dve, lambda e: e.tensor_copy(out=identb.ap, in_=ident.ap), [identb], [ident])
        with ExitStack() as es0:
            zrow = sb(es0, nc, "zrow", [1, RWC], F32)
            fw.op(dve, lambda e: e.memset(zrow.ap, 0.0), [zrow], [])
            for r in (0, C + 1, C + 2, T + 3):
                fw.dma(u_scr[r:r + 1, :], zrow.ap, [u_guard], [zrow])
            fw.barrier()

        for i in range(NT):
            src = ctx_in[i * 128:(i + 1) * 128, :] if i < NCT else x_in[(i - NCT) * 128:(i - NCT + 1) * 128, :]
            fw.dma(xs[i * 128:(i + 1) * 128, :], src, [xs_t[i]], [])

        for l in range(DEPTH):
            last = l == DEPTH - 1
            with ExitStack() as es:
                ccol = sb(es, nc, "ccol", [128, 16], F32)
                csil = sb(es, nc, "csil", [128, 16], F32)
                crep = sb(es, nc, "crep", [128, 16, 128], F32)
                fw.dma(ccol.ap, ccol_in, [ccol], [])
                fw.op(act, lambda e: e.activation(out=csil.ap, in_=ccol.ap, func=AF.Silu), [csil], [ccol])
                fw.op(dve, lambda e: e.tensor_copy(out=crep.ap, in_=csil.ap.unsqueeze(2).to_broadcast([128, 16, 128])), [crep], [csil])
                wm_pool = Pool(es, nc, "wm", [128, 8, 512], F32, 2)
                bm_pool = Pool(es, nc, "bm", [128, 512], F32, 2)
                modraw = [[sb(es, nc, "mr%d_%d" % (s, j), [128, D], F32) for j in range(6)] for s in range(2)]
                gmix = sb(es, nc, "gmix", [128, D], F32)
                gffn = sb(es, nc, "gffn", [128, D], F32)
                fw.dma(gmix.ap, norm_mix_g[l].partition_broadcast(128), [gmix], [])
                fw.dma(gffn.ap, norm_ffn_g[l].partition_broadcast(128), [gffn], [])
                for j in range(12):
                    wm = wm_pool.next()
                    bm = bm_pool.next()
                    fw.dma(wm.ap, w_mod[l][:, j * 512:(j + 1) * 512].rearrange("(kc p) n -> p kc n", p=128), [wm], [])
                    fw.dma(bm.ap, b_mod[l][j * 512:(j + 1) * 512].partition_broadcast(128), [bm], [])
                    for s in range(2):
                        ps = psum()
                        for kc in range(8):
                            fw.op(pe, lambda e, kc=kc, s=s, ps=ps, wm=wm: e.matmul(ps.ap, lhsT=crep.ap[:, s * 8 + kc, :], rhs=wm.ap[:, kc, :], start=(kc == 0), stop=(kc == 7)), [ps], [crep, wm])
                        dst = modraw[s][j // 2]
                        fw.op(dve, lambda e, ps=ps, dst=dst, bm=bm, j=j: e.tensor_tensor(out=dst.ap[:, (j % 2) * 512:(j % 2 + 1) * 512], in0=ps.ap, in1=bm.ap, op=ALU.add), [dst], [ps, bm])
                for s in range(2):
                    sh_a, sc_a, ga, sh_f, sc_f, gf = modraw[s]
                    fw.op(dve, lambda e, sc_a=sc_a: e.scalar_tensor_tensor(out=sc_a.ap, in0=sc_a.ap, scalar=1.0, in1=gmix.ap, op0=ALU.add, op1=ALU.mult), [sc_a], [sc_a, gmix])
                    fw.op(dve, lambda e, sc_f=sc_f: e.scalar_tensor_tensor(out=sc_f.ap, in0=sc_f.ap, scalar=1.0, in1=gffn.ap, op0=ALU.add, op1=ALU.mult), [sc_f], [sc_f, gffn])
                    for idx, t in enumerate((sc_a, sh_a, ga, sc_f, sh_f, gf)):
                        fw.dma(modbuf[l, s * 6 + idx], t.ap, [mod_t[l][s * 6 + idx]], [t])
                fw.barrier()
            if stop_after == "S1":
                break

            with ExitStack() as es_att:
                QT = sb(es_att, nc, "QT", [128, 4, T], BF16)
                KT = sb(es_att, nc, "KT", [128, T], BF16)
                VV = sb(es_att, nc, "VV", [128, NT, 128], BF16)
                qt_t = [TT() for _ in range(NT)]
                with ExitStack() as es:
                    winb = sb(es, nc, "winb", [128, 8, IN_COLS], BF16)
                    wst_pool = Pool(es, nc, "wst", [128, 8, 384], F32, 2)
                    for j in range(7):
                        wst = wst_pool.next()
                        fw.dma(wst.ap, w_in[l][:, j * 384:(j + 1) * 384].rearrange("(kc p) n -> p kc n", p=128), [wst], [])
                        fw.op(pool, lambda e, wst=wst, j=j: e.tensor_copy(out=winb.ap[:, :, j * 384:(j + 1) * 384], in_=wst.ap), [winb], [wst])
                    modA = [sb(es, nc, "modA%d" % s, [128, D], F32) for s in range(2)]
                    modB = [sb(es, nc, "modB%d" % s, [128, D], F32) for s in range(2)]
                    for s in range(2):
                        fw.dma(modA[s].ap, modbuf[l, s * 6 + 0], [modA[s]], [mod_t[l][s * 6 + 0]])
                        fw.dma(modB[s].ap, modbuf[l, s * 6 + 1], [modB[s]], [mod_t[l][s * 6 + 1]])
                    x_pool = Pool(es, nc, "xt", [128, D], F32, 2)
                    junk = sb(es, nc, "junk", [128, D], F32)
                    st_pool = Pool(es, nc, "st", [128, 4], F32, 2)
                    h_pool = Pool(es, nc, "ht", [128, D], BF16, 2)
                    hT_pool = Pool(es, nc, "hT", [128, 8, 128], BF16, 2)
                    pr_pool = Pool(es, nc, "pr", [128, IN_COLS], F32, 2)
                    cos_pool = Pool(es, nc, "cos", [128, 640], F32, 2)
                    sin_pool = Pool(es, nc, "sin", [128, 640], F32, 2)
                    rp_pool = Pool(es, nc, "rp", [128, 640], F32, 2)
                    rq_pool = Pool(es, nc, "rq", [128, 640], BF16, 2)

                    def s2_load(i):
                        xt = x_pool.next()
                        fw.dma(xt.ap, xs[i * 128:(i + 1) * 128, :], [xt], [xs_t[i]])
                        cs = sn = None
                        if i >= NCT:
                            cs = cos_pool.next()
                            sn = sin_pool.next()
                            r0 = (i - NCT) * 128
                            fw.dma(cs.ap, rope_cos[r0:r0 + 128, :], [cs], [])
                            fw.dma(sn.ap, rope_sin[r0:r0 + 128, :], [sn], [])
                        return xt, cs, sn

                    nxt = s2_load(0)
                    for i in range(NT):
                        xt, cs, sn = nxt
                        if i + 1 < NT:
                            nxt = s2_load(i + 1)
                        s = 1 if i < NCT else 0
                        st = st_pool.next()
                        fw.op(act, lambda e, xt=xt, st=st: e.activation(out=junk.ap, in_=xt.ap, func=AF.Square, accum_out=st.ap[:, 0:1]), [junk, st], [xt])
                        fw.op(dve, lambda e, st=st: e.tensor_scalar(out=st.ap[:, 1:2], in0=st.ap[:, 0:1], scalar1=1.0 / D, scalar2=1e-6, op0=ALU.mult, op1=ALU.add), [st], [st])
                        fw.op(act, lambda e, st=st: e.activation(out=st.ap[:, 3:4], in_=st.ap[:, 1:2], func=AF.Sqrt), [st], [st])
                        fw.op(dve, lambda e, st=st: e.reciprocal(out=st.ap[:, 2:3], in_=st.ap[:, 3:4]), [st], [st])
                        fw.op(dve, lambda e, xt=xt, st=st, s=s: e.scalar_tensor_tensor(out=xt.ap, in0=xt.ap, scalar=st.ap[:, 2:3], in1=modA[s].ap, op0=ALU.mult, op1=ALU.mult), [xt], [xt, st, modA[s]])
                        ht = h_pool.next()
                        fw.op(dve, lambda e, xt=xt, ht=ht, s=s: e.tensor_tensor(out=ht.ap, in0=xt.ap, in1=modB[s].ap, op=ALU.add), [ht], [xt, modB[s]])
                        hT = hT_pool.next()
                        for half in range(2):
                            ps = psum()
                            psb = ps.ap.bitcast(BF16)
                            for kk in range(4):
                                kc = half * 4 + kk
                                fw.op(pe, lambda e, kc=kc, kk=kk, psb=psb, ht=ht: e.transpose(out=psb[:, kk * 128:(kk + 1) * 128], in_=ht.ap[:, kc * 128:(kc + 1) * 128], identity=identb.ap), [ps], [ht, identb])
                            fw.op(act if half else dve, lambda e, psb=psb, hT=hT, half=half: (e.copy if half else e.tensor_copy)(out=hT.ap[:, half * 4:(half + 1) * 4, :], in_=psb[:, 0:512].rearrange("p (a b) -> p a b", a=4)), [hT], [ps])
                        pr = pr_pool.next()
                        col = 0
                        ci = 0
                        while col < IN_COLS:
                            n = min(512, IN_COLS - col)
                            ps = psum()
                            for kc in range(8):
                                fw.op(pe, lambda e, kc=kc, ps=ps, hT=hT, col=col, n=n: e.matmul(ps.ap[:, 0:n], lhsT=hT.ap[:, kc, :], rhs=winb.ap[:, kc, col:col + n], start=(kc == 0), stop=(kc == 7)), [ps], [hT, winb])
                            if ci % 2 == 0:
                                fw.op(act, lambda e, ps=ps, pr=pr, col=col, n=n: e.copy(out=pr.ap[:, col:col + n], in_=ps.ap[:, 0:n]), [pr], [ps])
                            else:
                                fw.op(dve, lambda e, ps=ps, pr=pr, col=col, n=n: e.tensor_copy(out=pr.ap[:, col:col + n], in_=ps.ap[:, 0:n]), [pr], [ps])
                            col += n
                            ci += 1
                        if "proj" in dbg and l == 0:
                            fw.dma(dbg["proj"][i * 128:(i + 1) * 128, :], pr.ap, [], [pr])
                        fw.dma(u_scr[urow(i):urow(i) + 128, :], pr.ap[:, 768:IN_COLS], [u_t[i]], [pr])
                        rq = rq_pool.next()
                        if i >= NCT:
                            rp = rp_pool.next()
                            v5 = lambda ap: ap.rearrange("p (m h f) -> p m h f", h=2, f=16)
                            fw.op(dve, lambda e, rp=rp, pr=pr, sn=sn: e.tensor_tensor(out=v5(rp.ap)[:, :, 0, :], in0=v5(pr.ap[:, 0:640])[:, :, 1, :], in1=v5(sn.ap)[:, :, 0, :], op=ALU.mult), [rp], [pr, sn])
                            fw.op(dve, lambda e, rp=rp, pr=pr, sn=sn: e.tensor_tensor(out=v5(rp.ap)[:, :, 1, :], in0=v5(pr.ap[:, 0:640])[:, :, 0, :], in1=v5(sn.ap)[:, :, 1, :], op=ALU.mult), [rp], [pr, sn])
                            fw.op(pool, lambda e, cs=cs, pr=pr: e.tensor_tensor(out=cs.ap, in0=pr.ap[:, 0:640], in1=cs.ap, op=ALU.mult), [cs], [pr, cs])
                            gq = lambda ap: ap.rearrange("p (g j d) -> p g j d", g=2, j=4)
                            jq = lambda ap: ap.rearrange("p (j g d) -> p g j d", g=2, j=4)
                            fw.op(dve, lambda e, rq=rq, rp=rp, cs=cs: e.tensor_tensor(out=jq(rq.ap[:, 0:512]), in0=gq(rp.ap[:, 0:512]), in1=gq(cs.ap[:, 0:512]), op=ALU.add), [rq], [rp, cs])
                            fw.op(dve, lambda e, rq=rq, rp=rp, cs=cs: e.tensor_tensor(out=rq.ap[:, 512:640], in0=rp.ap[:, 512:640], in1=cs.ap[:, 512:640], op=ALU.add), [rq], [rp, cs])
                        else:
                            gq = lambda ap: ap.rearrange("p (g j d) -> p g j d", g=2, j=4)
                            jq = lambda ap: ap.rearrange("p (j g d) -> p g j d", g=2, j=4)
                            fw.op(dve, lambda e, rq=rq, pr=pr: e.tensor_copy(out=jq(rq.ap[:, 0:512]), in_=gq(pr.ap[:, 0:512])), [rq], [pr])
                            fw.op(dve, lambda e, rq=rq, pr=pr: e.tensor_copy(out=rq.ap[:, 512:640], in_=pr.ap[:, 512:640]), [rq], [pr])
                        fw.op(act, lambda e, pr=pr, i=i: e.copy(out=VV.ap[:, i, :], in_=pr.ap[:, 640:768]), [qt_t[i]], [pr])
                        ps = psum()
                        psb = ps.ap.bitcast(BF16)
                        for j in range(4):
                            fw.op(pe, lambda e, j=j, psb=psb, rq=rq: e.transpose(out=psb[:, j * 128:(j + 1) * 128], in_=rq.ap[:, j * 128:(j + 1) * 128], identity=identb.ap), [ps], [rq, identb])
                        fw.op(pe, lambda e, psb=psb, rq=rq: e.transpose(out=psb[:, 512:640], in_=rq.ap[:, 512:640], identity=identb.ap), [ps], [rq, identb])
                        fw.op(dve, lambda e, psb=psb, i=i: e.tensor_copy(out=QT.ap[:, :, i * 128:(i + 1) * 128], in_=psb[:, 0:512].rearrange("p (j t) -> p j t", j=4)), [qt_t[i]], [ps])
                        fw.op(act, lambda e, psb=psb, i=i: e.copy(out=KT.ap[:, i * 128:(i + 1) * 128], in_=psb[:, 512:640]), [qt_t[i]], [ps])
                    fw.barrier()
                if stop_after == "S2":
                    break
                with ExitStack() as es:
                    sinkb = sb(es, nc, "sinkb", [128, 8], F32)
                    fw.dma(sinkb.ap, att_sink[l].partition_broadcast(128), [sinkb], [])
                    mb = sb(es, nc, "mb", [128, 384], F32)
                    fw.op(dve, lambda e: e.memset(mb.ap, 0.0), [mb], [])
                    fw.op(dve, lambda e: e.tensor_scalar(out=mb.ap[:, 0:128], in0=masks.ap[:, 1, :], scalar1=-1.0, scalar2=-NEG, op0=ALU.add, op1=ALU.mult), [mb], [masks])
                    fw.op(dve, lambda e: e.tensor_scalar(out=mb.ap[:, 256:384], in0=masks.ap[:, 3, :], scalar1=-1.0, scalar2=-NEG, op0=ALU.add, op1=ALU.mult), [mb], [masks])
                    s_pool = Pool(es, nc, "ssb", [128, 640], F32, 3)
                    p_pool = Pool(es, nc, "psb", [128, 640], BF16, 4)
                    pT_pool = Pool(es, nc, "pTs", [128, 640], BF16, 2)
                    ast_pool = Pool(es, nc, "ast", [128, 8], F32, 6)
                    mixa_pool = Pool(es, nc, "mixa", [128, 512], F32, 3)
                    tiles3 = list(range(NCT, NT) if last else range(NT))
                    tinfo = {}
                    for i in tiles3:
                        if i < NCT:
                            nloc, lo, hi, moff = 0, 0, 0, 0
                            kblocks = list(range(NCT))
                        else:
                            lo, hi = max(NCT, i - 1), min(NT - 1, i + 1)
                            nloc = (hi - lo + 1) * 128
                            moff = 0 if lo == i - 1 else 128
                            kblocks = list(range(lo, hi + 1)) + list(range(NCT))
                        tinfo[i] = dict(nloc=nloc, lo=lo, hi=hi, moff=moff, kblocks=kblocks, ncols=nloc + C, mixa=None, ops=PS[6 + (i % 2)])

                    def att_front(i, h):
                        ti = tinfo[i]
                        nloc, lo, hi, moff, ncols = ti["nloc"], ti["lo"], ti["hi"], ti["moff"], ti["ncols"]
                        if ti["mixa"] is None:
                            ti["mixa"] = mixa_pool.next()
                        g, j = h // 4, h % 4
                        lhsT = QT.ap[g * 64:(g + 1) * 64, j, i * 128:(i + 1) * 128]
                        ssb = s_pool.next()
                        st = ast_pool.next()
                        if nloc:
                            ps1 = psum()
                            fw.op(pe, lambda e: e.matmul(ps1.ap[:, 0:nloc], lhsT=lhsT, rhs=KT.ap[g * 64:(g + 1) * 64, lo * 128:(hi + 1) * 128], start=True, stop=True), [ps1], [qt_t[i]])
                            fw.op(dve, lambda e: e.scalar_tensor_tensor(out=ssb.ap[:, 0:nloc], in0=ps1.ap[:, 0:nloc], scalar=ATT_SCALE, in1=mb.ap[:, moff:moff + nloc], op0=ALU.mult, op1=ALU.add), [ssb], [ps1, mb])
                        ps2 = psum()
                        fw.op(pe, lambda e: e.matmul(ps2.ap[:, 0:C], lhsT=lhsT, rhs=KT.ap[g * 64:(g + 1) * 64, 0:C], start=True, stop=True), [ps2], [qt_t[i]])
                        fw.op(act, lambda e: e.activation(out=ssb.ap[:, nloc:ncols], in_=ps2.ap[:, 0:C], func=AF.Copy, scale=ATT_SCALE), [ssb], [ps2])
                        fw.op(dve, lambda e: e.reduce_max(out=st.ap[:, 0:1], in_=ssb.ap[:, 0:ncols], axis=AX.X), [st], [ssb])
                        fw.op(dve, lambda e: e.tensor_scalar(out=st.ap[:, 1:2], in0=st.ap[:, 0:1], scalar1=sinkb.ap[:, h:h + 1], scalar2=-1.0, op0=ALU.max, op1=ALU.mult), [st], [st, sinkb])
                        pb = p_pool.next()
                        fw.op(act, lambda e: e.activation(out=pb.ap[:, 0:ncols], in_=ssb.ap[:, 0:ncols], func=AF.Exp, bias=st.ap[:, 1:2], accum_out=st.ap[:, 2:3]), [pb, st], [ssb, st])
                        fw.op(act, lambda e: e.activation(out=st.ap[:, 3:4], in_=sinkb.ap[:, h:h + 1], func=AF.Exp, bias=st.ap[:, 1:2]), [st], [st, sinkb])
                        fw.op(dve, lambda e: e.tensor_tensor(out=st.ap[:, 4:5], in0=st.ap[:, 2:3], in1=st.ap[:, 3:4], op=ALU.add), [st], [st])
                        fw.op(dve, lambda e: e.reciprocal(out=st.ap[:, 5:6], in_=st.ap[:, 4:5]), [st], [st])
                        return dict(i=i, h=h, g=g, pb=pb, st=st)

                    def att_back(cx):
                        i, h, g, pb, st = cx["i"], cx["h"], cx["g"], cx["pb"], cx["st"]
                        ti = tinfo[i]
                        ncols, kblocks, mixa, ops = ti["ncols"], ti["kblocks"], ti["mixa"], ti["ops"]
                        ps3 = psum()
                        ps3b = ps3.ap.bitcast(BF16)
                        nblk = ncols // 128
                        for b in range(nblk):
                            fw.op(pe, lambda e, b=b: e.transpose(out=ps3b[:, b * 128:(b + 1) * 128], in_=pb.ap[:, b * 128:(b + 1) * 128], identity=identb.ap), [ps3], [pb, identb])
                        pT = pT_pool.next()
                        if h % 2 == 0:
                            fw.op(dve, lambda e: e.tensor_copy(out=pT.ap[:, 0:ncols], in_=ps3b[:, 0:ncols]), [pT], [ps3])
                        else:
                            fw.op(act, lambda e: e.copy(out=pT.ap[:, 0:ncols], in_=ps3b[:, 0:ncols]), [pT], [ps3])
                        nb = len(kblocks)
                        for b, kb in enumerate(kblocks):
                            fw.op(pe, lambda e, b=b, kb=kb: e.matmul(ops.ap[:, h * 64:(h + 1) * 64], lhsT=pT.ap[:, b * 128:(b + 1) * 128], rhs=VV.ap[:, kb, g * 64:(g + 1) * 64], start=(b == 0), stop=(b == nb - 1)), [ops], [pT, qt_t[kb]])
                        fw.op(act, lambda e: e.activation(out=mixa.ap[:, h * 64:(h + 1) * 64], in_=ops.ap[:, h * 64:(h + 1) * 64], func=AF.Copy, scale=st.ap[:, 5:6]), [mixa], [ops, st])
                        if h == 7:
                            fw.dma(mix[i * 128:(i + 1) * 128, 0:512], mixa.ap, [mix_a[i]], [mixa])

                    units = [(i, h) for i in tiles3 for h in range(8)]
                    DEPTH_P = 2
                    q_ = [att_front(*units[k]) for k in range(min(DEPTH_P, len(units)))]
                    for n in range(len(units)):
                        if n + DEPTH_P < len(units):
                            q_.append(att_front(*units[n + DEPTH_P]))
                        att_back(q_.pop(0))
                    fw.barrier()
            if stop_after == "S3":
                if "mix" in dbg:
                    fw.barrier()
                    for i in range(NT):
                        fw.dma(dbg["mix"][i * 128:(i + 1) * 128, :], mix[i * 128:(i + 1) * 128, :], [], [mix_a[i], mix_r[i]])
                break
            FV, FR, FG, FKK, FBON = 0, 512, 1024, 1536, 2048
            FLW = lambda d_: 2560 + d_ * 1536
            FB = lambda d_: 2560 + d_ * 1536 + 512
            FKD = lambda d_: 2560 + d_ * 1536 + 1024
            NF = 5632
            with ExitStack() as es:
                def bc(name, src, n):
                    t_ = sb(es, nc, name, [128, n], F32)
                    fw.dma(t_.ap, src.partition_broadcast(128), [t_], [])
                    return t_
                mup = bc("mup", mu_prev[l], RWC)
                mun = bc("mun", mu_next[l], RWC)
                w0b = bc("w0b", decay_w0[l].rearrange("d c -> (d c)"), 1024)
                a0b = bc("a0b", iclr_a0[l].rearrange("d c -> (d c)"), 1024)
                kkb = bc("kkb", k_k[l], RW)
                kab = bc("kab", k_a[l], RW)
                rkb = bc("rkb", r_k[l], RW)
                w2t = sb(es, nc, "w2t", [128, RW], F32)
                a2t = sb(es, nc, "a2t", [128, RW], F32)
                g2t = sb(es, nc, "g2t", [128, RW], F32)
                fw.dma(w2t.ap, decay_w2[l].rearrange("d r c -> (d r) c"), [w2t], [])
                fw.dma(a2t.ap, iclr_a2[l].rearrange("d r c -> (d r) c"), [a2t], [])
                fw.dma(g2t.ap, gate_g2[l], [g2t], [])
                if l > 0:
                    v0b = bc("v0b", vres_v0[0], RW)
                    v1t = sb(es, nc, "v1t", [128, 4, 32], F32)
                    fw.dma(v1t.ap, vres_v1[0].rearrange("(c p) n -> p c n", p=128), [v1t], [])
                    v2t = sb(es, nc, "v2t", [32, RW], F32)
                    fw.dma(v2t.ap, vres_v2[0], [v2t], [])
                uc_pool = Pool(es, nc, "uc", [128, RWC], F32, 2)
                up_pool = Pool(es, nc, "up", [128, RWC], F32, 2)
                un_pool = Pool(es, nc, "un", [128, RWC], F32, 2)
                vf_pool = Pool(es, nc, "vf", [128, RW], F32, 2)
                li_pool = Pool(es, nc, "li", [128, 384], F32, 2)
                liT_pool = Pool(es, nc, "liT", [128, 3, 128], F32, 2)
                fo_pool = Pool(es, nc, "fo", [128, NF], F32, 2)
                tmp_pool = Pool(es, nc, "ftmp", [128, RW], F32, 6)
                a_pool = Pool(es, nc, "fa", [128, RW], F32, 4)
                sm_pool = Pool(es, nc, "fsm", [128, 64], F32, 2)
                vT_pool = Pool(es, nc, "fvT", [128, 4, 128], F32, 2)
                h3 = lambda ap: ap.rearrange("p (h d) -> p h d", h=8)

                def s4a_load(i):
                    uc, up, un = uc_pool.next(), up_pool.next(), un_pool.next()
                    deps = [u_t[i], u_guard] + ([u_t[i - 1]] if i > 0 else []) + ([u_t[i + 1]] if i + 1 < NT else [])
                    r0 = urow(i)
                    fw.dma(uc.ap, u_scr[r0:r0 + 128, :], [uc], deps)
                    fw.dma(up.ap, u_scr[r0 - 1:r0 + 127, :], [up], deps)
                    fw.dma(un.ap, u_scr[r0 + 1:r0 + 129, :], [un], deps)
                    vf = None
                    if l > 0:
                        vf = vf_pool.next()
                        fw.dma(vf.ap, vfirst[i * 128:(i + 1) * 128, :], [vf], [vf_t[i]])
                    return uc, up, un, vf

                nxt = s4a_load(0)
                for i in range(NT):
                    uc, up, un, vf = nxt
                    if i + 1 < NT:
                        nxt = s4a_load(i + 1)
                    fw.op(dve, lambda e: e.tensor_tensor(out=up.ap, in0=up.ap, in1=uc.ap, op=ALU.subtract), [up], [up, uc])
                    fw.op(pool, lambda e: e.tensor_tensor(out=un.ap, in0=un.ap, in1=uc.ap, op=ALU.subtract), [un], [un, uc])
                    fw.op(dve, lambda e: e.tensor_tensor(out=up.ap, in0=up.ap, in1=mup.ap, op=ALU.mult), [up], [up, mup])
                    fw.op(pool, lambda e: e.tensor_tensor(out=un.ap, in0=un.ap, in1=mun.ap, op=ALU.mult), [un], [un, mun])
                    fw.op(dve, lambda e: e.tensor_tensor(out=uc.ap, in0=uc.ap, in1=up.ap, op=ALU.add), [uc], [uc, up])
                    fw.op(dve, lambda e: e.tensor_tensor(out=uc.ap, in0=uc.ap, in1=un.ap, op=ALU.add), [uc], [uc, un])
                    U = uc.ap
                    r_, k_, v_ = U[:, 0:512], U[:, 512:1024], U[:, 1024:1536]
                    li = li_pool.next()
                    fw.op(act, lambda e: e.activation(out=li.ap[:, 0:128], in_=U[:, 1536:1664], func=AF.Tanh), [li], [uc])
                    fw.op(act, lambda e: e.copy(out=li.ap[:, 128:256], in_=U[:, 1664:1792]), [li], [uc])
                    fw.op(act, lambda e: e.activation(out=li.ap[:, 256:384], in_=U[:, 1792:1920], func=AF.Sigmoid), [li], [uc])
                    ps = psum()
                    for q in range(3):
                        fw.op(pe, lambda e, q=q, ps=ps: e.transpose(out=ps.ap[:, q * 128:(q + 1) * 128], in_=li.ap[:, q * 128:(q + 1) * 128], identity=ident.ap), [ps], [li, ident])
                    liT = liT_pool.next()
                    fw.op(dve, lambda e, ps=ps: e.tensor_copy(out=liT.ap, in_=ps.ap[:, 0:384].rearrange("p (a b) -> p a b", a=3)), [liT], [ps])
                    fo = fo_pool.next()
                    FO = fo.ap
                    a_d = []
                    for d_ in range(2):
                        hp = slice(d_ * 64, (d_ + 1) * 64)
                        ps = psum()
                        fw.op(pe, lambda e, ps=ps, hp=hp: e.matmul(ps.ap, lhsT=liT.ap[hp, 0, :], rhs=w2t.ap[hp, :], start=True, stop=True), [ps], [liT, w2t])
                        t1 = tmp_pool.next()
                        fw.op(dve, lambda e, ps=ps, t1=t1, d_=d_: e.tensor_tensor(out=t1.ap, in0=ps.ap, in1=w0b.ap[:, d_ * 512:(d_ + 1) * 512], op=ALU.add), [t1], [ps, w0b])
                        fw.op(act, lambda e, t1=t1: e.activation(out=t1.ap, in_=t1.ap, func=AF.Sigmoid), [t1], [t1])
                        fw.op(act, lambda e, t1=t1, d_=d_: e.activation(out=FO[:, FLW(d_):FLW(d_) + 512], in_=t1.ap, func=AF.Copy, scale=-0.6065306597126334), [fo], [t1])
                        ps = psum()
                        fw.op(pe, lambda e, ps=ps, hp=hp: e.matmul(ps.ap, lhsT=liT.ap[hp, 1, :], rhs=a2t.ap[hp, :], start=True, stop=True), [ps], [liT, a2t])
                        ad = a_pool.next()
                        fw.op(dve, lambda e, ps=ps, ad=ad, d_=d_: e.tensor_tensor(out=ad.ap, in0=ps.ap, in1=a0b.ap[:, d_ * 512:(d_ + 1) * 512], op=ALU.add), [ad], [ps, a0b])
                        fw.op(act, lambda e, ad=ad: e.activation(out=ad.ap, in_=ad.ap, func=AF.Sigmoid), [ad], [ad])
                        a_d.append(ad)
                    ps = psum()
                    fw.op(pe, lambda e, ps=ps: e.matmul(ps.ap, lhsT=liT.ap[:, 2, :], rhs=g2t.ap, start=True, stop=True), [ps], [liT, g2t])
                    fw.op(act, lambda e, ps=ps: e.copy(out=FO[:, FG:FG + 512], in_=ps.ap), [fo], [ps])
                    if l == 0:
                        fw.op(act, lambda e: e.copy(out=FO[:, FV:FV + 512], in_=v_), [fo], [uc])
                        fw.dma(vfirst[i * 128:(i + 1) * 128, :], v_, [vf_t[i]], [uc])
                    else:
                        ps = psum()
                        for c_ in range(4):
                            fw.op(pe, lambda e, c_=c_, ps=ps: e.transpose(out=ps.ap[:, c_ * 128:(c_ + 1) * 128], in_=U[:, 1024 + c_ * 128:1024 + (c_ + 1) * 128], identity=ident.ap), [ps], [uc, ident])
                        vT = vT_pool.next()
                        fw.op(dve, lambda e, ps=ps, vT=vT: e.tensor_copy(out=vT.ap, in_=ps.ap.rearrange("p (a b) -> p a b", a=4)), [vT], [ps])
                        ps = psum()
                        for c_ in range(4):
                            fw.op(pe, lambda e, c_=c_, ps=ps, vT=vT: e.matmul(ps.ap[:, 0:32], lhsT=vT.ap[:, c_, :], rhs=v1t.ap[:, c_, :], start=(c_ == 0), stop=(c_ == 3)), [ps], [vT, v1t])
                        sm = sm_pool.next()
                        fw.op(dve, lambda e, ps=ps, sm=sm: e.tensor_copy(out=sm.ap[:, 0:32], in_=ps.ap[:, 0:32]), [sm], [ps])
                        ps = psum()
                        fw.op(pe, lambda e, ps=ps, sm=sm: e.transpose(out=ps.ap[0:32, 0:128], in_=sm.ap[:, 0:32], identity=ident.ap), [ps], [sm, ident])
                        smT = tmp_pool.next()
                        fw.op(dve, lambda e, ps=ps, smT=smT: e.tensor_copy(out=smT.ap[0:32, 0:128], in_=ps.ap[0:32, 0:128]), [smT], [ps])
                        ps = psum()
                        fw.op(pe, lambda e, ps=ps, smT=smT: e.matmul(ps.ap, lhsT=smT.ap[0:32, 0:128], rhs=v2t.ap, start=True, stop=True), [ps], [smT, v2t])
                        t1 = tmp_pool.next()
                        fw.op(dve, lambda e, ps=ps, t1=t1: e.tensor_tensor(out=t1.ap, in0=ps.ap, in1=v0b.ap, op=ALU.add), [t1], [ps, v0b])
                        fw.op(act, lambda e, t1=t1: e.activation(out=t1.ap, in_=t1.ap, func=AF.Sigmoid), [t1], [t1])
                        fw.op(dve, lambda e, vf=vf: e.tensor_tensor(out=vf.ap, in0=vf.ap, in1=v_, op=ALU.subtract), [vf], [vf, uc])
                        fw.op(dve, lambda e, vf=vf, t1=t1: e.tensor_tensor(out=vf.ap, in0=vf.ap, in1=t1.ap, op=ALU.mult), [vf], [vf, t1])
                        fw.op(dve, lambda e, vf=vf: e.tensor_tensor(out=FO[:, FV:FV + 512], in0=vf.ap, in1=v_, op=ALU.add), [fo], [vf, uc])
                    fw.op(act, lambda e: e.copy(out=FO[:, FR:FR + 512], in_=r_), [fo], [uc])
                    kk0 = tmp_pool.next()
                    sq = tmp_pool.next()
                    sm = sm_pool.next()
                    fw.op(dve, lambda e, kk0=kk0: e.tensor_tensor(out=kk0.ap, in0=k_, in1=kkb.ap, op=ALU.mult), [kk0], [uc, kkb])
                    fw.op(act, lambda e, kk0=kk0, sq=sq: e.activation(out=sq.ap, in_=kk0.ap, func=AF.Square), [sq], [kk0])
                    fw.op(dve, lambda e, sq=sq, sm=sm: e.reduce_sum(out=sm.ap[:, 0:8], in_=h3(sq.ap), axis=AX.X), [sm], [sq])
                    fw.op(act, lambda e, sm=sm: e.activation(out=sm.ap[:, 8:16], in_=sm.ap[:, 0:8], func=AF.Sqrt), [sm], [sm])
                    fw.op(dve, lambda e, sm=sm: e.tensor_scalar(out=sm.ap[:, 8:16], in0=sm.ap[:, 8:16], scalar1=1e-12, scalar2=None, op0=ALU.max), [sm], [sm])
                    fw.op(dve, lambda e, sm=sm: e.reciprocal(out=sm.ap[:, 16:24], in_=sm.ap[:, 8:16]), [sm], [sm])
                    fw.op(dve, lambda e, sm=sm, kk0=kk0: e.tensor_tensor(out=h3(FO[:, FKK:FKK + 512]), in0=h3(kk0.ap), in1=sm.ap[:, 16:24].unsqueeze(2).to_broadcast([128, 8, 64]), op=ALU.mult), [fo], [kk0, sm])
                    for d_ in range(2):
                        t1 = tmp_pool.next()
                        fw.op(dve, lambda e, t1=t1, d_=d_: e.scalar_tensor_tensor(out=t1.ap, in0=a_d[d_].ap, scalar=-1.0, in1=kab.ap, op0=ALU.add, op1=ALU.mult), [t1], [a_d[d_], kab])
                        fw.op(dve, lambda e, t1=t1, d_=d_: e.scalar_tensor_tensor(out=FO[:, FKD(d_):FKD(d_) + 512], in0=t1.ap, scalar=1.0, in1=k_, op0=ALU.add, op1=ALU.mult), [fo], [t1, uc])
                        fw.op(pool, lambda e, d_=d_: e.tensor_tensor(out=FO[:, FB(d_):FB(d_) + 512], in0=FO[:, FKK:FKK + 512], in1=a_d[d_].ap, op=ALU.mult), [fo], [fo, a_d[d_]])
                    t1 = tmp_pool.next()
                    sm2 = sm_pool.next()
                    fw.op(pool, lambda e, t1=t1: e.tensor_tensor(out=t1.ap, in0=FO[:, FKD(0):FKD(0) + 512], in1=FO[:, FKD(1):FKD(1) + 512], op=ALU.add), [t1], [fo])
                    fw.op(dve, lambda e, t1=t1: e.tensor_tensor(out=t1.ap, in0=t1.ap, in1=r_, op=ALU.mult), [t1], [t1, uc])
                    fw.op(pool, lambda e, t1=t1: e.tensor_tensor(out=t1.ap, in0=t1.ap, in1=rkb.ap, op=ALU.mult), [t1], [t1, rkb])
                    fw.op(dve, lambda e, t1=t1, sm2=sm2: e.reduce_sum(out=sm2.ap[:, 0:8], in_=h3(t1.ap), axis=AX.X), [sm2], [t1])
                    fw.op(dve, lambda e, sm2=sm2: e.tensor_tensor(out=h3(FO[:, FBON:FBON + 512]), in0=h3(FO[:, FV:FV + 512]), in1=sm2.ap[:, 0:8].unsqueeze(2).to_broadcast([128, 8, 64]), op=ALU.mult), [fo], [fo, sm2])
                    fw.dma(feat[i * 128:(i + 1) * 128, :], FO, [feat_t[i]], [fo])
                    if "feat" in dbg and l == 0:
                        fw.dma(dbg["feat"][i * 128:(i + 1) * 128, :], FO[:, 0:4096], [], [fo])
                fw.barrier()
            if stop_after == "S4a":
                break
            with ExitStack() as es:
                ones = sb(es, nc, "ones", [128, 1], F32)
                fw.op(dve, lambda e: e.memset(ones.ap, 1.0), [ones], [])
                Mq1, MnT = [], []
                for d_ in range(2):
                    m1_ = sb(es, nc, "Mq1", [128, 256], F32)
                    fw.op(dve, lambda e, m1_=m1_, d_=d_: e.tensor_scalar(out=m1_.ap[:, 0:128], in0=masks.ap[:, 2 * d_, :], scalar1=-1.0, scalar2=None, op0=ALU.mult), [m1_], [masks])
                    fw.op(dve, lambda e, m1_=m1_, d_=d_: e.tensor_copy(out=m1_.ap[:, 128:256], in_=masks.ap[:, 2 * d_ + 1, :]), [m1_], [masks])
                    Mq1.append(m1_)
                    mt_ = sb(es, nc, "MnT", [128, 128], F32)
                    fw.op(dve, lambda e, mt_=mt_, d_=d_: e.tensor_scalar(out=mt_.ap, in0=masks.ap[:, 2 * (1 - d_), :], scalar1=-1.0, scalar2=None, op0=ALU.mult), [mt_], [masks])
                    MnT.append(mt_)
                Mq2 = [masks.ap[:, 2 * d_:2 * d_ + 2, :].rearrange("p a t -> p (a t)") for d_ in range(2)]
                PSQB = int(os.environ.get('S4B_PSQB', '4'))
                PSQ = [TT(PS[PSQB + k // 4].ap[:, (k % 4) * 128:(k % 4 + 1) * 128]) for k in range(16)]
                pq = [0]

                def psq():
                    t_ = PSQ[pq[0] % 16]
                    pq[0] += 1
                    return t_
                p4 = [0]

                def psumR():
                    t_ = PS[p4[0] % 8]
                    p4[0] += 1
                    return t_
                fa_pool = Pool(es, nc, "sfa", [128, 3, 512], F32, 4)
                fb_pool = Pool(es, nc, "sfb", [128, 3, 512], F32, 4)
                tm_pool = Pool(es, nc, "stm", [128, 4, 512], BF16, 2)
                vb_pool = Pool(es, nc, "svb", [128, 512], BF16, 2)
                ex_pool = Pool(es, nc, "sex", [128, 3, 512], F32, 2)
                fm_pool = Pool(es, nc, "sfm", [128, 4, 4, 128], BF16, 2)
                el_pool = Pool(es, nc, "sel", [128, 4], F32, 2)
                g1_pool = Pool(es, nc, "sg1", [128, 2, 4, 128], BF16, 4)
                a2_pool = Pool(es, nc, "sa2", [128, 256], BF16, 16)
                inv_pool = Pool(es, nc, "sinv", [128, 4, 128], BF16, 32)
                invf_pool = Pool(es, nc, "sinvf", [128, 4, 128], F32, 10)
                xs_pool = Pool(es, nc, "sxs", [128, 4, 64], BF16, 4)
                nu_pool = Pool(es, nc, "snu", [128, 512], BF16, 2)
                ys_pool = Pool(es, nc, "sys", [128, 512], F32, 2)
                ST = [[[sb(es, nc, "ST", [128, 64], F32) for c_ in range(4)] for pp in range(2)] for d_ in range(2)]
                STb = [[[sb(es, nc, "STb", [128, 64], BF16) for c_ in range(4)] for pp in range(2)] for d_ in range(2)]
                for d_ in range(2):
                    for c_ in range(4):
                        fw.op(dve, lambda e, d_=d_, c_=c_: e.memset(ST[d_][0][c_].ap, 0.0), [ST[d_][0][c_]], [])
                        fw.op(dve, lambda e, d_=d_, c_=c_: e.memset(STb[d_][0][c_].ap, 0.0), [STb[d_][0][c_]], [])
                orders = [list(range(NT)), [1, 0] + list(range(NT - 1, NCT - 1, -1))]
                evi = [0]

                def evac(fn_dve, fn_act, outs, ins):
                    if evi[0] % 2:
                        fw.op(dve, fn_dve, outs, ins)
                    else:
                        fw.op(act, fn_act, outs, ins)

                def s4b_load(d_, i):
                    fa, fb = fa_pool.next(), fb_pool.next()
                    r0 = i * 128
                    fw.dma(fa.ap, feat[r0:r0 + 128, FLW(d_):FLW(d_) + 1536].rearrange("p (a c) -> p a c", a=3), [fa], [feat_t[i]])
                    fw.dma(fb.ap[:, 0:2, :], feat[r0:r0 + 128, 0:1024].rearrange("p (a c) -> p a c", a=2), [fb], [feat_t[i]])
                    fw.dma(fb.ap[:, 2, :], feat[r0:r0 + 128, FKK:FKK + 512], [fb], [feat_t[i]])
                    return fa, fb

                def step_prep(d_, j, fa, fb):
                    i = orders[d_][j]
                    pp = j % 2
                    STo, STn = ST[d_][pp], ST[d_][1 - pp]
                    SBo, SBn = STb[d_][pp], STb[d_][1 - pp]
                    vb = vb_pool.next()
                    fw.op(act, lambda e: e.copy(out=vb.ap, in_=fb.ap[:, 0, :]), [vb], [fb])
                    lw, b_, kd = fa.ap[:, 0, :], fa.ap[:, 1, :], fa.ap[:, 2, :]
                    v_, r_, kk = fb.ap[:, 0, :], fb.ap[:, 1, :], fb.ap[:, 2, :]
                    psL = psumR()
                    fw.op(pe, lambda e: e.matmul(psL.ap, lhsT=masks.ap[:, 1 + 2 * d_, :], rhs=lw, start=True, stop=True), [psL], [masks, fa])
                    ex = ex_pool.next()
                    fw.op(act, lambda e: e.activation(out=ex.ap[:, 0, :], in_=psL.ap, func=AF.Exp), [ex], [psL])
                    fw.op(act, lambda e: e.activation(out=ex.ap[:, 1, :], in_=psL.ap, func=AF.Exp, scale=-1.0), [ex], [psL])
                    fw.op(dve, lambda e: e.tensor_tensor(out=ex.ap[:, 2, :], in0=psL.ap, in1=lw, op=ALU.subtract), [ex], [psL, fa])
                    fw.op(act, lambda e: e.activation(out=ex.ap[:, 2, :], in_=ex.ap[:, 2, :], func=AF.Exp), [ex], [ex])
                    tm = tm_pool.next()
                    fw.op(dve, lambda e: e.tensor_tensor(out=tm.ap[:, 0, :], in0=kk, in1=ex.ap[:, 2, :], op=ALU.mult), [tm], [fb, ex])
                    fw.op(pool, lambda e: e.tensor_tensor(out=tm.ap[:, 1, :], in0=r_, in1=ex.ap[:, 0, :], op=ALU.mult), [tm], [fb, ex])
                    fw.op(dve, lambda e: e.tensor_tensor(out=tm.ap[:, 2, :], in0=b_, in1=ex.ap[:, 1, :], op=ALU.mult), [tm], [fa, ex])
                    fw.op(pool, lambda e: e.tensor_tensor(out=tm.ap[:, 3, :], in0=kd, in1=ex.ap[:, 1, :], op=ALU.mult), [tm], [fa, ex])
                    psE = psumR()
                    for c_ in range(4):
                        fw.op(pe, lambda e, c_=c_: e.matmul(psE.ap[:, c_:c_ + 1], lhsT=lw[:, c_ * 128:(c_ + 1) * 128], rhs=ones.ap, start=True, stop=True), [psE], [fa, ones])
                    el = el_pool.next()
                    fw.op(act, lambda e: e.activation(out=el.ap, in_=psE.ap[:, 0:4], func=AF.Exp), [el], [psE])
                    fm = fm_pool.next()
                    for c_ in range(4):
                        psT = psumR()
                        for q in range(4):
                            fw.op(pe, lambda e, c_=c_, q=q, psT=psT: e.transpose(out=psT.ap.bitcast(BF16)[:, q * 128:(q + 1) * 128], in_=tm.ap[:, q, c_ * 128:(c_ + 1) * 128], identity=identb.ap), [psT], [tm, identb])
                        evi[0] += 1
                        evac(lambda e, c_=c_, psT=psT: e.tensor_copy(out=fm.ap[:, c_, :, :], in_=psT.ap.bitcast(BF16)[:, 0:512].rearrange("p (a b) -> p a b", a=4)),
                             lambda e, c_=c_, psT=psT: e.copy(out=fm.ap[:, c_, :, :], in_=psT.ap.bitcast(BF16)[:, 0:512].rearrange("p (a b) -> p a b", a=4)), [fm], [psT])
                    H = []
                    for h in range(8):
                        c_ = h // 2
                        hp = slice((h % 2) * 64, (h % 2) * 64 + 64)
                        H.append(dict(c=c_, hp=hp, par=h % 2, k=h // 2, al=fm.ap[hp, c_, 0, :], rh=fm.ap[hp, c_, 1, :], be=fm.ap[hp, c_, 2, :], ka=fm.ap[hp, c_, 3, :],
                                      ar=fm.ap[hp, c_, 0:2, :].rearrange("p a t -> p (a t)"), vh=vb.ap[:, h * 64:(h + 1) * 64]))
                    G1 = [g1_pool.next() for _ in range(2)]
                    for h in range(8):
                        u = H[h]
                        psG = psumR()
                        fw.op(pe, lambda e, u=u, psG=psG: e.matmul(psG.ap[:, 0:256], lhsT=u["be"], rhs=u["ar"], start=True, stop=True), [psG], [fm])
                        fw.op(pe, lambda e, u=u, psG=psG: e.matmul(psG.ap[:, 256:512], lhsT=u["ka"], rhs=u["ar"], start=True, stop=True), [psG], [fm])
                        a2 = a2_pool.next()
                        g1 = G1[u["par"]]
                        fw.op(dve, lambda e, u=u, psG=psG, g1=g1: e.tensor_tensor(out=g1.ap[:, :, u["k"], :], in0=psG.ap[:, 0:256].rearrange("p (a t) -> p a t", a=2), in1=Mq1[d_].ap.rearrange("p (a t) -> p a t", a=2), op=ALU.mult), [g1], [psG, Mq1[d_]])
                        fw.op(dve, lambda e, a2=a2, psG=psG: e.tensor_tensor(out=a2.ap, in0=psG.ap[:, 256:512], in1=Mq2[d_], op=ALU.mult), [a2], [psG, masks])
                        u.update(a2=a2, abr=g1.ap[:, 1, u["k"], :], g1=g1)
                    GP = []
                    for par in range(2):
                        g1 = G1[par]
                        Mb = inv_pool.next()
                        fw.op(dve, lambda e, Mb=Mb, g1=g1: e.tensor_tensor(out=Mb.ap, in0=g1.ap[:, 0, :, :], in1=ident.ap.unsqueeze(1).to_broadcast([128, 4, 128]), op=ALU.add), [Mb], [g1, ident])
                        bank = psumR()
                        bkb = bank.ap.bitcast(BF16)
                        for k_ in range(4):
                            fw.op(pe, lambda e, k_=k_, bkb=bkb, g1=g1: e.transpose(out=bkb[:, k_ * 128:(k_ + 1) * 128], in_=g1.ap[:, 0, k_, :], identity=identb.ap), [bank], [g1, identb])
                        AT = inv_pool.next()
                        evi[0] += 1
                        evac(lambda e, AT=AT, bkb=bkb: e.tensor_copy(out=f4(AT.ap), in_=bkb[:, 0:512]), lambda e, AT=AT, bkb=bkb: e.copy(out=f4(AT.ap), in_=bkb[:, 0:512]), [AT], [bank])
                        GP.append(dict(A=g1, Aap=g1.ap[:, 0, :, :], AT=AT, Mb=Mb))
                    return dict(d_=d_, i=i, STo=STo, STn=STn, tm=tm, el=el, fm=fm, fb=fb, v_=v_, H=H, GP=GP, SBo=SBo, SBn=SBn, vb=vb)

                f4 = lambda ap: ap.rearrange("p k t -> p (k t)")

                def inv_all(GP):
                    for it in range(6):
                        for g in GP:
                            bank = psumR()
                            for k_ in range(4):
                                fw.op(pe, lambda e, k_=k_, bank=bank, g=g: e.matmul(bank.ap[:, k_ * 128:(k_ + 1) * 128], lhsT=g["Aap"][:, k_, :], rhs=g["AT"].ap[:, k_, :], start=True, stop=True), [bank], [g["A"], g["AT"]])
                            ATn = inv_pool.next()
                            evi[0] += 1
                            evac(lambda e, ATn=ATn, bank=bank: e.tensor_copy(out=f4(ATn.ap), in_=bank.ap), lambda e, ATn=ATn, bank=bank: e.copy(out=f4(ATn.ap), in_=bank.ap), [ATn], [bank])
                            g["ATn"] = ATn
                            if it < 5:
                                bank = psumR()
                                for k_ in range(4):
                                    fw.op(pe, lambda e, k_=k_, bank=bank, g=g: e.matmul(bank.ap[:, k_ * 128:(k_ + 1) * 128], lhsT=g["AT"].ap[:, k_, :], rhs=g["Aap"][:, k_, :], start=True, stop=True), [bank], [g["A"], g["AT"]])
                                An = inv_pool.next()
                                evi[0] += 1
                                evac(lambda e, An=An, bank=bank: e.tensor_copy(out=f4(An.ap), in_=bank.ap), lambda e, An=An, bank=bank: e.copy(out=f4(An.ap), in_=bank.ap), [An], [bank])
                                g["An"] = An
                        for g in GP:
                            g["AT"] = g["ATn"]
                            if it < 5:
                                g["A"], g["Aap"] = g["An"], g["An"].ap
                        for g in GP:
                            bank = psumR()
                            for k_ in range(4):
                                fw.op(pe, lambda e, k_=k_, bank=bank, g=g: e.matmul(bank.ap[:, k_ * 128:(k_ + 1) * 128], lhsT=identb.ap, rhs=g["Mb"].ap[:, k_, :], start=True, stop=False), [bank], [identb, g["Mb"]])
                                fw.op(pe, lambda e, k_=k_, bank=bank, g=g: e.matmul(bank.ap[:, k_ * 128:(k_ + 1) * 128], lhsT=g["AT"].ap[:, k_, :], rhs=g["Mb"].ap[:, k_, :], start=False, stop=True), [bank], [g["AT"], g["Mb"]])
                            Mbn = inv_pool.next()
                            evi[0] += 1
                            evac(lambda e, Mbn=Mbn, bank=bank: e.tensor_copy(out=f4(Mbn.ap), in_=bank.ap), lambda e, Mbn=Mbn, bank=bank: e.copy(out=f4(Mbn.ap), in_=bank.ap), [Mbn], [bank])
                            g["Mb"] = Mbn

                def step_state(cx):
                    d_, i, STo, STn, tm, el, fm, fb, v_, H, GP, SBo, SBn, vb = (cx[k] for k in ('d_', 'i', 'STo', 'STn', 'tm', 'el', 'fm', 'fb', 'v_', 'H', 'GP', 'SBo', 'SBn', 'vb'))
                    NU = nu_pool.next()
                    q4 = lambda ap: ap.rearrange("p (k t) -> p k t", k=4)[:, :, 0:64]
                    for par, g in enumerate(GP):
                        bank = psumR()
                        for k_ in range(4):
                            u = H[par + 2 * k_]
                            o = bank.ap[:, k_ * 128:k_ * 128 + 64]
                            fw.op(pe, lambda e, u=u, o=o: e.matmul(o, lhsT=u["al"], rhs=SBo[u["c"]].ap[u["hp"], :], start=True, stop=False), [bank], [fm, SBo[u["c"]]])
                            fw.op(pe, lambda e, u=u, o=o: e.matmul(o, lhsT=u["a2"].ap[:, 0:128], rhs=u["vh"], start=False, stop=True), [bank], [u["a2"], vb])
                        xsg = xs_pool.next()
                        evi[0] += 1
                        evac(lambda e, xsg=xsg, bank=bank: e.tensor_copy(out=xsg.ap, in_=q4(bank.ap)), lambda e, xsg=xsg, bank=bank: e.copy(out=xsg.ap, in_=q4(bank.ap)), [xsg], [bank])
                        g["xs"] = xsg
                    for par, g in enumerate(GP):
                        bank = psumR()
                        for k_ in range(4):
                            fw.op(pe, lambda e, k_=k_, bank=bank, g=g: e.matmul(bank.ap[:, k_ * 128:k_ * 128 + 64], lhsT=g["Mb"].ap[:, k_, :], rhs=g["xs"].ap[:, k_, :], start=True, stop=True), [bank], [g["Mb"], g["xs"]])
                        nuv = NU.ap.rearrange("p (c two d) -> p c two d", two=2, d=64)[:, :, par, :]
                        fw.op(act, lambda e, bank=bank, nuv=nuv: e.activation(out=nuv, in_=q4(bank.ap), func=AF.Copy, scale=-1.0), [NU], [bank])
                    ysb = ys_pool.next()
                    for par in (0, 1):
                        psY = psumR()
                        for k_ in range(4):
                            h = par + 2 * k_
                            u = H[h]
                            ysl = psY.ap[:, k_ * 64:(k_ + 1) * 64]
                            fw.op(pe, lambda e, u=u, ysl=ysl: e.matmul(ysl, lhsT=u["rh"], rhs=SBo[u["c"]].ap[u["hp"], :], start=True, stop=False), [psY], [fm, SBo[u["c"]]])
                            fw.op(pe, lambda e, u=u, ysl=ysl, h=h: e.matmul(ysl, lhsT=u["abr"], rhs=NU.ap[:, h * 64:(h + 1) * 64], start=False, stop=False), [psY], [u["g1"], NU])
                            fw.op(pe, lambda e, u=u, ysl=ysl: e.matmul(ysl, lhsT=u["a2"].ap[:, 128:256], rhs=u["vh"], start=False, stop=True), [psY], [u["a2"], vb])
                        yv = ysb.ap.rearrange("p (c two d) -> p c two d", two=2, d=64)[:, :, par, :]
                        fw.op(act, lambda e, psY=psY, yv=yv: e.copy(out=yv, in_=psY.ap[:, 0:256].rearrange("p (c d) -> p c d", d=64)), [ysb], [psY])
                    fw.dma(y_scr[d_, i * 128:(i + 1) * 128, :], ysb.ap, [y_t[d_][i]], [ysb])
                    psS = psumR()
                    for c_ in range(4):
                        so = psS.ap[:, c_ * 128:(c_ + 1) * 128]
                        fw.op(pe, lambda e, c_=c_, so=so: e.matmul(so, lhsT=tm.ap[:, 2, c_ * 128:(c_ + 1) * 128], rhs=NU.ap[:, c_ * 128:(c_ + 1) * 128], start=True, stop=False), [psS], [tm, NU])
                        fw.op(pe, lambda e, c_=c_, so=so: e.matmul(so, lhsT=tm.ap[:, 3, c_ * 128:(c_ + 1) * 128], rhs=vb.ap[:, c_ * 128:(c_ + 1) * 128], start=False, stop=True), [psS], [tm, vb])
                        fw.op(act, lambda e, c_=c_: e.activation(out=STn[c_].ap, in_=STo[c_].ap, func=AF.Copy, scale=el.ap[:, c_:c_ + 1]), [STn[c_]], [STo[c_], el])
                    for c_ in range(4):
                        so = psS.ap[:, c_ * 128:(c_ + 1) * 128]
                        for hh in range(2):
                            hp = slice(hh * 64, hh * 64 + 64)
                            fw.op(dve, lambda e, c_=c_, hp=hp, so=so: e.scalar_tensor_tensor(out=STn[c_].ap[hp, :], in0=so[hp, hp], scalar=el.ap[hp, c_:c_ + 1], in1=STn[c_].ap[hp, :], op0=ALU.mult, op1=ALU.add), [STn[c_]], [psS, el, STn[c_]])
                    for c_ in range(4):
                        fw.op(act, lambda e, c_=c_: e.copy(out=SBn[c_].ap, in_=STn[c_].ap), [SBn[c_]], [STn[c_]])

                nsteps = NT if not os.environ.get('S4B_STEPS') else int(os.environ['S4B_STEPS'])
                nxt = [s4b_load(d_, orders[d_][0]) for d_ in range(2)]
                for j in range(nsteps):
                    cur = list(nxt)
                    cxs = []
                    for d_ in range(2):
                        cxs.append(step_prep(d_, j, *cur[d_]))
                        if j + 1 < nsteps:
                            nxt[d_] = s4b_load(d_, orders[d_][j + 1])
                    inv_all([g for cx in cxs for g in cx["GP"]])
                    for cx in cxs:
                        step_state(cx)
                fw.barrier()
            if stop_after == "S4b":
                break
            with ExitStack() as es:
                lnw = sb(es, nc, "lnw", [128, RW], F32)
                lnb = sb(es, nc, "lnb", [128, RW], F32)
                fw.dma(lnw.ap, ln_x_w[l].partition_broadcast(128), [lnw], [])
                fw.dma(lnb.ap, ln_x_b[l].partition_broadcast(128), [lnb], [])
                y0_pool = Pool(es, nc, "cy0", [128, RW], F32, 2)
                y1_pool = Pool(es, nc, "cy1", [128, RW], F32, 2)
                gb_pool = Pool(es, nc, "cgb", [128, 2, RW], F32, 2)
                sq_pool = Pool(es, nc, "csq", [128, RW], F32, 2)
                cs_pool = Pool(es, nc, "ccs", [128, 32], F32, 2)
                h3 = lambda ap: ap.rearrange("p (h d) -> p h d", h=8)
                bc8 = lambda ap: ap.unsqueeze(2).to_broadcast([128, 8, 64])
                for i in (range(NCT, NT) if last else range(NT)):
                    y0, y1, gb = y0_pool.next(), y1_pool.next(), gb_pool.next()
                    r0 = i * 128
                    fw.dma(y0.ap, y_scr[0, r0:r0 + 128, :], [y0], [y_t[0][i]])
                    fw.dma(y1.ap, y_scr[1, r0:r0 + 128, :], [y1], [y_t[1][i]])
                    fw.dma(gb.ap[:, 0, :], feat[r0:r0 + 128, FG:FG + 512], [gb], [feat_t[i]])
                    fw.dma(gb.ap[:, 1, :], feat[r0:r0 + 128, FBON:FBON + 512], [gb], [feat_t[i]])
                    cs_ = cs_pool.next()
                    sq = sq_pool.next()
                    fw.op(dve, lambda e: e.tensor_tensor(out=y0.ap, in0=y0.ap, in1=y1.ap, op=ALU.add), [y0], [y0, y1])
                    fw.op(dve, lambda e: e.reduce_sum(out=cs_.ap[:, 0:8], in_=h3(y0.ap), axis=AX.X), [cs_], [y0])
                    fw.op(dve, lambda e: e.tensor_scalar(out=cs_.ap[:, 0:8], in0=cs_.ap[:, 0:8], scalar1=1.0 / 64, scalar2=None, op0=ALU.mult), [cs_], [cs_])
                    fw.op(dve, lambda e: e.tensor_tensor(out=h3(y0.ap), in0=h3(y0.ap), in1=bc8(cs_.ap[:, 0:8]), op=ALU.subtract), [y0], [y0, cs_])
                    fw.op(act, lambda e: e.activation(out=sq.ap, in_=y0.ap, func=AF.Square), [sq], [y0])
                    fw.op(dve, lambda e: e.reduce_sum(out=cs_.ap[:, 8:16], in_=h3(sq.ap), axis=AX.X), [cs_], [sq])
                    fw.op(dve, lambda e: e.tensor_scalar(out=cs_.ap[:, 8:16], in0=cs_.ap[:, 8:16], scalar1=1.0 / 64, scalar2=64e-5, op0=ALU.mult, op1=ALU.add), [cs_], [cs_])
                    fw.op(act, lambda e: e.activation(out=cs_.ap[:, 16:24], in_=cs_.ap[:, 8:16], func=AF.Sqrt), [cs_], [cs_])
                    fw.op(dve, lambda e: e.reciprocal(out=cs_.ap[:, 24:32], in_=cs_.ap[:, 16:24]), [cs_], [cs_])
                    fw.op(dve, lambda e: e.tensor_tensor(out=h3(y0.ap), in0=h3(y0.ap), in1=bc8(cs_.ap[:, 24:32]), op=ALU.mult), [y0], [y0, cs_])
                    fw.op(pool, lambda e: e.tensor_tensor(out=y0.ap, in0=y0.ap, in1=lnw.ap, op=ALU.mult), [y0], [y0, lnw])
                    fw.op(dve, lambda e: e.tensor_tensor(out=y0.ap, in0=y0.ap, in1=lnb.ap, op=ALU.add), [y0], [y0, lnb])
                    fw.op(pool, lambda e: e.tensor_tensor(out=y0.ap, in0=y0.ap, in1=gb.ap[:, 1, :], op=ALU.add), [y0], [y0, gb])
                    fw.op(dve, lambda e: e.tensor_tensor(out=y0.ap, in0=y0.ap, in1=gb.ap[:, 0, :], op=ALU.mult), [y0], [y0, gb])
                    fw.dma(mix[r0:r0 + 128, 512:1024], y0.ap, [mix_r[i]], [y0])
                fw.barrier()
            if stop_after == "S4":
                if "mix" in dbg:
                    for i in range(NT):
                        fw.dma(dbg["mix"][i * 128:(i + 1) * 128, :], mix[i * 128:(i + 1) * 128, :], [], [mix_a[i], mix_r[i]])
                    fw.barrier()
                break
            tiles5 = list(range(NCT, NT)) if last else list(range(NT))
            with ExitStack() as es:
                woutb = sb(es, nc, "woutb", [128, 8, D], BF16)
                wst_pool = Pool(es, nc, "wst5", [128, 8, 256], F32, 2)
                for j in range(4):
                    wst = wst_pool.next()
                    fw.dma(wst.ap, w_out[l][:, j * 256:(j + 1) * 256].rearrange("(kc p) n -> p kc n", p=128), [wst], [])
                    fw.op(pool, lambda e, wst=wst, j=j: e.tensor_copy(out=woutb.ap[:, :, j * 256:(j + 1) * 256], in_=wst.ap), [woutb], [wst])
                modG = [sb(es, nc, "modG%d" % s_, [128, D], F32) for s_ in range(2)]
                for s_ in range(2):
                    fw.dma(modG[s_].ap, modbuf[l, s_ * 6 + 2], [modG[s_]], [mod_t[l][s_ * 6 + 2]])
                mx_pool = Pool(es, nc, "mx", [128, D], F32, 2)
                mxb_pool = Pool(es, nc, "mxb", [128, D], BF16, 2)
                mT_pool = Pool(es, nc, "mT", [128, 8, 128], BF16, 2)
                x5_pool = Pool(es, nc, "x5", [128, D], F32, 2)

                def s5_load(i):
                    mx = mx_pool.next()
                    fw.dma(mx.ap, mix[i * 128:(i + 1) * 128, :], [mx], [mix_a[i], mix_r[i]])
                    xt = x5_pool.next()
                    fw.dma(xt.ap, xs[i * 128:(i + 1) * 128, :], [xt], [xs_t[i]])
                    return mx, xt

                nxt = s5_load(tiles5[0])
                for ti, i in enumerate(tiles5):
                    mx, xt = nxt
                    if ti + 1 < len(tiles5):
                        nxt = s5_load(tiles5[ti + 1])
                    s_ = 1 if i < NCT else 0
                    mxb = mxb_pool.next()
                    fw.op(act, lambda e, mx=mx, mxb=mxb: e.copy(out=mxb.ap, in_=mx.ap), [mxb], [mx])
                    mT = mT_pool.next()
                    for half in range(2):
                        ps = psum()
                        psb = ps.ap.bitcast(BF16)
                        for kk in range(4):
                            kc = half * 4 + kk
                            fw.op(pe, lambda e, kc=kc, kk=kk, psb=psb, mxb=mxb: e.transpose(out=psb[:, kk * 128:(kk + 1) * 128], in_=mxb.ap[:, kc * 128:(kc + 1) * 128], identity=identb.ap), [ps], [mxb, identb])
                        fw.op(act if half else dve, lambda e, psb=psb, mT=mT, half=half: (e.copy if half else e.tensor_copy)(out=mT.ap[:, half * 4:(half + 1) * 4, :], in_=psb[:, 0:512].rearrange("p (a b) -> p a b", a=4)), [mT], [ps])
                    for half in range(2):
                        ps = psum()
                        hs = slice(half * 512, (half + 1) * 512)
                        for kc in range(8):
                            fw.op(pe, lambda e, kc=kc, ps=ps, mT=mT, hs=hs: e.matmul(ps.ap, lhsT=mT.ap[:, kc, :], rhs=woutb.ap[:, kc, hs], start=(kc == 0), stop=(kc == 7)), [ps], [mT, woutb])
                        fw.op(dve, lambda e, ps=ps, mx=mx, hs=hs, s_=s_: e.tensor_tensor(out=mx.ap[:, hs], in0=ps.ap, in1=modG[s_].ap[:, hs], op=ALU.mult), [mx], [ps, modG[s_]])
                    fw.op(dve, lambda e, xt=xt, mx=mx: e.tensor_tensor(out=xt.ap, in0=xt.ap, in1=mx.ap, op=ALU.add), [xt], [xt, mx])
                    fw.dma(xs[i * 128:(i + 1) * 128, :], xt.ap, [xs_t[i]], [xt])
                    if "xmix" in dbg and l == 0:
                        fw.dma(dbg["xmix"][i * 128:(i + 1) * 128, :], xt.ap, [], [xt])
                fw.barrier()
            if stop_after == "S5":
                break

            tiles6 = list(range(NCT, NT)) if last else list(range(NT))
            GS = 8
            ng_ = -(-len(tiles6) // GS)
            gsz = [len(tiles6) // ng_ + (1 if k < len(tiles6) % ng_ else 0) for k in range(ng_)]
            groups, k0_ = [], 0
            for z in gsz:
                groups.append(tiles6[k0_:k0_ + z])
                k0_ += z
            with ExitStack() as es:
                nseg = 1 if last else 2
                mA = [sb(es, nc, "mA%d" % s_, [128, D], F32) for s_ in range(nseg)]
                mB = [sb(es, nc, "mB%d" % s_, [128, D], F32) for s_ in range(nseg)]
                mG = [sb(es, nc, "mG%d" % s_, [128, D], F32) for s_ in range(nseg)]
                for s_ in range(nseg):
                    fw.dma(mA[s_].ap, modbuf[l, s_ * 6 + 3], [mA[s_]], [mod_t[l][s_ * 6 + 3]])
                    fw.dma(mB[s_].ap, modbuf[l, s_ * 6 + 4], [mB[s_]], [mod_t[l][s_ * 6 + 4]])
                    fw.dma(mG[s_].ap, modbuf[l, s_ * 6 + 5], [mG[s_]], [mod_t[l][s_ * 6 + 5]])
                rwt = sb(es, nc, "rwt", [128, 8, NE], F32)
                fw.dma(rwt.ap, router_w.rearrange("(kc p) n -> p kc n", p=128), [rwt], [])
                rbt = sb(es, nc, "rbt", [128, NE], F32)
                fw.dma(rbt.ap, router_b.partition_broadcast(128), [rbt], [])
                fgt = None
                if last:
                    fgt = sb(es, nc, "fgt", [128, D], F32)
                    fw.dma(fgt.ap, final_g.partition_broadcast(128), [fgt], [])
                HT2 = [sb(es, nc, "HT", [128, 8, GS * 128], BF16) for _ in range(2)]
                accs = [sb(es, nc, "acc%d" % k, [128, D], F32) for k in range(GS)]
                gates2 = [sb(es, nc, "gates", [128, GS, NE], F32) for _ in range(2)]
                Wg2 = [sb(es, nc, "Wg", [128, 8, D], BF16) for _ in range(2)]
                Wu2 = [sb(es, nc, "Wu", [128, 8, D], BF16) for _ in range(2)]
                Wd = sb(es, nc, "Wd", [128, 8, D], BF16)
                actb = sb(es, nc, "actb", [128, 8, GS * 128], BF16)
                x6_pool = Pool(es, nc, "x6", [128, D], F32, 2)
                hT32_pool = Pool(es, nc, "hT32", [128, 8, 128], F32, 1)
                hooks = {}
                rt_pool = Pool(es, nc, "rt", [128, 96], F32, 2)
                sg_pool = Pool(es, nc, "sg", [128, 512], F32, 2)
                st6_pool = Pool(es, nc, "st6", [128, 4], F32, 2)

                def rms(xt, st, junk6):
                    jap = junk6.ap if len(junk6.ap.shape) == 2 else junk6.ap.rearrange("p a b -> p (a b)")
                    fw.op(act, lambda e: e.activation(out=jap, in_=xt.ap, func=AF.Square, accum_out=st.ap[:, 0:1]), [junk6, st], [xt])
                    fw.op(dve, lambda e: e.tensor_scalar(out=st.ap[:, 1:2], in0=st.ap[:, 0:1], scalar1=1.0 / D, scalar2=1e-6, op0=ALU.mult, op1=ALU.add), [st], [st])
                    fw.op(act, lambda e: e.activation(out=st.ap[:, 3:4], in_=st.ap[:, 1:2], func=AF.Sqrt), [st], [st])
                    fw.op(dve, lambda e: e.reciprocal(out=st.ap[:, 2:3], in_=st.ap[:, 3:4]), [st], [st])

                def load_w(dst, src):
                    for j in range(2):
                        fw.dma(dst.ap[:, :, j * 512:(j + 1) * 512], src[:, j * 512:(j + 1) * 512].rearrange("(kc p) n -> p kc n", p=128), [dst], [], q=pool)

                seq = [(gi, ex) for gi in range(len(groups)) for ex in range(NE)]
                load_w(Wg2[0], e_gate[l, 0])
                load_w(Wu2[0], e_up[l, 0])

                def moe_A_front(gi, grp, t):
                    i = grp[t]
                    if True:
                        s_ = 1 if i < NCT else 0
                        xt = x6_pool.next()
                        fw.dma(xt.ap, xs[i * 128:(i + 1) * 128, :], [xt], [xs_t[i]])
                        st = st6_pool.next()
                        hT32 = hT32_pool.next()
                        rms(xt, st, hT32)
                        fw.op(dve, lambda e, xt=xt, st=st, s_=s_: e.scalar_tensor_tensor(out=xt.ap, in0=xt.ap, scalar=st.ap[:, 2:3], in1=mA[s_].ap, op0=ALU.mult, op1=ALU.mult), [xt], [xt, st, mA[s_]])
                        fw.op(dve, lambda e, xt=xt, s_=s_: e.tensor_tensor(out=xt.ap, in0=xt.ap, in1=mB[s_].ap, op=ALU.add), [xt], [xt, mB[s_]])
                        return dict(xt=xt, hT32=hT32, t=t, i=i)

                def moe_A_back(gi, grp, cx):
                    HT, gates = HT2[gi % 2], gates2[gi % 2]
                    xt, hT32, t, i = cx["xt"], cx["hT32"], cx["t"], cx["i"]
                    if True:
                        for half in range(2):
                            ps = psum()
                            for kk in range(4):
                                kc = half * 4 + kk
                                fw.op(pe, lambda e, kc=kc, kk=kk, ps=ps, xt=xt: e.transpose(out=ps.ap[:, kk * 128:(kk + 1) * 128], in_=xt.ap[:, kc * 128:(kc + 1) * 128], identity=ident.ap), [ps], [xt, ident])
                            fw.op(dve, lambda e, ps=ps, hT32=hT32, half=half: e.tensor_copy(out=hT32.ap[:, half * 4:(half + 1) * 4, :], in_=ps.ap.rearrange("p (a b) -> p a b", a=4)), [hT32], [ps])
                            fw.op(act, lambda e, hT32=hT32, half=half, t=t: e.copy(out=HT.ap[:, half * 4:(half + 1) * 4, t * 128:(t + 1) * 128], in_=hT32.ap[:, half * 4:(half + 1) * 4, :]), [HT], [hT32])
                        ps = psum()
                        for kc in range(8):
                            fw.op(pe, lambda e, kc=kc, ps=ps, hT32=hT32: e.matmul(ps.ap[:, 0:NE], lhsT=hT32.ap[:, kc, :], rhs=rwt.ap[:, kc, :], start=(kc == 0), stop=(kc == 7)), [ps], [hT32, rwt])
                        rt = rt_pool.next()
                        R = rt.ap
                        sc_, bi_ = R[:, 0:16], R[:, 16:32]
                        g4 = lambda ap: ap.rearrange("p (g m) -> p g m", g=4)
                        fw.op(act, lambda e, ps=ps, sc_=sc_: e.activation(out=sc_, in_=ps.ap[:, 0:NE], func=AF.Sigmoid), [rt], [ps])
                        fw.op(dve, lambda e, sc_=sc_, bi_=bi_: e.tensor_tensor(out=bi_, in0=sc_, in1=rbt.ap, op=ALU.add), [rt], [rt, rbt])
                        a = [g4(bi_)[:, :, m] for m in range(4)]
                        hi01, lo01, hi23, lo23 = R[:, 32:36], R[:, 36:40], R[:, 40:44], R[:, 44:48]
                        top1, m1, m2, top2 = R[:, 48:52], R[:, 52:56], R[:, 56:60], R[:, 60:64]
                        gs_, gmx, gm_, rd = R[:, 64:68], R[:, 68:69], R[:, 72:76], R[:, 69:70]
                        sel = R[:, 80:96]
                        tt = lambda o, x0, x1, op: fw.op(dve, lambda e: e.tensor_tensor(out=o, in0=x0, in1=x1, op=op), [rt], [rt])
                        tt(hi01, a[0], a[1], ALU.max)
                        tt(lo01, a[0], a[1], ALU.min)
                        tt(hi23, a[2], a[3], ALU.max)
                        tt(lo23, a[2], a[3], ALU.min)
                        tt(top1, hi01, hi23, ALU.max)
                        tt(m1, hi01, hi23, ALU.min)
                        tt(m2, lo01, lo23, ALU.max)
                        tt(top2, m1, m2, ALU.max)
                        tt(gs_, top1, top2, ALU.add)
                        fw.op(dve, lambda e, gs_=gs_, gmx=gmx: e.reduce_max(out=gmx, in_=gs_, axis=AX.X), [rt], [rt])
                        fw.op(dve, lambda e, gs_=gs_, gmx=gmx, gm_=gm_: e.tensor_scalar(out=gm_, in0=gs_, scalar1=gmx, scalar2=None, op0=ALU.is_ge), [rt], [rt])
                        tt(g4(sel), g4(bi_), top2.unsqueeze(2).to_broadcast([128, 4, 4]), ALU.is_ge)
                        tt(g4(sel), g4(sel), gm_.unsqueeze(2).to_broadcast([128, 4, 4]), ALU.mult)
                        tt(sel, sel, sc_, ALU.mult)
                        fw.op(dve, lambda e, sel=sel, rd=rd: e.reduce_sum(out=rd, in_=sel, axis=AX.X), [rt], [rt])
                        fw.op(dve, lambda e, rd=rd: e.reciprocal(out=rd, in_=rd), [rt], [rt])
                        fw.op(dve, lambda e, sel=sel, rd=rd, t=t: e.tensor_scalar(out=gates.ap[:, t, :], in0=sel, scalar1=rd, scalar2=None, op0=ALU.mult), [gates], [rt])
                        if "gates" in dbg and l == 0:
                            fw.dma(dbg["gates"][i * 128:(i + 1) * 128, :], gates.ap[:, t, :], [], [gates])

                def moe_B(gi, grp, exs):
                    ntok = len(grp) * 128
                    HT, gates = HT2[gi % 2], gates2[gi % 2]
                    for ex in exs:
                        pre = hooks.get((gi, ex, 0))
                        cxh = pre() if pre else None
                        qi = gi * NE + ex
                        Wg, Wu = Wg2[qi % 2], Wu2[qi % 2]
                        load_w(Wd, e_down[l, ex])
                        if qi + 1 < len(seq):
                            nex = seq[qi + 1][1]
                            load_w(Wg2[(qi + 1) % 2], e_gate[l, nex])
                            load_w(Wu2[(qi + 1) % 2], e_up[l, nex])
                        for t0 in range(0, ntok, 512):
                            ntk = min(512, ntok - t0)
                            for fc in range(8):
                                psg = psum()
                                psu = psum()
                                for kc in range(8):
                                    fw.op(pe, lambda e, kc=kc, fc=fc, psg=psg, t0=t0, ntk=ntk: e.matmul(psg.ap[:, 0:ntk], lhsT=Wg.ap[:, kc, fc * 128:(fc + 1) * 128], rhs=HT.ap[:, kc, t0:t0 + ntk], start=(kc == 0), stop=(kc == 7)), [psg], [Wg, HT])
                                for kc in range(8):
                                    fw.op(pe, lambda e, kc=kc, fc=fc, psu=psu, t0=t0, ntk=ntk: e.matmul(psu.ap[:, 0:ntk], lhsT=Wu.ap[:, kc, fc * 128:(fc + 1) * 128], rhs=HT.ap[:, kc, t0:t0 + ntk], start=(kc == 0), stop=(kc == 7)), [psu], [Wu, HT])
                                sg = sg_pool.next()
                                fw.op(act, lambda e, psg=psg, sg=sg, ntk=ntk: e.activation(out=sg.ap[:, 0:ntk], in_=psg.ap[:, 0:ntk], func=AF.Silu), [sg], [psg])
                                fw.op(dve, lambda e, psu=psu, sg=sg, fc=fc, t0=t0, ntk=ntk: e.tensor_tensor(out=actb.ap[:, fc, t0:t0 + ntk], in0=sg.ap[:, 0:ntk], in1=psu.ap[:, 0:ntk], op=ALU.mult), [actb], [sg, psu])
                        for t in range(len(grp)):
                            for half in range(2):
                                hs = slice(half * 512, (half + 1) * 512)
                                ps = psum()
                                for fc in range(8):
                                    fw.op(pe, lambda e, fc=fc, ps=ps, t=t, hs=hs: e.matmul(ps.ap, lhsT=actb.ap[:, fc, t * 128:(t + 1) * 128], rhs=Wd.ap[:, fc, hs], start=(fc == 0), stop=(fc == 7)), [ps], [actb, Wd])
                                if ex == 0:
                                    fw.op(dve, lambda e, ps=ps, t=t, hs=hs, ex=ex: e.tensor_scalar(out=accs[t].ap[:, hs], in0=ps.ap, scalar1=gates.ap[:, t, ex:ex + 1], scalar2=None, op0=ALU.mult), [accs[t]], [ps, gates])
                                else:
                                    fw.op(dve, lambda e, ps=ps, t=t, hs=hs, ex=ex: e.scalar_tensor_tensor(out=accs[t].ap[:, hs], in0=ps.ap, scalar=gates.ap[:, t, ex:ex + 1], in1=accs[t].ap[:, hs], op0=ALU.mult, op1=ALU.add), [accs[t]], [ps, gates, accs[t]])
                        post = hooks.get((gi, ex, 1))
                        if post:
                            post(cxh)

                def moe_C(gi, grp):
                    ntok = len(grp) * 128
                    HT, gates = HT2[gi % 2], gates2[gi % 2]
                    for t, i in enumerate(grp):
                        s_ = 1 if i < NCT else 0
                        xt = x6_pool.next()
                        fw.dma(xt.ap, xs[i * 128:(i + 1) * 128, :], [xt], [xs_t[i]])
                        fw.op(dve, lambda e, t=t, s_=s_: e.tensor_tensor(out=accs[t].ap, in0=accs[t].ap, in1=mG[s_].ap, op=ALU.mult), [accs[t]], [accs[t], mG[s_]])
                        fw.op(dve, lambda e, xt=xt, t=t: e.tensor_tensor(out=xt.ap, in0=xt.ap, in1=accs[t].ap, op=ALU.add), [xt], [xt, accs[t]])
                        if "xout" in dbg and l == 0:
                            fw.dma(dbg["xout"][i * 128:(i + 1) * 128, :], xt.ap, [], [xt])
                        if not last:
                            fw.dma(xs[i * 128:(i + 1) * 128, :], xt.ap, [xs_t[i]], [xt])
                        else:
                            st = st6_pool.next()
                            rms(xt, st, accs[t])
                            fw.op(dve, lambda e, xt=xt, st=st: e.scalar_tensor_tensor(out=xt.ap, in0=xt.ap, scalar=st.ap[:, 2:3], in1=fgt.ap, op0=ALU.mult, op1=ALU.mult), [xt], [xt, st, fgt])
                            fw.dma(out[(i - NCT) * 128:(i - NCT + 1) * 128, :], xt.ap, [], [xt])

                hooks = {}
                for t in range(len(groups[0])):
                    moe_A_back(0, groups[0], moe_A_front(0, groups[0], t))
                for gi, grp in enumerate(groups):
                    if gi + 1 < len(groups):
                        ng = groups[gi + 1]
                        for t in range(len(ng)):
                            ex = 4 + t
                            hooks[(gi, ex, 0)] = (lambda gi=gi, ng=ng, t=t: moe_A_front(gi + 1, ng, t))
                            hooks[(gi, ex, 1)] = (lambda cx, gi=gi, ng=ng: moe_A_back(gi + 1, ng, cx))
                    moe_B(gi, grp, range(NE))
                    moe_C(gi, grp)
                fw.barrier()
            if stop_after == "S6":
                break
        fw.barrier()
    return nc


def prep_inputs(inputs):
    f = lambda a: np.ascontiguousarray(np.asarray(a, dtype=np.float32))
    shared = {}
    for k in ("w_mod", "b_mod", "norm_mix_g", "norm_ffn_g", "w_in", "w_out", "att_sink", "shift_mu_prev", "shift_mu_next",
              "decay_w0", "decay_w2", "iclr_a0", "iclr_a2", "vres_v0", "vres_v1", "vres_v2", "gate_g2", "k_k", "k_a",
              "ln_x_w", "ln_x_b", "router_w", "router_b", "expert_w_gate", "expert_w_up", "expert_w_down", "final_norm_g"):
        shared[k] = f(inputs[k])
    shared["r_k"] = f(inputs["r_k"]).reshape(DEPTH, RW)
    shared["ident"] = np.eye(128, dtype=np.float32)
    s = np.arange(128)[:, None]
    t = np.arange(128)[None, :]
    shared["masks"] = np.ascontiguousarray(np.stack([s < t, s <= t, s > t, s >= t], axis=1).astype(np.float32))
    pos = np.arange(L)
    row = (pos // 64).astype(np.float32)
    colp = (pos % 64).astype(np.float32)
    inv = (10000.0 ** (-np.arange(16, dtype=np.float32) / 16)).astype(np.float32)
    ang = np.concatenate([row[:, None] * inv, row[:, None] * inv, colp[:, None] * inv, colp[:, None] * inv], axis=1)
    cos = np.cos(ang).astype(np.float32)
    sin = np.sin(ang).astype(np.float32)
    sgn = np.concatenate([-np.ones(16), np.ones(16), -np.ones(16), np.ones(16)]).astype(np.float32)
    shared["rope_cos"] = np.ascontiguousarray(np.tile(cos, (1, 10)))
    shared["rope_sin"] = np.ascontiguousarray(np.tile(sin * sgn, (1, 10)))
    x = f(inputs["x"])
    ctx = f(inputs["ctx"])
    c = f(inputs["c"])
    cc = f(inputs["c_ctx"])
    maps = []
    for b in range(8):
        m = dict(shared)
        m["x"] = x[b]
        m["ctx"] = ctx[b]
        m["ccol"] = np.ascontiguousarray(np.concatenate([c[b].reshape(8, 128).T, cc.reshape(8, 128).T], axis=1))
        maps.append(m)
    return maps


def kernel(**inputs):
    maps = prep_inputs(inputs)
    nc = build()
    res = run_bass_kernel_spmd(nc, maps, core_ids=list(range(8)))
    return np.stack([r["out"] for r in res.results], axis=0).astype(np.float32)
```

```python
from contextlib import ExitStack
import os
import numpy as np
import ml_dtypes
import concourse.bass as bass
import concourse.mybir as mybir
from concourse.bass_utils import run_bass_kernel_spmd

F32 = mybir.dt.float32
BF16 = mybir.dt.bfloat16
AF = mybir.ActivationFunctionType
ALU = mybir.AluOpType
AX = mybir.AxisListType

D = 1024
L = 4096
C = 256
T = L + C
NT = T // 128
NCT = C // 128
DEPTH = 2
IN_COLS = 2688
RW = 512
RWC = 1920
NE = 16
ATT_SCALE = 0.125
NEG = -30000.0


class Eng:
    def __init__(self, fw, e, name, ring=0):
        self.fw = fw
        self.e = e
        self.name = name
        self.count = 0
        self.seen = {}
        self.sem = fw.nc.alloc_semaphore("s_" + name)
        self.ring = [fw.nc.alloc_semaphore("r_%s%d" % (name, i)) for i in range(ring)]
        self.ndma = 0

    def wait_tok(self, tok):
        sem, val, key = tok
        if self.seen.get(key, 0) >= val:
            return
        self.e.wait_ge(sem, val)
        self.seen[key] = val


class TT:
    def __init__(self, ap=None):
        self.ap = ap
        self.w = None
        self.r = {}


class FW:
    def __init__(self, nc):
        self.nc = nc
        self.pe = Eng(self, nc.tensor, "pe")
        self.act = Eng(self, nc.scalar, "act")
        self.dve = Eng(self, nc.vector, "dve")
        self.pool = Eng(self, nc.gpsimd, "pool", ring=12)
        self.sp = Eng(self, nc.sync, "sp", ring=24)
        self.engs = [self.pe, self.act, self.dve, self.pool, self.sp]
        self.ninst = 0

    def _deps(self, eng, outs, ins):
        for t in ins:
            if t.w is not None and not (eng is self.pe and t.w[2] == "pe"):
                eng.wait_tok(t.w)
        for t in outs:
            if t.w is not None and not (eng is self.pe and t.w[2] == "pe"):
                eng.wait_tok(t.w)
            for tok in t.r.values():
                if not (eng is self.pe and tok[2] == "pe"):
                    eng.wait_tok(tok)

    def _mark(self, tok, outs, ins):
        for t in ins:
            t.r[tok[2]] = tok
        for t in outs:
            t.w = tok
            t.r = {}

    def op(self, eng, fn, outs, ins):
        self._deps(eng, outs, ins)
        inst = fn(eng.e)
        eng.count += 1
        inst.then_inc(eng.sem, 1)
        self._mark((eng.sem, eng.count, eng.name), outs, ins)
        self.ninst += 1

    def dma(self, out_ap, in_ap, outs, ins, q=None):
        q = q or self.sp
        self._deps(q, outs, ins)
        K = len(q.ring)
        slot = q.ndma % K
        rnd = q.ndma // K
        key = "%s_r%d" % (q.name, slot)
        if rnd > 0:
            q.wait_tok((q.ring[slot], 16 * rnd, key))
        q.e.dma_start(out=out_ap, in_=in_ap).then_inc(q.ring[slot], 16)
        q.ndma += 1
        self._mark((q.ring[slot], 16 * (rnd + 1), key), outs, ins)
        self.ninst += 1

    def all_toks(self):
        toks = []
        for e in self.engs:
            if e.count:
                toks.append((e.sem, e.count, e.name))
            K = len(e.ring)
            for slot in range(min(K, e.ndma)):
                n = (e.ndma - 1 - slot) // K + 1
                toks.append((e.ring[slot], 16 * n, "%s_r%d" % (e.name, slot)))
        return toks

    def barrier(self):
        toks = self.all_toks()
        for e in self.engs:
            for tok in toks:
                if e is self.pe and tok[2] == "pe":
                    continue
                e.wait_tok(tok)


_uid = [0]


def _nm(name):
    _uid[0] += 1
    return "sb_%s_%d" % (name, _uid[0])


class Pool:
    def __init__(self, es, nc, name, shape, dtype, bufs):
        self.t = [TT(es.enter_context(nc.sbuf_tensor(_nm(name), shape, dtype)).ap()) for i in range(bufs)]
        self.i = 0

    def next(self):
        t = self.t[self.i % len(self.t)]
        self.i += 1
        return t


def sb(es, nc, name, shape, dtype):
    return TT(es.enter_context(nc.sbuf_tensor(_nm(name), shape, dtype)).ap())


def build(debug_outs=(), stop_after=None):
    nc = bass.Bass("TRN2", target_bir_lowering=False)
    fw = FW(nc)
    pe, act, dve, pool, sp = fw.pe, fw.act, fw.dve, fw.pool, fw.sp

    def din(name, shape, dt=F32):
        return nc.dram_tensor(name, list(shape), dt, kind="ExternalInput").ap()

    def dscr(name, shape, dt=F32):
        return nc.dram_tensor(name, list(shape), dt, kind="Internal").ap()

    x_in = din("x", [L, D])
    ctx_in = din("ctx", [C, D])
    ccol_in = din("ccol", [128, 16])
    w_mod = din("w_mod", [DEPTH, D, 6 * D])
    b_mod = din("b_mod", [DEPTH, 6 * D])
    norm_mix_g = din("norm_mix_g", [DEPTH, D])
    norm_ffn_g = din("norm_ffn_g", [DEPTH, D])
    w_in = din("w_in", [DEPTH, D, IN_COLS])
    w_out = din("w_out", [DEPTH, D, D])
    att_sink = din("att_sink", [DEPTH, 8])
    mu_prev = din("shift_mu_prev", [DEPTH, RWC])
    mu_next = din("shift_mu_next", [DEPTH, RWC])
    decay_w0 = din("decay_w0", [DEPTH, 2, RW])
    decay_w2 = din("decay_w2", [DEPTH, 2, 64, RW])
    iclr_a0 = din("iclr_a0", [DEPTH, 2, RW])
    iclr_a2 = din("iclr_a2", [DEPTH, 2, 64, RW])
    vres_v0 = din("vres_v0", [1, RW])
    vres_v1 = din("vres_v1", [1, RW, 32])
    vres_v2 = din("vres_v2", [1, 32, RW])
    gate_g2 = din("gate_g2", [DEPTH, 128, RW])
    k_k = din("k_k", [DEPTH, RW])
    k_a = din("k_a", [DEPTH, RW])
    r_k = din("r_k", [DEPTH, RW])
    ln_x_w = din("ln_x_w", [DEPTH, RW])
    ln_x_b = din("ln_x_b", [DEPTH, RW])
    router_w = din("router_w", [D, NE])
    router_b = din("router_b", [NE])
    e_gate = din("expert_w_gate", [DEPTH, NE, D, D])
    e_up = din("expert_w_up", [DEPTH, NE, D, D])
    e_down = din("expert_w_down", [DEPTH, NE, D, D])
    final_g = din("final_norm_g", [D])
    ident_in = din("ident", [128, 128])
    masks_in = din("masks", [128, 4, 128])
    rope_cos = din("rope_cos", [L, 640])
    rope_sin = din("rope_sin", [L, 640])
    out = nc.dram_tensor("out", [L, D], F32, kind="ExternalOutput").ap()

    xs = dscr("xs", [T, D])
    xs_t = [TT() for _ in range(NT)]
    modbuf = dscr("modbuf", [DEPTH, 12, 128, D])
    mod_t = [[TT() for _ in range(12)] for _ in range(DEPTH)]
    u_scr = dscr("u_scr", [T + 4, RWC])
    u_t = [TT() for _ in range(NT)]
    u_guard = TT()
    mix = dscr("mix", [T, D])
    mix_a = [TT() for _ in range(NT)]
    mix_r = [TT() for _ in range(NT)]
    vfirst = dscr("vfirst", [T, RW])
    vf_t = [TT() for _ in range(NT)]
    feat = dscr("feat", [T, 5632])
    feat_t = [TT() for _ in range(NT)]
    y_scr = dscr("y_scr", [2, T, RW])
    y_t = [[TT() for _ in range(NT)] for _ in range(2)]
    dbg = {}
    for name, shape in debug_outs:
        dbg[name] = nc.dram_tensor("dbg_" + name, list(shape), F32, kind="ExternalOutput").ap()

    def urow(i):
        return 1 + i * 128 if i < NCT else 3 + i * 128

    PS = [TT(nc.alloc_psum_tensor("psb%d" % i, [128, 512], F32).ap()) for i in range(8)]
    psi = [0]

    def psum():
        t = PS[psi[0] % 6]
        psi[0] += 1
        return t

    with ExitStack() as g_es:
        ident = sb(g_es, nc, "ident", [128, 128], F32)
        identb = sb(g_es, nc, "identb", [128, 128], BF16)
        masks = sb(g_es, nc, "masks", [128, 4, 128], F32)
        fw.dma(ident.ap, ident_in, [ident], [])
        fw.dma(masks.ap, masks_in, [masks], [])
        fw.op(dve, lambda e: e.tensor_copy(out=identb.ap, in_=ident.ap), [identb], [ident])
        with ExitStack() as es0:
            zrow = sb(es0, nc, "zrow", [1, RWC], F32)
            fw.op(dve, lambda e: e.memset(zrow.ap, 0.0), [zrow], [])
            for r in (0, C + 1, C + 2, T + 3):
                fw.dma(u_scr[r:r + 1, :], zrow.ap, [u_guard], [zrow])
            fw.barrier()

        for i in range(NT):
            src = ctx_in[i * 128:(i + 1) * 128, :] if i < NCT else x_in[(i - NCT) * 128:(i - NCT + 1) * 128, :]
            fw.dma(xs[i * 128:(i + 1) * 128, :], src, [xs_t[i]], [])

        for l in range(DEPTH):
            last = l == DEPTH - 1
            with ExitStack() as es:
                ccol = sb(es, nc, "ccol", [128, 16], F32)
                csil = sb(es, nc, "csil", [128, 16], F32)
                crep = sb(es, nc, "crep", [128, 16, 128], F32)
                fw.dma(ccol.ap, ccol_in, [ccol], [])
                fw.op(act, lambda e: e.activation(out=csil.ap, in_=ccol.ap, func=AF.Silu), [csil], [ccol])
                fw.op(dve, lambda e: e.tensor_copy(out=crep.ap, in_=csil.ap.unsqueeze(2).to_broadcast([128, 16, 128])), [crep], [csil])
                wm_pool = Pool(es, nc, "wm", [128, 8, 512], F32, 2)
                bm_pool = Pool(es, nc, "bm", [128, 512], F32, 2)
                modraw = [[sb(es, nc, "mr%d_%d" % (s, j), [128, D], F32) for j in range(6)] for s in range(2)]
                gmix = sb(es, nc, "gmix", [128, D], F32)
                gffn = sb(es, nc, "gffn", [128, D], F32)
                fw.dma(gmix.ap, norm_mix_g[l].partition_broadcast(128), [gmix], [])
                fw.dma(gffn.ap, norm_ffn_g[l].partition_broadcast(128), [gffn], [])
                for j in range(12):
                    wm = wm_pool.next()
                    bm = bm_pool.next()
                    fw.dma(wm.ap, w_mod[l][:, j * 512:(j + 1) * 512].rearrange("(kc p) n -> p kc n", p=128), [wm], [])
                    fw.dma(bm.ap, b_mod[l][j * 512:(j + 1) * 512].partition_broadcast(128), [bm], [])
                    for s in range(2):
                        ps = psum()
                        for kc in range(8):
                            fw.op(pe, lambda e, kc=kc, s=s, ps=ps, wm=wm: e.matmul(ps.ap, lhsT=crep.ap[:, s * 8 + kc, :], rhs=wm.ap[:, kc, :], start=(kc == 0), stop=(kc == 7)), [ps], [crep, wm])
                        dst = modraw[s][j // 2]
                        fw.op(dve, lambda e, ps=ps, dst=dst, bm=bm, j=j: e.tensor_tensor(out=dst.ap[:, (j % 2) * 512:(j % 2 + 1) * 512], in0=ps.ap, in1=bm.ap, op=ALU.add), [dst], [ps, bm])
                for s in range(2):
                    sh_a, sc_a, ga, sh_f, sc_f, gf = modraw[s]
                    fw.op(dve, lambda e, sc_a=sc_a: e.scalar_tensor_tensor(out=sc_a.ap, in0=sc_a.ap, scalar=1.0, in1=gmix.ap, op0=ALU.add, op1=ALU.mult), [sc_a], [sc_a, gmix])
                    fw.op(dve, lambda e, sc_f=sc_f: e.scalar_tensor_tensor(out=sc_f.ap, in0=sc_f.ap, scalar=1.0, in1=gffn.ap, op0=ALU.add, op1=ALU.mult), [sc_f], [sc_f, gffn])
                    for idx, t in enumerate((sc_a, sh_a, ga, sc_f, sh_f, gf)):
                        fw.dma(modbuf[l, s * 6 + idx], t.ap, [mod_t[l][s * 6 + idx]], [t])
                fw.barrier()
            if stop_after == "S1":
                break

            with ExitStack() as es_att:
                QT = sb(es_att, nc, "QT", [128, 4, T], BF16)
                KT = sb(es_att, nc, "KT", [128, T], BF16)
                VV = sb(es_att, nc, "VV", [128, NT, 128], BF16)
                qt_t = [TT() for _ in range(NT)]
                with ExitStack() as es:
                    winb = sb(es, nc, "winb", [128, 8, IN_COLS], BF16)
                    wst_pool = Pool(es, nc, "wst", [128, 8, 384], F32, 1)
                    for j in range(7):
                        wst = wst_pool.next()
                        fw.dma(wst.ap, w_in[l][:, j * 384:(j + 1) * 384].rearrange("(kc p) n -> p kc n", p=128), [wst], [])
                        fw.op(pool, lambda e, wst=wst, j=j: e.tensor_copy(out=winb.ap[:, :, j * 384:(j + 1) * 384], in_=wst.ap), [winb], [wst])
                    modA = [sb(es, nc, "modA%d" % s, [128, D], F32) for s in range(2)]
                    modB = [sb(es, nc, "modB%d" % s, [128, D], F32) for s in range(2)]
                    for s in range(2):
                        fw.dma(modA[s].ap, modbuf[l, s * 6 + 0], [modA[s]], [mod_t[l][s * 6 + 0]])
                        fw.dma(modB[s].ap, modbuf[l, s * 6 + 1], [modB[s]], [mod_t[l][s * 6 + 1]])
                    x_pool = Pool(es, nc, "xt", [128, D], F32, 3)
                    junk = sb(es, nc, "junk", [128, D], F32)
                    st_pool = Pool(es, nc, "st", [128, 4], F32, 2)
                    h_pool = Pool(es, nc, "ht", [128, D], BF16, 2)
                    hT_pool = Pool(es, nc, "hT", [128, 8, 128], BF16, 3)
                    pr_pool = Pool(es, nc, "pr", [128, IN_COLS], F32, 2)
                    cos_pool = Pool(es, nc, "cos", [128, 640], F32, 3)
                    sin_pool = Pool(es, nc, "sin", [128, 640], F32, 3)
                    rp_pool = Pool(es, nc, "rp", [128, 640], F32, 2)
                    rq_pool = Pool(es, nc, "rq", [128, 640], BF16, 2)

                    def s2_load(i):
                        xt = x_pool.next()
                        fw.dma(xt.ap, xs[i * 128:(i + 1) * 128, :], [xt], [xs_t[i]])
                        cs = sn = None
                        if i >= NCT:
                            cs = cos_pool.next()
                            sn = sin_pool.next()
                            r0 = (i - NCT) * 128
                            fw.dma(cs.ap, rope_cos[r0:r0 + 128, :], [cs], [])
                            fw.dma(sn.ap, rope_sin[r0:r0 + 128, :], [sn], [])
                        return xt, cs, sn

                    def s2_front(i, xt, cs, sn):
                        s = 1 if i < NCT else 0
                        st = st_pool.next()
                        fw.op(act, lambda e, xt=xt, st=st: e.activation(out=junk.ap, in_=xt.ap, func=AF.Square, accum_out=st.ap[:, 0:1]), [junk, st], [xt])
                        fw.op(dve, lambda e, st=st: e.tensor_scalar(out=st.ap[:, 1:2], in0=st.ap[:, 0:1], scalar1=1.0 / D, scalar2=1e-6, op0=ALU.mult, op1=ALU.add), [st], [st])
                        fw.op(act, lambda e, st=st: e.activation(out=st.ap[:, 3:4], in_=st.ap[:, 1:2], func=AF.Sqrt), [st], [st])
                        fw.op(dve, lambda e, st=st: e.reciprocal(out=st.ap[:, 2:3], in_=st.ap[:, 3:4]), [st], [st])
                        fw.op(dve, lambda e, xt=xt, st=st, s=s: e.scalar_tensor_tensor(out=xt.ap, in0=xt.ap, scalar=st.ap[:, 2:3], in1=modA[s].ap, op0=ALU.mult, op1=ALU.mult), [xt], [xt, st, modA[s]])
                        ht = h_pool.next()
                        fw.op(dve, lambda e, xt=xt, ht=ht, s=s: e.tensor_tensor(out=ht.ap, in0=xt.ap, in1=modB[s].ap, op=ALU.add), [ht], [xt, modB[s]])
                        hT = hT_pool.next()
                        for half in range(2):
                            ps = psum()
                            psb = ps.ap.bitcast(BF16)
                            for kk in range(4):
                                kc = half * 4 + kk
                                fw.op(pe, lambda e, kc=kc, kk=kk, psb=psb, ht=ht: e.transpose(out=psb[:, kk * 128:(kk + 1) * 128], in_=ht.ap[:, kc * 128:(kc + 1) * 128], identity=identb.ap), [ps], [ht, identb])
                            fw.op(act if half else dve, lambda e, psb=psb, hT=hT, half=half: (e.copy if half else e.tensor_copy)(out=hT.ap[:, half * 4:(half + 1) * 4, :], in_=psb[:, 0:512].rearrange("p (a b) -> p a b", a=4)), [hT], [ps])
                        return dict(i=i, hT=hT, cs=cs, sn=sn)

                    def s2_B(cx):
                        i, hT, cs, sn = cx['i'], cx['hT'], cx['cs'], cx['sn']
                        pr = pr_pool.next()
                        col = 0
                        ci = 0
                        while col < IN_COLS:
                            n = min(512, IN_COLS - col)
                            ps = psum()
                            for kc in range(8):
                                fw.op(pe, lambda e, kc=kc, ps=ps, hT=hT, col=col, n=n: e.matmul(ps.ap[:, 0:n], lhsT=hT.ap[:, kc, :], rhs=winb.ap[:, kc, col:col + n], start=(kc == 0), stop=(kc == 7)), [ps], [hT, winb])
                            fw.op(act, lambda e, ps=ps, pr=pr, col=col, n=n: e.copy(out=pr.ap[:, col:col + n], in_=ps.ap[:, 0:n]), [pr], [ps])
                            col += n
                            ci += 1
                        if "proj" in dbg and l == 0:
                            fw.dma(dbg["proj"][i * 128:(i + 1) * 128, :], pr.ap, [], [pr])
                        fw.dma(u_scr[urow(i):urow(i) + 128, :], pr.ap[:, 768:IN_COLS], [u_t[i]], [pr])
                        cx['pr'] = pr

                    def s2_C(cx):
                        i, cs, sn, pr = cx['i'], cx['cs'], cx['sn'], cx['pr']
                        rq = rq_pool.next()
                        if i >= NCT:
                            rp = rp_pool.next()
                            v5 = lambda ap: ap.rearrange("p (m h f) -> p m h f", h=2, f=16)
                            fw.op(dve, lambda e, rp=rp, pr=pr, sn=sn: e.tensor_tensor(out=v5(rp.ap)[:, :, 0, :], in0=v5(pr.ap[:, 0:640])[:, :, 1, :], in1=v5(sn.ap)[:, :, 0, :], op=ALU.mult), [rp], [pr, sn])
                            fw.op(dve, lambda e, rp=rp, pr=pr, sn=sn: e.tensor_tensor(out=v5(rp.ap)[:, :, 1, :], in0=v5(pr.ap[:, 0:640])[:, :, 0, :], in1=v5(sn.ap)[:, :, 1, :], op=ALU.mult), [rp], [pr, sn])
                            fw.op(pool, lambda e, cs=cs, pr=pr: e.tensor_tensor(out=cs.ap, in0=pr.ap[:, 0:640], in1=cs.ap, op=ALU.mult), [cs], [pr, cs])
                            gq = lambda ap: ap.rearrange("p (g j d) -> p g j d", g=2, j=4)
                            jq = lambda ap: ap.rearrange("p (j g d) -> p g j d", g=2, j=4)
                            fw.op(dve, lambda e, rq=rq, rp=rp, cs=cs: e.tensor_tensor(out=jq(rq.ap[:, 0:512]), in0=gq(rp.ap[:, 0:512]), in1=gq(cs.ap[:, 0:512]), op=ALU.add), [rq], [rp, cs])
                            fw.op(dve, lambda e, rq=rq, rp=rp, cs=cs: e.tensor_tensor(out=rq.ap[:, 512:640], in0=rp.ap[:, 512:640], in1=cs.ap[:, 512:640], op=ALU.add), [rq], [rp, cs])
                        else:
                            gq = lambda ap: ap.rearrange("p (g j d) -> p g j d", g=2, j=4)
                            jq = lambda ap: ap.rearrange("p (j g d) -> p g j d", g=2, j=4)
                            fw.op(dve, lambda e, rq=rq, pr=pr: e.tensor_copy(out=jq(rq.ap[:, 0:512]), in_=gq(pr.ap[:, 0:512])), [rq], [pr])
                            fw.op(dve, lambda e, rq=rq, pr=pr: e.tensor_copy(out=rq.ap[:, 512:640], in_=pr.ap[:, 512:640]), [rq], [pr])
                        fw.op(act, lambda e, pr=pr, i=i: e.copy(out=VV.ap[:, i, :], in_=pr.ap[:, 640:768]), [qt_t[i]], [pr])
                        ps = psum()
                        psb = ps.ap.bitcast(BF16)
                        for j in range(4):
                            fw.op(pe, lambda e, j=j, psb=psb, rq=rq: e.transpose(out=psb[:, j * 128:(j + 1) * 128], in_=rq.ap[:, j * 128:(j + 1) * 128], identity=identb.ap), [ps], [rq, identb])
                        fw.op(pe, lambda e, psb=psb, rq=rq: e.transpose(out=psb[:, 512:640], in_=rq.ap[:, 512:640], identity=identb.ap), [ps], [rq, identb])
                        fw.op(dve, lambda e, psb=psb, i=i: e.tensor_copy(out=QT.ap[:, :, i * 128:(i + 1) * 128], in_=psb[:, 0:512].rearrange("p (j t) -> p j t", j=4)), [qt_t[i]], [ps])
                        fw.op(act, lambda e, psb=psb, i=i: e.copy(out=KT.ap[:, i * 128:(i + 1) * 128], in_=psb[:, 512:640]), [qt_t[i]], [ps])

                    lds = {0: s2_load(0), 1: s2_load(1)}
                    cxn = s2_front(0, *lds.pop(0))
                    for i in range(NT):
                        cxc = cxn
                        if i + 2 < NT:
                            lds[i + 2] = s2_load(i + 2)
                        s2_B(cxc)
                        if i + 1 < NT:
                            cxn = s2_front(i + 1, *lds.pop(i + 1))
                        s2_C(cxc)
                    fw.barrier()
                if stop_after == "S2":
                    break
                with ExitStack() as es:
                    sinkb = sb(es, nc, "sinkb", [128, 8], F32)
                    fw.dma(sinkb.ap, att_sink[l].partition_broadcast(128), [sinkb], [])
                    mb = sb(es, nc, "mb", [128, 384], F32)
                    fw.op(dve, lambda e: e.memset(mb.ap, 0.0), [mb], [])
                    fw.op(dve, lambda e: e.tensor_scalar(out=mb.ap[:, 0:128], in0=masks.ap[:, 1, :], scalar1=-1.0, scalar2=-NEG, op0=ALU.add, op1=ALU.mult), [mb], [masks])
                    fw.op(dve, lambda e: e.tensor_scalar(out=mb.ap[:, 256:384], in0=masks.ap[:, 3, :], scalar1=-1.0, scalar2=-NEG, op0=ALU.add, op1=ALU.mult), [mb], [masks])
                    s_pool = Pool(es, nc, "ssb", [128, 640], F32, 3)
                    p_pool = Pool(es, nc, "psb", [128, 640], BF16, 4)
                    pT_pool = Pool(es, nc, "pTs", [128, 640], BF16, 2)
                    ast_pool = Pool(es, nc, "ast", [128, 8], F32, 6)
                    mixa_pool = Pool(es, nc, "mixa", [128, 512], F32, 3)
                    tiles3 = list(range(NCT, NT) if last else range(NT))
                    tinfo = {}
                    for i in tiles3:
                        if i < NCT:
                            nloc, lo, hi, moff = 0, 0, 0, 0
                            kblocks = list(range(NCT))
                        else:
                            lo, hi = max(NCT, i - 1), min(NT - 1, i + 1)
                            nloc = (hi - lo + 1) * 128
                            moff = 0 if lo == i - 1 else 128
                            kblocks = list(range(lo, hi + 1)) + list(range(NCT))
                        tinfo[i] = dict(nloc=nloc, lo=lo, hi=hi, moff=moff, kblocks=kblocks, ncols=nloc + C, mixa=None, ops=PS[6 + (i % 2)])

                    def att_front(i, h):
                        ti = tinfo[i]
                        nloc, lo, hi, moff, ncols = ti["nloc"], ti["lo"], ti["hi"], ti["moff"], ti["ncols"]
                        if ti["mixa"] is None:
                            ti["mixa"] = mixa_pool.next()
                        g, j = h // 4, h % 4
                        lhsT = QT.ap[g * 64:(g + 1) * 64, j, i * 128:(i + 1) * 128]
                        ssb = s_pool.next()
                        st = ast_pool.next()
                        if nloc:
                            ps1 = psum()
                            fw.op(pe, lambda e: e.matmul(ps1.ap[:, 0:nloc], lhsT=lhsT, rhs=KT.ap[g * 64:(g + 1) * 64, lo * 128:(hi + 1) * 128], start=True, stop=True), [ps1], [qt_t[i]])
                            fw.op(dve, lambda e: e.scalar_tensor_tensor(out=ssb.ap[:, 0:nloc], in0=ps1.ap[:, 0:nloc], scalar=ATT_SCALE, in1=mb.ap[:, moff:moff + nloc], op0=ALU.mult, op1=ALU.add), [ssb], [ps1, mb])
                        ps2 = psum()
                        fw.op(pe, lambda e: e.matmul(ps2.ap[:, 0:C], lhsT=lhsT, rhs=KT.ap[g * 64:(g + 1) * 64, 0:C], start=True, stop=True), [ps2], [qt_t[i]])
                        fw.op(act, lambda e: e.activation(out=ssb.ap[:, nloc:ncols], in_=ps2.ap[:, 0:C], func=AF.Copy, scale=ATT_SCALE), [ssb], [ps2])
                        fw.op(dve, lambda e: e.reduce_max(out=st.ap[:, 0:1], in_=ssb.ap[:, 0:ncols], axis=AX.X), [st], [ssb])
                        fw.op(dve, lambda e: e.tensor_scalar(out=st.ap[:, 1:2], in0=st.ap[:, 0:1], scalar1=sinkb.ap[:, h:h + 1], scalar2=-1.0, op0=ALU.max, op1=ALU.mult), [st], [st, sinkb])
                        pb = p_pool.next()
                        fw.op(act, lambda e: e.activation(out=pb.ap[:, 0:ncols], in_=ssb.ap[:, 0:ncols], func=AF.Exp, bias=st.ap[:, 1:2], accum_out=st.ap[:, 2:3]), [pb, st], [ssb, st])
                        fw.op(act, lambda e: e.activation(out=st.ap[:, 3:4], in_=sinkb.ap[:, h:h + 1], func=AF.Exp, bias=st.ap[:, 1:2]), [st], [st, sinkb])
                        fw.op(dve, lambda e: e.tensor_tensor(out=st.ap[:, 4:5], in0=st.ap[:, 2:3], in1=st.ap[:, 3:4], op=ALU.add), [st], [st])
                        fw.op(dve, lambda e: e.reciprocal(out=st.ap[:, 5:6], in_=st.ap[:, 4:5]), [st], [st])
                        return dict(i=i, h=h, g=g, pb=pb, st=st)

                    def att_back(cx):
                        i, h, g, pb, st = cx["i"], cx["h"], cx["g"], cx["pb"], cx["st"]
                        ti = tinfo[i]
                        ncols, kblocks, mixa, ops = ti["ncols"], ti["kblocks"], ti["mixa"], ti["ops"]
                        ps3 = psum()
                        ps3b = ps3.ap.bitcast(BF16)
                        nblk = ncols // 128
                        for b in range(nblk):
                            fw.op(pe, lambda e, b=b: e.transpose(out=ps3b[:, b * 128:(b + 1) * 128], in_=pb.ap[:, b * 128:(b + 1) * 128], identity=identb.ap), [ps3], [pb, identb])
                        pT = pT_pool.next()
                        if h % 2 == 0:
                            fw.op(dve, lambda e: e.tensor_copy(out=pT.ap[:, 0:ncols], in_=ps3b[:, 0:ncols]), [pT], [ps3])
                        else:
                            fw.op(act, lambda e: e.copy(out=pT.ap[:, 0:ncols], in_=ps3b[:, 0:ncols]), [pT], [ps3])
                        nb = len(kblocks)
                        for b, kb in enumerate(kblocks):
                            fw.op(pe, lambda e, b=b, kb=kb: e.matmul(ops.ap[:, h * 64:(h + 1) * 64], lhsT=pT.ap[:, b * 128:(b + 1) * 128], rhs=VV.ap[:, kb, g * 64:(g + 1) * 64], start=(b == 0), stop=(b == nb - 1)), [ops], [pT, qt_t[kb]])
                        fw.op(act, lambda e: e.activation(out=mixa.ap[:, h * 64:(h + 1) * 64], in_=ops.ap[:, h * 64:(h + 1) * 64], func=AF.Copy, scale=st.ap[:, 5:6]), [mixa], [ops, st])
                        if h == 7:
                            fw.dma(mix[i * 128:(i + 1) * 128, 0:512], mixa.ap, [mix_a[i]], [mixa])

                    units = [(i, h) for i in tiles3 for h in range(8)]
                    DEPTH_P = 2
                    q_ = [att_front(*units[k]) for k in range(min(DEPTH_P, len(units)))]
                    for n in range(len(units)):
                        if n + DEPTH_P < len(units):
                            q_.append(att_front(*units[n + DEPTH_P]))
                        att_back(q_.pop(0))
                    fw.barrier()
            if stop_after == "S3":
                if "mix" in dbg:
                    fw.barrier()
                    for i in range(NT):
                        fw.dma(dbg["mix"][i * 128:(i + 1) * 128, :], mix[i * 128:(i + 1) * 128, :], [], [mix_a[i], mix_r[i]])
                break
            FV, FR, FG, FKK, FBON = 0, 512, 1024, 1536, 2048
            FLW = lambda d_: 2560 + d_ * 1536
            FB = lambda d_: 2560 + d_ * 1536 + 512
            FKD = lambda d_: 2560 + d_ * 1536 + 1024
            NF = 5632
            with ExitStack() as es:
                def bc(name, src, n):
                    t_ = sb(es, nc, name, [128, n], F32)
                    fw.dma(t_.ap, src.partition_broadcast(128), [t_], [])
                    return t_
                mup = bc("mup", mu_prev[l], RWC)
                mun = bc("mun", mu_next[l], RWC)
                w0b = bc("w0b", decay_w0[l].rearrange("d c -> (d c)"), 1024)
                a0b = bc("a0b", iclr_a0[l].rearrange("d c -> (d c)"), 1024)
                kkb = bc("kkb", k_k[l], RW)
                kab = bc("kab", k_a[l], RW)
                rkb = bc("rkb", r_k[l], RW)
                w2t = sb(es, nc, "w2t", [128, RW], F32)
                a2t = sb(es, nc, "a2t", [128, RW], F32)
                g2t = sb(es, nc, "g2t", [128, RW], F32)
                fw.dma(w2t.ap, decay_w2[l].rearrange("d r c -> (d r) c"), [w2t], [])
                fw.dma(a2t.ap, iclr_a2[l].rearrange("d r c -> (d r) c"), [a2t], [])
                fw.dma(g2t.ap, gate_g2[l], [g2t], [])
                if l > 0:
                    v0b = bc("v0b", vres_v0[0], RW)
                    v1t = sb(es, nc, "v1t", [128, 4, 32], F32)
                    fw.dma(v1t.ap, vres_v1[0].rearrange("(c p) n -> p c n", p=128), [v1t], [])
                    v2t = sb(es, nc, "v2t", [32, RW], F32)
                    fw.dma(v2t.ap, vres_v2[0], [v2t], [])
                uc_pool = Pool(es, nc, "uc", [128, RWC], F32, 3)
                up_pool = Pool(es, nc, "up", [128, RWC], F32, 2)
                un_pool = Pool(es, nc, "un", [128, RWC], F32, 2)
                vf_pool = Pool(es, nc, "vf", [128, RW], F32, 3)
                li_pool = Pool(es, nc, "li", [128, 384], F32, 2)
                liT_pool = Pool(es, nc, "liT", [128, 3, 128], F32, 2)
                fo_pool = Pool(es, nc, "fo", [128, NF], F32, 2)
                tmp_pool = Pool(es, nc, "ftmp", [128, RW], F32, 6)
                a_pool = Pool(es, nc, "fa", [128, RW], F32, 4)
                sm_pool = Pool(es, nc, "fsm", [128, 64], F32, 2)
                vT_pool = Pool(es, nc, "fvT", [128, 4, 128], F32, 2)
                h3 = lambda ap: ap.rearrange("p (h d) -> p h d", h=8)

                def s4a_load(i):
                    uc, up, un = uc_pool.next(), up_pool.next(), un_pool.next()
                    deps = [u_t[i], u_guard] + ([u_t[i - 1]] if i > 0 else []) + ([u_t[i + 1]] if i + 1 < NT else [])
                    r0 = urow(i)
                    fw.dma(uc.ap, u_scr[r0:r0 + 128, :], [uc], deps)
                    fw.dma(up.ap, u_scr[r0 - 1:r0 + 127, :], [up], deps)
                    fw.dma(un.ap, u_scr[r0 + 1:r0 + 129, :], [un], deps)
                    vf = None
                    if l > 0:
                        vf = vf_pool.next()
                        fw.dma(vf.ap, vfirst[i * 128:(i + 1) * 128, :], [vf], [vf_t[i]])
                    return uc, up, un, vf

                def s4a_front(i, uc, up, un, vf):
                    fw.op(dve, lambda e: e.tensor_tensor(out=up.ap, in0=up.ap, in1=uc.ap, op=ALU.subtract), [up], [up, uc])
                    fw.op(pool, lambda e: e.tensor_tensor(out=un.ap, in0=un.ap, in1=uc.ap, op=ALU.subtract), [un], [un, uc])
                    fw.op(dve, lambda e: e.tensor_tensor(out=up.ap, in0=up.ap, in1=mup.ap, op=ALU.mult), [up], [up, mup])
                    fw.op(pool, lambda e: e.tensor_tensor(out=un.ap, in0=un.ap, in1=mun.ap, op=ALU.mult), [un], [un, mun])
                    fw.op(dve, lambda e: e.tensor_tensor(out=uc.ap, in0=uc.ap, in1=up.ap, op=ALU.add), [uc], [uc, up])
                    fw.op(dve, lambda e: e.tensor_tensor(out=uc.ap, in0=uc.ap, in1=un.ap, op=ALU.add), [uc], [uc, un])
                    return (i, uc, vf)

                def s4a_back(i, uc, vf):
                    U = uc.ap
                    r_, k_, v_ = U[:, 0:512], U[:, 512:1024], U[:, 1024:1536]
                    li = li_pool.next()
                    fw.op(act, lambda e: e.activation(out=li.ap[:, 0:128], in_=U[:, 1536:1664], func=AF.Tanh), [li], [uc])
                    fw.op(act, lambda e: e.copy(out=li.ap[:, 128:256], in_=U[:, 1664:1792]), [li], [uc])
                    fw.op(act, lambda e: e.activation(out=li.ap[:, 256:384], in_=U[:, 1792:1920], func=AF.Sigmoid), [li], [uc])
                    ps = psum()
                    for q in range(3):
                        fw.op(pe, lambda e, q=q, ps=ps: e.transpose(out=ps.ap[:, q * 128:(q + 1) * 128], in_=li.ap[:, q * 128:(q + 1) * 128], identity=ident.ap), [ps], [li, ident])
                    liT = liT_pool.next()
                    fw.op(dve, lambda e, ps=ps: e.tensor_copy(out=liT.ap, in_=ps.ap[:, 0:384].rearrange("p (a b) -> p a b", a=3)), [liT], [ps])
                    fo = fo_pool.next()
                    FO = fo.ap
                    a_d = []
                    for d_ in range(2):
                        hp = slice(d_ * 64, (d_ + 1) * 64)
                        ps = psum()
                        fw.op(pe, lambda e, ps=ps, hp=hp: e.matmul(ps.ap, lhsT=liT.ap[hp, 0, :], rhs=w2t.ap[hp, :], start=True, stop=True), [ps], [liT, w2t])
                        t1 = tmp_pool.next()
                        fw.op(dve, lambda e, ps=ps, t1=t1, d_=d_: e.tensor_tensor(out=t1.ap, in0=ps.ap, in1=w0b.ap[:, d_ * 512:(d_ + 1) * 512], op=ALU.add), [t1], [ps, w0b])
                        fw.op(act, lambda e, t1=t1: e.activation(out=t1.ap, in_=t1.ap, func=AF.Sigmoid), [t1], [t1])
                        fw.op(act, lambda e, t1=t1, d_=d_: e.activation(out=FO[:, FLW(d_):FLW(d_) + 512], in_=t1.ap, func=AF.Copy, scale=-0.6065306597126334), [fo], [t1])
                        ps = psum()
                        fw.op(pe, lambda e, ps=ps, hp=hp: e.matmul(ps.ap, lhsT=liT.ap[hp, 1, :], rhs=a2t.ap[hp, :], start=True, stop=True), [ps], [liT, a2t])
                        ad = a_pool.next()
                        fw.op(dve, lambda e, ps=ps, ad=ad, d_=d_: e.tensor_tensor(out=ad.ap, in0=ps.ap, in1=a0b.ap[:, d_ * 512:(d_ + 1) * 512], op=ALU.add), [ad], [ps, a0b])
                        fw.op(act, lambda e, ad=ad: e.activation(out=ad.ap, in_=ad.ap, func=AF.Sigmoid), [ad], [ad])
                        a_d.append(ad)
                    ps = psum()
                    fw.op(pe, lambda e, ps=ps: e.matmul(ps.ap, lhsT=liT.ap[:, 2, :], rhs=g2t.ap, start=True, stop=True), [ps], [liT, g2t])
                    fw.op(act, lambda e, ps=ps: e.copy(out=FO[:, FG:FG + 512], in_=ps.ap), [fo], [ps])
                    if l == 0:
                        fw.op(act, lambda e: e.copy(out=FO[:, FV:FV + 512], in_=v_), [fo], [uc])
                        fw.dma(vfirst[i * 128:(i + 1) * 128, :], v_, [vf_t[i]], [uc])
                    else:
                        ps = psum()
                        for c_ in range(4):
                            fw.op(pe, lambda e, c_=c_, ps=ps: e.transpose(out=ps.ap[:, c_ * 128:(c_ + 1) * 128], in_=U[:, 1024 + c_ * 128:1024 + (c_ + 1) * 128], identity=ident.ap), [ps], [uc, ident])
                        vT = vT_pool.next()
                        fw.op(dve, lambda e, ps=ps, vT=vT: e.tensor_copy(out=vT.ap, in_=ps.ap.rearrange("p (a b) -> p a b", a=4)), [vT], [ps])
                        ps = psum()
                        for c_ in range(4):
                            fw.op(pe, lambda e, c_=c_, ps=ps, vT=vT: e.matmul(ps.ap[:, 0:32], lhsT=vT.ap[:, c_, :], rhs=v1t.ap[:, c_, :], start=(c_ == 0), stop=(c_ == 3)), [ps], [vT, v1t])
                        sm = sm_pool.next()
                        fw.op(dve, lambda e, ps=ps, sm=sm: e.tensor_copy(out=sm.ap[:, 0:32], in_=ps.ap[:, 0:32]), [sm], [ps])
                        ps = psum()
                        fw.op(pe, lambda e, ps=ps, sm=sm: e.transpose(out=ps.ap[0:32, 0:128], in_=sm.ap[:, 0:32], identity=ident.ap), [ps], [sm, ident])
                        smT = tmp_pool.next()
                        fw.op(dve, lambda e, ps=ps, smT=smT: e.tensor_copy(out=smT.ap[0:32, 0:128], in_=ps.ap[0:32, 0:128]), [smT], [ps])
                        ps = psum()
                        fw.op(pe, lambda e, ps=ps, smT=smT: e.matmul(ps.ap, lhsT=smT.ap[0:32, 0:128], rhs=v2t.ap, start=True, stop=True), [ps], [smT, v2t])
                        t1 = tmp_pool.next()
                        fw.op(dve, lambda e, ps=ps, t1=t1: e.tensor_tensor(out=t1.ap, in0=ps.ap, in1=v0b.ap, op=ALU.add), [t1], [ps, v0b])
                        fw.op(act, lambda e, t1=t1: e.activation(out=t1.ap, in_=t1.ap, func=AF.Sigmoid), [t1], [t1])
                        fw.op(dve, lambda e, vf=vf: e.tensor_tensor(out=vf.ap, in0=vf.ap, in1=v_, op=ALU.subtract), [vf], [vf, uc])
                        fw.op(dve, lambda e, vf=vf, t1=t1: e.tensor_tensor(out=vf.ap, in0=vf.ap, in1=t1.ap, op=ALU.mult), [vf], [vf, t1])
                        fw.op(dve, lambda e, vf=vf: e.tensor_tensor(out=FO[:, FV:FV + 512], in0=vf.ap, in1=v_, op=ALU.add), [fo], [vf, uc])
                    fw.op(act, lambda e: e.copy(out=FO[:, FR:FR + 512], in_=r_), [fo], [uc])
                    kk0 = tmp_pool.next()
                    sq = tmp_pool.next()
                    sm = sm_pool.next()
                    fw.op(dve, lambda e, kk0=kk0: e.tensor_tensor(out=kk0.ap, in0=k_, in1=kkb.ap, op=ALU.mult), [kk0], [uc, kkb])
                    fw.op(act, lambda e, kk0=kk0, sq=sq: e.activation(out=sq.ap, in_=kk0.ap, func=AF.Square), [sq], [kk0])
                    fw.op(dve, lambda e, sq=sq, sm=sm: e.reduce_sum(out=sm.ap[:, 0:8], in_=h3(sq.ap), axis=AX.X), [sm], [sq])
                    fw.op(act, lambda e, sm=sm: e.activation(out=sm.ap[:, 8:16], in_=sm.ap[:, 0:8], func=AF.Sqrt), [sm], [sm])
                    fw.op(dve, lambda e, sm=sm: e.tensor_scalar(out=sm.ap[:, 8:16], in0=sm.ap[:, 8:16], scalar1=1e-12, scalar2=None, op0=ALU.max), [sm], [sm])
                    fw.op(dve, lambda e, sm=sm: e.reciprocal(out=sm.ap[:, 16:24], in_=sm.ap[:, 8:16]), [sm], [sm])
                    fw.op(dve, lambda e, sm=sm, kk0=kk0: e.tensor_tensor(out=h3(FO[:, FKK:FKK + 512]), in0=h3(kk0.ap), in1=sm.ap[:, 16:24].unsqueeze(2).to_broadcast([128, 8, 64]), op=ALU.mult), [fo], [kk0, sm])
                    for d_ in range(2):
                        t1 = tmp_pool.next()
                        fw.op(dve, lambda e, t1=t1, d_=d_: e.scalar_tensor_tensor(out=t1.ap, in0=a_d[d_].ap, scalar=-1.0, in1=kab.ap, op0=ALU.add, op1=ALU.mult), [t1], [a_d[d_], kab])
                        fw.op(dve, lambda e, t1=t1, d_=d_: e.scalar_tensor_tensor(out=FO[:, FKD(d_):FKD(d_) + 512], in0=t1.ap, scalar=1.0, in1=k_, op0=ALU.add, op1=ALU.mult), [fo], [t1, uc])
                        fw.op(pool, lambda e, d_=d_: e.tensor_tensor(out=FO[:, FB(d_):FB(d_) + 512], in0=FO[:, FKK:FKK + 512], in1=a_d[d_].ap, op=ALU.mult), [fo], [fo, a_d[d_]])
                    t1 = tmp_pool.next()
                    sm2 = sm_pool.next()
                    fw.op(pool, lambda e, t1=t1: e.tensor_tensor(out=t1.ap, in0=FO[:, FKD(0):FKD(0) + 512], in1=FO[:, FKD(1):FKD(1) + 512], op=ALU.add), [t1], [fo])
                    fw.op(dve, lambda e, t1=t1: e.tensor_tensor(out=t1.ap, in0=t1.ap, in1=r_, op=ALU.mult), [t1], [t1, uc])
                    fw.op(pool, lambda e, t1=t1: e.tensor_tensor(out=t1.ap, in0=t1.ap, in1=rkb.ap, op=ALU.mult), [t1], [t1, rkb])
                    fw.op(dve, lambda e, t1=t1, sm2=sm2: e.reduce_sum(out=sm2.ap[:, 0:8], in_=h3(t1.ap), axis=AX.X), [sm2], [t1])
                    fw.op(dve, lambda e, sm2=sm2: e.tensor_tensor(out=h3(FO[:, FBON:FBON + 512]), in0=h3(FO[:, FV:FV + 512]), in1=sm2.ap[:, 0:8].unsqueeze(2).to_broadcast([128, 8, 64]), op=ALU.mult), [fo], [fo, sm2])
                    fw.dma(feat[i * 128:(i + 1) * 128, :], FO, [feat_t[i]], [fo])
                    if "feat" in dbg and l == 0:
                        fw.dma(dbg["feat"][i * 128:(i + 1) * 128, :], FO[:, 0:4096], [], [fo])

                lds = {0: s4a_load(0), 1: s4a_load(1)}
                cxn = s4a_front(0, *lds.pop(0))
                for i in range(NT):
                    cxc = cxn
                    if i + 2 < NT:
                        lds[i + 2] = s4a_load(i + 2)
                    if i + 1 < NT:
                        cxn = s4a_front(i + 1, *lds.pop(i + 1))
                    s4a_back(*cxc)
                fw.barrier()
            if stop_after == "S4a":
                break
            with ExitStack() as es:
                ones = sb(es, nc, "ones", [128, 1], F32)
                fw.op(dve, lambda e: e.memset(ones.ap, 1.0), [ones], [])
                Mq1, MnT = [], []
                for d_ in range(2):
                    m1_ = sb(es, nc, "Mq1", [128, 256], F32)
                    fw.op(dve, lambda e, m1_=m1_, d_=d_: e.tensor_scalar(out=m1_.ap[:, 0:128], in0=masks.ap[:, 2 * d_, :], scalar1=-1.0, scalar2=None, op0=ALU.mult), [m1_], [masks])
                    fw.op(dve, lambda e, m1_=m1_, d_=d_: e.tensor_copy(out=m1_.ap[:, 128:256], in_=masks.ap[:, 2 * d_ + 1, :]), [m1_], [masks])
                    Mq1.append(m1_)
                    mt_ = sb(es, nc, "MnT", [128, 128], F32)
                    fw.op(dve, lambda e, mt_=mt_, d_=d_: e.tensor_scalar(out=mt_.ap, in0=masks.ap[:, 2 * (1 - d_), :], scalar1=-1.0, scalar2=None, op0=ALU.mult), [mt_], [masks])
                    MnT.append(mt_)
                Mq2 = [masks.ap[:, 2 * d_:2 * d_ + 2, :].rearrange("p a t -> p (a t)") for d_ in range(2)]
                PSQB = int(os.environ.get('S4B_PSQB', '4'))
                PSQ = [TT(PS[PSQB + k // 4].ap[:, (k % 4) * 128:(k % 4 + 1) * 128]) for k in range(16)]
                pq = [0]

                def psq():
                    t_ = PSQ[pq[0] % 16]
                    pq[0] += 1
                    return t_
                p4 = [0]

                def psumR():
                    t_ = PS[p4[0] % 8]
                    p4[0] += 1
                    return t_
                fa_pool = Pool(es, nc, "sfa", [128, 3, 512], F32, 4)
                fb_pool = Pool(es, nc, "sfb", [128, 3, 512], F32, 4)
                tm_pool = Pool(es, nc, "stm", [128, 4, 512], BF16, 2)
                vb_pool = Pool(es, nc, "svb", [128, 512], BF16, 2)
                ex_pool = Pool(es, nc, "sex", [128, 3, 512], F32, 2)
                fm_pool = Pool(es, nc, "sfm", [128, 4, 4, 128], BF16, 2)
                el_pool = Pool(es, nc, "sel", [128, 4], F32, 2)
                g1_pool = Pool(es, nc, "sg1", [128, 2, 4, 128], BF16, 4)
                a2_pool = Pool(es, nc, "sa2", [128, 256], BF16, 16)
                inv_pool = Pool(es, nc, "sinv", [128, 4, 128], BF16, 32)
                invf_pool = Pool(es, nc, "sinvf", [128, 4, 128], F32, 10)
                xs_pool = Pool(es, nc, "sxs", [128, 4, 64], BF16, 4)
                nu_pool = Pool(es, nc, "snu", [128, 512], BF16, 2)
                ys_pool = Pool(es, nc, "sys", [128, 512], F32, 2)
                ST = [[[sb(es, nc, "ST", [128, 64], F32) for c_ in range(4)] for pp in range(2)] for d_ in range(2)]
                STb = [[[sb(es, nc, "STb", [128, 64], BF16) for c_ in range(4)] for pp in range(2)] for d_ in range(2)]
                for d_ in range(2):
                    for c_ in range(4):
                        fw.op(dve, lambda e, d_=d_, c_=c_: e.memset(ST[d_][0][c_].ap, 0.0), [ST[d_][0][c_]], [])
                        fw.op(dve, lambda e, d_=d_, c_=c_: e.memset(STb[d_][0][c_].ap, 0.0), [STb[d_][0][c_]], [])
                orders = [list(range(NT)), [1, 0] + list(range(NT - 1, NCT - 1, -1))]
                evi = [0]

                def evac(fn_dve, fn_act, outs, ins):
                    if evi[0] % 2:
                        fw.op(dve, fn_dve, outs, ins)
                    else:
                        fw.op(act, fn_act, outs, ins)

                def s4b_load(d_, i):
                    fa, fb = fa_pool.next(), fb_pool.next()
                    r0 = i * 128
                    fw.dma(fa.ap, feat[r0:r0 + 128, FLW(d_):FLW(d_) + 1536].rearrange("p (a c) -> p a c", a=3), [fa], [feat_t[i]])
                    fw.dma(fb.ap[:, 0:2, :], feat[r0:r0 + 128, 0:1024].rearrange("p (a c) -> p a c", a=2), [fb], [feat_t[i]])
                    fw.dma(fb.ap[:, 2, :], feat[r0:r0 + 128, FKK:FKK + 512], [fb], [feat_t[i]])
                    return fa, fb

                def step_prep(d_, j, fa, fb):
                    i = orders[d_][j]
                    pp = j % 2
                    STo, STn = ST[d_][pp], ST[d_][1 - pp]
                    SBo, SBn = STb[d_][pp], STb[d_][1 - pp]
                    vb = vb_pool.next()
                    fw.op(act, lambda e: e.copy(out=vb.ap, in_=fb.ap[:, 0, :]), [vb], [fb])
                    lw, b_, kd = fa.ap[:, 0, :], fa.ap[:, 1, :], fa.ap[:, 2, :]
                    v_, r_, kk = fb.ap[:, 0, :], fb.ap[:, 1, :], fb.ap[:, 2, :]
                    psL = psumR()
                    fw.op(pe, lambda e: e.matmul(psL.ap, lhsT=masks.ap[:, 1 + 2 * d_, :], rhs=lw, start=True, stop=True), [psL], [masks, fa])
                    ex = ex_pool.next()
                    fw.op(act, lambda e: e.activation(out=ex.ap[:, 0, :], in_=psL.ap, func=AF.Exp), [ex], [psL])
                    fw.op(act, lambda e: e.activation(out=ex.ap[:, 1, :], in_=psL.ap, func=AF.Exp, scale=-1.0), [ex], [psL])
                    fw.op(dve, lambda e: e.tensor_tensor(out=ex.ap[:, 2, :], in0=psL.ap, in1=lw, op=ALU.subtract), [ex], [psL, fa])
                    fw.op(act, lambda e: e.activation(out=ex.ap[:, 2, :], in_=ex.ap[:, 2, :], func=AF.Exp), [ex], [ex])
                    tm = tm_pool.next()
                    fw.op(dve, lambda e: e.tensor_tensor(out=tm.ap[:, 0, :], in0=kk, in1=ex.ap[:, 2, :], op=ALU.mult), [tm], [fb, ex])
                    fw.op(pool, lambda e: e.tensor_tensor(out=tm.ap[:, 1, :], in0=r_, in1=ex.ap[:, 0, :], op=ALU.mult), [tm], [fb, ex])
                    fw.op(dve, lambda e: e.tensor_tensor(out=tm.ap[:, 2, :], in0=b_, in1=ex.ap[:, 1, :], op=ALU.mult), [tm], [fa, ex])
                    fw.op(pool, lambda e: e.tensor_tensor(out=tm.ap[:, 3, :], in0=kd, in1=ex.ap[:, 1, :], op=ALU.mult), [tm], [fa, ex])
                    psE = psumR()
                    for c_ in range(4):
                        fw.op(pe, lambda e, c_=c_: e.matmul(psE.ap[:, c_:c_ + 1], lhsT=lw[:, c_ * 128:(c_ + 1) * 128], rhs=ones.ap, start=True, stop=True), [psE], [fa, ones])
                    el = el_pool.next()
                    fw.op(act, lambda e: e.activation(out=el.ap, in_=psE.ap[:, 0:4], func=AF.Exp), [el], [psE])
                    fm = fm_pool.next()
                    for c_ in range(4):
                        psT = psumR()
                        for q in range(4):
                            fw.op(pe, lambda e, c_=c_, q=q, psT=psT: e.transpose(out=psT.ap.bitcast(BF16)[:, q * 128:(q + 1) * 128], in_=tm.ap[:, q, c_ * 128:(c_ + 1) * 128], identity=identb.ap), [psT], [tm, identb])
                        evi[0] += 1
                        evac(lambda e, c_=c_, psT=psT: e.tensor_copy(out=fm.ap[:, c_, :, :], in_=psT.ap.bitcast(BF16)[:, 0:512].rearrange("p (a b) -> p a b", a=4)),
                             lambda e, c_=c_, psT=psT: e.copy(out=fm.ap[:, c_, :, :], in_=psT.ap.bitcast(BF16)[:, 0:512].rearrange("p (a b) -> p a b", a=4)), [fm], [psT])
                    H = []
                    for h in range(8):
                        c_ = h // 2
                        hp = slice((h % 2) * 64, (h % 2) * 64 + 64)
                        H.append(dict(c=c_, hp=hp, par=h % 2, k=h // 2, al=fm.ap[hp, c_, 0, :], rh=fm.ap[hp, c_, 1, :], be=fm.ap[hp, c_, 2, :], ka=fm.ap[hp, c_, 3, :],
                                      ar=fm.ap[hp, c_, 0:2, :].rearrange("p a t -> p (a t)"), vh=vb.ap[:, h * 64:(h + 1) * 64]))
                    G1 = [g1_pool.next() for _ in range(2)]
                    for h in range(8):
                        u = H[h]
                        psG = psumR()
                        fw.op(pe, lambda e, u=u, psG=psG: e.matmul(psG.ap[:, 0:256], lhsT=u["be"], rhs=u["ar"], start=True, stop=True), [psG], [fm])
                        fw.op(pe, lambda e, u=u, psG=psG: e.matmul(psG.ap[:, 256:512], lhsT=u["ka"], rhs=u["ar"], start=True, stop=True), [psG], [fm])
                        a2 = a2_pool.next()
                        g1 = G1[u["par"]]
                        fw.op(dve, lambda e, u=u, psG=psG, g1=g1: e.tensor_tensor(out=g1.ap[:, :, u["k"], :], in0=psG.ap[:, 0:256].rearrange("p (a t) -> p a t", a=2), in1=Mq1[d_].ap.rearrange("p (a t) -> p a t", a=2), op=ALU.mult), [g1], [psG, Mq1[d_]])
                        fw.op(dve, lambda e, a2=a2, psG=psG: e.tensor_tensor(out=a2.ap, in0=psG.ap[:, 256:512], in1=Mq2[d_], op=ALU.mult), [a2], [psG, masks])
                        u.update(a2=a2, abr=g1.ap[:, 1, u["k"], :], g1=g1)
                    GP = []
                    for par in range(2):
                        g1 = G1[par]
                        Mb = inv_pool.next()
                        fw.op(dve, lambda e, Mb=Mb, g1=g1: e.tensor_tensor(out=Mb.ap, in0=g1.ap[:, 0, :, :], in1=ident.ap.unsqueeze(1).to_broadcast([128, 4, 128]), op=ALU.add), [Mb], [g1, ident])
                        bank = psumR()
                        bkb = bank.ap.bitcast(BF16)
                        for k_ in range(4):
                            fw.op(pe, lambda e, k_=k_, bkb=bkb, g1=g1: e.transpose(out=bkb[:, k_ * 128:(k_ + 1) * 128], in_=g1.ap[:, 0, k_, :], identity=identb.ap), [bank], [g1, identb])
                        AT = inv_pool.next()
                        evi[0] += 1
                        evac(lambda e, AT=AT, bkb=bkb: e.tensor_copy(out=f4(AT.ap), in_=bkb[:, 0:512]), lambda e, AT=AT, bkb=bkb: e.copy(out=f4(AT.ap), in_=bkb[:, 0:512]), [AT], [bank])
                        GP.append(dict(A=g1, Aap=g1.ap[:, 0, :, :], AT=AT, Mb=Mb))
                    return dict(d_=d_, i=i, STo=STo, STn=STn, tm=tm, el=el, fm=fm, fb=fb, v_=v_, H=H, GP=GP, SBo=SBo, SBn=SBn, vb=vb)

                f4 = lambda ap: ap.rearrange("p k t -> p (k t)")

                def inv_all(GP):
                    for it in range(6):
                        for g in GP:
                            bank = psumR()
                            for k_ in range(4):
                                fw.op(pe, lambda e, k_=k_, bank=bank, g=g: e.matmul(bank.ap[:, k_ * 128:(k_ + 1) * 128], lhsT=g["Aap"][:, k_, :], rhs=g["AT"].ap[:, k_, :], start=True, stop=True), [bank], [g["A"], g["AT"]])
                            ATn = inv_pool.next()
                            evi[0] += 1
                            evac(lambda e, ATn=ATn, bank=bank: e.tensor_copy(out=f4(ATn.ap), in_=bank.ap), lambda e, ATn=ATn, bank=bank: e.copy(out=f4(ATn.ap), in_=bank.ap), [ATn], [bank])
                            g["ATn"] = ATn
                            if it < 5:
                                bank = psumR()
                                for k_ in range(4):
                                    fw.op(pe, lambda e, k_=k_, bank=bank, g=g: e.matmul(bank.ap[:, k_ * 128:(k_ + 1) * 128], lhsT=g["AT"].ap[:, k_, :], rhs=g["Aap"][:, k_, :], start=True, stop=True), [bank], [g["A"], g["AT"]])
                                An = inv_pool.next()
                                evi[0] += 1
                                evac(lambda e, An=An, bank=bank: e.tensor_copy(out=f4(An.ap), in_=bank.ap), lambda e, An=An, bank=bank: e.copy(out=f4(An.ap), in_=bank.ap), [An], [bank])
                                g["An"] = An
                        for g in GP:
                            g["AT"] = g["ATn"]
                            if it < 5:
                                g["A"], g["Aap"] = g["An"], g["An"].ap
                        for g in GP:
                            bank = psumR()
                            for k_ in range(4):
                                fw.op(pe, lambda e, k_=k_, bank=bank, g=g: e.matmul(bank.ap[:, k_ * 128:(k_ + 1) * 128], lhsT=identb.ap, rhs=g["Mb"].ap[:, k_, :], start=True, stop=False), [bank], [identb, g["Mb"]])
                                fw.op(pe, lambda e, k_=k_, bank=bank, g=g: e.matmul(bank.ap[:, k_ * 128:(k_ + 1) * 128], lhsT=g["AT"].ap[:, k_, :], rhs=g["Mb"].ap[:, k_, :], start=False, stop=True), [bank], [g["AT"], g["Mb"]])
                            Mbn = inv_pool.next()
                            evi[0] += 1
                            evac(lambda e, Mbn=Mbn, bank=bank: e.tensor_copy(out=f4(Mbn.ap), in_=bank.ap), lambda e, Mbn=Mbn, bank=bank: e.copy(out=f4(Mbn.ap), in_=bank.ap), [Mbn], [bank])
                            g["Mb"] = Mbn

                def step_state(cx):
                    d_, i, STo, STn, tm, el, fm, fb, v_, H, GP, SBo, SBn, vb = (cx[k] for k in ('d_', 'i', 'STo', 'STn', 'tm', 'el', 'fm', 'fb', 'v_', 'H', 'GP', 'SBo', 'SBn', 'vb'))
                    NU = nu_pool.next()
                    q4 = lambda ap: ap.rearrange("p (k t) -> p k t", k=4)[:, :, 0:64]
                    for par, g in enumerate(GP):
                        bank = psumR()
                        for k_ in range(4):
                            u = H[par + 2 * k_]
                            o = bank.ap[:, k_ * 128:k_ * 128 + 64]
                            fw.op(pe, lambda e, u=u, o=o: e.matmul(o, lhsT=u["al"], rhs=SBo[u["c"]].ap[u["hp"], :], start=True, stop=False), [bank], [fm, SBo[u["c"]]])
                            fw.op(pe, lambda e, u=u, o=o: e.matmul(o, lhsT=u["a2"].ap[:, 0:128], rhs=u["vh"], start=False, stop=True), [bank], [u["a2"], vb])
                        xsg = xs_pool.next()
                        evi[0] += 1
                        evac(lambda e, xsg=xsg, bank=bank: e.tensor_copy(out=xsg.ap, in_=q4(bank.ap)), lambda e, xsg=xsg, bank=bank: e.copy(out=xsg.ap, in_=q4(bank.ap)), [xsg], [bank])
                        g["xs"] = xsg
                    for par, g in enumerate(GP):
                        bank = psumR()
                        for k_ in range(4):
                            fw.op(pe, lambda e, k_=k_, bank=bank, g=g: e.matmul(bank.ap[:, k_ * 128:k_ * 128 + 64], lhsT=g["Mb"].ap[:, k_, :], rhs=g["xs"].ap[:, k_, :], start=True, stop=True), [bank], [g["Mb"], g["xs"]])
                        nuv = NU.ap.rearrange("p (c two d) -> p c two d", two=2, d=64)[:, :, par, :]
                        fw.op(act, lambda e, bank=bank, nuv=nuv: e.activation(out=nuv, in_=q4(bank.ap), func=AF.Copy, scale=-1.0), [NU], [bank])
                    ysb = ys_pool.next()
                    for par in (0, 1):
                        psY = psumR()
                        for k_ in range(4):
                            h = par + 2 * k_
                            u = H[h]
                            ysl = psY.ap[:, k_ * 64:(k_ + 1) * 64]
                            fw.op(pe, lambda e, u=u, ysl=ysl: e.matmul(ysl, lhsT=u["rh"], rhs=SBo[u["c"]].ap[u["hp"], :], start=True, stop=False), [psY], [fm, SBo[u["c"]]])
                            fw.op(pe, lambda e, u=u, ysl=ysl, h=h: e.matmul(ysl, lhsT=u["abr"], rhs=NU.ap[:, h * 64:(h + 1) * 64], start=False, stop=False), [psY], [u["g1"], NU])
                            fw.op(pe, lambda e, u=u, ysl=ysl: e.matmul(ysl, lhsT=u["a2"].ap[:, 128:256], rhs=u["vh"], start=False, stop=True), [psY], [u["a2"], vb])
                        yv = ysb.ap.rearrange("p (c two d) -> p c two d", two=2, d=64)[:, :, par, :]
                        fw.op(act, lambda e, psY=psY, yv=yv: e.copy(out=yv, in_=psY.ap[:, 0:256].rearrange("p (c d) -> p c d", d=64)), [ysb], [psY])
                    fw.dma(y_scr[d_, i * 128:(i + 1) * 128, :], ysb.ap, [y_t[d_][i]], [ysb])
                    psS = psumR()
                    for c_ in range(4):
                        so = psS.ap[:, c_ * 128:(c_ + 1) * 128]
                        fw.op(pe, lambda e, c_=c_, so=so: e.matmul(so, lhsT=tm.ap[:, 2, c_ * 128:(c_ + 1) * 128], rhs=NU.ap[:, c_ * 128:(c_ + 1) * 128], start=True, stop=False), [psS], [tm, NU])
                        fw.op(pe, lambda e, c_=c_, so=so: e.matmul(so, lhsT=tm.ap[:, 3, c_ * 128:(c_ + 1) * 128], rhs=vb.ap[:, c_ * 128:(c_ + 1) * 128], start=False, stop=True), [psS], [tm, vb])
                        fw.op(act, lambda e, c_=c_: e.activation(out=STn[c_].ap, in_=STo[c_].ap, func=AF.Copy, scale=el.ap[:, c_:c_ + 1]), [STn[c_]], [STo[c_], el])
                    for c_ in range(4):
                        so = psS.ap[:, c_ * 128:(c_ + 1) * 128]
                        for hh in range(2):
                            hp = slice(hh * 64, hh * 64 + 64)
                            fw.op(dve, lambda e, c_=c_, hp=hp, so=so: e.scalar_tensor_tensor(out=STn[c_].ap[hp, :], in0=so[hp, hp], scalar=el.ap[hp, c_:c_ + 1], in1=STn[c_].ap[hp, :], op0=ALU.mult, op1=ALU.add), [STn[c_]], [psS, el, STn[c_]])
                    for c_ in range(4):
                        fw.op(act, lambda e, c_=c_: e.copy(out=SBn[c_].ap, in_=STn[c_].ap), [SBn[c_]], [STn[c_]])

                nsteps = NT if not os.environ.get('S4B_STEPS') else int(os.environ['S4B_STEPS'])
                nxt = [s4b_load(d_, orders[d_][0]) for d_ in range(2)]
                for j in range(nsteps):
                    cur = list(nxt)
                    cxs = []
                    for d_ in range(2):
                        cxs.append(step_prep(d_, j, *cur[d_]))
                        if j + 1 < nsteps:
                            nxt[d_] = s4b_load(d_, orders[d_][j + 1])
                    inv_all([g for cx in cxs for g in cx["GP"]])
                    for cx in cxs:
                        step_state(cx)
                fw.barrier()
            if stop_after == "S4b":
                break
            with ExitStack() as es:
                lnw = sb(es, nc, "lnw", [128, RW], F32)
                lnb = sb(es, nc, "lnb", [128, RW], F32)
                fw.dma(lnw.ap, ln_x_w[l].partition_broadcast(128), [lnw], [])
                fw.dma(lnb.ap, ln_x_b[l].partition_broadcast(128), [lnb], [])
                y0_pool = Pool(es, nc, "cy0", [128, RW], F32, 2)
                y1_pool = Pool(es, nc, "cy1", [128, RW], F32, 2)
                gb_pool = Pool(es, nc, "cgb", [128, 2, RW], F32, 2)
                sq_pool = Pool(es, nc, "csq", [128, RW], F32, 2)
                cs_pool = Pool(es, nc, "ccs", [128, 32], F32, 2)
                h3 = lambda ap: ap.rearrange("p (h d) -> p h d", h=8)
                bc8 = lambda ap: ap.unsqueeze(2).to_broadcast([128, 8, 64])
                for i in (range(NCT, NT) if last else range(NT)):
                    y0, y1, gb = y0_pool.next(), y1_pool.next(), gb_pool.next()
                    r0 = i * 128
                    fw.dma(y0.ap, y_scr[0, r0:r0 + 128, :], [y0], [y_t[0][i]])
                    fw.dma(y1.ap, y_scr[1, r0:r0 + 128, :], [y1], [y_t[1][i]])
                    fw.dma(gb.ap[:, 0, :], feat[r0:r0 + 128, FG:FG + 512], [gb], [feat_t[i]])
                    fw.dma(gb.ap[:, 1, :], feat[r0:r0 + 128, FBON:FBON + 512], [gb], [feat_t[i]])
                    cs_ = cs_pool.next()
                    sq = sq_pool.next()
                    fw.op(dve, lambda e: e.tensor_tensor(out=y0.ap, in0=y0.ap, in1=y1.ap, op=ALU.add), [y0], [y0, y1])
                    fw.op(dve, lambda e: e.reduce_sum(out=cs_.ap[:, 0:8], in_=h3(y0.ap), axis=AX.X), [cs_], [y0])
                    fw.op(dve, lambda e: e.tensor_scalar(out=cs_.ap[:, 0:8], in0=cs_.ap[:, 0:8], scalar1=1.0 / 64, scalar2=None, op0=ALU.mult), [cs_], [cs_])
                    fw.op(dve, lambda e: e.tensor_tensor(out=h3(y0.ap), in0=h3(y0.ap), in1=bc8(cs_.ap[:, 0:8]), op=ALU.subtract), [y0], [y0, cs_])
                    fw.op(act, lambda e: e.activation(out=sq.ap, in_=y0.ap, func=AF.Square), [sq], [y0])
                    fw.op(dve, lambda e: e.reduce_sum(out=cs_.ap[:, 8:16], in_=h3(sq.ap), axis=AX.X), [cs_], [sq])
                    fw.op(dve, lambda e: e.tensor_scalar(out=cs_.ap[:, 8:16], in0=cs_.ap[:, 8:16], scalar1=1.0 / 64, scalar2=64e-5, op0=ALU.mult, op1=ALU.add), [cs_], [cs_])
                    fw.op(act, lambda e: e.activation(out=cs_.ap[:, 16:24], in_=cs_.ap[:, 8:16], func=AF.Sqrt), [cs_], [cs_])
                    fw.op(dve, lambda e: e.reciprocal(out=cs_.ap[:, 24:32], in_=cs_.ap[:, 16:24]), [cs_], [cs_])
                    fw.op(dve, lambda e: e.tensor_tensor(out=h3(y0.ap), in0=h3(y0.ap), in1=bc8(cs_.ap[:, 24:32]), op=ALU.mult), [y0], [y0, cs_])
                    fw.op(pool, lambda e: e.tensor_tensor(out=y0.ap, in0=y0.ap, in1=lnw.ap, op=ALU.mult), [y0], [y0, lnw])
                    fw.op(dve, lambda e: e.tensor_tensor(out=y0.ap, in0=y0.ap, in1=lnb.ap, op=ALU.add), [y0], [y0, lnb])
                    fw.op(pool, lambda e: e.tensor_tensor(out=y0.ap, in0=y0.ap, in1=gb.ap[:, 1, :], op=ALU.add), [y0], [y0, gb])
                    fw.op(dve, lambda e: e.tensor_tensor(out=y0.ap, in0=y0.ap, in1=gb.ap[:, 0, :], op=ALU.mult), [y0], [y0, gb])
                    fw.dma(mix[r0:r0 + 128, 512:1024], y0.ap, [mix_r[i]], [y0])
                fw.barrier()
            if stop_after == "S4":
                if "mix" in dbg:
                    for i in range(NT):
                        fw.dma(dbg["mix"][i * 128:(i + 1) * 128, :], mix[i * 128:(i + 1) * 128, :], [], [mix_a[i], mix_r[i]])
                    fw.barrier()
                break
            tiles5 = list(range(NCT, NT)) if last else list(range(NT))
            with ExitStack() as es:
                woutb = sb(es, nc, "woutb", [128, 8, D], BF16)
                wst_pool = Pool(es, nc, "wst5", [128, 8, 256], F32, 2)
                for j in range(4):
                    wst = wst_pool.next()
                    fw.dma(wst.ap, w_out[l][:, j * 256:(j + 1) * 256].rearrange("(kc p) n -> p kc n", p=128), [wst], [])
                    fw.op(pool, lambda e, wst=wst, j=j: e.tensor_copy(out=woutb.ap[:, :, j * 256:(j + 1) * 256], in_=wst.ap), [woutb], [wst])
                modG = [sb(es, nc, "modG%d" % s_, [128, D], F32) for s_ in range(2)]
                for s_ in range(2):
                    fw.dma(modG[s_].ap, modbuf[l, s_ * 6 + 2], [modG[s_]], [mod_t[l][s_ * 6 + 2]])
                mx_pool = Pool(es, nc, "mx", [128, D], F32, 2)
                mxb_pool = Pool(es, nc, "mxb", [128, D], BF16, 2)
                mT_pool = Pool(es, nc, "mT", [128, 8, 128], BF16, 2)
                x5_pool = Pool(es, nc, "x5", [128, D], F32, 2)

                def s5_load(i):
                    mx = mx_pool.next()
                    fw.dma(mx.ap, mix[i * 128:(i + 1) * 128, :], [mx], [mix_a[i], mix_r[i]])
                    xt = x5_pool.next()
                    fw.dma(xt.ap, xs[i * 128:(i + 1) * 128, :], [xt], [xs_t[i]])
                    return mx, xt

                nxt = s5_load(tiles5[0])
                for ti, i in enumerate(tiles5):
                    mx, xt = nxt
                    if ti + 1 < len(tiles5):
                        nxt = s5_load(tiles5[ti + 1])
                    s_ = 1 if i < NCT else 0
                    mxb = mxb_pool.next()
                    fw.op(act, lambda e, mx=mx, mxb=mxb: e.copy(out=mxb.ap, in_=mx.ap), [mxb], [mx])
                    mT = mT_pool.next()
                    for half in range(2):
                        ps = psum()
                        psb = ps.ap.bitcast(BF16)
                        for kk in range(4):
                            kc = half * 4 + kk
                            fw.op(pe, lambda e, kc=kc, kk=kk, psb=psb, mxb=mxb: e.transpose(out=psb[:, kk * 128:(kk + 1) * 128], in_=mxb.ap[:, kc * 128:(kc + 1) * 128], identity=identb.ap), [ps], [mxb, identb])
                        fw.op(act if half else dve, lambda e, psb=psb, mT=mT, half=half: (e.copy if half else e.tensor_copy)(out=mT.ap[:, half * 4:(half + 1) * 4, :], in_=psb[:, 0:512].rearrange("p (a b) -> p a b", a=4)), [mT], [ps])
                    for half in range(2):
                        ps = psum()
                        hs = slice(half * 512, (half + 1) * 512)
                        for kc in range(8):
                            fw.op(pe, lambda e, kc=kc, ps=ps, mT=mT, hs=hs: e.matmul(ps.ap, lhsT=mT.ap[:, kc, :], rhs=woutb.ap[:, kc, hs], start=(kc == 0), stop=(kc == 7)), [ps], [mT, woutb])
                        fw.op(dve, lambda e, ps=ps, mx=mx, hs=hs, s_=s_: e.tensor_tensor(out=mx.ap[:, hs], in0=ps.ap, in1=modG[s_].ap[:, hs], op=ALU.mult), [mx], [ps, modG[s_]])
                    fw.op(dve, lambda e, xt=xt, mx=mx: e.tensor_tensor(out=xt.ap, in0=xt.ap, in1=mx.ap, op=ALU.add), [xt], [xt, mx])
                    fw.dma(xs[i * 128:(i + 1) * 128, :], xt.ap, [xs_t[i]], [xt])
                    if "xmix" in dbg and l == 0:
                        fw.dma(dbg["xmix"][i * 128:(i + 1) * 128, :], xt.ap, [], [xt])
                fw.barrier()
            if stop_after == "S5":
                break

            tiles6 = list(range(NCT, NT)) if last else list(range(NT))
            GS = 8
            ng_ = -(-len(tiles6) // GS)
            gsz = [len(tiles6) // ng_ + (1 if k < len(tiles6) % ng_ else 0) for k in range(ng_)]
            groups, k0_ = [], 0
            for z in gsz:
                groups.append(tiles6[k0_:k0_ + z])
                k0_ += z
            with ExitStack() as es:
                nseg = 1 if last else 2
                mA = [sb(es, nc, "mA%d" % s_, [128, D], F32) for s_ in range(nseg)]
                mB = [sb(es, nc, "mB%d" % s_, [128, D], F32) for s_ in range(nseg)]
                mG = [sb(es, nc, "mG%d" % s_, [128, D], F32) for s_ in range(nseg)]
                for s_ in range(nseg):
                    fw.dma(mA[s_].ap, modbuf[l, s_ * 6 + 3], [mA[s_]], [mod_t[l][s_ * 6 + 3]])
                    fw.dma(mB[s_].ap, modbuf[l, s_ * 6 + 4], [mB[s_]], [mod_t[l][s_ * 6 + 4]])
                    fw.dma(mG[s_].ap, modbuf[l, s_ * 6 + 5], [mG[s_]], [mod_t[l][s_ * 6 + 5]])
                rwt = sb(es, nc, "rwt", [128, 8, NE], F32)
                fw.dma(rwt.ap, router_w.rearrange("(kc p) n -> p kc n", p=128), [rwt], [])
                rbt = sb(es, nc, "rbt", [128, NE], F32)
                fw.dma(rbt.ap, router_b.partition_broadcast(128), [rbt], [])
                fgt = None
                if last:
                    fgt = sb(es, nc, "fgt", [128, D], F32)
                    fw.dma(fgt.ap, final_g.partition_broadcast(128), [fgt], [])
                HT2 = [sb(es, nc, "HT", [128, 8, GS * 128], BF16) for _ in range(2)]
                accs = [sb(es, nc, "acc%d" % k, [128, D], F32) for k in range(GS)]
                gates2 = [sb(es, nc, "gates", [128, GS, NE], F32) for _ in range(2)]
                Wg2 = [sb(es, nc, "Wg", [128, 8, D], BF16) for _ in range(2)]
                Wu2 = [sb(es, nc, "Wu", [128, 8, D], BF16) for _ in range(2)]
                Wd = sb(es, nc, "Wd", [128, 8, D], BF16)
                actb = sb(es, nc, "actb", [128, 8, GS * 128], BF16)
                x6_pool = Pool(es, nc, "x6", [128, D], F32, 2)
                hT32_pool = Pool(es, nc, "hT32", [128, 8, 128], F32, 1)
                hooks = {}
                rt_pool = Pool(es, nc, "rt", [128, 96], F32, 2)
                sg_pool = Pool(es, nc, "sg", [128, 512], F32, 2)
                st6_pool = Pool(es, nc, "st6", [128, 4], F32, 2)

                def rms(xt, st, junk6):
                    jap = junk6.ap if len(junk6.ap.shape) == 2 else junk6.ap.rearrange("p a b -> p (a b)")
                    fw.op(act, lambda e: e.activation(out=jap, in_=xt.ap, func=AF.Square, accum_out=st.ap[:, 0:1]), [junk6, st], [xt])
                    fw.op(dve, lambda e: e.tensor_scalar(out=st.ap[:, 1:2], in0=st.ap[:, 0:1], scalar1=1.0 / D, scalar2=1e-6, op0=ALU.mult, op1=ALU.add), [st], [st])
                    fw.op(act, lambda e: e.activation(out=st.ap[:, 3:4], in_=st.ap[:, 1:2], func=AF.Sqrt), [st], [st])
                    fw.op(dve, lambda e: e.reciprocal(out=st.ap[:, 2:3], in_=st.ap[:, 3:4]), [st], [st])

                def load_w(dst, src):
                    for j in range(2):
                        fw.dma(dst.ap[:, :, j * 512:(j + 1) * 512], src[:, j * 512:(j + 1) * 512].rearrange("(kc p) n -> p kc n", p=128), [dst], [], q=pool)

                seq = [(gi, ex) for gi in range(len(groups)) for ex in range(NE)]
                load_w(Wg2[0], e_gate[l, 0])
                load_w(Wu2[0], e_up[l, 0])

                def moe_A_front(gi, grp, t):
                    i = grp[t]
                    if True:
                        s_ = 1 if i < NCT else 0
                        xt = x6_pool.next()
                        fw.dma(xt.ap, xs[i * 128:(i + 1) * 128, :], [xt], [xs_t[i]])
                        st = st6_pool.next()
                        hT32 = hT32_pool.next()
                        rms(xt, st, hT32)
                        fw.op(dve, lambda e, xt=xt, st=st, s_=s_: e.scalar_tensor_tensor(out=xt.ap, in0=xt.ap, scalar=st.ap[:, 2:3], in1=mA[s_].ap, op0=ALU.mult, op1=ALU.mult), [xt], [xt, st, mA[s_]])
                        fw.op(dve, lambda e, xt=xt, s_=s_: e.tensor_tensor(out=xt.ap, in0=xt.ap, in1=mB[s_].ap, op=ALU.add), [xt], [xt, mB[s_]])
                        return dict(xt=xt, hT32=hT32, t=t, i=i)

                def moe_A_back(gi, grp, cx):
                    HT, gates = HT2[gi % 2], gates2[gi % 2]
                    xt, hT32, t, i = cx["xt"], cx["hT32"], cx["t"], cx["i"]
                    if True:
                        for half in range(2):
                            ps = psum()
                            for kk in range(4):
                                kc = half * 4 + kk
                                fw.op(pe, lambda e, kc=kc, kk=kk, ps=ps, xt=xt: e.transpose(out=ps.ap[:, kk * 128:(kk + 1) * 128], in_=xt.ap[:, kc * 128:(kc + 1) * 128], identity=ident.ap), [ps], [xt, ident])
                            fw.op(dve, lambda e, ps=ps, hT32=hT32, half=half: e.tensor_copy(out=hT32.ap[:, half * 4:(half + 1) * 4, :], in_=ps.ap.rearrange("p (a b) -> p a b", a=4)), [hT32], [ps])
                            fw.op(act, lambda e, hT32=hT32, half=half, t=t: e.copy(out=HT.ap[:, half * 4:(half + 1) * 4, t * 128:(t + 1) * 128], in_=hT32.ap[:, half * 4:(half + 1) * 4, :]), [HT], [hT32])
                        ps = psum()
                        for kc in range(8):
                            fw.op(pe, lambda e, kc=kc, ps=ps, hT32=hT32: e.matmul(ps.ap[:, 0:NE], lhsT=hT32.ap[:, kc, :], rhs=rwt.ap[:, kc, :], start=(kc == 0), stop=(kc == 7)), [ps], [hT32, rwt])
                        rt = rt_pool.next()
                        R = rt.ap
                        sc_, bi_ = R[:, 0:16], R[:, 16:32]
                        g4 = lambda ap: ap.rearrange("p (g m) -> p g m", g=4)
                        fw.op(act, lambda e, ps=ps, sc_=sc_: e.activation(out=sc_, in_=ps.ap[:, 0:NE], func=AF.Sigmoid), [rt], [ps])
                        fw.op(dve, lambda e, sc_=sc_, bi_=bi_: e.tensor_tensor(out=bi_, in0=sc_, in1=rbt.ap, op=ALU.add), [rt], [rt, rbt])
                        a = [g4(bi_)[:, :, m] for m in range(4)]
                        hi01, lo01, hi23, lo23 = R[:, 32:36], R[:, 36:40], R[:, 40:44], R[:, 44:48]
                        top1, m1, m2, top2 = R[:, 48:52], R[:, 52:56], R[:, 56:60], R[:, 60:64]
                        gs_, gmx, gm_, rd = R[:, 64:68], R[:, 68:69], R[:, 72:76], R[:, 69:70]
                        sel = R[:, 80:96]
                        tt = lambda o, x0, x1, op: fw.op(dve, lambda e: e.tensor_tensor(out=o, in0=x0, in1=x1, op=op), [rt], [rt])
                        tt(hi01, a[0], a[1], ALU.max)
                        tt(lo01, a[0], a[1], ALU.min)
                        tt(hi23, a[2], a[3], ALU.max)
                        tt(lo23, a[2], a[3], ALU.min)
                        tt(top1, hi01, hi23, ALU.max)
                        tt(m1, hi01, hi23, ALU.min)
                        tt(m2, lo01, lo23, ALU.max)
                        tt(top2, m1, m2, ALU.max)
                        tt(gs_, top1, top2, ALU.add)
                        fw.op(dve, lambda e, gs_=gs_, gmx=gmx: e.reduce_max(out=gmx, in_=gs_, axis=AX.X), [rt], [rt])
                        fw.op(dve, lambda e, gs_=gs_, gmx=gmx, gm_=gm_: e.tensor_scalar(out=gm_, in0=gs_, scalar1=gmx, scalar2=None, op0=ALU.is_ge), [rt], [rt])
                        tt(g4(sel), g4(bi_), top2.unsqueeze(2).to_broadcast([128, 4, 4]), ALU.is_ge)
                        tt(g4(sel), g4(sel), gm_.unsqueeze(2).to_broadcast([128, 4, 4]), ALU.mult)
                        tt(sel, sel, sc_, ALU.mult)
                        fw.op(dve, lambda e, sel=sel, rd=rd: e.reduce_sum(out=rd, in_=sel, axis=AX.X), [rt], [rt])
                        fw.op(dve, lambda e, rd=rd: e.reciprocal(out=rd, in_=rd), [rt], [rt])
                        fw.op(dve, lambda e, sel=sel, rd=rd, t=t: e.tensor_scalar(out=gates.ap[:, t, :], in0=sel, scalar1=rd, scalar2=None, op0=ALU.mult), [gates], [rt])
                        if "gates" in dbg and l == 0:
                            fw.dma(dbg["gates"][i * 128:(i + 1) * 128, :], gates.ap[:, t, :], [], [gates])

                def moe_B(gi, grp, exs):
                    ntok = len(grp) * 128
                    HT, gates = HT2[gi % 2], gates2[gi % 2]
                    for ex in exs:
                        pre = hooks.get((gi, ex, 0))
                        cxh = pre() if pre else None
                        qi = gi * NE + ex
                        Wg, Wu = Wg2[qi % 2], Wu2[qi % 2]
                        load_w(Wd, e_down[l, ex])
                        if qi + 1 < len(seq):
                            nex = seq[qi + 1][1]
                            load_w(Wg2[(qi + 1) % 2], e_gate[l, nex])
                            load_w(Wu2[(qi + 1) % 2], e_up[l, nex])
                        for t0 in range(0, ntok, 512):
                            ntk = min(512, ntok - t0)
                            for fc in range(8):
                                psg = psum()
                                psu = psum()
                                for kc in range(8):
                                    fw.op(pe, lambda e, kc=kc, fc=fc, psg=psg, t0=t0, ntk=ntk: e.matmul(psg.ap[:, 0:ntk], lhsT=Wg.ap[:, kc, fc * 128:(fc + 1) * 128], rhs=HT.ap[:, kc, t0:t0 + ntk], start=(kc == 0), stop=(kc == 7)), [psg], [Wg, HT])
                                for kc in range(8):
                                    fw.op(pe, lambda e, kc=kc, fc=fc, psu=psu, t0=t0, ntk=ntk: e.matmul(psu.ap[:, 0:ntk], lhsT=Wu.ap[:, kc, fc * 128:(fc + 1) * 128], rhs=HT.ap[:, kc, t0:t0 + ntk], start=(kc == 0), stop=(kc == 7)), [psu], [Wu, HT])
                                sg = sg_pool.next()
                                fw.op(act, lambda e, psg=psg, sg=sg, ntk=ntk: e.activation(out=sg.ap[:, 0:ntk], in_=psg.ap[:, 0:ntk], func=AF.Silu), [sg], [psg])
                                fw.op(dve, lambda e, psu=psu, sg=sg, fc=fc, t0=t0, ntk=ntk: e.tensor_tensor(out=actb.ap[:, fc, t0:t0 + ntk], in0=sg.ap[:, 0:ntk], in1=psu.ap[:, 0:ntk], op=ALU.mult), [actb], [sg, psu])
                        for t in range(len(grp)):
                            for half in range(2):
                                hs = slice(half * 512, (half + 1) * 512)
                                ps = psum()
                                for fc in range(8):
                                    fw.op(pe, lambda e, fc=fc, ps=ps, t=t, hs=hs: e.matmul(ps.ap, lhsT=actb.ap[:, fc, t * 128:(t + 1) * 128], rhs=Wd.ap[:, fc, hs], start=(fc == 0), stop=(fc == 7)), [ps], [actb, Wd])
                                if ex == 0:
                                    fw.op(dve, lambda e, ps=ps, t=t, hs=hs, ex=ex: e.tensor_scalar(out=accs[t].ap[:, hs], in0=ps.ap, scalar1=gates.ap[:, t, ex:ex + 1], scalar2=None, op0=ALU.mult), [accs[t]], [ps, gates])
                                else:
                                    fw.op(dve, lambda e, ps=ps, t=t, hs=hs, ex=ex: e.scalar_tensor_tensor(out=accs[t].ap[:, hs], in0=ps.ap, scalar=gates.ap[:, t, ex:ex + 1], in1=accs[t].ap[:, hs], op0=ALU.mult, op1=ALU.add), [accs[t]], [ps, gates, accs[t]])
                        post = hooks.get((gi, ex, 1))
                        if post:
                            post(cxh)

                def moe_C(gi, grp):
                    ntok = len(grp) * 128
                    HT, gates = HT2[gi % 2], gates2[gi % 2]
                    for t, i in enumerate(grp):
                        s_ = 1 if i < NCT else 0
                        xt = x6_pool.next()
                        fw.dma(xt.ap, xs[i * 128:(i + 1) * 128, :], [xt], [xs_t[i]])
                        fw.op(dve, lambda e, t=t, s_=s_: e.tensor_tensor(out=accs[t].ap, in0=accs[t].ap, in1=mG[s_].ap, op=ALU.mult), [accs[t]], [accs[t], mG[s_]])
                        fw.op(dve, lambda e, xt=xt, t=t: e.tensor_tensor(out=xt.ap, in0=xt.ap, in1=accs[t].ap, op=ALU.add), [xt], [xt, accs[t]])
                        if "xout" in dbg and l == 0:
                            fw.dma(dbg["xout"][i * 128:(i + 1) * 128, :], xt.ap, [], [xt])
                        if not last:
                            fw.dma(xs[i * 128:(i + 1) * 128, :], xt.ap, [xs_t[i]], [xt])
                        else:
                            st = st6_pool.next()
                            rms(xt, st, accs[t])
                            fw.op(dve, lambda e, xt=xt, st=st: e.scalar_tensor_tensor(out=xt.ap, in0=xt.ap, scalar=st.ap[:, 2:3], in1=fgt.ap, op0=ALU.mult, op1=ALU.mult), [xt], [xt, st, fgt])
                            fw.dma(out[(i - NCT) * 128:(i - NCT + 1) * 128, :], xt.ap, [], [xt])

                hooks = {}
                for t in range(len(groups[0])):
                    moe_A_back(0, groups[0], moe_A_front(0, groups[0], t))
                for gi, grp in enumerate(groups):
                    if gi + 1 < len(groups):
                        ng = groups[gi + 1]
                        for t in range(len(ng)):
                            ex = 4 + t
                            hooks[(gi, ex, 0)] = (lambda gi=gi, ng=ng, t=t: moe_A_front(gi + 1, ng, t))
                            hooks[(gi, ex, 1)] = (lambda cx, gi=gi, ng=ng: moe_A_back(gi + 1, ng, cx))
                    moe_B(gi, grp, range(NE))
                    moe_C(gi, grp)
                fw.barrier()
            if stop_after == "S6":
                break
        fw.barrier()
    return nc


def prep_inputs(inputs):
    f = lambda a: np.ascontiguousarray(np.asarray(a, dtype=np.float32))
    shared = {}
    for k in ("w_mod", "b_mod", "norm_mix_g", "norm_ffn_g", "w_in", "w_out", "att_sink", "shift_mu_prev", "shift_mu_next",
              "decay_w0", "decay_w2", "iclr_a0", "iclr_a2", "vres_v0", "vres_v1", "vres_v2", "gate_g2", "k_k", "k_a",
              "ln_x_w", "ln_x_b", "router_w", "router_b", "expert_w_gate", "expert_w_up", "expert_w_down", "final_norm_g"):
        shared[k] = f(inputs[k])
    shared["r_k"] = f(inputs["r_k"]).reshape(DEPTH, RW)
    shared["ident"] = np.eye(128, dtype=np.float32)
    s = np.arange(128)[:, None]
    t = np.arange(128)[None, :]
    shared["masks"] = np.ascontiguousarray(np.stack([s < t, s <= t, s > t, s >= t], axis=1).astype(np.float32))
    pos = np.arange(L)
    row = (pos // 64).astype(np.float32)
    colp = (pos % 64).astype(np.float32)
    inv = (10000.0 ** (-np.arange(16, dtype=np.float32) / 16)).astype(np.float32)
    ang = np.concatenate([row[:, None] * inv, row[:, None] * inv, colp[:, None] * inv, colp[:, None] * inv], axis=1)
    cos = np.cos(ang).astype(np.float32)
    sin = np.sin(ang).astype(np.float32)
    sgn = np.concatenate([-np.ones(16), np.ones(16), -np.ones(16), np.ones(16)]).astype(np.float32)
    shared["rope_cos"] = np.ascontiguousarray(np.tile(cos, (1, 10)))
    shared["rope_sin"] = np.ascontiguousarray(np.tile(sin * sgn, (1, 10)))
    x = f(inputs["x"])
    ctx = f(inputs["ctx"])
    c = f(inputs["c"])
    cc = f(inputs["c_ctx"])
    maps = []
    for b in range(8):
        m = dict(shared)
        m["x"] = x[b]
        m["ctx"] = ctx[b]
        m["ccol"] = np.ascontiguousarray(np.concatenate([c[b].reshape(8, 128).T, cc.reshape(8, 128).T], axis=1))
        maps.append(m)
    return maps


def kernel(**inputs):
    maps = prep_inputs(inputs)
    nc = build()
    res = run_bass_kernel_spmd(nc, maps, core_ids=list(range(8)))
    return np.stack([r["out"] for r in res.results], axis=0).astype(np.float32)
```

```python
from contextlib import ExitStack
import os
import numpy as np
import ml_dtypes
import concourse.bass as bass
import concourse.mybir as mybir
from concourse.bass_utils import run_bass_kernel_spmd

F32 = mybir.dt.float32
BF16 = mybir.dt.bfloat16
AF = mybir.ActivationFunctionType
ALU = mybir.AluOpType
AX = mybir.AxisListType

D = 1024
L = 4096
C = 256
T = L + C
NT = T // 128
NCT = C // 128
DEPTH = 2
IN_COLS = 2688
RW = 512
RWC = 1920
NE = 16
ATT_SCALE = 0.125
NEG = -30000.0


class Eng:
    def __init__(self, fw, e, name, ring=0):
        self.fw = fw
        self.e = e
        self.name = name
        self.count = 0
        self.seen = {}
        self.sem = fw.nc.alloc_semaphore("s_" + name)
        self.ring = [fw.nc.alloc_semaphore("r_%s%d" % (name, i)) for i in range(ring)]
        self.ndma = 0

    def wait_tok(self, tok):
        sem, val, key = tok
        if self.seen.get(key, 0) >= val:
            return
        self.e.wait_ge(sem, val)
        self.seen[key] = val


class TT:
    def __init__(self, ap=None):
        self.ap = ap
        self.w = None
        self.r = {}


class FW:
    def __init__(self, nc):
        self.nc = nc
        self.pe = Eng(self, nc.tensor, "pe")
        self.act = Eng(self, nc.scalar, "act")
        self.dve = Eng(self, nc.vector, "dve")
        self.pool = Eng(self, nc.gpsimd, "pool", ring=12)
        self.sp = Eng(self, nc.sync, "sp", ring=24)
        self.engs = [self.pe, self.act, self.dve, self.pool, self.sp]
        self.ninst = 0

    def _deps(self, eng, outs, ins):
        for t in ins:
            if t.w is not None and not (eng is self.pe and t.w[2] == "pe"):
                eng.wait_tok(t.w)
        for t in outs:
            if t.w is not None and not (eng is self.pe and t.w[2] == "pe"):
                eng.wait_tok(t.w)
            for tok in t.r.values():
                if not (eng is self.pe and tok[2] == "pe"):
                    eng.wait_tok(tok)

    def _mark(self, tok, outs, ins):
        for t in ins:
            t.r[tok[2]] = tok
        for t in outs:
            t.w = tok
            t.r = {}

    def op(self, eng, fn, outs, ins):
        self._deps(eng, outs, ins)
        inst = fn(eng.e)
        eng.count += 1
        inst.then_inc(eng.sem, 1)
        self._mark((eng.sem, eng.count, eng.name), outs, ins)
        self.ninst += 1

    def dma(self, out_ap, in_ap, outs, ins, q=None):
        q = q or self.sp
        self._deps(q, outs, ins)
        K = len(q.ring)
        slot = q.ndma % K
        rnd = q.ndma // K
        key = "%s_r%d" % (q.name, slot)
        if rnd > 0:
            q.wait_tok((q.ring[slot], 16 * rnd, key))
        q.e.dma_start(out=out_ap, in_=in_ap).then_inc(q.ring[slot], 16)
        q.ndma += 1
        self._mark((q.ring[slot], 16 * (rnd + 1), key), outs, ins)
        self.ninst += 1

    def all_toks(self):
        toks = []
        for e in self.engs:
            if e.count:
                toks.append((e.sem, e.count, e.name))
            K = len(e.ring)
            for slot in range(min(K, e.ndma)):
                n = (e.ndma - 1 - slot) // K + 1
                toks.append((e.ring[slot], 16 * n, "%s_r%d" % (e.name, slot)))
        return toks

    def barrier(self):
        toks = self.all_toks()
        for e in self.engs:
            for tok in toks:
                if e is self.pe and tok[2] == "pe":
                    continue
                e.wait_tok(tok)


_uid = [0]


def _nm(name):
    _uid[0] += 1
    return "sb_%s_%d" % (name, _uid[0])


class Pool:
    def __init__(self, es, nc, name, shape, dtype, bufs):
        self.t = [TT(es.enter_context(nc.sbuf_tensor(_nm(name), shape, dtype)).ap()) for i in range(bufs)]
        self.i = 0

    def next(self):
        t = self.t[self.i % len(self.t)]
        self.i += 1
        return t


def sb(es, nc, name, shape, dtype):
    return TT(es.enter_context(nc.sbuf_tensor(_nm(name), shape, dtype)).ap())


def build(debug_outs=(), stop_after=None):
    nc = bass.Bass("TRN2", target_bir_lowering=False)
    fw = FW(nc)
    pe, act, dve, pool, sp = fw.pe, fw.act, fw.dve, fw.pool, fw.sp

    def din(name, shape, dt=F32):
        return nc.dram_tensor(name, list(shape), dt, kind="ExternalInput").ap()

    def dscr(name, shape, dt=F32):
        return nc.dram_tensor(name, list(shape), dt, kind="Internal").ap()

    x_in = din("x", [L, D])
    ctx_in = din("ctx", [C, D])
    ccol_in = din("ccol", [128, 16])
    w_mod = din("w_mod", [DEPTH, D, 6 * D])
    b_mod = din("b_mod", [DEPTH, 6 * D])
    norm_mix_g = din("norm_mix_g", [DEPTH, D])
    norm_ffn_g = din("norm_ffn_g", [DEPTH, D])
    w_in = din("w_in", [DEPTH, D, IN_COLS])
    w_out = din("w_out", [DEPTH, D, D])
    att_sink = din("att_sink", [DEPTH, 8])
    mu_prev = din("shift_mu_prev", [DEPTH, RWC])
    mu_next = din("shift_mu_next", [DEPTH, RWC])
    decay_w0 = din("decay_w0", [DEPTH, 2, RW])
    decay_w2 = din("decay_w2", [DEPTH, 2, 64, RW])
    iclr_a0 = din("iclr_a0", [DEPTH, 2, RW])
    iclr_a2 = din("iclr_a2", [DEPTH, 2, 64, RW])
    vres_v0 = din("vres_v0", [1, RW])
    vres_v1 = din("vres_v1", [1, RW, 32])
    vres_v2 = din("vres_v2", [1, 32, RW])
    gate_g2 = din("gate_g2", [DEPTH, 128, RW])
    k_k = din("k_k", [DEPTH, RW])
    k_a = din("k_a", [DEPTH, RW])
    r_k = din("r_k", [DEPTH, RW])
    ln_x_w = din("ln_x_w", [DEPTH, RW])
    ln_x_b = din("ln_x_b", [DEPTH, RW])
    router_w = din("router_w", [D, NE])
    router_b = din("router_b", [NE])
    e_gate = din("expert_w_gate", [DEPTH, NE, D, D])
    e_up = din("expert_w_up", [DEPTH, NE, D, D])
    e_down = din("expert_w_down", [DEPTH, NE, D, D])
    final_g = din("final_norm_g", [D])
    ident_in = din("ident", [128, 128])
    masks_in = din("masks", [128, 4, 128])
    rope_cos = din("rope_cos", [L, 640])
    rope_sin = din("rope_sin", [L, 640])
    out = nc.dram_tensor("out", [L, D], F32, kind="ExternalOutput").ap()

    xs = dscr("xs", [T, D])
    xs_t = [TT() for _ in range(NT)]
    modbuf = dscr("modbuf", [DEPTH, 12, 128, D])
    mod_t = [[TT() for _ in range(12)] for _ in range(DEPTH)]
    u_scr = dscr("u_scr", [T + 4, RWC])
    u_t = [TT() for _ in range(NT)]
    u_guard = TT()
    mix = dscr("mix", [T, D])
    mix_a = [TT() for _ in range(NT)]
    mix_r = [TT() for _ in range(NT)]
    vfirst = dscr("vfirst", [T, RW])
    vf_t = [TT() for _ in range(NT)]
    feat = dscr("feat", [T, 5632])
    feat_t = [TT() for _ in range(NT)]
    y_scr = dscr("y_scr", [2, T, RW])
    y_t = [[TT() for _ in range(NT)] for _ in range(2)]
    dbg = {}
    for name, shape in debug_outs:
        dbg[name] = nc.dram_tensor("dbg_" + name, list(shape), F32, kind="ExternalOutput").ap()

    def urow(i):
        return 1 + i * 128 if i < NCT else 3 + i * 128

    PS = [TT(nc.alloc_psum_tensor("psb%d" % i, [128, 512], F32).ap()) for i in range(8)]
    psi = [0]

    def psum():
        t = PS[psi[0] % 6]
        psi[0] += 1
        return t

    with ExitStack() as g_es:
        ident = sb(g_es, nc, "ident", [128, 128], F32)
        identb = sb(g_es, nc, "identb", [128, 128], BF16)
        masks = sb(g_es, nc, "masks", [128, 4, 128], F32)
        fw.dma(ident.ap, ident_in, [ident], [])
        fw.dma(masks.ap, masks_in, [masks], [])
        fw.op(dve, lambda e: e.tensor_copy(out=identb.ap, in_=ident.ap), [identb], [ident])
        with ExitStack() as es0:
            zrow = sb(es0, nc, "zrow", [1, RWC], F32)
            fw.op(dve, lambda e: e.memset(zrow.ap, 0.0), [zrow], [])
            for r in (0, C + 1, C + 2, T + 3):
                fw.dma(u_scr[r:r + 1, :], zrow.ap, [u_guard], [zrow])
            fw.barrier()

        for i in range(NT):
            src = ctx_in[i * 128:(i + 1) * 128, :] if i < NCT else x_in[(i - NCT) * 128:(i - NCT + 1) * 128, :]
            fw.dma(xs[i * 128:(i + 1) * 128, :], src, [xs_t[i]], [])

        for l in range(DEPTH):
            last = l == DEPTH - 1
            with ExitStack() as es:
                ccol = sb(es, nc, "ccol", [128, 16], F32)
                csil = sb(es, nc, "csil", [128, 16], F32)
                crep = sb(es, nc, "crep", [128, 16, 128], F32)
                fw.dma(ccol.ap, ccol_in, [ccol], [])
                fw.op(act, lambda e: e.activation(out=csil.ap, in_=ccol.ap, func=AF.Silu), [csil], [ccol])
                fw.op(dve, lambda e: e.tensor_copy(out=crep.ap, in_=csil.ap.unsqueeze(2).to_broadcast([128, 16, 128])), [crep], [csil])
                wm_pool = Pool(es, nc, "wm", [128, 8, 512], F32, 2)
                bm_pool = Pool(es, nc, "bm", [128, 512], F32, 2)
                modraw = [[sb(es, nc, "mr%d_%d" % (s, j), [128, D], F32) for j in range(6)] for s in range(2)]
                gmix = sb(es, nc, "gmix", [128, D], F32)
                gffn = sb(es, nc, "gffn", [128, D], F32)
                fw.dma(gmix.ap, norm_mix_g[l].partition_broadcast(128), [gmix], [])
                fw.dma(gffn.ap, norm_ffn_g[l].partition_broadcast(128), [gffn], [])
                for j in range(12):
                    wm = wm_pool.next()
                    bm = bm_pool.next()
                    fw.dma(wm.ap, w_mod[l][:, j * 512:(j + 1) * 512].rearrange("(kc p) n -> p kc n", p=128), [wm], [])
                    fw.dma(bm.ap, b_mod[l][j * 512:(j + 1) * 512].partition_broadcast(128), [bm], [])
                    for s in range(2):
                        ps = psum()
                        for kc in range(8):
                            fw.op(pe, lambda e, kc=kc, s=s, ps=ps, wm=wm: e.matmul(ps.ap, lhsT=crep.ap[:, s * 8 + kc, :], rhs=wm.ap[:, kc, :], start=(kc == 0), stop=(kc == 7)), [ps], [crep, wm])
                        dst = modraw[s][j // 2]
                        fw.op(dve, lambda e, ps=ps, dst=dst, bm=bm, j=j: e.tensor_tensor(out=dst.ap[:, (j % 2) * 512:(j % 2 + 1) * 512], in0=ps.ap, in1=bm.ap, op=ALU.add), [dst], [ps, bm])
                for s in range(2):
                    sh_a, sc_a, ga, sh_f, sc_f, gf = modraw[s]
                    fw.op(dve, lambda e, sc_a=sc_a: e.scalar_tensor_tensor(out=sc_a.ap, in0=sc_a.ap, scalar=1.0, in1=gmix.ap, op0=ALU.add, op1=ALU.mult), [sc_a], [sc_a, gmix])
                    fw.op(dve, lambda e, sc_f=sc_f: e.scalar_tensor_tensor(out=sc_f.ap, in0=sc_f.ap, scalar=1.0, in1=gffn.ap, op0=ALU.add, op1=ALU.mult), [sc_f], [sc_f, gffn])
                    for idx, t in enumerate((sc_a, sh_a, ga, sc_f, sh_f, gf)):
                        fw.dma(modbuf[l, s * 6 + idx], t.ap, [mod_t[l][s * 6 + idx]], [t])
                fw.barrier()
            if stop_after == "S1":
                break

            with ExitStack() as es_att:
                QT = sb(es_att, nc, "QT", [128, 4, T], BF16)
                KT = sb(es_att, nc, "KT", [128, T], BF16)
                VV = sb(es_att, nc, "VV", [128, NT, 128], BF16)
                qt_t = [TT() for _ in range(NT)]
                with ExitStack() as es:
                    winb = sb(es, nc, "winb", [128, 8, IN_COLS], BF16)
                    wst_pool = Pool(es, nc, "wst", [128, 8, 384], F32, 1)
                    for j in range(7):
                        wst = wst_pool.next()
                        fw.dma(wst.ap, w_in[l][:, j * 384:(j + 1) * 384].rearrange("(kc p) n -> p kc n", p=128), [wst], [])
                        fw.op(pool, lambda e, wst=wst, j=j: e.tensor_copy(out=winb.ap[:, :, j * 384:(j + 1) * 384], in_=wst.ap), [winb], [wst])
                    modA = [sb(es, nc, "modA%d" % s, [128, D], F32) for s in range(2)]
                    modB = [sb(es, nc, "modB%d" % s, [128, D], F32) for s in range(2)]
                    for s in range(2):
                        fw.dma(modA[s].ap, modbuf[l, s * 6 + 0], [modA[s]], [mod_t[l][s * 6 + 0]])
                        fw.dma(modB[s].ap, modbuf[l, s * 6 + 1], [modB[s]], [mod_t[l][s * 6 + 1]])
                    x_pool = Pool(es, nc, "xt", [128, D], F32, 3)
                    junk = sb(es, nc, "junk", [128, D], F32)
                    st_pool = Pool(es, nc, "st", [128, 4], F32, 2)
                    h_pool = Pool(es, nc, "ht", [128, D], BF16, 2)
                    hT_pool = Pool(es, nc, "hT", [128, 8, 128], BF16, 3)
                    pr_pool = Pool(es, nc, "pr", [128, IN_COLS], F32, 2)
                    cos_pool = Pool(es, nc, "cos", [128, 640], F32, 3)
                    sin_pool = Pool(es, nc, "sin", [128, 640], F32, 3)
                    rp_pool = Pool(es, nc, "rp", [128, 640], F32, 2)
                    rq_pool = Pool(es, nc, "rq", [128, 640], BF16, 2)

                    def s2_load(i):
                        xt = x_pool.next()
                        fw.dma(xt.ap, xs[i * 128:(i + 1) * 128, :], [xt], [xs_t[i]])
                        cs = sn = None
                        if i >= NCT:
                            cs = cos_pool.next()
                            sn = sin_pool.next()
                            r0 = (i - NCT) * 128
                            fw.dma(cs.ap, rope_cos[r0:r0 + 128, :], [cs], [])
                            fw.dma(sn.ap, rope_sin[r0:r0 + 128, :], [sn], [])
                        return xt, cs, sn

                    def s2_front(i, xt, cs, sn):
                        s = 1 if i < NCT else 0
                        st = st_pool.next()
                        fw.op(act, lambda e, xt=xt, st=st: e.activation(out=junk.ap, in_=xt.ap, func=AF.Square, accum_out=st.ap[:, 0:1]), [junk, st], [xt])
                        fw.op(dve, lambda e, st=st: e.tensor_scalar(out=st.ap[:, 1:2], in0=st.ap[:, 0:1], scalar1=1.0 / D, scalar2=1e-6, op0=ALU.mult, op1=ALU.add), [st], [st])
                        fw.op(act, lambda e, st=st: e.activation(out=st.ap[:, 3:4], in_=st.ap[:, 1:2], func=AF.Sqrt), [st], [st])
                        fw.op(dve, lambda e, st=st: e.reciprocal(out=st.ap[:, 2:3], in_=st.ap[:, 3:4]), [st], [st])
                        fw.op(dve, lambda e, xt=xt, st=st, s=s: e.scalar_tensor_tensor(out=xt.ap, in0=xt.ap, scalar=st.ap[:, 2:3], in1=modA[s].ap, op0=ALU.mult, op1=ALU.mult), [xt], [xt, st, modA[s]])
                        ht = h_pool.next()
                        fw.op(dve, lambda e, xt=xt, ht=ht, s=s: e.tensor_tensor(out=ht.ap, in0=xt.ap, in1=modB[s].ap, op=ALU.add), [ht], [xt, modB[s]])
                        hT = hT_pool.next()
                        for half in range(2):
                            ps = psum()
                            psb = ps.ap.bitcast(BF16)
                            for kk in range(4):
                                kc = half * 4 + kk
                                fw.op(pe, lambda e, kc=kc, kk=kk, psb=psb, ht=ht: e.transpose(out=psb[:, kk * 128:(kk + 1) * 128], in_=ht.ap[:, kc * 128:(kc + 1) * 128], identity=identb.ap), [ps], [ht, identb])
                            fw.op(act if half else dve, lambda e, psb=psb, hT=hT, half=half: (e.copy if half else e.tensor_copy)(out=hT.ap[:, half * 4:(half + 1) * 4, :], in_=psb[:, 0:512].rearrange("p (a b) -> p a b", a=4)), [hT], [ps])
                        return dict(i=i, hT=hT, cs=cs, sn=sn)

                    def s2_B(cx):
                        i, hT, cs, sn = cx['i'], cx['hT'], cx['cs'], cx['sn']
                        pr = pr_pool.next()
                        col = 0
                        ci = 0
                        while col < IN_COLS:
                            n = min(512, IN_COLS - col)
                            ps = psum()
                            for kc in range(8):
                                fw.op(pe, lambda e, kc=kc, ps=ps, hT=hT, col=col, n=n: e.matmul(ps.ap[:, 0:n], lhsT=hT.ap[:, kc, :], rhs=winb.ap[:, kc, col:col + n], start=(kc == 0), stop=(kc == 7)), [ps], [hT, winb])
                            fw.op(act, lambda e, ps=ps, pr=pr, col=col, n=n: e.copy(out=pr.ap[:, col:col + n], in_=ps.ap[:, 0:n]), [pr], [ps])
                            col += n
                            ci += 1
                        if "proj" in dbg and l == 0:
                            fw.dma(dbg["proj"][i * 128:(i + 1) * 128, :], pr.ap, [], [pr])
                        fw.dma(u_scr[urow(i):urow(i) + 128, :], pr.ap[:, 768:IN_COLS], [u_t[i]], [pr])
                        cx['pr'] = pr

                    def s2_C(cx):
                        i, cs, sn, pr = cx['i'], cx['cs'], cx['sn'], cx['pr']
                        rq = rq_pool.next()
                        if i >= NCT:
                            rp = rp_pool.next()
                            v5 = lambda ap: ap.rearrange("p (m h f) -> p m h f", h=2, f=16)
                            fw.op(dve, lambda e, rp=rp, pr=pr, sn=sn: e.tensor_tensor(out=v5(rp.ap)[:, :, 0, :], in0=v5(pr.ap[:, 0:640])[:, :, 1, :], in1=v5(sn.ap)[:, :, 0, :], op=ALU.mult), [rp], [pr, sn])
                            fw.op(dve, lambda e, rp=rp, pr=pr, sn=sn: e.tensor_tensor(out=v5(rp.ap)[:, :, 1, :], in0=v5(pr.ap[:, 0:640])[:, :, 0, :], in1=v5(sn.ap)[:, :, 1, :], op=ALU.mult), [rp], [pr, sn])
                            fw.op(pool, lambda e, cs=cs, pr=pr: e.tensor_tensor(out=cs.ap, in0=pr.ap[:, 0:640], in1=cs.ap, op=ALU.mult), [cs], [pr, cs])
                            gq = lambda ap: ap.rearrange("p (g j d) -> p g j d", g=2, j=4)
                            jq = lambda ap: ap.rearrange("p (j g d) -> p g j d", g=2, j=4)
                            fw.op(dve, lambda e, rq=rq, rp=rp, cs=cs: e.tensor_tensor(out=jq(rq.ap[:, 0:512]), in0=gq(rp.ap[:, 0:512]), in1=gq(cs.ap[:, 0:512]), op=ALU.add), [rq], [rp, cs])
                            fw.op(dve, lambda e, rq=rq, rp=rp, cs=cs: e.tensor_tensor(out=rq.ap[:, 512:640], in0=rp.ap[:, 512:640], in1=cs.ap[:, 512:640], op=ALU.add), [rq], [rp, cs])
                        else:
                            gq = lambda ap: ap.rearrange("p (g j d) -> p g j d", g=2, j=4)
                            jq = lambda ap: ap.rearrange("p (j g d) -> p g j d", g=2, j=4)
                            fw.op(dve, lambda e, rq=rq, pr=pr: e.tensor_copy(out=jq(rq.ap[:, 0:512]), in_=gq(pr.ap[:, 0:512])), [rq], [pr])
                            fw.op(dve, lambda e, rq=rq, pr=pr: e.tensor_copy(out=rq.ap[:, 512:640], in_=pr.ap[:, 512:640]), [rq], [pr])
                        fw.op(act, lambda e, pr=pr, i=i: e.copy(out=VV.ap[:, i, :], in_=pr.ap[:, 640:768]), [qt_t[i]], [pr])
                        ps = psum()
                        psb = ps.ap.bitcast(BF16)
                        for j in range(4):
                            fw.op(pe, lambda e, j=j, psb=psb, rq=rq: e.transpose(out=psb[:, j * 128:(j + 1) * 128], in_=rq.ap[:, j * 128:(j + 1) * 128], identity=identb.ap), [ps], [rq, identb])
                        fw.op(pe, lambda e, psb=psb, rq=rq: e.transpose(out=psb[:, 512:640], in_=rq.ap[:, 512:640], identity=identb.ap), [ps], [rq, identb])
                        fw.op(dve, lambda e, psb=psb, i=i: e.tensor_copy(out=QT.ap[:, :, i * 128:(i + 1) * 128], in_=psb[:, 0:512].rearrange("p (j t) -> p j t", j=4)), [qt_t[i]], [ps])
                        fw.op(act, lambda e, psb=psb, i=i: e.copy(out=KT.ap[:, i * 128:(i + 1) * 128], in_=psb[:, 512:640]), [qt_t[i]], [ps])

                    lds = {0: s2_load(0), 1: s2_load(1)}
                    cxn = s2_front(0, *lds.pop(0))
                    for i in range(NT):
                        cxc = cxn
                        if i + 2 < NT:
                            lds[i + 2] = s2_load(i + 2)
                        s2_B(cxc)
                        if i + 1 < NT:
                            cxn = s2_front(i + 1, *lds.pop(i + 1))
                        s2_C(cxc)
                    fw.barrier()
                if stop_after == "S2":
                    break
                with ExitStack() as es:
                    sinkb = sb(es, nc, "sinkb", [128, 8], F32)
                    fw.dma(sinkb.ap, att_sink[l].partition_broadcast(128), [sinkb], [])
                    mb = sb(es, nc, "mb", [128, 384], F32)
                    fw.op(dve, lambda e: e.memset(mb.ap, 0.0), [mb], [])
                    fw.op(dve, lambda e: e.tensor_scalar(out=mb.ap[:, 0:128], in0=masks.ap[:, 1, :], scalar1=-1.0, scalar2=-NEG, op0=ALU.add, op1=ALU.mult), [mb], [masks])
                    fw.op(dve, lambda e: e.tensor_scalar(out=mb.ap[:, 256:384], in0=masks.ap[:, 3, :], scalar1=-1.0, scalar2=-NEG, op0=ALU.add, op1=ALU.mult), [mb], [masks])
                    s_pool = Pool(es, nc, "ssb", [128, 640], F32, 3)
                    p_pool = Pool(es, nc, "psb", [128, 640], BF16, 4)
                    pT_pool = Pool(es, nc, "pTs", [128, 640], BF16, 2)
                    ast_pool = Pool(es, nc, "ast", [128, 8], F32, 6)
                    mixa_pool = Pool(es, nc, "mixa", [128, 512], F32, 3)
                    tiles3 = list(range(NCT, NT) if last else range(NT))
                    tinfo = {}
                    for i in tiles3:
                        if i < NCT:
                            nloc, lo, hi, moff = 0, 0, 0, 0
                            kblocks = list(range(NCT))
                        else:
                            lo, hi = max(NCT, i - 1), min(NT - 1, i + 1)
                            nloc = (hi - lo + 1) * 128
                            moff = 0 if lo == i - 1 else 128
                            kblocks = list(range(lo, hi + 1)) + list(range(NCT))
                        tinfo[i] = dict(nloc=nloc, lo=lo, hi=hi, moff=moff, kblocks=kblocks, ncols=nloc + C, mixa=None, ops=PS[6 + (i % 2)])

                    def att_front(i, h):
                        ti = tinfo[i]
                        nloc, lo, hi, moff, ncols = ti["nloc"], ti["lo"], ti["hi"], ti["moff"], ti["ncols"]
                        if ti["mixa"] is None:
                            ti["mixa"] = mixa_pool.next()
                        g, j = h // 4, h % 4
                        lhsT = QT.ap[g * 64:(g + 1) * 64, j, i * 128:(i + 1) * 128]
                        ssb = s_pool.next()
                        st = ast_pool.next()
                        if nloc:
                            ps1 = psum()
                            fw.op(pe, lambda e: e.matmul(ps1.ap[:, 0:nloc], lhsT=lhsT, rhs=KT.ap[g * 64:(g + 1) * 64, lo * 128:(hi + 1) * 128], start=True, stop=True), [ps1], [qt_t[i]])
                            fw.op(dve, lambda e: e.scalar_tensor_tensor(out=ssb.ap[:, 0:nloc], in0=ps1.ap[:, 0:nloc], scalar=ATT_SCALE, in1=mb.ap[:, moff:moff + nloc], op0=ALU.mult, op1=ALU.add), [ssb], [ps1, mb])
                        ps2 = psum()
                        fw.op(pe, lambda e: e.matmul(ps2.ap[:, 0:C], lhsT=lhsT, rhs=KT.ap[g * 64:(g + 1) * 64, 0:C], start=True, stop=True), [ps2], [qt_t[i]])
                        fw.op(act, lambda e: e.activation(out=ssb.ap[:, nloc:ncols], in_=ps2.ap[:, 0:C], func=AF.Copy, scale=ATT_SCALE), [ssb], [ps2])
                        fw.op(dve, lambda e: e.reduce_max(out=st.ap[:, 0:1], in_=ssb.ap[:, 0:ncols], axis=AX.X), [st], [ssb])
                        fw.op(dve, lambda e: e.tensor_scalar(out=st.ap[:, 1:2], in0=st.ap[:, 0:1], scalar1=sinkb.ap[:, h:h + 1], scalar2=-1.0, op0=ALU.max, op1=ALU.mult), [st], [st, sinkb])
                        return dict(i=i, h=h, g=g, ssb=ssb, st=st)

                    def att_mid(cx):
                        i, h, g, ssb, st = cx["i"], cx["h"], cx["g"], cx["ssb"], cx["st"]
                        ncols = tinfo[i]["ncols"]
                        pb = p_pool.next()
                        fw.op(act, lambda e: e.activation(out=pb.ap[:, 0:ncols], in_=ssb.ap[:, 0:ncols], func=AF.Exp, bias=st.ap[:, 1:2], accum_out=st.ap[:, 2:3]), [pb, st], [ssb, st])
                        fw.op(act, lambda e: e.activation(out=st.ap[:, 3:4], in_=sinkb.ap[:, h:h + 1], func=AF.Exp, bias=st.ap[:, 1:2]), [st], [st, sinkb])
                        fw.op(dve, lambda e: e.tensor_tensor(out=st.ap[:, 4:5], in0=st.ap[:, 2:3], in1=st.ap[:, 3:4], op=ALU.add), [st], [st])
                        fw.op(dve, lambda e: e.reciprocal(out=st.ap[:, 5:6], in_=st.ap[:, 4:5]), [st], [st])
                        return dict(i=i, h=h, g=g, pb=pb, st=st)

                    def att_back(cx):
                        i, h, g, pb, st = cx["i"], cx["h"], cx["g"], cx["pb"], cx["st"]
                        ti = tinfo[i]
                        ncols, kblocks, mixa, ops = ti["ncols"], ti["kblocks"], ti["mixa"], ti["ops"]
                        ps3 = psum()
                        ps3b = ps3.ap.bitcast(BF16)
                        nblk = ncols // 128
                        for b in range(nblk):
                            fw.op(pe, lambda e, b=b: e.transpose(out=ps3b[:, b * 128:(b + 1) * 128], in_=pb.ap[:, b * 128:(b + 1) * 128], identity=identb.ap), [ps3], [pb, identb])
                        pT = pT_pool.next()
                        if h % 2 == 0:
                            fw.op(dve, lambda e: e.tensor_copy(out=pT.ap[:, 0:ncols], in_=ps3b[:, 0:ncols]), [pT], [ps3])
                        else:
                            fw.op(act, lambda e: e.copy(out=pT.ap[:, 0:ncols], in_=ps3b[:, 0:ncols]), [pT], [ps3])
                        nb = len(kblocks)
                        for b, kb in enumerate(kblocks):
                            fw.op(pe, lambda e, b=b, kb=kb: e.matmul(ops.ap[:, h * 64:(h + 1) * 64], lhsT=pT.ap[:, b * 128:(b + 1) * 128], rhs=VV.ap[:, kb, g * 64:(g + 1) * 64], start=(b == 0), stop=(b == nb - 1)), [ops], [pT, qt_t[kb]])
                        fw.op(act, lambda e: e.activation(out=mixa.ap[:, h * 64:(h + 1) * 64], in_=ops.ap[:, h * 64:(h + 1) * 64], func=AF.Copy, scale=st.ap[:, 5:6]), [mixa], [ops, st])
                        if h == 7:
                            fw.dma(mix[i * 128:(i + 1) * 128, 0:512], mixa.ap, [mix_a[i]], [mixa])

                    units = [(i, h) for i in tiles3 for h in range(8)]
                    NU_ = len(units)
                    fr = {0: att_front(*units[0])}
                    md = {0: att_mid(fr.pop(0))}
                    if NU_ > 1:
                        fr[1] = att_front(*units[1])
                    for n in range(NU_):
                        att_back(md.pop(n))
                        if n + 1 < NU_:
                            md[n + 1] = att_mid(fr.pop(n + 1))
                        if n + 2 < NU_:
                            fr[n + 2] = att_front(*units[n + 2])
                    fw.barrier()
            if stop_after == "S3":
                if "mix" in dbg:
                    fw.barrier()
                    for i in range(NT):
                        fw.dma(dbg["mix"][i * 128:(i + 1) * 128, :], mix[i * 128:(i + 1) * 128, :], [], [mix_a[i], mix_r[i]])
                break
            FV, FR, FG, FKK, FBON = 0, 512, 1024, 1536, 2048
            FLW = lambda d_: 2560 + d_ * 1536
            FB = lambda d_: 2560 + d_ * 1536 + 512
            FKD = lambda d_: 2560 + d_ * 1536 + 1024
            NF = 5632
            with ExitStack() as es:
                def bc(name, src, n):
                    t_ = sb(es, nc, name, [128, n], F32)
                    fw.dma(t_.ap, src.partition_broadcast(128), [t_], [])
                    return t_
                mup = bc("mup", mu_prev[l], RWC)
                mun = bc("mun", mu_next[l], RWC)
                w0b = bc("w0b", decay_w0[l].rearrange("d c -> (d c)"), 1024)
                a0b = bc("a0b", iclr_a0[l].rearrange("d c -> (d c)"), 1024)
                kkb = bc("kkb", k_k[l], RW)
                kab = bc("kab", k_a[l], RW)
                rkb = bc("rkb", r_k[l], RW)
                w2t = sb(es, nc, "w2t", [128, RW], F32)
                a2t = sb(es, nc, "a2t", [128, RW], F32)
                g2t = sb(es, nc, "g2t", [128, RW], F32)
                fw.dma(w2t.ap, decay_w2[l].rearrange("d r c -> (d r) c"), [w2t], [])
                fw.dma(a2t.ap, iclr_a2[l].rearrange("d r c -> (d r) c"), [a2t], [])
                fw.dma(g2t.ap, gate_g2[l], [g2t], [])
                if l > 0:
                    v0b = bc("v0b", vres_v0[0], RW)
                    v1t = sb(es, nc, "v1t", [128, 4, 32], F32)
                    fw.dma(v1t.ap, vres_v1[0].rearrange("(c p) n -> p c n", p=128), [v1t], [])
                    v2t = sb(es, nc, "v2t", [32, RW], F32)
                    fw.dma(v2t.ap, vres_v2[0], [v2t], [])
                uc_pool = Pool(es, nc, "uc", [128, RWC], F32, 4)
                up_pool = Pool(es, nc, "up", [128, RWC], F32, 2)
                un_pool = Pool(es, nc, "un", [128, RWC], F32, 2)
                vf_pool = Pool(es, nc, "vf", [128, RW], F32, 4)
                li_pool = Pool(es, nc, "li", [128, 384], F32, 2)
                liT_pool = Pool(es, nc, "liT", [128, 3, 128], F32, 2)
                fo_pool = Pool(es, nc, "fo", [128, NF], F32, 2)
                tmp_pool = Pool(es, nc, "ftmp", [128, RW], F32, 6)
                a_pool = Pool(es, nc, "fa", [128, RW], F32, 4)
                sm_pool = Pool(es, nc, "fsm", [128, 64], F32, 2)
                vT_pool = Pool(es, nc, "fvT", [128, 4, 128], F32, 2)
                h3 = lambda ap: ap.rearrange("p (h d) -> p h d", h=8)

                def s4a_load(i):
                    uc, up, un = uc_pool.next(), up_pool.next(), un_pool.next()
                    deps = [u_t[i], u_guard] + ([u_t[i - 1]] if i > 0 else []) + ([u_t[i + 1]] if i + 1 < NT else [])
                    r0 = urow(i)
                    fw.dma(uc.ap, u_scr[r0:r0 + 128, :], [uc], deps)
                    fw.dma(up.ap, u_scr[r0 - 1:r0 + 127, :], [up], deps)
                    fw.dma(un.ap, u_scr[r0 + 1:r0 + 129, :], [un], deps)
                    vf = None
                    if l > 0:
                        vf = vf_pool.next()
                        fw.dma(vf.ap, vfirst[i * 128:(i + 1) * 128, :], [vf], [vf_t[i]])
                    return uc, up, un, vf

                def s4a_front(i, uc, up, un, vf):
                    fw.op(dve, lambda e: e.tensor_tensor(out=up.ap, in0=up.ap, in1=uc.ap, op=ALU.subtract), [up], [up, uc])
                    fw.op(pool, lambda e: e.tensor_tensor(out=un.ap, in0=un.ap, in1=uc.ap, op=ALU.subtract), [un], [un, uc])
                    fw.op(dve, lambda e: e.tensor_tensor(out=up.ap, in0=up.ap, in1=mup.ap, op=ALU.mult), [up], [up, mup])
                    fw.op(pool, lambda e: e.tensor_tensor(out=un.ap, in0=un.ap, in1=mun.ap, op=ALU.mult), [un], [un, mun])
                    fw.op(dve, lambda e: e.tensor_tensor(out=uc.ap, in0=uc.ap, in1=up.ap, op=ALU.add), [uc], [uc, up])
                    fw.op(dve, lambda e: e.tensor_tensor(out=uc.ap, in0=uc.ap, in1=un.ap, op=ALU.add), [uc], [uc, un])
                    return (i, uc, vf)

                def s4a_back1(i, uc, vf):
                    U = uc.ap
                    r_, k_, v_ = U[:, 0:512], U[:, 512:1024], U[:, 1024:1536]
                    li = li_pool.next()
                    fw.op(act, lambda e: e.activation(out=li.ap[:, 0:128], in_=U[:, 1536:1664], func=AF.Tanh), [li], [uc])
                    fw.op(act, lambda e: e.copy(out=li.ap[:, 128:256], in_=U[:, 1664:1792]), [li], [uc])
                    fw.op(act, lambda e: e.activation(out=li.ap[:, 256:384], in_=U[:, 1792:1920], func=AF.Sigmoid), [li], [uc])
                    ps = psum()
                    for q in range(3):
                        fw.op(pe, lambda e, q=q, ps=ps: e.transpose(out=ps.ap[:, q * 128:(q + 1) * 128], in_=li.ap[:, q * 128:(q + 1) * 128], identity=ident.ap), [ps], [li, ident])
                    liT = liT_pool.next()
                    fw.op(dve, lambda e, ps=ps: e.tensor_copy(out=liT.ap, in_=ps.ap[:, 0:384].rearrange("p (a b) -> p a b", a=3)), [liT], [ps])
                    fo = fo_pool.next()
                    FO = fo.ap
                    a_d = []
                    for d_ in range(2):
                        hp = slice(d_ * 64, (d_ + 1) * 64)
                        ps = psum()
                        fw.op(pe, lambda e, ps=ps, hp=hp: e.matmul(ps.ap, lhsT=liT.ap[hp, 0, :], rhs=w2t.ap[hp, :], start=True, stop=True), [ps], [liT, w2t])
                        t1 = tmp_pool.next()
                        fw.op(dve, lambda e, ps=ps, t1=t1, d_=d_: e.tensor_tensor(out=t1.ap, in0=ps.ap, in1=w0b.ap[:, d_ * 512:(d_ + 1) * 512], op=ALU.add), [t1], [ps, w0b])
                        fw.op(act, lambda e, t1=t1: e.activation(out=t1.ap, in_=t1.ap, func=AF.Sigmoid), [t1], [t1])
                        fw.op(act, lambda e, t1=t1, d_=d_: e.activation(out=FO[:, FLW(d_):FLW(d_) + 512], in_=t1.ap, func=AF.Copy, scale=-0.6065306597126334), [fo], [t1])
                        ps = psum()
                        fw.op(pe, lambda e, ps=ps, hp=hp: e.matmul(ps.ap, lhsT=liT.ap[hp, 1, :], rhs=a2t.ap[hp, :], start=True, stop=True), [ps], [liT, a2t])
                        ad = a_pool.next()
                        fw.op(dve, lambda e, ps=ps, ad=ad, d_=d_: e.tensor_tensor(out=ad.ap, in0=ps.ap, in1=a0b.ap[:, d_ * 512:(d_ + 1) * 512], op=ALU.add), [ad], [ps, a0b])
                        fw.op(act, lambda e, ad=ad: e.activation(out=ad.ap, in_=ad.ap, func=AF.Sigmoid), [ad], [ad])
                        a_d.append(ad)
                    ps = psum()
                    fw.op(pe, lambda e, ps=ps: e.matmul(ps.ap, lhsT=liT.ap[:, 2, :], rhs=g2t.ap, start=True, stop=True), [ps], [liT, g2t])
                    fw.op(act, lambda e, ps=ps: e.copy(out=FO[:, FG:FG + 512], in_=ps.ap), [fo], [ps])
                    if l == 0:
                        fw.op(act, lambda e: e.copy(out=FO[:, FV:FV + 512], in_=v_), [fo], [uc])
                        fw.dma(vfirst[i * 128:(i + 1) * 128, :], v_, [vf_t[i]], [uc])
                    else:
                        ps = psum()
                        for c_ in range(4):
                            fw.op(pe, lambda e, c_=c_, ps=ps: e.transpose(out=ps.ap[:, c_ * 128:(c_ + 1) * 128], in_=U[:, 1024 + c_ * 128:1024 + (c_ + 1) * 128], identity=ident.ap), [ps], [uc, ident])
                        vT = vT_pool.next()
                        fw.op(dve, lambda e, ps=ps, vT=vT: e.tensor_copy(out=vT.ap, in_=ps.ap.rearrange("p (a b) -> p a b", a=4)), [vT], [ps])
                        ps = psum()
                        for c_ in range(4):
                            fw.op(pe, lambda e, c_=c_, ps=ps, vT=vT: e.matmul(ps.ap[:, 0:32], lhsT=vT.ap[:, c_, :], rhs=v1t.ap[:, c_, :], start=(c_ == 0), stop=(c_ == 3)), [ps], [vT, v1t])
                        sm = sm_pool.next()
                        fw.op(dve, lambda e, ps=ps, sm=sm: e.tensor_copy(out=sm.ap[:, 0:32], in_=ps.ap[:, 0:32]), [sm], [ps])
                        ps = psum()
                        fw.op(pe, lambda e, ps=ps, sm=sm: e.transpose(out=ps.ap[0:32, 0:128], in_=sm.ap[:, 0:32], identity=ident.ap), [ps], [sm, ident])
                        smT = tmp_pool.next()
                        fw.op(dve, lambda e, ps=ps, smT=smT: e.tensor_copy(out=smT.ap[0:32, 0:128], in_=ps.ap[0:32, 0:128]), [smT], [ps])
                        ps = psum()
                        fw.op(pe, lambda e, ps=ps, smT=smT: e.matmul(ps.ap, lhsT=smT.ap[0:32, 0:128], rhs=v2t.ap, start=True, stop=True), [ps], [smT, v2t])
                        t1 = tmp_pool.next()
                        fw.op(dve, lambda e, ps=ps, t1=t1: e.tensor_tensor(out=t1.ap, in0=ps.ap, in1=v0b.ap, op=ALU.add), [t1], [ps, v0b])
                        fw.op(act, lambda e, t1=t1: e.activation(out=t1.ap, in_=t1.ap, func=AF.Sigmoid), [t1], [t1])
                        fw.op(dve, lambda e, vf=vf: e.tensor_tensor(out=vf.ap, in0=vf.ap, in1=v_, op=ALU.subtract), [vf], [vf, uc])
                        fw.op(dve, lambda e, vf=vf, t1=t1: e.tensor_tensor(out=vf.ap, in0=vf.ap, in1=t1.ap, op=ALU.mult), [vf], [vf, t1])
                        fw.op(dve, lambda e, vf=vf: e.tensor_tensor(out=FO[:, FV:FV + 512], in0=vf.ap, in1=v_, op=ALU.add), [fo], [vf, uc])
                    fw.op(act, lambda e: e.copy(out=FO[:, FR:FR + 512], in_=r_), [fo], [uc])
                    return dict(i=i, uc=uc, fo=fo, a_d=a_d)

                def s4a_back2(cx):
                    i, uc, fo, a_d = cx['i'], cx['uc'], cx['fo'], cx['a_d']
                    U = uc.ap
                    FO = fo.ap
                    r_, k_, v_ = U[:, 0:512], U[:, 512:1024], U[:, 1024:1536]
                    kk0 = tmp_pool.next()
                    sq = tmp_pool.next()
                    sm = sm_pool.next()
                    fw.op(dve, lambda e, kk0=kk0: e.tensor_tensor(out=kk0.ap, in0=k_, in1=kkb.ap, op=ALU.mult), [kk0], [uc, kkb])
                    fw.op(act, lambda e, kk0=kk0, sq=sq: e.activation(out=sq.ap, in_=kk0.ap, func=AF.Square), [sq], [kk0])
                    fw.op(dve, lambda e, sq=sq, sm=sm: e.reduce_sum(out=sm.ap[:, 0:8], in_=h3(sq.ap), axis=AX.X), [sm], [sq])
                    fw.op(act, lambda e, sm=sm: e.activation(out=sm.ap[:, 8:16], in_=sm.ap[:, 0:8], func=AF.Sqrt), [sm], [sm])
                    fw.op(dve, lambda e, sm=sm: e.tensor_scalar(out=sm.ap[:, 8:16], in0=sm.ap[:, 8:16], scalar1=1e-12, scalar2=None, op0=ALU.max), [sm], [sm])
                    fw.op(dve, lambda e, sm=sm: e.reciprocal(out=sm.ap[:, 16:24], in_=sm.ap[:, 8:16]), [sm], [sm])
                    fw.op(dve, lambda e, sm=sm, kk0=kk0: e.tensor_tensor(out=h3(FO[:, FKK:FKK + 512]), in0=h3(kk0.ap), in1=sm.ap[:, 16:24].unsqueeze(2).to_broadcast([128, 8, 64]), op=ALU.mult), [fo], [kk0, sm])
                    for d_ in range(2):
                        t1 = tmp_pool.next()
                        fw.op(dve, lambda e, t1=t1, d_=d_: e.scalar_tensor_tensor(out=t1.ap, in0=a_d[d_].ap, scalar=-1.0, in1=kab.ap, op0=ALU.add, op1=ALU.mult), [t1], [a_d[d_], kab])
                        fw.op(dve, lambda e, t1=t1, d_=d_: e.scalar_tensor_tensor(out=FO[:, FKD(d_):FKD(d_) + 512], in0=t1.ap, scalar=1.0, in1=k_, op0=ALU.add, op1=ALU.mult), [fo], [t1, uc])
                        fw.op(pool, lambda e, d_=d_: e.tensor_tensor(out=FO[:, FB(d_):FB(d_) + 512], in0=FO[:, FKK:FKK + 512], in1=a_d[d_].ap, op=ALU.mult), [fo], [fo, a_d[d_]])
                    t1 = tmp_pool.next()
                    sm2 = sm_pool.next()
                    fw.op(pool, lambda e, t1=t1: e.tensor_tensor(out=t1.ap, in0=FO[:, FKD(0):FKD(0) + 512], in1=FO[:, FKD(1):FKD(1) + 512], op=ALU.add), [t1], [fo])
                    fw.op(dve, lambda e, t1=t1: e.tensor_tensor(out=t1.ap, in0=t1.ap, in1=r_, op=ALU.mult), [t1], [t1, uc])
                    fw.op(pool, lambda e, t1=t1: e.tensor_tensor(out=t1.ap, in0=t1.ap, in1=rkb.ap, op=ALU.mult), [t1], [t1, rkb])
                    fw.op(dve, lambda e, t1=t1, sm2=sm2: e.reduce_sum(out=sm2.ap[:, 0:8], in_=h3(t1.ap), axis=AX.X), [sm2], [t1])
                    fw.op(dve, lambda e, sm2=sm2: e.tensor_tensor(out=h3(FO[:, FBON:FBON + 512]), in0=h3(FO[:, FV:FV + 512]), in1=sm2.ap[:, 0:8].unsqueeze(2).to_broadcast([128, 8, 64]), op=ALU.mult), [fo], [fo, sm2])
                    fw.dma(feat[i * 128:(i + 1) * 128, :], FO, [feat_t[i]], [fo])
                    if "feat" in dbg and l == 0:
                        fw.dma(dbg["feat"][i * 128:(i + 1) * 128, :], FO[:, 0:4096], [], [fo])

                lds = {k: s4a_load(k) for k in range(2)}
                fr = {0: s4a_front(0, *lds.pop(0)), 1: s4a_front(1, *lds.pop(1))}
                lds[2] = s4a_load(2)
                b1s = {0: s4a_back1(*fr.pop(0))}
                for i in range(NT):
                    if i + 3 < NT:
                        lds[i + 3] = s4a_load(i + 3)
                    s4a_back2(b1s.pop(i))
                    if i + 1 < NT:
                        b1s[i + 1] = s4a_back1(*fr.pop(i + 1))
                    if i + 2 < NT:
                        fr[i + 2] = s4a_front(i + 2, *lds.pop(i + 2))
                fw.barrier()
            if stop_after == "S4a":
                break
            with ExitStack() as es:
                ones = sb(es, nc, "ones", [128, 1], F32)
                fw.op(dve, lambda e: e.memset(ones.ap, 1.0), [ones], [])
                Mq1, MnT = [], []
                for d_ in range(2):
                    m1_ = sb(es, nc, "Mq1", [128, 256], F32)
                    fw.op(dve, lambda e, m1_=m1_, d_=d_: e.tensor_scalar(out=m1_.ap[:, 0:128], in0=masks.ap[:, 2 * d_, :], scalar1=-1.0, scalar2=None, op0=ALU.mult), [m1_], [masks])
                    fw.op(dve, lambda e, m1_=m1_, d_=d_: e.tensor_copy(out=m1_.ap[:, 128:256], in_=masks.ap[:, 2 * d_ + 1, :]), [m1_], [masks])
                    Mq1.append(m1_)
                    mt_ = sb(es, nc, "MnT", [128, 128], F32)
                    fw.op(dve, lambda e, mt_=mt_, d_=d_: e.tensor_scalar(out=mt_.ap, in0=masks.ap[:, 2 * (1 - d_), :], scalar1=-1.0, scalar2=None, op0=ALU.mult), [mt_], [masks])
                    MnT.append(mt_)
                Mq2 = [masks.ap[:, 2 * d_:2 * d_ + 2, :].rearrange("p a t -> p (a t)") for d_ in range(2)]
                PSQB = int(os.environ.get('S4B_PSQB', '4'))
                PSQ = [TT(PS[PSQB + k // 4].ap[:, (k % 4) * 128:(k % 4 + 1) * 128]) for k in range(16)]
                pq = [0]

                def psq():
                    t_ = PSQ[pq[0] % 16]
                    pq[0] += 1
                    return t_
                p4 = [0]

                def psumR():
                    t_ = PS[p4[0] % 8]
                    p4[0] += 1
                    return t_
                fa_pool = Pool(es, nc, "sfa", [128, 3, 512], F32, 4)
                fb_pool = Pool(es, nc, "sfb", [128, 3, 512], F32, 4)
                tm_pool = Pool(es, nc, "stm", [128, 4, 512], BF16, 2)
                vb_pool = Pool(es, nc, "svb", [128, 512], BF16, 2)
                ex_pool = Pool(es, nc, "sex", [128, 3, 512], F32, 2)
                fm_pool = Pool(es, nc, "sfm", [128, 4, 4, 128], BF16, 2)
                el_pool = Pool(es, nc, "sel", [128, 4], F32, 2)
                g1_pool = Pool(es, nc, "sg1", [128, 2, 4, 128], BF16, 4)
                a2_pool = Pool(es, nc, "sa2", [128, 256], BF16, 16)
                inv_pool = Pool(es, nc, "sinv", [128, 4, 128], BF16, 32)
                invf_pool = Pool(es, nc, "sinvf", [128, 4, 128], F32, 10)
                xs_pool = Pool(es, nc, "sxs", [128, 4, 64], BF16, 4)
                nu_pool = Pool(es, nc, "snu", [128, 512], BF16, 2)
                ys_pool = Pool(es, nc, "sys", [128, 512], F32, 2)
                ST = [[[sb(es, nc, "ST", [128, 64], F32) for c_ in range(4)] for pp in range(2)] for d_ in range(2)]
                STb = [[[sb(es, nc, "STb", [128, 64], BF16) for c_ in range(4)] for pp in range(2)] for d_ in range(2)]
                for d_ in range(2):
                    for c_ in range(4):
                        fw.op(dve, lambda e, d_=d_, c_=c_: e.memset(ST[d_][0][c_].ap, 0.0), [ST[d_][0][c_]], [])
                        fw.op(dve, lambda e, d_=d_, c_=c_: e.memset(STb[d_][0][c_].ap, 0.0), [STb[d_][0][c_]], [])
                orders = [list(range(NT)), [1, 0] + list(range(NT - 1, NCT - 1, -1))]
                evi = [0]

                def evac(fn_dve, fn_act, outs, ins):
                    if evi[0] % 2:
                        fw.op(dve, fn_dve, outs, ins)
                    else:
                        fw.op(act, fn_act, outs, ins)

                def s4b_load(d_, i):
                    fa, fb = fa_pool.next(), fb_pool.next()
                    r0 = i * 128
                    fw.dma(fa.ap, feat[r0:r0 + 128, FLW(d_):FLW(d_) + 1536].rearrange("p (a c) -> p a c", a=3), [fa], [feat_t[i]])
                    fw.dma(fb.ap[:, 0:2, :], feat[r0:r0 + 128, 0:1024].rearrange("p (a c) -> p a c", a=2), [fb], [feat_t[i]])
                    fw.dma(fb.ap[:, 2, :], feat[r0:r0 + 128, FKK:FKK + 512], [fb], [feat_t[i]])
                    return fa, fb

                def step_prep(d_, j, fa, fb):
                    i = orders[d_][j]
                    pp = j % 2
                    STo, STn = ST[d_][pp], ST[d_][1 - pp]
                    SBo, SBn = STb[d_][pp], STb[d_][1 - pp]
                    vb = vb_pool.next()
                    fw.op(act, lambda e: e.copy(out=vb.ap, in_=fb.ap[:, 0, :]), [vb], [fb])
                    lw, b_, kd = fa.ap[:, 0, :], fa.ap[:, 1, :], fa.ap[:, 2, :]
                    v_, r_, kk = fb.ap[:, 0, :], fb.ap[:, 1, :], fb.ap[:, 2, :]
                    psL = psumR()
                    fw.op(pe, lambda e: e.matmul(psL.ap, lhsT=masks.ap[:, 1 + 2 * d_, :], rhs=lw, start=True, stop=True), [psL], [masks, fa])
                    ex = ex_pool.next()
                    fw.op(act, lambda e: e.activation(out=ex.ap[:, 0, :], in_=psL.ap, func=AF.Exp), [ex], [psL])
                    fw.op(act, lambda e: e.activation(out=ex.ap[:, 1, :], in_=psL.ap, func=AF.Exp, scale=-1.0), [ex], [psL])
                    fw.op(dve, lambda e: e.tensor_tensor(out=ex.ap[:, 2, :], in0=psL.ap, in1=lw, op=ALU.subtract), [ex], [psL, fa])
                    fw.op(act, lambda e: e.activation(out=ex.ap[:, 2, :], in_=ex.ap[:, 2, :], func=AF.Exp), [ex], [ex])
                    tm = tm_pool.next()
                    fw.op(dve, lambda e: e.tensor_tensor(out=tm.ap[:, 0, :], in0=kk, in1=ex.ap[:, 2, :], op=ALU.mult), [tm], [fb, ex])
                    fw.op(pool, lambda e: e.tensor_tensor(out=tm.ap[:, 1, :], in0=r_, in1=ex.ap[:, 0, :], op=ALU.mult), [tm], [fb, ex])
                    fw.op(dve, lambda e: e.tensor_tensor(out=tm.ap[:, 2, :], in0=b_, in1=ex.ap[:, 1, :], op=ALU.mult), [tm], [fa, ex])
                    fw.op(pool, lambda e: e.tensor_tensor(out=tm.ap[:, 3, :], in0=kd, in1=ex.ap[:, 1, :], op=ALU.mult), [tm], [fa, ex])
                    psE = psumR()
                    for c_ in range(4):
                        fw.op(pe, lambda e, c_=c_: e.matmul(psE.ap[:, c_:c_ + 1], lhsT=lw[:, c_ * 128:(c_ + 1) * 128], rhs=ones.ap, start=True, stop=True), [psE], [fa, ones])
                    el = el_pool.next()
                    fw.op(act, lambda e: e.activation(out=el.ap, in_=psE.ap[:, 0:4], func=AF.Exp), [el], [psE])
                    fm = fm_pool.next()
                    for c_ in range(4):
                        psT = psumR()
                        for q in range(4):
                            fw.op(pe, lambda e, c_=c_, q=q, psT=psT: e.transpose(out=psT.ap.bitcast(BF16)[:, q * 128:(q + 1) * 128], in_=tm.ap[:, q, c_ * 128:(c_ + 1) * 128], identity=identb.ap), [psT], [tm, identb])
                        evi[0] += 1
                        evac(lambda e, c_=c_, psT=psT: e.tensor_copy(out=fm.ap[:, c_, :, :], in_=psT.ap.bitcast(BF16)[:, 0:512].rearrange("p (a b) -> p a b", a=4)),
                             lambda e, c_=c_, psT=psT: e.copy(out=fm.ap[:, c_, :, :], in_=psT.ap.bitcast(BF16)[:, 0:512].rearrange("p (a b) -> p a b", a=4)), [fm], [psT])
                    H = []
                    for h in range(8):
                        c_ = h // 2
                        hp = slice((h % 2) * 64, (h % 2) * 64 + 64)
                        H.append(dict(c=c_, hp=hp, par=h % 2, k=h // 2, al=fm.ap[hp, c_, 0, :], rh=fm.ap[hp, c_, 1, :], be=fm.ap[hp, c_, 2, :], ka=fm.ap[hp, c_, 3, :],
                                      ar=fm.ap[hp, c_, 0:2, :].rearrange("p a t -> p (a t)"), vh=vb.ap[:, h * 64:(h + 1) * 64]))
                    G1 = [g1_pool.next() for _ in range(2)]
                    for h in range(8):
                        u = H[h]
                        psG = psumR()
                        fw.op(pe, lambda e, u=u, psG=psG: e.matmul(psG.ap[:, 0:256], lhsT=u["be"], rhs=u["ar"], start=True, stop=True), [psG], [fm])
                        fw.op(pe, lambda e, u=u, psG=psG: e.matmul(psG.ap[:, 256:512], lhsT=u["ka"], rhs=u["ar"], start=True, stop=True), [psG], [fm])
                        a2 = a2_pool.next()
                        g1 = G1[u["par"]]
                        fw.op(dve, lambda e, u=u, psG=psG, g1=g1: e.tensor_tensor(out=g1.ap[:, :, u["k"], :], in0=psG.ap[:, 0:256].rearrange("p (a t) -> p a t", a=2), in1=Mq1[d_].ap.rearrange("p (a t) -> p a t", a=2), op=ALU.mult), [g1], [psG, Mq1[d_]])
                        fw.op(dve, lambda e, a2=a2, psG=psG: e.tensor_tensor(out=a2.ap, in0=psG.ap[:, 256:512], in1=Mq2[d_], op=ALU.mult), [a2], [psG, masks])
                        u.update(a2=a2, abr=g1.ap[:, 1, u["k"], :], g1=g1)
                    GP = []
                    for par in range(2):
                        g1 = G1[par]
                        Mb = inv_pool.next()
                        fw.op(dve, lambda e, Mb=Mb, g1=g1: e.tensor_tensor(out=Mb.ap, in0=g1.ap[:, 0, :, :], in1=ident.ap.unsqueeze(1).to_broadcast([128, 4, 128]), op=ALU.add), [Mb], [g1, ident])
                        bank = psumR()
                        bkb = bank.ap.bitcast(BF16)
                        for k_ in range(4):
                            fw.op(pe, lambda e, k_=k_, bkb=bkb, g1=g1: e.transpose(out=bkb[:, k_ * 128:(k_ + 1) * 128], in_=g1.ap[:, 0, k_, :], identity=identb.ap), [bank], [g1, identb])
                        AT = inv_pool.next()
                        evi[0] += 1
                        evac(lambda e, AT=AT, bkb=bkb: e.tensor_copy(out=f4(AT.ap), in_=bkb[:, 0:512]), lambda e, AT=AT, bkb=bkb: e.copy(out=f4(AT.ap), in_=bkb[:, 0:512]), [AT], [bank])
                        GP.append(dict(A=g1, Aap=g1.ap[:, 0, :, :], AT=AT, Mb=Mb))
                    return dict(d_=d_, i=i, STo=STo, STn=STn, tm=tm, el=el, fm=fm, fb=fb, v_=v_, H=H, GP=GP, SBo=SBo, SBn=SBn, vb=vb)

                f4 = lambda ap: ap.rearrange("p k t -> p (k t)")

                def inv_all(GP):
                    for it in range(6):
                        for g in GP:
                            bank = psumR()
                            for k_ in range(4):
                                fw.op(pe, lambda e, k_=k_, bank=bank, g=g: e.matmul(bank.ap[:, k_ * 128:(k_ + 1) * 128], lhsT=g["Aap"][:, k_, :], rhs=g["AT"].ap[:, k_, :], start=True, stop=True), [bank], [g["A"], g["AT"]])
                            ATn = inv_pool.next()
                            evi[0] += 1
                            evac(lambda e, ATn=ATn, bank=bank: e.tensor_copy(out=f4(ATn.ap), in_=bank.ap), lambda e, ATn=ATn, bank=bank: e.copy(out=f4(ATn.ap), in_=bank.ap), [ATn], [bank])
                            g["ATn"] = ATn
                            if it < 5:
                                bank = psumR()
                                for k_ in range(4):
                                    fw.op(pe, lambda e, k_=k_, bank=bank, g=g: e.matmul(bank.ap[:, k_ * 128:(k_ + 1) * 128], lhsT=g["AT"].ap[:, k_, :], rhs=g["Aap"][:, k_, :], start=True, stop=True), [bank], [g["A"], g["AT"]])
                                An = inv_pool.next()
                                evi[0] += 1
                                evac(lambda e, An=An, bank=bank: e.tensor_copy(out=f4(An.ap), in_=bank.ap), lambda e, An=An, bank=bank: e.copy(out=f4(An.ap), in_=bank.ap), [An], [bank])
                                g["An"] = An
                        for g in GP:
                            g["AT"] = g["ATn"]
                            if it < 5:
                                g["A"], g["Aap"] = g["An"], g["An"].ap
                        for g in GP:
                            bank = psumR()
                            for k_ in range(4):
                                fw.op(pe, lambda e, k_=k_, bank=bank, g=g: e.matmul(bank.ap[:, k_ * 128:(k_ + 1) * 128], lhsT=identb.ap, rhs=g["Mb"].ap[:, k_, :], start=True, stop=False), [bank], [identb, g["Mb"]])
                                fw.op(pe, lambda e, k_=k_, bank=bank, g=g: e.matmul(bank.ap[:, k_ * 128:(k_ + 1) * 128], lhsT=g["AT"].ap[:, k_, :], rhs=g["Mb"].ap[:, k_, :], start=False, stop=True), [bank], [g["AT"], g["Mb"]])
                            Mbn = inv_pool.next()
                            evi[0] += 1
                            evac(lambda e, Mbn=Mbn, bank=bank: e.tensor_copy(out=f4(Mbn.ap), in_=bank.ap), lambda e, Mbn=Mbn, bank=bank: e.copy(out=f4(Mbn.ap), in_=bank.ap), [Mbn], [bank])
                            g["Mb"] = Mbn

                def step_state(cx):
                    d_, i, STo, STn, tm, el, fm, fb, v_, H, GP, SBo, SBn, vb = (cx[k] for k in ('d_', 'i', 'STo', 'STn', 'tm', 'el', 'fm', 'fb', 'v_', 'H', 'GP', 'SBo', 'SBn', 'vb'))
                    NU = nu_pool.next()
                    q4 = lambda ap: ap.rearrange("p (k t) -> p k t", k=4)[:, :, 0:64]
                    for par, g in enumerate(GP):
                        bank = psumR()
                        for k_ in range(4):
                            u = H[par + 2 * k_]
                            o = bank.ap[:, k_ * 128:k_ * 128 + 64]
                            fw.op(pe, lambda e, u=u, o=o: e.matmul(o, lhsT=u["al"], rhs=SBo[u["c"]].ap[u["hp"], :], start=True, stop=False), [bank], [fm, SBo[u["c"]]])
                            fw.op(pe, lambda e, u=u, o=o: e.matmul(o, lhsT=u["a2"].ap[:, 0:128], rhs=u["vh"], start=False, stop=True), [bank], [u["a2"], vb])
                        xsg = xs_pool.next()
                        evi[0] += 1
                        evac(lambda e, xsg=xsg, bank=bank: e.tensor_copy(out=xsg.ap, in_=q4(bank.ap)), lambda e, xsg=xsg, bank=bank: e.copy(out=xsg.ap, in_=q4(bank.ap)), [xsg], [bank])
                        g["xs"] = xsg
                    for par, g in enumerate(GP):
                        bank = psumR()
                        for k_ in range(4):
                            fw.op(pe, lambda e, k_=k_, bank=bank, g=g: e.matmul(bank.ap[:, k_ * 128:k_ * 128 + 64], lhsT=g["Mb"].ap[:, k_, :], rhs=g["xs"].ap[:, k_, :], start=True, stop=True), [bank], [g["Mb"], g["xs"]])
                        nuv = NU.ap.rearrange("p (c two d) -> p c two d", two=2, d=64)[:, :, par, :]
                        fw.op(act, lambda e, bank=bank, nuv=nuv: e.activation(out=nuv, in_=q4(bank.ap), func=AF.Copy, scale=-1.0), [NU], [bank])
                    ysb = ys_pool.next()
                    for par in (0, 1):
                        psY = psumR()
                        for k_ in range(4):
                            h = par + 2 * k_
                            u = H[h]
                            ysl = psY.ap[:, k_ * 64:(k_ + 1) * 64]
                            fw.op(pe, lambda e, u=u, ysl=ysl: e.matmul(ysl, lhsT=u["rh"], rhs=SBo[u["c"]].ap[u["hp"], :], start=True, stop=False), [psY], [fm, SBo[u["c"]]])
                            fw.op(pe, lambda e, u=u, ysl=ysl, h=h: e.matmul(ysl, lhsT=u["abr"], rhs=NU.ap[:, h * 64:(h + 1) * 64], start=False, stop=False), [psY], [u["g1"], NU])
                            fw.op(pe, lambda e, u=u, ysl=ysl: e.matmul(ysl, lhsT=u["a2"].ap[:, 128:256], rhs=u["vh"], start=False, stop=True), [psY], [u["a2"], vb])
                        yv = ysb.ap.rearrange("p (c two d) -> p c two d", two=2, d=64)[:, :, par, :]
                        fw.op(act, lambda e, psY=psY, yv=yv: e.copy(out=yv, in_=psY.ap[:, 0:256].rearrange("p (c d) -> p c d", d=64)), [ysb], [psY])
                    fw.dma(y_scr[d_, i * 128:(i + 1) * 128, :], ysb.ap, [y_t[d_][i]], [ysb])
                    psS = psumR()
                    for c_ in range(4):
                        so = psS.ap[:, c_ * 128:(c_ + 1) * 128]
                        fw.op(pe, lambda e, c_=c_, so=so: e.matmul(so, lhsT=tm.ap[:, 2, c_ * 128:(c_ + 1) * 128], rhs=NU.ap[:, c_ * 128:(c_ + 1) * 128], start=True, stop=False), [psS], [tm, NU])
                        fw.op(pe, lambda e, c_=c_, so=so: e.matmul(so, lhsT=tm.ap[:, 3, c_ * 128:(c_ + 1) * 128], rhs=vb.ap[:, c_ * 128:(c_ + 1) * 128], start=False, stop=True), [psS], [tm, vb])
                        fw.op(act, lambda e, c_=c_: e.activation(out=STn[c_].ap, in_=STo[c_].ap, func=AF.Copy, scale=el.ap[:, c_:c_ + 1]), [STn[c_]], [STo[c_], el])
                    for c_ in range(4):
                        so = psS.ap[:, c_ * 128:(c_ + 1) * 128]
                        for hh in range(2):
                            hp = slice(hh * 64, hh * 64 + 64)
                            fw.op(dve, lambda e, c_=c_, hp=hp, so=so: e.scalar_tensor_tensor(out=STn[c_].ap[hp, :], in0=so[hp, hp], scalar=el.ap[hp, c_:c_ + 1], in1=STn[c_].ap[hp, :], op0=ALU.mult, op1=ALU.add), [STn[c_]], [psS, el, STn[c_]])
                    for c_ in range(4):
                        fw.op(act, lambda e, c_=c_: e.copy(out=SBn[c_].ap, in_=STn[c_].ap), [SBn[c_]], [STn[c_]])

                nsteps = NT if not os.environ.get('S4B_STEPS') else int(os.environ['S4B_STEPS'])
                nxt = [s4b_load(d_, orders[d_][0]) for d_ in range(2)]
                for j in range(nsteps):
                    cur = list(nxt)
                    cxs = []
                    for d_ in range(2):
                        cxs.append(step_prep(d_, j, *cur[d_]))
                        if j + 1 < nsteps:
                            nxt[d_] = s4b_load(d_, orders[d_][j + 1])
                    inv_all([g for cx in cxs for g in cx["GP"]])
                    for cx in cxs:
                        step_state(cx)
                fw.barrier()
            if stop_after == "S4b":
                break
            with ExitStack() as es:
                lnw = sb(es, nc, "lnw", [128, RW], F32)
                lnb = sb(es, nc, "lnb", [128, RW], F32)
                fw.dma(lnw.ap, ln_x_w[l].partition_broadcast(128), [lnw], [])
                fw.dma(lnb.ap, ln_x_b[l].partition_broadcast(128), [lnb], [])
                y0_pool = Pool(es, nc, "cy0", [128, RW], F32, 2)
                y1_pool = Pool(es, nc, "cy1", [128, RW], F32, 2)
                gb_pool = Pool(es, nc, "cgb", [128, 2, RW], F32, 2)
                sq_pool = Pool(es, nc, "csq", [128, RW], F32, 2)
                cs_pool = Pool(es, nc, "ccs", [128, 32], F32, 2)
                h3 = lambda ap: ap.rearrange("p (h d) -> p h d", h=8)
                bc8 = lambda ap: ap.unsqueeze(2).to_broadcast([128, 8, 64])
                for i in (range(NCT, NT) if last else range(NT)):
                    y0, y1, gb = y0_pool.next(), y1_pool.next(), gb_pool.next()
                    r0 = i * 128
                    fw.dma(y0.ap, y_scr[0, r0:r0 + 128, :], [y0], [y_t[0][i]])
                    fw.dma(y1.ap, y_scr[1, r0:r0 + 128, :], [y1], [y_t[1][i]])
                    fw.dma(gb.ap[:, 0, :], feat[r0:r0 + 128, FG:FG + 512], [gb], [feat_t[i]])
                    fw.dma(gb.ap[:, 1, :], feat[r0:r0 + 128, FBON:FBON + 512], [gb], [feat_t[i]])
                    cs_ = cs_pool.next()
                    sq = sq_pool.next()
                    fw.op(dve, lambda e: e.tensor_tensor(out=y0.ap, in0=y0.ap, in1=y1.ap, op=ALU.add), [y0], [y0, y1])
                    fw.op(dve, lambda e: e.reduce_sum(out=cs_.ap[:, 0:8], in_=h3(y0.ap), axis=AX.X), [cs_], [y0])
                    fw.op(dve, lambda e: e.tensor_scalar(out=cs_.ap[:, 0:8], in0=cs_.ap[:, 0:8], scalar1=1.0 / 64, scalar2=None, op0=ALU.mult), [cs_], [cs_])
                    fw.op(dve, lambda e: e.tensor_tensor(out=h3(y0.ap), in0=h3(y0.ap), in1=bc8(cs_.ap[:, 0:8]), op=ALU.subtract), [y0], [y0, cs_])
                    fw.op(act, lambda e: e.activation(out=sq.ap, in_=y0.ap, func=AF.Square), [sq], [y0])
                    fw.op(dve, lambda e: e.reduce_sum(out=cs_.ap[:, 8:16], in_=h3(sq.ap), axis=AX.X), [cs_], [sq])
                    fw.op(dve, lambda e: e.tensor_scalar(out=cs_.ap[:, 8:16], in0=cs_.ap[:, 8:16], scalar1=1.0 / 64, scalar2=64e-5, op0=ALU.mult, op1=ALU.add), [cs_], [cs_])
                    fw.op(act, lambda e: e.activation(out=cs_.ap[:, 16:24], in_=cs_.ap[:, 8:16], func=AF.Sqrt), [cs_], [cs_])
                    fw.op(dve, lambda e: e.reciprocal(out=cs_.ap[:, 24:32], in_=cs_.ap[:, 16:24]), [cs_], [cs_])
                    fw.op(dve, lambda e: e.tensor_tensor(out=h3(y0.ap), in0=h3(y0.ap), in1=bc8(cs_.ap[:, 24:32]), op=ALU.mult), [y0], [y0, cs_])
                    fw.op(pool, lambda e: e.tensor_tensor(out=y0.ap, in0=y0.ap, in1=lnw.ap, op=ALU.mult), [y0], [y0, lnw])
                    fw.op(dve, lambda e: e.tensor_tensor(out=y0.ap, in0=y0.ap, in1=lnb.ap, op=ALU.add), [y0], [y0, lnb])
                    fw.op(pool, lambda e: e.tensor_tensor(out=y0.ap, in0=y0.ap, in1=gb.ap[:, 1, :], op=ALU.add), [y0], [y0, gb])
                    fw.op(dve, lambda e: e.tensor_tensor(out=y0.ap, in0=y0.ap, in1=gb.ap[:, 0, :], op=ALU.mult), [y0], [y0, gb])
                    fw.dma(mix[r0:r0 + 128, 512:1024], y0.ap, [mix_r[i]], [y0])
                fw.barrier()
            if stop_after == "S4":
                if "mix" in dbg:
                    for i in range(NT):
                        fw.dma(dbg["mix"][i * 128:(i + 1) * 128, :], mix[i * 128:(i + 1) * 128, :], [], [mix_a[i], mix_r[i]])
                    fw.barrier()
                break
            tiles5 = list(range(NCT, NT)) if last else list(range(NT))
            with ExitStack() as es:
                woutb = sb(es, nc, "woutb", [128, 8, D], BF16)
                wst_pool = Pool(es, nc, "wst5", [128, 8, 256], F32, 2)
                for j in range(4):
                    wst = wst_pool.next()
                    fw.dma(wst.ap, w_out[l][:, j * 256:(j + 1) * 256].rearrange("(kc p) n -> p kc n", p=128), [wst], [])
                    fw.op(pool, lambda e, wst=wst, j=j: e.tensor_copy(out=woutb.ap[:, :, j * 256:(j + 1) * 256], in_=wst.ap), [woutb], [wst])
                modG = [sb(es, nc, "modG%d" % s_, [128, D], F32) for s_ in range(2)]
                for s_ in range(2):
                    fw.dma(modG[s_].ap, modbuf[l, s_ * 6 + 2], [modG[s_]], [mod_t[l][s_ * 6 + 2]])
                mx_pool = Pool(es, nc, "mx", [128, D], F32, 2)
                mxb_pool = Pool(es, nc, "mxb", [128, D], BF16, 2)
                mT_pool = Pool(es, nc, "mT", [128, 8, 128], BF16, 2)
                x5_pool = Pool(es, nc, "x5", [128, D], F32, 2)

                def s5_load(i):
                    mx = mx_pool.next()
                    fw.dma(mx.ap, mix[i * 128:(i + 1) * 128, :], [mx], [mix_a[i], mix_r[i]])
                    xt = x5_pool.next()
                    fw.dma(xt.ap, xs[i * 128:(i + 1) * 128, :], [xt], [xs_t[i]])
                    return mx, xt

                nxt = s5_load(tiles5[0])
                for ti, i in enumerate(tiles5):
                    mx, xt = nxt
                    if ti + 1 < len(tiles5):
                        nxt = s5_load(tiles5[ti + 1])
                    s_ = 1 if i < NCT else 0
                    mxb = mxb_pool.next()
                    fw.op(act, lambda e, mx=mx, mxb=mxb: e.copy(out=mxb.ap, in_=mx.ap), [mxb], [mx])
                    mT = mT_pool.next()
                    for half in range(2):
                        ps = psum()
                        psb = ps.ap.bitcast(BF16)
                        for kk in range(4):
                            kc = half * 4 + kk
                            fw.op(pe, lambda e, kc=kc, kk=kk, psb=psb, mxb=mxb: e.transpose(out=psb[:, kk * 128:(kk + 1) * 128], in_=mxb.ap[:, kc * 128:(kc + 1) * 128], identity=identb.ap), [ps], [mxb, identb])
                        fw.op(act if half else dve, lambda e, psb=psb, mT=mT, half=half: (e.copy if half else e.tensor_copy)(out=mT.ap[:, half * 4:(half + 1) * 4, :], in_=psb[:, 0:512].rearrange("p (a b) -> p a b", a=4)), [mT], [ps])
                    for half in range(2):
                        ps = psum()
                        hs = slice(half * 512, (half + 1) * 512)
                        for kc in range(8):
                            fw.op(pe, lambda e, kc=kc, ps=ps, mT=mT, hs=hs: e.matmul(ps.ap, lhsT=mT.ap[:, kc, :], rhs=woutb.ap[:, kc, hs], start=(kc == 0), stop=(kc == 7)), [ps], [mT, woutb])
                        fw.op(dve, lambda e, ps=ps, mx=mx, hs=hs, s_=s_: e.tensor_tensor(out=mx.ap[:, hs], in0=ps.ap, in1=modG[s_].ap[:, hs], op=ALU.mult), [mx], [ps, modG[s_]])
                    fw.op(dve, lambda e, xt=xt, mx=mx: e.tensor_tensor(out=xt.ap, in0=xt.ap, in1=mx.ap, op=ALU.add), [xt], [xt, mx])
                    fw.dma(xs[i * 128:(i + 1) * 128, :], xt.ap, [xs_t[i]], [xt])
                    if "xmix" in dbg and l == 0:
                        fw.dma(dbg["xmix"][i * 128:(i + 1) * 128, :], xt.ap, [], [xt])
                fw.barrier()
            if stop_after == "S5":
                break

            tiles6 = list(range(NCT, NT)) if last else list(range(NT))
            GS = 8
            ng_ = -(-len(tiles6) // GS)
            gsz = [len(tiles6) // ng_ + (1 if k < len(tiles6) % ng_ else 0) for k in range(ng_)]
            groups, k0_ = [], 0
            for z in gsz:
                groups.append(tiles6[k0_:k0_ + z])
                k0_ += z
            with ExitStack() as es:
                nseg = 1 if last else 2
                mA = [sb(es, nc, "mA%d" % s_, [128, D], F32) for s_ in range(nseg)]
                mB = [sb(es, nc, "mB%d" % s_, [128, D], F32) for s_ in range(nseg)]
                mG = [sb(es, nc, "mG%d" % s_, [128, D], F32) for s_ in range(nseg)]
                for s_ in range(nseg):
                    fw.dma(mA[s_].ap, modbuf[l, s_ * 6 + 3], [mA[s_]], [mod_t[l][s_ * 6 + 3]])
                    fw.dma(mB[s_].ap, modbuf[l, s_ * 6 + 4], [mB[s_]], [mod_t[l][s_ * 6 + 4]])
                    fw.dma(mG[s_].ap, modbuf[l, s_ * 6 + 5], [mG[s_]], [mod_t[l][s_ * 6 + 5]])
                rwt = sb(es, nc, "rwt", [128, 8, NE], F32)
                fw.dma(rwt.ap, router_w.rearrange("(kc p) n -> p kc n", p=128), [rwt], [])
                rbt = sb(es, nc, "rbt", [128, NE], F32)
                fw.dma(rbt.ap, router_b.partition_broadcast(128), [rbt], [])
                fgt = None
                if last:
                    fgt = sb(es, nc, "fgt", [128, D], F32)
                    fw.dma(fgt.ap, final_g.partition_broadcast(128), [fgt], [])
                HT2 = [sb(es, nc, "HT", [128, 8, GS * 128], BF16) for _ in range(2)]
                accs = [sb(es, nc, "acc%d" % k, [128, D], F32) for k in range(GS)]
                gates2 = [sb(es, nc, "gates", [128, GS, NE], F32) for _ in range(2)]
                Wg2 = [sb(es, nc, "Wg", [128, 8, D], BF16) for _ in range(2)]
                Wu2 = [sb(es, nc, "Wu", [128, 8, D], BF16) for _ in range(2)]
                Wd = sb(es, nc, "Wd", [128, 8, D], BF16)
                actb = sb(es, nc, "actb", [128, 8, GS * 128], BF16)
                x6_pool = Pool(es, nc, "x6", [128, D], F32, 2)
                hT32_pool = Pool(es, nc, "hT32", [128, 8, 128], F32, 1)
                hooks = {}
                rt_pool = Pool(es, nc, "rt", [128, 96], F32, 2)
                sg_pool = Pool(es, nc, "sg", [128, 512], F32, 2)
                st6_pool = Pool(es, nc, "st6", [128, 4], F32, 2)

                def rms(xt, st, junk6):
                    jap = junk6.ap if len(junk6.ap.shape) == 2 else junk6.ap.rearrange("p a b -> p (a b)")
                    fw.op(act, lambda e: e.activation(out=jap, in_=xt.ap, func=AF.Square, accum_out=st.ap[:, 0:1]), [junk6, st], [xt])
                    fw.op(dve, lambda e: e.tensor_scalar(out=st.ap[:, 1:2], in0=st.ap[:, 0:1], scalar1=1.0 / D, scalar2=1e-6, op0=ALU.mult, op1=ALU.add), [st], [st])
                    fw.op(act, lambda e: e.activation(out=st.ap[:, 3:4], in_=st.ap[:, 1:2], func=AF.Sqrt), [st], [st])
                    fw.op(dve, lambda e: e.reciprocal(out=st.ap[:, 2:3], in_=st.ap[:, 3:4]), [st], [st])

                def load_w(dst, src):
                    for j in range(2):
                        fw.dma(dst.ap[:, :, j * 512:(j + 1) * 512], src[:, j * 512:(j + 1) * 512].rearrange("(kc p) n -> p kc n", p=128), [dst], [], q=pool)

                seq = [(gi, ex) for gi in range(len(groups)) for ex in range(NE)]
                load_w(Wg2[0], e_gate[l, 0])
                load_w(Wu2[0], e_up[l, 0])

                def moe_A_front(gi, grp, t):
                    i = grp[t]
                    if True:
                        s_ = 1 if i < NCT else 0
                        xt = x6_pool.next()
                        fw.dma(xt.ap, xs[i * 128:(i + 1) * 128, :], [xt], [xs_t[i]])
                        st = st6_pool.next()
                        hT32 = hT32_pool.next()
                        rms(xt, st, hT32)
                        fw.op(dve, lambda e, xt=xt, st=st, s_=s_: e.scalar_tensor_tensor(out=xt.ap, in0=xt.ap, scalar=st.ap[:, 2:3], in1=mA[s_].ap, op0=ALU.mult, op1=ALU.mult), [xt], [xt, st, mA[s_]])
                        fw.op(dve, lambda e, xt=xt, s_=s_: e.tensor_tensor(out=xt.ap, in0=xt.ap, in1=mB[s_].ap, op=ALU.add), [xt], [xt, mB[s_]])
                        return dict(xt=xt, hT32=hT32, t=t, i=i)

                def moe_A_back(gi, grp, cx):
                    HT, gates = HT2[gi % 2], gates2[gi % 2]
                    xt, hT32, t, i = cx["xt"], cx["hT32"], cx["t"], cx["i"]
                    if True:
                        for half in range(2):
                            ps = psum()
                            for kk in range(4):
                                kc = half * 4 + kk
                                fw.op(pe, lambda e, kc=kc, kk=kk, ps=ps, xt=xt: e.transpose(out=ps.ap[:, kk * 128:(kk + 1) * 128], in_=xt.ap[:, kc * 128:(kc + 1) * 128], identity=ident.ap), [ps], [xt, ident])
                            fw.op(dve, lambda e, ps=ps, hT32=hT32, half=half: e.tensor_copy(out=hT32.ap[:, half * 4:(half + 1) * 4, :], in_=ps.ap.rearrange("p (a b) -> p a b", a=4)), [hT32], [ps])
                            fw.op(act, lambda e, hT32=hT32, half=half, t=t: e.copy(out=HT.ap[:, half * 4:(half + 1) * 4, t * 128:(t + 1) * 128], in_=hT32.ap[:, half * 4:(half + 1) * 4, :]), [HT], [hT32])
                        ps = psum()
                        for kc in range(8):
                            fw.op(pe, lambda e, kc=kc, ps=ps, hT32=hT32: e.matmul(ps.ap[:, 0:NE], lhsT=hT32.ap[:, kc, :], rhs=rwt.ap[:, kc, :], start=(kc == 0), stop=(kc == 7)), [ps], [hT32, rwt])
                        rt = rt_pool.next()
                        R = rt.ap
                        sc_, bi_ = R[:, 0:16], R[:, 16:32]
                        g4 = lambda ap: ap.rearrange("p (g m) -> p g m", g=4)
                        fw.op(act, lambda e, ps=ps, sc_=sc_: e.activation(out=sc_, in_=ps.ap[:, 0:NE], func=AF.Sigmoid), [rt], [ps])
                        fw.op(dve, lambda e, sc_=sc_, bi_=bi_: e.tensor_tensor(out=bi_, in0=sc_, in1=rbt.ap, op=ALU.add), [rt], [rt, rbt])
                        a = [g4(bi_)[:, :, m] for m in range(4)]
                        hi01, lo01, hi23, lo23 = R[:, 32:36], R[:, 36:40], R[:, 40:44], R[:, 44:48]
                        top1, m1, m2, top2 = R[:, 48:52], R[:, 52:56], R[:, 56:60], R[:, 60:64]
                        gs_, gmx, gm_, rd = R[:, 64:68], R[:, 68:69], R[:, 72:76], R[:, 69:70]
                        sel = R[:, 80:96]
                        tt = lambda o, x0, x1, op: fw.op(dve, lambda e: e.tensor_tensor(out=o, in0=x0, in1=x1, op=op), [rt], [rt])
                        tt(hi01, a[0], a[1], ALU.max)
                        tt(lo01, a[0], a[1], ALU.min)
                        tt(hi23, a[2], a[3], ALU.max)
                        tt(lo23, a[2], a[3], ALU.min)
                        tt(top1, hi01, hi23, ALU.max)
                        tt(m1, hi01, hi23, ALU.min)
                        tt(m2, lo01, lo23, ALU.max)
                        tt(top2, m1, m2, ALU.max)
                        tt(gs_, top1, top2, ALU.add)
                        fw.op(dve, lambda e, gs_=gs_, gmx=gmx: e.reduce_max(out=gmx, in_=gs_, axis=AX.X), [rt], [rt])
                        fw.op(dve, lambda e, gs_=gs_, gmx=gmx, gm_=gm_: e.tensor_scalar(out=gm_, in0=gs_, scalar1=gmx, scalar2=None, op0=ALU.is_ge), [rt], [rt])
                        tt(g4(sel), g4(bi_), top2.unsqueeze(2).to_broadcast([128, 4, 4]), ALU.is_ge)
                        tt(g4(sel), g4(sel), gm_.unsqueeze(2).to_broadcast([128, 4, 4]), ALU.mult)
                        tt(sel, sel, sc_, ALU.mult)
                        fw.op(dve, lambda e, sel=sel, rd=rd: e.reduce_sum(out=rd, in_=sel, axis=AX.X), [rt], [rt])
                        fw.op(dve, lambda e, rd=rd: e.reciprocal(out=rd, in_=rd), [rt], [rt])
                        fw.op(dve, lambda e, sel=sel, rd=rd, t=t: e.tensor_scalar(out=gates.ap[:, t, :], in0=sel, scalar1=rd, scalar2=None, op0=ALU.mult), [gates], [rt])
                        if "gates" in dbg and l == 0:
                            fw.dma(dbg["gates"][i * 128:(i + 1) * 128, :], gates.ap[:, t, :], [], [gates])

                def moe_B(gi, grp, exs):
                    ntok = len(grp) * 128
                    HT, gates = HT2[gi % 2], gates2[gi % 2]
                    for ex in exs:
                        pre = hooks.get((gi, ex, 0))
                        cxh = pre() if pre else None
                        qi = gi * NE + ex
                        Wg, Wu = Wg2[qi % 2], Wu2[qi % 2]
                        load_w(Wd, e_down[l, ex])
                        if qi + 1 < len(seq):
                            nex = seq[qi + 1][1]
                            load_w(Wg2[(qi + 1) % 2], e_gate[l, nex])
                            load_w(Wu2[(qi + 1) % 2], e_up[l, nex])
                        for t0 in range(0, ntok, 512):
                            ntk = min(512, ntok - t0)
                            for fc in range(8):
                                psg = psum()
                                psu = psum()
                                for kc in range(8):
                                    fw.op(pe, lambda e, kc=kc, fc=fc, psg=psg, t0=t0, ntk=ntk: e.matmul(psg.ap[:, 0:ntk], lhsT=Wg.ap[:, kc, fc * 128:(fc + 1) * 128], rhs=HT.ap[:, kc, t0:t0 + ntk], start=(kc == 0), stop=(kc == 7)), [psg], [Wg, HT])
                                for kc in range(8):
                                    fw.op(pe, lambda e, kc=kc, fc=fc, psu=psu, t0=t0, ntk=ntk: e.matmul(psu.ap[:, 0:ntk], lhsT=Wu.ap[:, kc, fc * 128:(fc + 1) * 128], rhs=HT.ap[:, kc, t0:t0 + ntk], start=(kc == 0), stop=(kc == 7)), [psu], [Wu, HT])
                                sg = sg_pool.next()
                                fw.op(act, lambda e, psg=psg, sg=sg, ntk=ntk: e.activation(out=sg.ap[:, 0:ntk], in_=psg.ap[:, 0:ntk], func=AF.Silu), [sg], [psg])
                                fw.op(dve, lambda e, psu=psu, sg=sg, fc=fc, t0=t0, ntk=ntk: e.tensor_tensor(out=actb.ap[:, fc, t0:t0 + ntk], in0=sg.ap[:, 0:ntk], in1=psu.ap[:, 0:ntk], op=ALU.mult), [actb], [sg, psu])
                        for t in range(len(grp)):
                            for half in range(2):
                                hs = slice(half * 512, (half + 1) * 512)
                                ps = psum()
                                for fc in range(8):
                                    fw.op(pe, lambda e, fc=fc, ps=ps, t=t, hs=hs: e.matmul(ps.ap, lhsT=actb.ap[:, fc, t * 128:(t + 1) * 128], rhs=Wd.ap[:, fc, hs], start=(fc == 0), stop=(fc == 7)), [ps], [actb, Wd])
                                if ex == 0:
                                    fw.op(dve, lambda e, ps=ps, t=t, hs=hs, ex=ex: e.tensor_scalar(out=accs[t].ap[:, hs], in0=ps.ap, scalar1=gates.ap[:, t, ex:ex + 1], scalar2=None, op0=ALU.mult), [accs[t]], [ps, gates])
                                else:
                                    fw.op(dve, lambda e, ps=ps, t=t, hs=hs, ex=ex: e.scalar_tensor_tensor(out=accs[t].ap[:, hs], in0=ps.ap, scalar=gates.ap[:, t, ex:ex + 1], in1=accs[t].ap[:, hs], op0=ALU.mult, op1=ALU.add), [accs[t]], [ps, gates, accs[t]])
                        post = hooks.get((gi, ex, 1))
                        if post:
                            post(cxh)

                def moe_C(gi, grp):
                    ntok = len(grp) * 128
                    HT, gates = HT2[gi % 2], gates2[gi % 2]
                    for t, i in enumerate(grp):
                        s_ = 1 if i < NCT else 0
                        xt = x6_pool.next()
                        fw.dma(xt.ap, xs[i * 128:(i + 1) * 128, :], [xt], [xs_t[i]])
                        fw.op(dve, lambda e, t=t, s_=s_: e.tensor_tensor(out=accs[t].ap, in0=accs[t].ap, in1=mG[s_].ap, op=ALU.mult), [accs[t]], [accs[t], mG[s_]])
                        fw.op(dve, lambda e, xt=xt, t=t: e.tensor_tensor(out=xt.ap, in0=xt.ap, in1=accs[t].ap, op=ALU.add), [xt], [xt, accs[t]])
                        if "xout" in dbg and l == 0:
                            fw.dma(dbg["xout"][i * 128:(i + 1) * 128, :], xt.ap, [], [xt])
                        if not last:
                            fw.dma(xs[i * 128:(i + 1) * 128, :], xt.ap, [xs_t[i]], [xt])
                        else:
                            st = st6_pool.next()
                            rms(xt, st, accs[t])
                            fw.op(dve, lambda e, xt=xt, st=st: e.scalar_tensor_tensor(out=xt.ap, in0=xt.ap, scalar=st.ap[:, 2:3], in1=fgt.ap, op0=ALU.mult, op1=ALU.mult), [xt], [xt, st, fgt])
                            fw.dma(out[(i - NCT) * 128:(i - NCT + 1) * 128, :], xt.ap, [], [xt])

                hooks = {}
                for t in range(len(groups[0])):
                    moe_A_back(0, groups[0], moe_A_front(0, groups[0], t))
                for gi, grp in enumerate(groups):
                    if gi + 1 < len(groups):
                        ng = groups[gi + 1]
                        for t in range(len(ng)):
                            ex = 4 + t
                            hooks[(gi, ex, 0)] = (lambda gi=gi, ng=ng, t=t: moe_A_front(gi + 1, ng, t))
                            hooks[(gi, ex, 1)] = (lambda cx, gi=gi, ng=ng: moe_A_back(gi + 1, ng, cx))
                    moe_B(gi, grp, range(NE))
                    moe_C(gi, grp)
                fw.barrier()
            if stop_after == "S6":
                break
        fw.barrier()
    return nc


def prep_inputs(inputs):
    f = lambda a: np.ascontiguousarray(np.asarray(a, dtype=np.float32))
    shared = {}
    for k in ("w_mod", "b_mod", "norm_mix_g", "norm_ffn_g", "w_in", "w_out", "att_sink", "shift_mu_prev", "shift_mu_next",
              "decay_w0", "decay_w2", "iclr_a0", "iclr_a2", "vres_v0", "vres_v1", "vres_v2", "gate_g2", "k_k", "k_a",
              "ln_x_w", "ln_x_b", "router_w", "router_b", "expert_w_gate", "expert_w_up", "expert_w_down", "final_norm_g"):
        shared[k] = f(inputs[k])
    shared["r_k"] = f(inputs["r_k"]).reshape(DEPTH, RW)
    shared["ident"] = np.eye(128, dtype=np.float32)
    s = np.arange(128)[:, None]
    t = np.arange(128)[None, :]
    shared["masks"] = np.ascontiguousarray(np.stack([s < t, s <= t, s > t, s >= t], axis=1).astype(np.float32))
    pos = np.arange(L)
    row = (pos // 64).astype(np.float32)
    colp = (pos % 64).astype(np.float32)
    inv = (10000.0 ** (-np.arange(16, dtype=np.float32) / 16)).astype(np.float32)
    ang = np.concatenate([row[:, None] * inv, row[:, None] * inv, colp[:, None] * inv, colp[:, None] * inv], axis=1)
    cos = np.cos(ang).astype(np.float32)
    sin = np.sin(ang).astype(np.float32)
    sgn = np.concatenate([-np.ones(16), np.ones(16), -np.ones(16), np.ones(16)]).astype(np.float32)
    shared["rope_cos"] = np.ascontiguousarray(np.tile(cos, (1, 10)))
    shared["rope_sin"] = np.ascontiguousarray(np.tile(sin * sgn, (1, 10)))
    x = f(inputs["x"])
    ctx = f(inputs["ctx"])
    c = f(inputs["c"])
    cc = f(inputs["c_ctx"])
    maps = []
    for b in range(8):
        m = dict(shared)
        m["x"] = x[b]
        m["ctx"] = ctx[b]
        m["ccol"] = np.ascontiguousarray(np.concatenate([c[b].reshape(8, 128).T, cc.reshape(8, 128).T], axis=1))
        maps.append(m)
    return maps


def kernel(**inputs):
    maps = prep_inputs(inputs)
    nc = build()
    res = run_bass_kernel_spmd(nc, maps, core_ids=list(range(8)))
    return np.stack([r["out"] for r in res.results], axis=0).astype(np.float32)
```
